# Optimizing a Trainium2 kernel written in Bass

```python
import math
import jax, jax.numpy as jnp
from jax import lax
import numpy as np

D_MODEL = 1024
BATCH = 16
SEQ = 4096
DEPTH = 1

NORM_EPS = 1e-6
DA_QK_DIM = 64
DA_V_DIM = 2 * DA_QK_DIM
DA_HEADS = D_MODEL // DA_V_DIM
DA_QK_W = DA_HEADS * 2 * DA_QK_DIM
DA_V_W = DA_HEADS * DA_V_DIM
ROPE_THETA = 500000.0
ROPE_DIM = DA_QK_DIM // 4
Q_BLOCK = 128
ML_HEADS = 8
ML_QK_DIM = 64
ML_V_DIM = D_MODEL // ML_HEADS
ML_QK_W = ML_HEADS * ML_QK_DIM
ML_V_W = ML_HEADS * ML_V_DIM
ML_CHUNK = 64
CONV_WIDTH = 4
PEER_HEADS = 8
PEER_N_KEYS = 128
PEER_N_EXPERTS = PEER_N_KEYS * PEER_N_KEYS
PEER_QUERY_DIM = 256
PEER_HALF = PEER_QUERY_DIM // 2
PEER_TOPK = 16
PEER_TOKEN_BLOCK = 128
IN_SIZES = (DA_QK_W, DA_QK_W, DA_V_W, ML_QK_W, ML_QK_W, ML_V_W, ML_V_W, ML_HEADS, ML_HEADS, D_MODEL, D_MODEL)
IN_WIDTH = sum(IN_SIZES)

kernel_name = "hybrid_diffattn_mlstm_peer_block"


def rmsnorm(x, w):
    xf = x.astype(jnp.float32)
    y = xf * lax.rsqrt(jnp.mean(xf * xf, axis=-1, keepdims=True) + NORM_EPS)
    return (y * w.astype(jnp.float32)).astype(x.dtype)


def split_cols(t, sizes):
    idx = np.cumsum(np.array(sizes))[:-1].tolist()
    return jnp.split(t, idx, axis=-1)


def partial_rope(x, pos):
    half = ROPE_DIM // 2
    inv = ROPE_THETA ** (-jnp.arange(half, dtype=jnp.float32) * 2.0 / ROPE_DIM)
    ang = pos.astype(jnp.float32)[:, None] * inv[None, :]
    cos = jnp.cos(ang)[None, :, None, None, :]
    sin = jnp.sin(ang)[None, :, None, None, :]
    xr = x[..., :ROPE_DIM].astype(jnp.float32)
    x1, x2 = xr[..., :half], xr[..., half:]
    rot = jnp.concatenate([x1 * cos - x2 * sin, x2 * cos + x1 * sin], axis=-1)
    return jnp.concatenate([rot.astype(x.dtype), x[..., ROPE_DIM:]], axis=-1)


def diff_attention(q, k, v, lam, subln_w, lam_init):
    B, S, H, _, dk = q.shape
    scale = dk ** -0.5
    outs = []
    for s0 in range(0, S, Q_BLOCK):
        s1 = s0 + Q_BLOCK
        qb, kb, vb = q[:, s0:s1], k[:, :s1], v[:, :s1]
        sc = jnp.einsum('bqhcd,bkhcd->bhcqk', qb, kb).astype(jnp.float32) * scale
        mask = jnp.arange(s1)[None, :] <= jnp.arange(s0, s1)[:, None]
        p = jax.nn.softmax(jnp.where(mask, sc, -jnp.inf), axis=-1)
        a = p[:, :, 0] - lam * p[:, :, 1]
        outs.append(jnp.einsum('bhqk,bkhd->bqhd', a.astype(v.dtype), vb))
    o = jnp.concatenate(outs, axis=1)
    o = rmsnorm(o, subln_w) * (1.0 - lam_init)
    return o.reshape(B, S, H * v.shape[-1])


def causal_conv(x, w, b):
    K = w.shape[0]
    S = x.shape[1]
    xp = jnp.pad(x, ((0, 0), (K - 1, 0), (0, 0)))
    y = b
    for j in range(K):
        y = y + xp[:, j:j + S] * w[j]
    return y


def mlstm_chunkwise(q, k, v, i_pre, f_pre):
    B, H, S, dk = q.shape
    dv = v.shape[-1]
    L = ML_CHUNK
    NC = S // L
    f32 = jnp.float32
    qc = q.astype(f32).reshape(B, H, NC, L, dk)
    kc = k.astype(f32).reshape(B, H, NC, L, dk)
    vc = v.astype(f32).reshape(B, H, NC, L, dv)
    log_f = jax.nn.log_sigmoid(f_pre.astype(f32)).reshape(B, H, NC, L)
    ig = i_pre.astype(f32).reshape(B, H, NC, L)
    b = jnp.cumsum(log_f, axis=-1)
    g = b[..., -1]
    a = g[..., None] - b + ig
    m_loc = jnp.max(a, axis=-1)
    w_loc = jnp.exp(a - m_loc[..., None])
    wk = w_loc[..., None] * kc
    C_loc = jnp.einsum('bhcld,bhcle->bhcde', wk, vc)
    n_loc = jnp.sum(wk, axis=-2)

    def step(carry, inp):
        C, n, m = carry
        g_c, m_loc_c, C_loc_c, n_loc_c = inp
        m_new = jnp.maximum(g_c + m, m_loc_c)
        dec = jnp.exp(g_c + m - m_new)
        inc = jnp.exp(m_loc_c - m_new)
        C_new = dec[..., None, None] * C + inc[..., None, None] * C_loc_c
        n_new = dec[..., None] * n + inc[..., None] * n_loc_c
        return (C_new, n_new, m_new), (C, n, m)

    init = (jnp.zeros((B, H, dk, dv), f32), jnp.zeros((B, H, dk), f32), jnp.zeros((B, H), f32))
    xs = (jnp.moveaxis(g, 2, 0), jnp.moveaxis(m_loc, 2, 0), jnp.moveaxis(C_loc, 2, 0), jnp.moveaxis(n_loc, 2, 0))
    _, (C_prev, n_prev, m_prev) = lax.scan(step, init, xs)
    C_prev = jnp.moveaxis(C_prev, 0, 2)
    n_prev = jnp.moveaxis(n_prev, 0, 2)
    m_prev = jnp.moveaxis(m_prev, 0, 2)
    Dm = b[..., :, None] - b[..., None, :] + ig[..., None, :]
    causal = jnp.tril(jnp.ones((L, L), dtype=bool))
    Dm = jnp.where(causal, Dm, -jnp.inf)
    m_inter = b + m_prev[..., None]
    m_j = jnp.maximum(jnp.max(Dm, axis=-1), m_inter)
    W = jnp.exp(Dm - m_j[..., None])
    qk = jnp.einsum('bhcjd,bhckd->bhcjk', qc, kc) * W
    inter_w = jnp.exp(m_inter - m_j)
    num = jnp.einsum('bhcjk,bhcke->bhcje', qk, vc) + inter_w[..., None] * jnp.einsum('bhcjd,bhcde->bhcje', qc, C_prev)
    den = jnp.sum(qk, axis=-1) + inter_w * jnp.einsum('bhcjd,bhcd->bhcj', qc, n_prev)
    h = num / jnp.maximum(jnp.abs(den), jnp.exp(-m_j))[..., None]
    return h.reshape(B, H, S, dv).astype(v.dtype)


def peer_ffn(h, w_q, sub_keys, U, V):
    B, S, D = h.shape
    T = B * S
    hf = h.reshape(T, D)
    q = (hf @ w_q).reshape(T, PEER_HEADS, 2, PEER_HALF)
    s = jnp.einsum('thpd,hpnd->thpn', q, sub_keys).astype(jnp.float32)
    v_top, i_top = lax.top_k(s, PEER_TOPK)
    cand = v_top[:, :, 0, :, None] + v_top[:, :, 1, None, :]
    cand_idx = i_top[:, :, 0, :, None] * PEER_N_KEYS + i_top[:, :, 1, None, :]
    cand = cand.reshape(T, PEER_HEADS, PEER_TOPK * PEER_TOPK)
    cand_idx = cand_idx.reshape(T, PEER_HEADS, PEER_TOPK * PEER_TOPK)
    sc, sel = lax.top_k(cand, PEER_TOPK)
    idx = jnp.take_along_axis(cand_idx, sel, axis=-1)
    gate = jax.nn.softmax(sc, axis=-1).astype(h.dtype)
    nb = T // PEER_TOKEN_BLOCK

    def block(args):
        hb, ib, gb = args
        act = jax.nn.gelu(jnp.einsum('thed,td->the', U[ib], hb), approximate=False)
        return jnp.einsum('the,thed->td', gb * act, V[ib])

    y = lax.map(block, (hf.reshape(nb, PEER_TOKEN_BLOCK, D),
                        idx.reshape(nb, PEER_TOKEN_BLOCK, PEER_HEADS, PEER_TOPK),
                        gate.reshape(nb, PEER_TOKEN_BLOCK, PEER_HEADS, PEER_TOPK)))
    return y.reshape(B, S, D)


def setup_inputs(seed: int = 0) -> dict:
    key = jax.random.key(seed)
    ks = jax.random.split(key, 24)
    nrm = jax.random.normal
    D = D_MODEL
    f32 = jnp.float32
    return {
        "x": nrm(ks[0], (BATCH, SEQ, D), f32),
        "c": nrm(ks[1], (BATCH, D), f32),
        "ada_w": nrm(ks[2], (DEPTH, D, 6 * D), f32) * D ** -0.5,
        "ada_b": nrm(ks[3], (DEPTH, 6 * D), f32) * 0.01,
        "norm1_w": 1.0 + 0.02 * nrm(ks[4], (DEPTH, D), f32),
        "norm2_w": 1.0 + 0.02 * nrm(ks[5], (DEPTH, D), f32),
        "w_in": nrm(ks[6], (DEPTH, D, IN_WIDTH), f32) * D ** -0.5,
        "conv_w": nrm(ks[7], (DEPTH, CONV_WIDTH, 2 * ML_QK_W), f32) * CONV_WIDTH ** -0.5,
        "conv_b": nrm(ks[8], (DEPTH, 2 * ML_QK_W), f32) * 0.01,
        "ml_i_bias": nrm(ks[9], (DEPTH, ML_HEADS), f32) * 0.1,
        "ml_f_bias": jnp.linspace(3.0, 6.0, ML_HEADS, dtype=f32)[None, :] + 0.1 * nrm(ks[10], (DEPTH, ML_HEADS), f32),
        "ml_norm_w": 1.0 + 0.02 * nrm(ks[11], (DEPTH, ML_V_W), f32),
        "lam_q1": 0.1 * nrm(ks[12], (DEPTH, DA_QK_DIM), f32),
        "lam_k1": 0.1 * nrm(ks[13], (DEPTH, DA_QK_DIM), f32),
        "lam_q2": 0.1 * nrm(ks[14], (DEPTH, DA_QK_DIM), f32),
        "lam_k2": 0.1 * nrm(ks[15], (DEPTH, DA_QK_DIM), f32),
        "subln_w": 1.0 + 0.02 * nrm(ks[16], (DEPTH, DA_V_DIM), f32),
        "w_out": nrm(ks[17], (DEPTH, D, D), f32) * D ** -0.5,
        "peer_wq": nrm(ks[18], (DEPTH, D, PEER_HEADS * PEER_QUERY_DIM), f32) * D ** -0.5,
        "peer_keys": nrm(ks[19], (DEPTH, PEER_HEADS, 2, PEER_N_KEYS, PEER_HALF), f32) * PEER_HALF ** -0.5,
        "peer_u": nrm(ks[20], (DEPTH, PEER_N_EXPERTS, D), f32) * D ** -0.5,
        "peer_v": nrm(ks[21], (DEPTH, PEER_N_EXPERTS, D), f32) * 0.5,
        "final_norm_w": 1.0 + 0.02 * nrm(ks[22], (D,), f32),
    }


def _to_heads(t, B, S, d):
    return t.reshape(B, S, ML_HEADS, d).transpose(0, 2, 1, 3)


def reference(x, c, ada_w, ada_b, norm1_w, norm2_w, w_in, conv_w, conv_b, ml_i_bias, ml_f_bias, ml_norm_w,
              lam_q1, lam_k1, lam_q2, lam_k2, subln_w, w_out, peer_wq, peer_keys, peer_u, peer_v, final_norm_w):
    B, S, D = x.shape
    pos = jnp.arange(S, dtype=jnp.int32)
    cond = jax.nn.silu(c)
    for l in range(DEPTH):
        mod = cond @ ada_w[l] + ada_b[l]
        sh1, sc1, gt1, sh2, sc2, gt2 = jnp.split(mod[:, None, :], 6, axis=-1)
        h = rmsnorm(x, norm1_w[l]) * (1.0 + sc1) + sh1
        qa, ka, va, qm, km, vm, om, ip, fp, ga, gm = split_cols(h @ w_in[l], IN_SIZES)
        qa = partial_rope(qa.reshape(B, S, DA_HEADS, 2, DA_QK_DIM), pos)
        ka = partial_rope(ka.reshape(B, S, DA_HEADS, 2, DA_QK_DIM), pos)
        va = va.reshape(B, S, DA_HEADS, DA_V_DIM)
        lam_init = 0.8 - 0.6 * math.exp(-0.3 * l)
        lam = (jnp.exp(jnp.sum(lam_q1[l].astype(jnp.float32) * lam_k1[l].astype(jnp.float32)))
               - jnp.exp(jnp.sum(lam_q2[l].astype(jnp.float32) * lam_k2[l].astype(jnp.float32))) + lam_init)
        y_a = diff_attention(qa, ka, va, lam, subln_w[l], lam_init)
        qk = jax.nn.silu(causal_conv(jnp.concatenate([qm, km], axis=-1), conv_w[l], conv_b[l]))
        qm, km = qk[..., :ML_QK_W], qk[..., ML_QK_W:]
        hm = mlstm_chunkwise(_to_heads(qm, B, S, ML_QK_DIM),
                             _to_heads(km, B, S, ML_QK_DIM) * (ML_QK_DIM ** -0.5),
                             _to_heads(vm, B, S, ML_V_DIM),
                             (ip + ml_i_bias[l]).transpose(0, 2, 1),
                             (fp + ml_f_bias[l]).transpose(0, 2, 1))
        hm = rmsnorm(hm.transpose(0, 2, 1, 3), ml_norm_w[l].reshape(ML_HEADS, ML_V_DIM))
        y_m = hm.reshape(B, S, ML_V_W) * jax.nn.sigmoid(om)
        merged = jax.nn.sigmoid(ga) * y_a + jax.nn.sigmoid(gm) * y_m
        x = x + gt1 * (merged @ w_out[l])
        h2 = rmsnorm(x, norm2_w[l]) * (1.0 + sc2) + sh2
        x = x + gt2 * peer_ffn(h2, peer_wq[l], peer_keys[l], peer_u[l], peer_v[l])
    return rmsnorm(x, final_norm_w)
```

```python
import math
from contextlib import ExitStack

import numpy as np
import ml_dtypes
import concourse.bass as bass
import concourse.mybir as mybir
from concourse.bass_utils import run_bass_kernel_spmd

F32 = mybir.dt.float32
BF16 = mybir.dt.bfloat16
U32 = mybir.dt.uint32
ALU = mybir.AluOpType
AF = mybir.ActivationFunctionType
AX = mybir.AxisListType

D = 1024
EPS = 1e-6
IN_W = 8208
NEG = -30000.0


class Tok:
    __slots__ = ("sem", "val", "eng")

    def __init__(self, sem, val, eng):
        self.sem, self.val, self.eng = sem, val, eng


class Eng:
    def __init__(self, kb, name, eng):
        self.kb, self.name, self.eng = kb, name, eng
        self.sem = None
        self.count = 0
        self.seen = {}
        self.last = None
        self.nsem = 0

    def wait(self, tok):
        if tok is None:
            return
        key = id(tok.sem)
        if self.seen.get(key, 0) >= tok.val:
            return
        self.seen[key] = tok.val
        self.eng.wait_ge(tok.sem, tok.val)

    def signal(self, instr):
        if self.sem is None or self.count >= 30000:
            self.sem = self.kb.es.enter_context(self.kb.nc.semaphore(f"s_{self.name}_{self.nsem}"))
            self.nsem += 1
            self.count = 0
        self.count += 1
        instr.then_inc(self.sem, 1)
        t = Tok(self.sem, self.count, self.name)
        self.last = t
        return t


class KB:
    def __init__(self, nc):
        self.nc = nc
        self.es = ExitStack()
        self.e = {
            "pe": Eng(self, "pe", nc.tensor),
            "act": Eng(self, "act", nc.scalar),
            "dve": Eng(self, "dve", nc.vector),
            "pool": Eng(self, "pool", nc.gpsimd),
            "sp": Eng(self, "sp", nc.sync),
        }
        self.res = {}
        self.dsems = []
        self.dvals = []
        self.dnext = 0
        self.ND = 40
        self.all_dma = []

    def _deps(self, r, w):
        deps = []
        for k in r:
            st = self.res.get(k)
            if st is not None and st[0] is not None:
                deps.append(st[0])
        for k in w:
            st = self.res.get(k)
            if st is not None:
                if st[0] is not None:
                    deps.append(st[0])
                deps.extend(st[1])
        return deps

    def _record(self, tok, r, w):
        for k in r:
            st = self.res.setdefault(k, [None, []])
            if tok.eng != "dma":
                st[1] = [t for t in st[1] if t.eng != tok.eng]
            st[1].append(tok)
        for k in w:
            self.res[k] = [tok, []]

    def op(self, en, fn, r=(), w=()):
        E = self.e[en]
        for t in self._deps(r, w):
            if en == "pe" and t.eng == "pe":
                continue
            E.wait(t)
        instr = fn(E.eng)
        tok = E.signal(instr)
        self._record(tok, r, w)
        return tok

    def P(self, fn, r=(), w=()):
        return self.op("pe", fn, r, w)

    def A(self, fn, r=(), w=()):
        return self.op("act", fn, r, w)

    def V(self, fn, r=(), w=()):
        return self.op("dve", fn, r, w)

    def G(self, fn, r=(), w=()):
        return self.op("pool", fn, r, w)

    def dma(self, out, in_, r=(), w=(), q="sp", **kw):
        E = self.e[q]
        for t in self._deps(r, w):
            E.wait(t)
        if len(self.dsems) < self.ND:
            self.dsems.append(self.es.enter_context(self.nc.semaphore(f"s_dma_{len(self.dsems)}")))
            self.dvals.append(0)
        i = self.dnext
        self.dnext = (self.dnext + 1) % self.ND
        sem = self.dsems[i]
        if self.dvals[i] > 0:
            E.wait(Tok(sem, self.dvals[i], "dma"))
        self.dvals[i] += 16
        instr = E.eng.dma_start(out=out, in_=in_, **kw)
        instr.then_inc(sem, 16)
        tok = Tok(sem, self.dvals[i], "dma")
        self._record(tok, r, w)
        return tok

    def barrier(self):
        toks = [E.last for E in self.e.values() if E.last is not None]
        toks += [Tok(s, v, "dma") for s, v in zip(self.dsems, self.dvals) if v > 0]
        for E in self.e.values():
            for t in toks:
                if t.eng == E.name:
                    continue
                E.wait(t)
        self.res = {}


class Cfg:
    def __init__(self, S=4096, NB=2, debug=False):
        self.S, self.NB, self.debug = S, NB, debug
        self.NT = S // 128


def host_consts(cfg):
    NT = cfg.NT
    p = np.arange(128)
    ident = np.eye(128, dtype=np.float32)
    tri = (p[:, None] <= p[None, :]).astype(np.float32)
    negm = np.where(p[:, None] > p[None, :], NEG, 0.0).astype(np.float32)
    cm = (p[:, None] <= p[None, :]).astype(np.float32)
    iota = np.broadcast_to(np.arange(128, dtype=np.float32)[None, :], (128, 128)).copy()
    ones = np.ones((128, 128), np.float32)
    half = 8
    inv = (500000.0 ** (-np.arange(half, dtype=np.float32) * 2.0 / 16)).astype(np.float32)
    pos = (np.arange(NT)[None, :] * 128 + p[:, None]).astype(np.float32)
    ang = pos[:, :, None] * inv[None, None, :]
    cos = np.cos(ang).astype(np.float32).reshape(128, NT * 8)
    sin = np.sin(ang).astype(np.float32).reshape(128, NT * 8)
    a16 = np.broadcast_to((np.arange(16, dtype=np.float32) * 16)[None, :], (128, 16)).copy()
    i16 = np.broadcast_to(np.arange(16, dtype=np.float32)[None, :], (128, 16)).copy()
    return np.concatenate([ident, tri, negm, cm, iota, ones, a16, i16, cos, sin], axis=1).astype(np.float32)


def build(cfg):
    S, NB, NT = cfg.S, cfg.NB, cfg.NT
    NG = S // 512
    T = NB * S
    nc = bass.Bass("TRN2", target_bir_lowering=False)

    def din(name, shape, dt=F32):
        return nc.dram_tensor(name, list(shape), dt, kind="ExternalInput").ap()

    def dscr(name, shape, dt):
        kind = "ExternalOutput" if cfg.debug else "Internal"
        return nc.dram_tensor(name, list(shape), dt, kind=kind).ap()

    x = din("x", [NB, S, D])
    c_in = din("c", [NB, D])
    ada_w = din("ada_w", [D, 6 * D])
    ada_b = din("ada_b", [1, 6 * D])
    norm1_w = din("norm1_w", [1, D])
    norm2_w = din("norm2_w", [1, D])
    w_in = din("w_in", [D, IN_W])
    conv_w = din("conv_w", [4, 1024])
    conv_b = din("conv_b", [1, 1024])
    ml_ib = din("ml_i_bias", [1, 8])
    ml_fb = din("ml_f_bias", [1, 8])
    ml_nw = din("ml_norm_w", [1, D])
    lam_q1 = din("lam_q1", [1, 64])
    lam_k1 = din("lam_k1", [1, 64])
    lam_q2 = din("lam_q2", [1, 64])
    lam_k2 = din("lam_k2", [1, 64])
    subln_w = din("subln_w", [1, 128])
    w_out = din("w_out", [D, D])
    peer_wq = din("peer_wq", [D, 2048])
    peer_keys = din("peer_keys", [16, 128, 128])
    peer_u = din("peer_u", [16384, D])
    peer_v = din("peer_v", [16384, D])
    fin_w = din("final_norm_w", [1, D])
    NCST = 128 * 6 + 32 + 2 * NT * 8
    cst_d = din("cst", [128, NCST])
    out = nc.dram_tensor("out", [NB, S, D], F32, kind="ExternalOutput").ap()

    UT = dscr("UT", [128, 128, 1024], BF16)
    VB = dscr("VB", [128, 128, 1024], BF16)
    MOD = dscr("MOD", [NB, 6 * D], F32)
    QT = dscr("QT", [NB, 8, 128, S], BF16)
    KT = dscr("KT", [NB, 8, 128, S], BF16)
    VA = dscr("VA", [NB, S, D], BF16)
    QMT = dscr("QMT", [NB, 4, 128, S], BF16)
    KMT = dscr("KMT", [NB, 4, 128, S], BF16)
    VM = dscr("VM", [NB, S, D], BF16)
    OMS = dscr("OMS", [NB, S, D], BF16)
    GAS = dscr("GAS", [NB, S, D], BF16)
    GMS = dscr("GMS", [NB, S, D], BF16)
    GATES = dscr("GATES", [NB, S, 16], F32)
    YM = dscr("YM", [NB, S, D], BF16)
    MT = dscr("MT", [NB, 8, 128, S], BF16)
    X1 = dscr("X1", [NB, S, D], F32)
    H2T = dscr("H2T", [NB, 8, 128, S], BF16)
    IG = dscr("IG", [NB, S, 3, 128], F32)

    k = KB(nc)
    P, A, V, G, dma = k.P, k.A, k.V, k.G, k.dma

    with k.es:
        es0 = k.es

        uid = [0]

        def sbt(es, name, shape, dt):
            uid[0] += 1
            return es.enter_context(nc.sbuf_tensor(f"sb{uid[0]}_{name}", list(shape), dt))

        def pst(es, name, shape, dt):
            uid[0] += 1
            return es.enter_context(nc.psum_tensor(f"ps{uid[0]}_{name}", list(shape), dt))

        cst = sbt(es0, "cst", [128, NCST], F32)
        identb = sbt(es0, "identb", [128, 128], BF16)
        cmb = sbt(es0, "cmb", [128, 128], BF16)
        nmx = sbt(es0, "nmx", [128, 32], F32)
        lamt = sbt(es0, "lamt", [128, 4], F32)
        dma(cst[:], cst_d, w=["cst"])
        identf = cst[:, 0:128]
        trif = cst[:, 128:256]
        negmf = cst[:, 256:384]
        cmf = cst[:, 384:512]
        iotaf = cst[:, 512:640]
        onesf = cst[:, 640:768]
        a16f = cst[:, 768:784]
        i16f = cst[:, 784:800]
        cosf = cst[:, 800:800 + NT * 8]
        sinf = cst[:, 800 + NT * 8:800 + 2 * NT * 8]
        V(lambda e: e.tensor_copy(out=identb[:], in_=identf), r=["cst"], w=["identb"])
        V(lambda e: e.tensor_copy(out=cmb[:], in_=cmf), r=["cst"], w=["cmb"])

        with ExitStack() as ph:
            uf = [sbt(ph, f"uf{i}", [128, 1024], F32) for i in range(2)]
            ub = [sbt(ph, f"ub{i}", [128, 1024], BF16) for i in range(2)]
            uts = [sbt(ph, f"uts{i}", [128, 1024], BF16) for i in range(2)]
            vf = [sbt(ph, f"vf{i}", [128, 1024], F32) for i in range(2)]
            vb = [sbt(ph, f"vb{i}", [128, 1024], BF16) for i in range(2)]
            ptb = [pst(ph, f"ptbA{i}", [128, 1024], BF16) for i in range(2)]
            for i1 in range(128):
                s = i1 % 2
                dma(uf[s][:], peer_u[i1 * 128:(i1 + 1) * 128, :], w=[("uf", s)])
                dma(vf[s][:], peer_v[i1 * 128:(i1 + 1) * 128, :], w=[("vf", s)])
                V(lambda e: e.tensor_copy(out=ub[s][:], in_=uf[s][:]), r=[("uf", s)], w=[("ub", s)])
                for kc in range(8):
                    P(lambda e: e.transpose(out=ptb[s][:, kc * 128:(kc + 1) * 128], in_=ub[s][:, kc * 128:(kc + 1) * 128], identity=identb[:]),
                      r=[("ub", s), "identb"], w=[("ptbA", s)])
                A(lambda e: e.activation(out=uts[s][:], in_=ptb[s][:], func=AF.Copy), r=[("ptbA", s)], w=[("uts", s)])
                dma(UT[i1], uts[s][:], r=[("uts", s)], w=["UT"])
                G(lambda e: e.tensor_copy(out=vb[s][:], in_=vf[s][:]), r=[("vf", s)], w=[("vb", s)])
                dma(VB[i1], vb[s][:], r=[("vb", s)], w=["VB"])

            condT = sbt(ph, "condT", [128, 8, NB], F32)
            adab = sbt(ph, "adab", [1, 6 * D], F32)
            modrow = sbt(ph, "modrow", [1, NB, 6 * D], F32)
            wada = [sbt(ph, f"wada{i}", [128, 8, 512], F32) for i in range(2)]
            psm = [pst(ph, f"psm{i}", [128, 512], F32) for i in range(2)]
            for b in range(NB):
                dma(condT[:, :, b], c_in[b].rearrange("(kc p) -> p kc", p=128), w=["condT"], allow_slow_non_contiguous=True)
            dma(adab[:], ada_b, w=["adab"])
            A(lambda e: e.activation(out=condT[:], in_=condT[:], func=AF.Silu), r=["condT"], w=["condT"])
            adaw_v = ada_w.rearrange("(kc p) n -> p kc n", p=128)
            for ncx in range(12):
                s = ncx % 2
                dma(wada[s][:], adaw_v[:, :, ncx * 512:(ncx + 1) * 512], w=[("wada", s)])
                for b in range(NB):
                    pb = (ncx * NB + b) % 2
                    for kc in range(8):
                        P(lambda e: e.matmul(psm[pb][0:1, :], lhsT=condT[:, kc, b:b + 1], rhs=wada[s][:, kc, :], start=(kc == 0), stop=(kc == 7)),
                          r=["condT", ("wada", s)], w=[("psm", pb)])
                    V(lambda e: e.tensor_tensor(out=modrow[0:1, b, ncx * 512:(ncx + 1) * 512], in0=psm[pb][0:1, :], in1=adab[0:1, ncx * 512:(ncx + 1) * 512], op=ALU.add),
                      r=[("psm", pb), "adab"], w=["modrow"])
            for b in range(NB):
                dma(MOD[b:b + 1, :], modrow[0:1, b, :], r=["modrow"], w=["MOD"])

            lq = sbt(ph, "lq", [1, 4, 64], F32)
            lsc = sbt(ph, "lsc", [1, 8], F32)
            dma(lq[0:1, 0, :], lam_q1, w=["lq0"])
            dma(lq[0:1, 1, :], lam_k1, w=["lq1"])
            dma(lq[0:1, 2, :], lam_q2, w=["lq2"])
            dma(lq[0:1, 3, :], lam_k2, w=["lq3"])
            V(lambda e: e.tensor_tensor(out=lq[0:1, 0, :], in0=lq[0:1, 0, :], in1=lq[0:1, 1, :], op=ALU.mult), r=["lq0", "lq1"], w=["lq0"])
            V(lambda e: e.tensor_tensor(out=lq[0:1, 2, :], in0=lq[0:1, 2, :], in1=lq[0:1, 3, :], op=ALU.mult), r=["lq2", "lq3"], w=["lq2"])
            V(lambda e: e.reduce_sum(out=lsc[0:1, 0:1], in_=lq[0:1, 0, :], axis=AX.X), r=["lq0"], w=["lsc0"])
            V(lambda e: e.reduce_sum(out=lsc[0:1, 1:2], in_=lq[0:1, 2, :], axis=AX.X), r=["lq2"], w=["lsc1"])
            A(lambda e: e.activation(out=lsc[0:1, 2:4], in_=lsc[0:1, 0:2], func=AF.Exp), r=["lsc0", "lsc1"], w=["lsc2"])
            lam_init = 0.8 - 0.6 * math.exp(0.0)
            V(lambda e: e.tensor_tensor(out=lsc[0:1, 4:5], in0=lsc[0:1, 3:4], in1=lsc[0:1, 2:3], op=ALU.subtract), r=["lsc2"], w=["lsc4"])
            V(lambda e: e.tensor_scalar(out=lsc[0:1, 5:6], in0=lsc[0:1, 4:5], scalar1=-lam_init, scalar2=None, op0=ALU.add), r=["lsc4"], w=["lsc5"])
            P(lambda e: e.matmul(psm[0][:, 0:1], lhsT=onesf[0:1, :], rhs=lsc[0:1, 5:6], start=True, stop=True), r=["lsc5", "cst", ("psm", 0)], w=[("psm", 0)])
            V(lambda e: e.tensor_copy(out=lamt[:, 0:1], in_=psm[0][:, 0:1]), r=[("psm", 0)], w=["lamt"])
            k.barrier()

        win_v = w_in.rearrange("(kc p) n -> p kc n", p=128)
        for b in range(NB):
            with ExitStack() as ph:
                hT = sbt(ph, "hT", [128, 8, S], BF16)
                A1 = sbt(ph, "A1", [128, D], F32)
                B1 = sbt(ph, "B1", [128, D], F32)
                w1b = sbt(ph, "w1b", [128, D], F32)
                xt = [sbt(ph, f"xt{i}", [128, D], F32) for i in range(2)]
                hx = sbt(ph, "hx", [128, D], F32)
                hb = [sbt(ph, f"hb{i}", [128, D], BF16) for i in range(2)]
                junk = sbt(ph, "junkB", [128, D], BF16)
                st4 = sbt(ph, "st4", [128, 8], F32)
                ptb = [pst(ph, f"ptbB{i}", [128, 1024], BF16) for i in range(2)]
                psb = [pst(ph, f"psB{i}", [128, 512], F32) for i in range(4)]
                dma(A1[:], MOD[b:b + 1, D:2 * D].partition_broadcast(128), r=["MOD"], w=["A1"])
                dma(B1[:], MOD[b:b + 1, 0:D].partition_broadcast(128), r=["MOD"], w=["B1"])
                dma(w1b[:], norm1_w.partition_broadcast(128), w=["w1b"])
                V(lambda e: e.scalar_tensor_tensor(out=A1[:], in0=A1[:], scalar=1.0, in1=w1b[:], op0=ALU.add, op1=ALU.mult), r=["A1", "w1b"], w=["A1"])
                V(lambda e: e.memset(nmx[:], 0.0), w=["nmx"])
                for tt in range(NT):
                    s = tt % 2
                    dma(xt[s][:], x[b, tt * 128:(tt + 1) * 128, :], w=[("xt", s)])
                    A(lambda e: e.activation(out=junk[:], in_=xt[s][:], func=AF.Square, accum_out=st4[:, 0:1]), r=[("xt", s)], w=["junkB", "st0"])
                    V(lambda e: e.tensor_scalar(out=st4[:, 1:2], in0=st4[:, 0:1], scalar1=1.0 / D, scalar2=EPS, op0=ALU.mult, op1=ALU.add), r=["st0"], w=["st1"])
                    A(lambda e: e.activation(out=st4[:, 2:3], in_=st4[:, 1:2], func=AF.Sqrt), r=["st1"], w=["st2"])
                    V(lambda e: e.reciprocal(out=st4[:, 3:4], in_=st4[:, 2:3]), r=["st2"], w=["st3"])
                    V(lambda e: e.scalar_tensor_tensor(out=hx[:], in0=xt[s][:], scalar=st4[:, 3:4], in1=A1[:], op0=ALU.mult, op1=ALU.mult), r=[("xt", s), "st3", "A1"], w=["hx"])
                    G(lambda e: e.tensor_tensor(out=hb[s][:], in0=hx[:], in1=B1[:], op=ALU.add), r=["hx", "B1"], w=[("hb", s)])
                    for kc in range(8):
                        P(lambda e: e.transpose(out=ptb[s][:, kc * 128:(kc + 1) * 128], in_=hb[s][:, kc * 128:(kc + 1) * 128], identity=identb[:]),
                          r=[("hb", s), "identb"], w=[("ptbB", s)])
                    A(lambda e: e.activation(out=hT[:, :, tt * 128:(tt + 1) * 128], in_=ptb[s][:].rearrange("p (k t) -> p k t", k=8), func=AF.Copy),
                      r=[("ptbB", s)], w=[("hT", tt)])

                wf = [sbt(ph, f"wf{i}", [128, 8, 512], F32) for i in range(2)]
                wb = [sbt(ph, f"wb{i}", [128, 8, 512], BF16) for i in range(2)]
                qf = sbt(ph, "qf", [128, 512], F32)
                sqj = sbt(ph, "sqj", [128, 512], F32)
                rt = sbt(ph, "rt", [128, 4, 8, 8], F32)
                nsq = sbt(ph, "nsq", [128, 8], F32)
                qb = [sbt(ph, f"qb{i}", [128, 512], BF16) for i in range(2)]
                stage = [sbt(ph, f"stage{i}", [128, 4, 512], BF16) for i in range(2)]
                ob = [sbt(ph, f"ob{i}", [128, 512], BF16) for i in range(3)]
                xbuf = sbt(ph, "xbuf", [128, 4, 515], F32)
                cacc = sbt(ph, "cacc", [128, 512], F32)
                csil = sbt(ph, "csil", [128, 512], F32)
                cstg = [sbt(ph, f"cstg{i}", [128, 512], BF16) for i in range(2)]
                cw = sbt(ph, "cw", [128, 8, 4], F32)
                cbi = sbt(ph, "cbi", [128, 8], F32)
                gbias = sbt(ph, "gbias", [128, 16], F32)
                gz = sbt(ph, "gz", [128, 16], F32)
                gt = [sbt(ph, f"gtB{i}", [128, 16], F32) for i in range(2)]
                for j in range(4):
                    dma(cw[:, :, j], conv_w[j].rearrange("(blk p) -> p blk", p=128), w=["cw"], allow_slow_non_contiguous=True)
                dma(cbi[:], conv_b.rearrange("o (blk p) -> p (o blk)", p=128), w=["cbi"], allow_slow_non_contiguous=True)
                dma(gbias[:, 0:8], ml_ib.partition_broadcast(128), w=["gbias0"])
                dma(gbias[:, 8:16], ml_fb.partition_broadcast(128), w=["gbias1"])

                chunks = []
                for i in range(2):
                    chunks.append((i * 512, 512, "qa", i))
                for i in range(2):
                    chunks.append((1024 + i * 512, 512, "ka", i))
                for i in range(2):
                    chunks.append((2048 + i * 512, 512, "va", i))
                chunks.append((3072, 512, "qm", 0))
                chunks.append((3584, 512, "km", 0))
                for i in range(2):
                    chunks.append((4096 + i * 512, 512, "vm", i))
                for i in range(2):
                    chunks.append((5120 + i * 512, 512, "om", i))
                chunks.append((6144, 16, "gate", 0))
                for i in range(2):
                    chunks.append((6160 + i * 512, 512, "ga", i))
                for i in range(2):
                    chunks.append((7184 + i * 512, 512, "gm", i))

                pcount = 0
                ocount = 0
                qcount = 0
                for ci, (c0, ncol, kind, idx) in enumerate(chunks):
                    s = ci % 2
                    dma(wf[s][:, :, 0:ncol], win_v[:, :, c0:c0 + ncol], w=[("wf", s)])
                    G(lambda e: e.tensor_copy(out=wb[s][:, :, 0:ncol], in_=wf[s][:, :, 0:ncol]), r=[("wf", s)], w=[("wb", s)])
                    if kind in ("qm", "km"):
                        sc_ = 1.0 if kind == "qm" else 0.125
                        dst = QMT if kind == "qm" else KMT
                        boff = 0 if kind == "qm" else 4
                        for cb in range(4):
                            V(lambda e: e.memset(xbuf[:, cb, 0:3], 0.0), w=[("xbuf", cb)])
                        for tg in range(NG):
                            for cb in range(4):
                                pb = pcount % 4
                                pcount += 1
                                for kc in range(8):
                                    P(lambda e: e.matmul(psb[pb][:], lhsT=wb[s][:, kc, cb * 128:(cb + 1) * 128], rhs=hT[:, kc, tg * 512:(tg + 1) * 512], start=(kc == 0), stop=(kc == 7)),
                                      r=[("wb", s)] + [("hT", t_) for t_ in range(tg * 4, tg * 4 + 4)], w=[("psB", pb)])
                                if tg > 0:
                                    V(lambda e: e.tensor_copy(out=xbuf[:, cb, 0:3], in_=xbuf[:, cb, 512:515]), r=[("xbuf", cb)], w=[("xbuf", cb)])
                                A(lambda e: e.activation(out=xbuf[:, cb, 3:515], in_=psb[pb][:], func=AF.Copy), r=[("psB", pb)], w=[("xbuf", cb)])
                                V(lambda e: e.tensor_scalar(out=cacc[:], in0=xbuf[:, cb, 0:512], scalar1=cw[:, boff + cb, 0:1], scalar2=None, op0=ALU.mult), r=[("xbuf", cb), "cw"], w=["cacc"])
                                for j in range(1, 4):
                                    V(lambda e: e.scalar_tensor_tensor(out=cacc[:], in0=xbuf[:, cb, j:j + 512], scalar=cw[:, boff + cb, j:j + 1], in1=cacc[:], op0=ALU.mult, op1=ALU.add),
                                      r=[("xbuf", cb), "cw", "cacc"], w=["cacc"])
                                A(lambda e: e.activation(out=csil[:], in_=cacc[:], func=AF.Silu, bias=cbi[:, boff + cb:boff + cb + 1], scale=1.0), r=["cacc", "cbi"], w=["csil"])
                                cs = qcount % 2
                                qcount += 1
                                G(lambda e: e.tensor_scalar(out=cstg[cs][:], in0=csil[:], scalar1=sc_, scalar2=None, op0=ALU.mult), r=["csil"], w=[("cstg", cs)])
                                dma(dst[b, cb, :, tg * 512:(tg + 1) * 512], cstg[cs][:], r=[("cstg", cs)], w=["QKMT"])
                        continue
                    for tt in range(NT):
                        pb = pcount % 4
                        pcount += 1
                        for kc in range(8):
                            P(lambda e: e.matmul(psb[pb][:, 0:ncol], lhsT=hT[:, kc, tt * 128:(tt + 1) * 128], rhs=wb[s][:, kc, 0:ncol], start=(kc == 0), stop=(kc == 7)),
                              r=[("wb", s), ("hT", tt)], w=[("psB", pb)])
                        if kind in ("qa", "ka"):
                            A(lambda e: e.activation(out=qf[:], in_=psb[pb][:], func=AF.Copy), r=[("psB", pb)], w=["qf"])
                            qv = qf[:].rearrange("p (g d) -> p g d", g=8)
                            x1 = qv[:, :, 0:8]
                            x2 = qv[:, :, 8:16]
                            cosb = cosf[:, tt * 8:(tt + 1) * 8].unsqueeze(1).to_broadcast([128, 8, 8])
                            sinb = sinf[:, tt * 8:(tt + 1) * 8].unsqueeze(1).to_broadcast([128, 8, 8])
                            V(lambda e: e.tensor_tensor(out=rt[:, 0], in0=x1, in1=cosb, op=ALU.mult), r=["qf", "cst"], w=["rt0"])
                            V(lambda e: e.tensor_tensor(out=rt[:, 1], in0=x2, in1=sinb, op=ALU.mult), r=["qf", "cst"], w=["rt1"])
                            V(lambda e: e.tensor_tensor(out=rt[:, 2], in0=x2, in1=cosb, op=ALU.mult), r=["qf", "cst"], w=["rt2"])
                            V(lambda e: e.tensor_tensor(out=rt[:, 3], in0=x1, in1=sinb, op=ALU.mult), r=["qf", "cst"], w=["rt3"])
                            V(lambda e: e.tensor_tensor(out=x1, in0=rt[:, 0], in1=rt[:, 1], op=ALU.subtract), r=["rt0", "rt1", "rt2", "rt3"], w=["qf"])
                            V(lambda e: e.tensor_tensor(out=x2, in0=rt[:, 2], in1=rt[:, 3], op=ALU.add), r=["rt2", "rt3"], w=["qf"])
                            G(lambda e: e.tensor_tensor(out=sqj[:], in0=qf[:], in1=qf[:], op=ALU.mult), r=["qf"], w=["sqj"])
                            V(lambda e: e.reduce_sum(out=nsq[:], in_=sqj[:].rearrange("p (g d) -> p g d", g=8), axis=AX.X), r=["sqj"], w=["nsq"])
                            ncol0 = (0 if kind == "qa" else 16) + idx * 8
                            V(lambda e: e.tensor_tensor(out=nmx[:, ncol0:ncol0 + 8], in0=nmx[:, ncol0:ncol0 + 8], in1=nsq[:], op=ALU.max), r=["nsq", "nmx"], w=["nmx"])
                            qs = qcount % 2
                            qcount += 1
                            A(lambda e: e.activation(out=qb[qs][:], in_=qf[:], func=AF.Copy, scale=(0.125 if kind == "qa" else 1.0)), r=["qf"], w=[("qb", qs)])
                            ps_ = tt % 2
                            for hd in range(4):
                                P(lambda e: e.transpose(out=ptb[ps_][:, hd * 128:(hd + 1) * 128], in_=qb[qs][:, hd * 128:(hd + 1) * 128], identity=identb[:]),
                                  r=[("qb", qs), "identb"], w=[("ptbB", ps_)])
                            sg = (tt // 4) % 2
                            V(lambda e: e.tensor_copy(out=stage[sg][:, :, (tt % 4) * 128:(tt % 4 + 1) * 128], in_=ptb[ps_][:, 0:512].rearrange("p (h t) -> p h t", h=4)),
                              r=[("ptbB", ps_)], w=[("stage", sg)])
                            if tt % 4 == 3:
                                dstT = QT if kind == "qa" else KT
                                t0 = (tt // 4) * 512
                                dma(dstT[b, idx * 4:(idx + 1) * 4, :, t0:t0 + 512].rearrange("h p t -> p h t"), stage[sg][:], r=[("stage", sg)], w=["QKT"])
                        elif kind in ("va", "vm"):
                            os_ = ocount % 3
                            ocount += 1
                            A(lambda e: e.activation(out=ob[os_][:], in_=psb[pb][:], func=AF.Copy), r=[("psB", pb)], w=[("ob", os_)])
                            dstv = VA if kind == "va" else VM
                            dma(dstv[b, tt * 128:(tt + 1) * 128, idx * 512:(idx + 1) * 512], ob[os_][:], r=[("ob", os_)], w=["VAVM"])
                        elif kind in ("om", "ga", "gm"):
                            os_ = ocount % 3
                            ocount += 1
                            A(lambda e: e.activation(out=ob[os_][:], in_=psb[pb][:], func=AF.Sigmoid), r=[("psB", pb)], w=[("ob", os_)])
                            dsts = {"om": OMS, "ga": GAS, "gm": GMS}[kind]
                            dma(dsts[b, tt * 128:(tt + 1) * 128, idx * 512:(idx + 1) * 512], ob[os_][:], r=[("ob", os_)], w=["SIGS"])
                        else:
                            gs = tt % 2
                            V(lambda e: e.tensor_tensor(out=gz[:], in0=psb[pb][:, 0:16], in1=gbias[:], op=ALU.add), r=[("psB", pb), "gbias0", "gbias1"], w=["gz"])
                            A(lambda e: e.activation(out=gz[:, 8:16], in_=gz[:, 8:16], func=AF.Exp, scale=-1.0), r=["gz"], w=["gz"])
                            A(lambda e: e.activation(out=gz[:, 8:16], in_=gz[:, 8:16], func=AF.Ln, bias=1.0, scale=1.0), r=["gz"], w=["gz"])
                            V(lambda e: e.tensor_copy(out=gt[gs][:, 0:8], in_=gz[:, 0:8]), r=["gz"], w=[("gtB", gs)])
                            V(lambda e: e.tensor_scalar(out=gt[gs][:, 8:16], in0=gz[:, 8:16], scalar1=-1.0, scalar2=None, op0=ALU.mult), r=["gz", ("gtB", gs)], w=[("gtB", gs)])
                            dma(GATES[b, tt * 128:(tt + 1) * 128, :], gt[gs][:], r=[("gtB", gs)], w=["GATES"])
                k.barrier()

            with ExitStack() as ph:
                qmt = sbt(ph, "qmt", [128, 4, S], BF16)
                kmt = sbt(ph, "kmt", [128, 4, S], BF16)
                Cst = sbt(ph, "Cst", [128, 4, 129], F32)
                Cbf = sbt(ph, "Cbf", [128, 4, 129], BF16)
                mlw = sbt(ph, "mlw", [128, D], F32)
                gtl = [sbt(ph, f"gtl{i}", [128, 16], F32) for i in range(2)]
                vmt = [sbt(ph, f"vmt{i}", [128, 8, 129], BF16) for i in range(2)]
                omt = [sbt(ph, f"omt{i}", [128, D], BF16) for i in range(2)]
                gmt = [sbt(ph, f"gmt{i}", [128, D], BF16) for i in range(2)]
                sm = sbt(ph, "sm", [128, 8, 8], F32)
                Kw = sbt(ph, "Kw", [128, 4, 128], BF16)
                Bd = [sbt(ph, f"Bd{i}", [128, 128], F32) for i in range(2)]
                WT = [sbt(ph, f"WT{i}", [128, 128], F32) for i in range(2)]
                PT = [sbt(ph, f"PT{i}", [128, 128], BF16) for i in range(2)]
                Isb = [sbt(ph, f"Isb{i}", [128, 129], F32) for i in range(2)]
                tot = sbt(ph, "tot", [128, 8, 129], F32)
                hn = sbt(ph, "hn", [128, 8, 128], F32)
                hj = sbt(ph, "hj", [128, 8, 128], F32)
                n8 = sbt(ph, "n8", [128, 6, 8], F32)
                ymb = [sbt(ph, f"ymb{i}", [128, D], BF16) for i in range(2)]
                psG = pst(ph, "psG", [128, 512], F32)
                psD = [pst(ph, f"psD{i}", [128, 512], F32) for i in range(2)]
                psS = [pst(ph, f"psS{i}", [128, 512], F32) for i in range(2)]
                psI = pst(ph, "psI", [128, 512], F32)
                psJ = pst(ph, "psJ", [128, 512], F32)
                psK = pst(ph, "psK", [128, 1024], BF16)
                dma(qmt[:], QMT[b].rearrange("c p t -> p c t"), w=["qmt"])
                dma(kmt[:], KMT[b].rearrange("c p t -> p c t"), w=["kmt"])
                dma(mlw[:], ml_nw.partition_broadcast(128), w=["mlw"])
                V(lambda e: e.memset(Cst[:], 0.0), w=["Cst"])
                V(lambda e: e.memset(Cbf[:], 0.0), w=["Cbf"])
                for i in range(2):
                    V(lambda e: e.memset(vmt[i][:, :, 128:129], 1.0), w=[("vmt", i)])
                for tt in range(NT):
                    s = tt % 2
                    tsl = slice(tt * 128, (tt + 1) * 128)
                    dma(gtl[s][:], GATES[b, tsl, :], w=[("gtl", s)])
                    dma(vmt[s][:, :, 0:128], VM[b, tsl, :].rearrange("p (h e) -> p h e", h=8), w=[("vmt", s)])
                    dma(omt[s][:], OMS[b, tsl, :], w=[("omt", s)])
                    dma(gmt[s][:], GMS[b, tsl, :], w=[("gmt", s)])
                    lf = gtl[s][:, 8:16]
                    ig = gtl[s][:, 0:8]
                    P(lambda e: e.matmul(psG[:, 0:8], lhsT=trif, rhs=lf, start=True, stop=True), r=[("gtl", s), "cst"], w=["psG"])
                    P(lambda e: e.matmul(psG[:, 8:16], lhsT=onesf, rhs=lf, start=True, stop=True), r=[("gtl", s), "cst"], w=["psG"])
                    V(lambda e: e.tensor_copy(out=sm[:, 0:2, :], in_=psG[:, 0:16].rearrange("p (a h) -> p a h", a=2)), r=["psG"], w=["sm01"])
                    V(lambda e: e.tensor_tensor(out=sm[:, 2, :], in0=ig, in1=sm[:, 0, :], op=ALU.subtract), r=[("gtl", s), "sm01"], w=["sm2"])
                    V(lambda e: e.tensor_tensor(out=sm[:, 6, :], in0=sm[:, 1, :], in1=sm[:, 2, :], op=ALU.add), r=["sm01", "sm2"], w=["sm6"])
                    A(lambda e: e.activation(out=sm[:, 3, :], in_=sm[:, 0, :], func=AF.Exp), r=["sm01"], w=["sm3"])
                    A(lambda e: e.activation(out=sm[:, 4, :], in_=sm[:, 6, :], func=AF.Exp), r=["sm6"], w=["sm4"])
                    A(lambda e: e.activation(out=sm[:, 5, :], in_=sm[:, 1, :], func=AF.Exp), r=["sm01"], w=["sm5"])
                    for cb in range(4):
                        P(lambda e: e.transpose(out=psK[:, cb * 128:(cb + 1) * 128], in_=kmt[:, cb, tsl], identity=identb[:]), r=["kmt", "identb"], w=["psK"])
                    V(lambda e: e.tensor_tensor(out=Kw[:].rearrange("p c (h d) -> p (c h) d", h=2), in0=psK[:, 0:512].rearrange("p (g d) -> p g d", g=8),
                                                in1=sm[:, 4, :].unsqueeze(2).to_broadcast([128, 8, 64]), op=ALU.mult), r=["psK", "sm4"], w=["Kw"])
                    for h in range(8):
                        cb, r0 = h // 2, (h % 2) * 64
                        hs = h % 2
                        G(lambda e: e.tensor_scalar(out=Bd[hs][:], in0=identf, scalar1=sm[:, 0, h:h + 1], scalar2=None, op0=ALU.mult), r=["cst", "sm01"], w=[("Bd", hs)])
                        P(lambda e: e.matmul(psD[hs][:, 0:128], lhsT=onesf, rhs=Bd[hs][:], start=True, stop=False), r=[("Bd", hs), "cst"], w=[("psD", hs)])
                        P(lambda e: e.matmul(psD[hs][:, 0:128], lhsT=identf, rhs=negmf, start=False, stop=True), r=["cst"], w=[("psD", hs)])
                        A(lambda e: e.activation(out=WT[hs][:], in_=psD[hs][:, 0:128], func=AF.Exp, bias=sm[:, 2, h:h + 1], scale=1.0), r=[("psD", hs), "sm2"], w=[("WT", hs)])
                        P(lambda e: e.matmul(psS[hs][:, 0:128], lhsT=kmt[r0:r0 + 64, cb, tsl], rhs=qmt[r0:r0 + 64, cb, tsl], start=True, stop=True), r=["kmt", "qmt"], w=[("psS", hs)])
                        V(lambda e: e.tensor_tensor(out=PT[hs][:], in0=psS[hs][:, 0:128], in1=WT[hs][:], op=ALU.mult), r=[("psS", hs), ("WT", hs)], w=[("PT", hs)])
                        P(lambda e: e.matmul(psI[:, 0:129], lhsT=PT[hs][:], rhs=vmt[s][:, h, :], start=True, stop=True), r=[("PT", hs), ("vmt", s)], w=["psI"])
                        P(lambda e: e.matmul(psJ[:, 0:129], lhsT=qmt[r0:r0 + 64, cb, tsl], rhs=Cbf[r0:r0 + 64, cb, :], start=True, stop=True), r=["qmt", ("Cbf", h)], w=["psJ"])
                        A(lambda e: e.activation(out=Isb[hs][:], in_=psI[:, 0:129], func=AF.Copy), r=["psI"], w=[("Isb", hs)])
                        V(lambda e: e.scalar_tensor_tensor(out=tot[:, h, :], in0=psJ[:, 0:129], scalar=sm[:, 3, h:h + 1], in1=Isb[hs][:], op0=ALU.mult, op1=ALU.add),
                          r=["psJ", "sm3", ("Isb", hs)], w=[("tot", h)])
                        P(lambda e: e.matmul(psJ[:, 256:385], lhsT=Kw[:, cb, :], rhs=vmt[s][:, h, :], start=True, stop=True), r=["Kw", ("vmt", s), "psJ"], w=["psJ"])
                        V(lambda e: e.scalar_tensor_tensor(out=Cst[r0:r0 + 64, cb, :], in0=Cst[r0:r0 + 64, cb, :], scalar=sm[r0:r0 + 64, 5, h:h + 1], in1=psJ[r0:r0 + 64, 256:385], op0=ALU.mult, op1=ALU.add),
                          r=["psJ", "sm5", ("Cst", h)], w=[("Cst", h)])
                        G(lambda e: e.tensor_copy(out=Cbf[r0:r0 + 64, cb, :], in_=Cst[r0:r0 + 64, cb, :]), r=[("Cst", h)], w=[("Cbf", h)])
                    allt = [("tot", h) for h in range(8)]
                    den = tot[:, :, 128]
                    V(lambda e: e.scalar_tensor_tensor(out=n8[:, 0, :], in0=den, scalar=-1.0, in1=den, op0=ALU.mult, op1=ALU.max), r=allt, w=["n80"])
                    V(lambda e: e.tensor_scalar(out=n8[:, 0, :], in0=n8[:, 0, :], scalar1=1.0, scalar2=None, op0=ALU.max), r=["n80"], w=["n80"])
                    V(lambda e: e.reciprocal(out=n8[:, 1, :], in_=n8[:, 0, :]), r=["n80"], w=["n81"])
                    V(lambda e: e.tensor_tensor(out=hn[:], in0=tot[:, :, 0:128], in1=n8[:, 1, :].unsqueeze(2).to_broadcast([128, 8, 128]), op=ALU.mult), r=allt + ["n81"], w=["hn"])
                    G(lambda e: e.tensor_tensor(out=hj[:], in0=hn[:], in1=hn[:], op=ALU.mult), r=["hn"], w=["hj"])
                    V(lambda e: e.reduce_sum(out=n8[:, 2, :], in_=hj[:], axis=AX.X), r=["hj"], w=["n82"])
                    V(lambda e: e.tensor_scalar(out=n8[:, 3, :], in0=n8[:, 2, :], scalar1=1.0 / 128, scalar2=EPS, op0=ALU.mult, op1=ALU.add), r=["n82"], w=["n83"])
                    A(lambda e: e.activation(out=n8[:, 4, :], in_=n8[:, 3, :], func=AF.Sqrt), r=["n83"], w=["n84"])
                    V(lambda e: e.reciprocal(out=n8[:, 5, :], in_=n8[:, 4, :]), r=["n84"], w=["n85"])
                    V(lambda e: e.tensor_tensor(out=hj[:], in0=hn[:], in1=n8[:, 5, :].unsqueeze(2).to_broadcast([128, 8, 128]), op=ALU.mult), r=["hn", "n85", "hj"], w=["hj"])
                    hjf = hj[:].rearrange("p h e -> p (h e)")
                    G(lambda e: e.tensor_tensor(out=hjf, in0=hjf, in1=mlw[:], op=ALU.mult), r=["hj", "mlw"], w=["hj"])
                    V(lambda e: e.tensor_tensor(out=hjf, in0=hjf, in1=omt[s][:], op=ALU.mult), r=["hj", ("omt", s)], w=["hj"])
                    G(lambda e: e.tensor_tensor(out=ymb[s][:], in0=hjf, in1=gmt[s][:], op=ALU.mult), r=["hj", ("gmt", s)], w=[("ymb", s)])
                    dma(YM[b, tsl, :], ymb[s][:], r=[("ymb", s)], w=["YM"])
                k.barrier()

            with ExitStack() as ph:
                qts = [sbt(ph, f"qts{i}", [128, S], BF16) for i in range(2)]
                kts = [sbt(ph, f"kts{i}", [128, S], BF16) for i in range(2)]
                vh = [sbt(ph, f"vh{i}", [128, NT, 129], BF16) for i in range(2)]
                negMb = sbt(ph, "negMb", [128, 16], F32)
                nw = sbt(ph, "nw", [16, 8], F32)
                dg = sbt(ph, "dg", [16, 16], F32)
                swb = sbt(ph, "swb", [128, 128], F32)
                PTa = [[sbt(ph, f"PTa{c}{i}", [128, 512], BF16) for i in range(2)] for c in range(2)]
                rr = sbt(ph, "rr", [128, 8], F32)
                o1 = sbt(ph, "o1", [128, 128], F32)
                o2 = sbt(ph, "o2", [128, 128], F32)
                oj = sbt(ph, "oj", [128, 128], F32)
                gat = [sbt(ph, f"gat{i}", [128, 128], BF16) for i in range(2)]
                ymt = [sbt(ph, f"ymt{i}", [128, 128], BF16) for i in range(2)]
                mb = [sbt(ph, f"mb{i}", [128, 128], BF16) for i in range(2)]
                mstage = [sbt(ph, f"mstage{i}", [128, 512], BF16) for i in range(2)]
                psS = [[pst(ph, f"psSa{c}{i}", [128, 512], F32) for i in range(2)] for c in range(2)]
                psA = [pst(ph, f"psA{i}", [128, 512], F32) for i in range(3)]
                psT = pst(ph, "psT", [128, 1024], BF16)
                P(lambda e: e.transpose(out=psA[0][0:16, 0:128], in_=nmx[:, 0:16], identity=identf), r=["nmx", "cst"], w=[("psA", 0)])
                P(lambda e: e.transpose(out=psA[0][0:16, 128:256], in_=nmx[:, 16:32], identity=identf), r=["nmx", "cst"], w=[("psA", 0)])
                V(lambda e: e.reduce_max(out=nw[:, 0:2], in_=psA[0][0:16, 0:256].rearrange("p (a t) -> p a t", a=2), axis=AX.X), r=[("psA", 0)], w=["nw0"])
                V(lambda e: e.tensor_tensor(out=nw[:, 2:3], in0=nw[:, 0:1], in1=nw[:, 1:2], op=ALU.mult), r=["nw0"], w=["nw2"])
                A(lambda e: e.activation(out=nw[:, 3:4], in_=nw[:, 2:3], func=AF.Sqrt), r=["nw2"], w=["nw3"])
                V(lambda e: e.tensor_scalar(out=nw[:, 4:5], in0=nw[:, 3:4], scalar1=-0.125, scalar2=None, op0=ALU.mult), r=["nw3"], w=["nw4"])
                V(lambda e: e.tensor_scalar(out=dg[:], in0=identf[0:16, 0:16], scalar1=nw[:, 4:5], scalar2=None, op0=ALU.mult), r=["nw4", "cst"], w=["dg"])
                P(lambda e: e.matmul(psA[1][:, 0:16], lhsT=onesf[0:16, :], rhs=dg[:], start=True, stop=True), r=["dg", "cst"], w=[("psA", 1)])
                V(lambda e: e.tensor_copy(out=negMb[:], in_=psA[1][:, 0:16]), r=[("psA", 1)], w=["negMb"])
                dma(swb[:], subln_w.partition_broadcast(128), w=["swb"])
                V(lambda e: e.tensor_scalar(out=swb[:], in0=swb[:], scalar1=(1.0 - lam_init), scalar2=None, op0=ALU.mult), r=["swb"], w=["swb"])
                for i in range(2):
                    V(lambda e: e.memset(vh[i][:, :, 128:129], 1.0), w=[("vh", i)])
                fcount = 0
                for hd in range(8):
                    s = hd % 2
                    dma(qts[s][:], QT[b, hd], w=[("qts", s)])
                    dma(kts[s][:], KT[b, hd], w=[("kts", s)])
                    for t8 in range(0, NT, 8):
                        te = min(NT, t8 + 8)
                        dma(vh[s][:, t8:te, 0:128], VA[b].rearrange("(tt p) c -> p tt c", p=128)[:, t8:te, hd * 128:(hd + 1) * 128], w=[("vh", s)])
                    blk = 0
                    for g in range(NG):
                        nkt = 4 * g + 4
                        started = [False, False, False]
                        for kt in range(nkt):
                            bs = blk % 2
                            blk += 1
                            for c in range(2):
                                P(lambda e: e.matmul(psS[c][bs][:], lhsT=kts[s][c * 64:(c + 1) * 64, kt * 128:(kt + 1) * 128], rhs=qts[s][c * 64:(c + 1) * 64, g * 512:(g + 1) * 512], start=True, stop=True),
                                  r=[("kts", s), ("qts", s)], w=[("psSa", c, bs)])
                                A(lambda e: e.activation(out=PTa[c][bs][:], in_=psS[c][bs][:], func=AF.Exp, bias=negMb[:, hd * 2 + c:hd * 2 + c + 1], scale=1.0),
                                  r=[("psSa", c, bs), "negMb"], w=[("PTa", c, bs)])
                            for qi in range(4):
                                qt_ = 4 * g + qi
                                if kt > qt_:
                                    continue
                                for c in range(2):
                                    if kt == qt_:
                                        eng = V if c == 0 else G
                                        eng(lambda e: e.tensor_tensor(out=PTa[c][bs][:, qi * 128:(qi + 1) * 128], in0=PTa[c][bs][:, qi * 128:(qi + 1) * 128], in1=cmb[:], op=ALU.mult),
                                            r=[("PTa", c, bs), "cmb"], w=[("PTa", c, bs)])
                                    ai = c * 4 + qi
                                    bank, slot = ai // 3, ai % 3
                                    st_ = not started[bank]
                                    started[bank] = True
                                    P(lambda e: e.matmul(psA[bank][:, slot * 129:(slot + 1) * 129], lhsT=PTa[c][bs][:, qi * 128:(qi + 1) * 128], rhs=vh[s][:, kt, :], start=st_, stop=(kt == qt_), skip_group_check=True),
                                      r=[("PTa", c, bs), ("vh", s)], w=[("psA", bank)])
                        ms = g % 2
                        for qi in range(4):
                            qt_ = 4 * g + qi
                            tsl = slice(qt_ * 128, (qt_ + 1) * 128)
                            fs = fcount % 2
                            fcount += 1
                            dma(gat[fs][:], GAS[b, tsl, hd * 128:(hd + 1) * 128], w=[("gat", fs)])
                            dma(ymt[fs][:], YM[b, tsl, hd * 128:(hd + 1) * 128], w=[("ymt", fs)])
                            a1 = psA[qi // 3][:, (qi % 3) * 129:(qi % 3 + 1) * 129]
                            a2i = 4 + qi
                            a2 = psA[a2i // 3][:, (a2i % 3) * 129:(a2i % 3 + 1) * 129]
                            rk = [("psA", qi // 3), ("psA", a2i // 3)]
                            V(lambda e: e.reciprocal(out=rr[:, 0:1], in_=a1[:, 128:129]), r=rk, w=["rr0"])
                            V(lambda e: e.reciprocal(out=rr[:, 1:2], in_=a2[:, 128:129]), r=rk, w=["rr1"])
                            V(lambda e: e.tensor_tensor(out=rr[:, 2:3], in0=rr[:, 1:2], in1=lamt[:, 0:1], op=ALU.mult), r=["rr1", "lamt"], w=["rr2"])
                            A(lambda e: e.activation(out=o1[:], in_=a1[:, 0:128], func=AF.Copy, scale=rr[:, 0:1]), r=rk + ["rr0"], w=["o1"])
                            V(lambda e: e.scalar_tensor_tensor(out=o2[:], in0=a2[:, 0:128], scalar=rr[:, 2:3], in1=o1[:], op0=ALU.mult, op1=ALU.add), r=rk + ["rr2", "o1"], w=["o2"])
                            A(lambda e: e.activation(out=oj[:], in_=o2[:], func=AF.Square, accum_out=rr[:, 3:4]), r=["o2"], w=["oj", "rr3"])
                            V(lambda e: e.tensor_scalar(out=rr[:, 4:5], in0=rr[:, 3:4], scalar1=1.0 / 128, scalar2=EPS, op0=ALU.mult, op1=ALU.add), r=["rr3"], w=["rr4"])
                            A(lambda e: e.activation(out=rr[:, 5:6], in_=rr[:, 4:5], func=AF.Sqrt), r=["rr4"], w=["rr5"])
                            V(lambda e: e.reciprocal(out=rr[:, 6:7], in_=rr[:, 5:6]), r=["rr5"], w=["rr6"])
                            V(lambda e: e.scalar_tensor_tensor(out=oj[:], in0=o2[:], scalar=rr[:, 6:7], in1=swb[:], op0=ALU.mult, op1=ALU.mult), r=["o2", "rr6", "swb", "oj"], w=["oj"])
                            G(lambda e: e.tensor_tensor(out=oj[:], in0=oj[:], in1=gat[fs][:], op=ALU.mult), r=["oj", ("gat", fs)], w=["oj"])
                            G(lambda e: e.tensor_tensor(out=mb[fs][:], in0=oj[:], in1=ymt[fs][:], op=ALU.add), r=["oj", ("ymt", fs)], w=[("mb", fs)])
                            P(lambda e: e.transpose(out=psT[:, qi * 128:(qi + 1) * 128], in_=mb[fs][:], identity=identb[:]), r=[("mb", fs), "identb"], w=["psT"])
                        V(lambda e: e.tensor_copy(out=mstage[ms][:], in_=psT[:, 0:512]), r=["psT"], w=[("mstage", ms)])
                        dma(MT[b, hd, :, g * 512:(g + 1) * 512], mstage[ms][:], r=[("mstage", ms)], w=["MT"])
                k.barrier()

        wq_v = peer_wq.rearrange("(kc p) n -> p kc n", p=128)
        wo_v = w_out.rearrange("(kc p) n -> p kc n", p=128)
        with ExitStack() as ph:
            wob = sbt(ph, "wob", [128, 8, D], BF16)
            wqb = sbt(ph, "wqb", [128, 8, 2048], BF16)
            keyT = sbt(ph, "keyT", [128, 16, 128], BF16)
            psT = [pst(ph, f"psTE{i}", [128, 1024], BF16) for i in range(2)]
            ph2 = ExitStack()
            wtmp = [sbt(ph2, f"wtmp{i}", [128, 8, 512], F32) for i in range(2)]
            ktmp = sbt(ph2, "ktmp", [128, 16, 128], F32)
            ktb = sbt(ph2, "ktb", [128, 16, 128], BF16)
            wi = 0
            for n0 in range(0, D, 512):
                s = wi % 2
                wi += 1
                dma(wtmp[s][:], wo_v[:, :, n0:n0 + 512], w=[("wtmp", s)])
                V(lambda e: e.tensor_copy(out=wob[:, :, n0:n0 + 512], in_=wtmp[s][:]), r=[("wtmp", s)], w=["wob"])
            for n0 in range(0, 2048, 512):
                s = wi % 2
                wi += 1
                dma(wtmp[s][:], wq_v[:, :, n0:n0 + 512], w=[("wtmp", s)])
                G(lambda e: e.tensor_copy(out=wqb[:, :, n0:n0 + 512], in_=wtmp[s][:]), r=[("wtmp", s)], w=["wqb"])
            dma(ktmp[:], peer_keys.rearrange("g n d -> n g d"), w=["ktmp"])
            V(lambda e: e.tensor_copy(out=ktb[:], in_=ktmp[:]), r=["ktmp"], w=["ktb"])
            for g8 in range(2):
                for j in range(8):
                    gi = g8 * 8 + j
                    P(lambda e: e.transpose(out=psT[g8][:, j * 128:(j + 1) * 128], in_=ktb[:, gi, :], identity=identb[:]), r=["ktb", "identb"], w=[("psTE", g8)])
                V(lambda e: e.tensor_copy(out=keyT[:, g8 * 8:(g8 + 1) * 8, :], in_=psT[g8][:].rearrange("p (g n) -> p g n", g=8)), r=[("psTE", g8)], w=["keyT"])
            k.barrier()
            ph2.close()
            gt1 = sbt(ph, "gt1", [128, D], F32)
            A2 = sbt(ph, "A2", [128, D], F32)
            B2 = sbt(ph, "B2", [128, D], F32)
            w2b = sbt(ph, "w2b", [128, D], F32)
            mt = [sbt(ph, f"mt{i}", [128, 8, 128], BF16) for i in range(2)]
            xt = [sbt(ph, f"xtE{i}", [128, D], F32) for i in range(2)]
            x1 = [sbt(ph, f"x1E{i}", [128, D], F32) for i in range(2)]
            junk = sbt(ph, "junkE", [128, D], BF16)
            hx = sbt(ph, "hxE", [128, D], F32)
            hb = sbt(ph, "hbE", [128, D], BF16)
            h2t = [sbt(ph, f"h2t{i}", [128, 8, 128], BF16) for i in range(2)]
            qpt = sbt(ph, "qpt", [128, 16, 128], BF16)
            sc = sbt(ph, "sc", [128, 16, 128], F32)
            wk = sbt(ph, "wkE", [128, 16, 128], F32)
            v16 = sbt(ph, "v16", [128, 16, 16], F32)
            i16u = sbt(ph, "i16u", [128, 16, 16], U32)
            i16v = sbt(ph, "i16v", [128, 16, 16], F32)
            cand = sbt(ph, "cand", [128, 8, 256], F32)
            cwk = sbt(ph, "cwk", [128, 8, 256], F32)
            c16 = sbt(ph, "c16", [128, 8, 16], F32)
            p16u = sbt(ph, "p16u", [128, 8, 16], U32)
            p16 = sbt(ph, "p16", [128, 8, 16], F32)
            d1 = sbt(ph, "d1", [128, 8, 16, 16], F32)
            d2 = sbt(ph, "d2", [128, 8, 16, 16], F32)
            ar = sbt(ph, "ar", [128, 8, 16], F32)
            br = sbt(ph, "br", [128, 8, 16], F32)
            igt = [sbt(ph, f"igt{i}", [128, 3, 128], F32) for i in range(2)]
            st4 = sbt(ph, "st4E", [128, 16], F32)
            psO = [pst(ph, f"psO{i}", [128, 512], F32) for i in range(2)]
            psQ = [pst(ph, f"psQ{i}", [128, 512], F32) for i in range(2)]
            psX = [pst(ph, f"psX{i}", [128, 512], F32) for i in range(2)]
            dma(w2b[:], norm2_w.partition_broadcast(128), w=["w2b"])
            for b in range(NB):
                dma(gt1[:], MOD[b:b + 1, 2 * D:3 * D].partition_broadcast(128), r=["MOD"], w=["gt1"])
                dma(B2[:], MOD[b:b + 1, 3 * D:4 * D].partition_broadcast(128), r=["MOD"], w=["B2"])
                dma(A2[:], MOD[b:b + 1, 4 * D:5 * D].partition_broadcast(128), r=["MOD"], w=["A2"])
                V(lambda e: e.scalar_tensor_tensor(out=A2[:], in0=A2[:], scalar=1.0, in1=w2b[:], op0=ALU.add, op1=ALU.mult), r=["A2", "w2b"], w=["A2"])
                for tt in range(NT):
                    s = tt % 2
                    tsl = slice(tt * 128, (tt + 1) * 128)
                    dma(mt[s][:], MT[b, :, :, tsl].rearrange("h p t -> p h t"), r=["MT"], w=[("mt", s)])
                    dma(xt[s][:], x[b, tsl, :], w=[("xtE", s)])
                    for nh in range(2):
                        for kc in range(8):
                            P(lambda e: e.matmul(psO[nh][:], lhsT=mt[s][:, kc, :], rhs=wob[:, kc, nh * 512:(nh + 1) * 512], start=(kc == 0), stop=(kc == 7)),
                              r=[("mt", s), "wob"], w=[("psO", nh)])
                        V(lambda e: e.tensor_tensor(out=x1[s][:, nh * 512:(nh + 1) * 512], in0=psO[nh][:], in1=gt1[:, nh * 512:(nh + 1) * 512], op=ALU.mult), r=[("psO", nh), "gt1"], w=[("x1E", s)])
                    G(lambda e: e.tensor_tensor(out=x1[s][:], in0=x1[s][:], in1=xt[s][:], op=ALU.add), r=[("x1E", s), ("xtE", s)], w=[("x1E", s)])
                    dma(X1[b, tsl, :], x1[s][:], r=[("x1E", s)], w=["X1"])
                    A(lambda e: e.activation(out=junk[:], in_=x1[s][:], func=AF.Square, accum_out=st4[:, 0:1]), r=[("x1E", s)], w=["junkE", "sE0"])
                    V(lambda e: e.tensor_scalar(out=st4[:, 1:2], in0=st4[:, 0:1], scalar1=1.0 / D, scalar2=EPS, op0=ALU.mult, op1=ALU.add), r=["sE0"], w=["sE1"])
                    A(lambda e: e.activation(out=st4[:, 2:3], in_=st4[:, 1:2], func=AF.Sqrt), r=["sE1"], w=["sE2"])
                    V(lambda e: e.reciprocal(out=st4[:, 3:4], in_=st4[:, 2:3]), r=["sE2"], w=["sE3"])
                    V(lambda e: e.scalar_tensor_tensor(out=hx[:], in0=x1[s][:], scalar=st4[:, 3:4], in1=A2[:], op0=ALU.mult, op1=ALU.mult), r=[("x1E", s), "sE3", "A2"], w=["hxE"])
                    G(lambda e: e.tensor_tensor(out=hb[:], in0=hx[:], in1=B2[:], op=ALU.add), r=["hxE", "B2"], w=["hbE"])
                    for kc in range(8):
                        P(lambda e: e.transpose(out=psT[0][:, kc * 128:(kc + 1) * 128], in_=hb[:, kc * 128:(kc + 1) * 128], identity=identb[:]), r=["hbE", "identb"], w=[("psTE", 0)])
                    A(lambda e: e.activation(out=h2t[s][:], in_=psT[0][:].rearrange("p (k t) -> p k t", k=8), func=AF.Copy), r=[("psTE", 0)], w=[("h2t", s)])
                    dma(H2T[b, :, :, tsl].rearrange("k p t -> p k t"), h2t[s][:], r=[("h2t", s)], w=["H2T"])
                    for gq in range(4):
                        pq = gq % 2
                        for j in range(4):
                            gi = gq * 4 + j
                            for kc in range(8):
                                P(lambda e: e.matmul(psQ[pq][:, j * 128:(j + 1) * 128], lhsT=wqb[:, kc, gi * 128:(gi + 1) * 128], rhs=h2t[s][:, kc, :], start=(kc == 0), stop=(kc == 7)),
                                  r=["wqb", ("h2t", s)], w=[("psQ", pq)])
                        A(lambda e: e.activation(out=qpt[:, gq * 4:(gq + 1) * 4, :], in_=psQ[pq][:].rearrange("p (g t) -> p g t", g=4), func=AF.Copy), r=[("psQ", pq)], w=["qpt"])
                    for gq in range(4):
                        px = gq % 2
                        for j in range(4):
                            gi = gq * 4 + j
                            P(lambda e: e.matmul(psX[px][:, j * 128:(j + 1) * 128], lhsT=qpt[:, gi, :], rhs=keyT[:, gi, :], start=True, stop=True), r=["qpt", "keyT"], w=[("psX", px)])
                        A(lambda e: e.activation(out=sc[:, gq * 4:(gq + 1) * 4, :], in_=psX[px][:].rearrange("p (g n) -> p g n", g=4), func=AF.Copy), r=[("psX", px)], w=["sc"])
                    for gi in range(16):
                        V(lambda e: e.max(out=v16[:, gi, 0:8], in_=sc[:, gi, :]), r=["sc"], w=["v16"])
                        V(lambda e: e.max_index(out=i16u[:, gi, 0:8], in_max=v16[:, gi, 0:8], in_values=sc[:, gi, :]), r=["sc", "v16"], w=["i16u"])
                        V(lambda e: e.match_replace(out=wk[:, gi, :], in_to_replace=v16[:, gi, 0:8], in_values=sc[:, gi, :], imm_value=-1e30), r=["sc", "v16"], w=["wkE"])
                        V(lambda e: e.max(out=v16[:, gi, 8:16], in_=wk[:, gi, :]), r=["wkE"], w=["v16"])
                        V(lambda e: e.max_index(out=i16u[:, gi, 8:16], in_max=v16[:, gi, 8:16], in_values=wk[:, gi, :]), r=["wkE", "v16"], w=["i16u"])
                    V(lambda e: e.tensor_copy(out=i16v[:], in_=i16u[:]), r=["i16u"], w=["i16v"])
                    v16v = v16[:].rearrange("p (h q) k -> p h q k", q=2)
                    i16vv = i16v[:].rearrange("p (h q) k -> p h q k", q=2)
                    V(lambda e: e.tensor_tensor(out=cand[:].rearrange("p h (a b) -> p h a b", a=16), in0=v16v[:, :, 0, :].unsqueeze(3).to_broadcast([128, 8, 16, 16]),
                                                in1=v16v[:, :, 1, :].unsqueeze(2).to_broadcast([128, 8, 16, 16]), op=ALU.add), r=["v16"], w=["cand"])
                    for h in range(8):
                        V(lambda e: e.max(out=c16[:, h, 0:8], in_=cand[:, h, :]), r=["cand"], w=["c16"])
                        V(lambda e: e.max_index(out=p16u[:, h, 0:8], in_max=c16[:, h, 0:8], in_values=cand[:, h, :]), r=["cand", "c16"], w=["p16u"])
                        V(lambda e: e.match_replace(out=cwk[:, h, :], in_to_replace=c16[:, h, 0:8], in_values=cand[:, h, :], imm_value=-1e30), r=["cand", "c16"], w=["cwk"])
                        V(lambda e: e.max(out=c16[:, h, 8:16], in_=cwk[:, h, :]), r=["cwk"], w=["c16"])
                        V(lambda e: e.max_index(out=p16u[:, h, 8:16], in_max=c16[:, h, 8:16], in_values=cwk[:, h, :]), r=["cwk", "c16"], w=["p16u"])
                    V(lambda e: e.tensor_copy(out=p16[:], in_=p16u[:]), r=["p16u"], w=["p16"])
                    p16b = p16[:].unsqueeze(3).to_broadcast([128, 8, 16, 16])
                    a16b = a16f.unsqueeze(1).unsqueeze(1).to_broadcast([128, 8, 16, 16])
                    i16b = i16f.unsqueeze(1).unsqueeze(1).to_broadcast([128, 8, 16, 16])
                    V(lambda e: e.tensor_tensor(out=d1[:], in0=p16b, in1=a16b, op=ALU.subtract), r=["p16", "cst"], w=["d1"])
                    V(lambda e: e.tensor_scalar(out=d2[:], in0=d1[:], scalar1=0.0, scalar2=None, op0=ALU.is_ge), r=["d1"], w=["d2"])
                    V(lambda e: e.tensor_scalar(out=d1[:], in0=d1[:], scalar1=15.5, scalar2=None, op0=ALU.is_lt), r=["d1", "d2"], w=["d1"])
                    V(lambda e: e.tensor_tensor(out=d1[:], in0=d1[:], in1=d2[:], op=ALU.mult), r=["d1", "d2"], w=["d1"])
                    G(lambda e: e.tensor_tensor(out=d2[:], in0=d1[:], in1=a16b, op=ALU.mult), r=["d1", "cst"], w=["d2"])
                    V(lambda e: e.reduce_sum(out=ar[:], in_=d2[:], axis=AX.X), r=["d2"], w=["ar"])
                    G(lambda e: e.tensor_tensor(out=d2[:], in0=d1[:], in1=i16vv[:, :, 0, :].unsqueeze(2).to_broadcast([128, 8, 16, 16]), op=ALU.mult), r=["d1", "i16v", "ar"], w=["d2"])
                    V(lambda e: e.reduce_sum(out=igt[s][:, 0, :].rearrange("p (h r) -> p h r", h=8), in_=d2[:], axis=AX.X), r=["d2"], w=[("igt", s)])
                    V(lambda e: e.tensor_tensor(out=br[:], in0=p16[:], in1=ar[:], op=ALU.subtract), r=["p16", "ar"], w=["br"])
                    V(lambda e: e.tensor_tensor(out=d1[:], in0=br[:].unsqueeze(3).to_broadcast([128, 8, 16, 16]), in1=i16b, op=ALU.is_equal), r=["br", "cst", "d2"], w=["d1"])
                    G(lambda e: e.tensor_tensor(out=d2[:], in0=d1[:], in1=i16vv[:, :, 1, :].unsqueeze(2).to_broadcast([128, 8, 16, 16]), op=ALU.mult), r=["d1", "i16v"], w=["d2"])
                    V(lambda e: e.reduce_sum(out=igt[s][:, 1, :].rearrange("p (h r) -> p h r", h=8), in_=d2[:], axis=AX.X), r=["d2", ("igt", s)], w=[("igt", s)])
                    V(lambda e: e.tensor_tensor(out=ar[:], in0=c16[:], in1=c16[:, :, 0:1].to_broadcast([128, 8, 16]), op=ALU.subtract), r=["c16", "br"], w=["ar"])
                    A(lambda e: e.activation(out=ar[:], in_=ar[:], func=AF.Exp), r=["ar"], w=["ar"])
                    V(lambda e: e.reduce_sum(out=st4[:, 8:16], in_=ar[:], axis=AX.X), r=["ar"], w=["sE8"])
                    V(lambda e: e.reciprocal(out=st4[:, 8:16], in_=st4[:, 8:16]), r=["sE8"], w=["sE8"])
                    V(lambda e: e.tensor_tensor(out=igt[s][:, 2, :].rearrange("p (h r) -> p h r", h=8), in0=ar[:], in1=st4[:, 8:16].unsqueeze(2).to_broadcast([128, 8, 16]), op=ALU.mult),
                      r=["ar", "sE8", ("igt", s)], w=[("igt", s)])
                    dma(IG[b, tsl, :, :], igt[s][:], r=[("igt", s)], w=["IG"])
            k.barrier()

        TG = 256
        NGR = T // TG
        X1f = X1.rearrange("b s d -> (b s) d")
        outf = out.rearrange("b s d -> (b s) d")
        IGf = IG.rearrange("b s a r -> (b s) a r")
        with ExitStack() as ph:
            Gm = sbt(ph, "Gm", [128, 128, TG], BF16)
            ust = [sbt(ph, f"ust{i}", [128, 4, 1024], BF16) for i in range(3)]
            vst = [sbt(ph, f"vst{i}", [128, 4, 1024], BF16) for i in range(3)]
            h2 = sbt(ph, "h2F", [128, 8, TG], BF16)
            igl = sbt(ph, "igl", [128, 2, 3, 128], F32)
            igT = sbt(ph, "igT", [128, 3, TG], F32)
            ohA = [sbt(ph, f"ohA{i}", [128, 16, 128], BF16) for i in range(2)]
            ohB = [sbt(ph, f"ohB{i}", [128, 16, 128], BF16) for i in range(2)]
            ohT = sbt(ph, "ohT", [128, 16, 128], BF16)
            act = [sbt(ph, f"actF{i}", [128, TG], BF16) for i in range(2)]
            ga = [sbt(ph, f"gaF{i}", [128, TG], BF16) for i in range(2)]
            gt2 = sbt(ph, "gt2", [128, D], F32)
            fwb = sbt(ph, "fwb", [128, D], F32)
            x1t = sbt(ph, "x1t", [128, 2, D], F32)
            yo = sbt(ph, "yo", [128, D], F32)
            junk = sbt(ph, "junkF", [128, D], BF16)
            st4 = sbt(ph, "st4F", [128, 8], F32)
            oo = [sbt(ph, f"oo{i}", [128, D], F32) for i in range(2)]
            psY = [pst(ph, f"psY{i}", [128, 512], F32) for i in range(4)]
            psSc = [pst(ph, f"psSc{i}", [128, 512], F32) for i in range(2)]
            psGb = [pst(ph, f"psGb{i}", [128, 512], F32) for i in range(2)]
            dma(fwb[:], fin_w.partition_broadcast(128), w=["fwb"])
            UTv = UT.rearrange("i p f -> p i f")
            VBv = VB.rearrange("i p f -> p i f")
            scount = 0
            gcount = 0
            for gr in range(NGR):
                t0 = gr * TG
                b = t0 // S
                s0 = t0 % S
                if s0 == 0:
                    dma(gt2[:], MOD[b:b + 1, 5 * D:6 * D].partition_broadcast(128), r=["MOD"], w=["gt2"])
                dma(h2[:], H2T[b, :, :, s0:s0 + TG].rearrange("k p t -> p k t"), w=["h2F"])
                dma(igl[:].rearrange("p j a r -> p j (a r)"), IGf[t0:t0 + TG].rearrange("(j p) a r -> p j (a r)", p=128), w=["igl"])
                dma(x1t[:], X1f[t0:t0 + TG, :].rearrange("(j p) d -> p j d", p=128), w=["x1t"])
                for j in range(2):
                    for a in range(3):
                        P(lambda e: e.transpose(out=psSc[0][:, 0:128], in_=igl[:, j, a, :], identity=identf),
                          r=["igl", "cst"], w=[("psSc", 0)])
                        V(lambda e: e.tensor_copy(out=igT[:, a, j * 128:(j + 1) * 128], in_=psSc[0][:, 0:128]), r=[("psSc", 0)], w=["igT"])
                for sub in range(TG // 16):
                    os_ = sub % 2
                    tq = slice(sub * 16, (sub + 1) * 16)
                    iob = iotaf.unsqueeze(1).to_broadcast([128, 16, 128])
                    V(lambda e: e.tensor_tensor(out=ohA[os_][:], in0=iob, in1=igT[:, 0, tq].unsqueeze(2).to_broadcast([128, 16, 128]), op=ALU.is_equal), r=["cst", "igT"], w=[("ohA", os_)])
                    V(lambda e: e.tensor_tensor(out=ohT[:], in0=iob, in1=igT[:, 1, tq].unsqueeze(2).to_broadcast([128, 16, 128]), op=ALU.is_equal), r=["cst", "igT"], w=["ohT"])
                    G(lambda e: e.tensor_tensor(out=ohB[os_][:], in0=ohT[:], in1=igT[:, 2, tq].unsqueeze(2).to_broadcast([128, 16, 128]), op=ALU.mult), r=["ohT", "igT"], w=[("ohB", os_)])
                    for q4 in range(4):
                        pg = gcount % 2
                        gcount += 1
                        for j in range(4):
                            tl = q4 * 4 + j
                            P(lambda e: e.matmul(psGb[pg][:, j * 128:(j + 1) * 128], lhsT=ohB[os_][:, tl, :], rhs=ohA[os_][:, tl, :], start=True, stop=True),
                              r=[("ohA", os_), ("ohB", os_)], w=[("psGb", pg)])
                        tb = sub * 16 + q4 * 4
                        A(lambda e: e.activation(out=Gm[:, :, tb:tb + 4].rearrange("p i t -> p t i"), in_=psGb[pg][:].rearrange("p (t i) -> p t i", t=4), func=AF.Copy),
                          r=[("psGb", pg)], w=["Gm"])
                for i4 in range(32):
                    ss_ = scount % 3
                    scount += 1
                    dma(ust[ss_][:], UTv[:, i4 * 4:(i4 + 1) * 4, :], r=["UT"], w=[("ust", ss_)])
                    dma(vst[ss_][:], VBv[:, i4 * 4:(i4 + 1) * 4, :], r=["VB"], w=[("vst", ss_)])
                    for j in range(4):
                        i1 = i4 * 4 + j
                        ps_ = i1 % 2
                        for kc in range(8):
                            P(lambda e: e.matmul(psSc[ps_][:, 0:TG], lhsT=ust[ss_][:, j, kc * 128:(kc + 1) * 128], rhs=h2[:, kc, :], start=(kc == 0), stop=(kc == 7)),
                              r=[("ust", ss_), "h2F"], w=[("psSc", ps_)])
                        A(lambda e: e.activation(out=act[ps_][:], in_=psSc[ps_][:, 0:TG], func=AF.Gelu), r=[("psSc", ps_)], w=[("actF", ps_)])
                        eng = V if (i1 % 2 == 0) else G
                        eng(lambda e: e.tensor_tensor(out=ga[ps_][:], in0=act[ps_][:], in1=Gm[:, i1, :], op=ALU.mult), r=[("actF", ps_), "Gm"], w=[("gaF", ps_)])
                        for tj in range(2):
                            for nh in range(2):
                                P(lambda e: e.matmul(psY[tj * 2 + nh][:], lhsT=ga[ps_][:, tj * 128:(tj + 1) * 128], rhs=vst[ss_][:, j, nh * 512:(nh + 1) * 512], start=(i1 == 0), stop=(i1 == 127)),
                                  r=[("gaF", ps_), ("vst", ss_)], w=[("psY", tj * 2 + nh)])
                for tj in range(2):
                    os2 = tj % 2
                    for nh in range(2):
                        V(lambda e: e.tensor_tensor(out=yo[:, nh * 512:(nh + 1) * 512], in0=psY[tj * 2 + nh][:], in1=gt2[:, nh * 512:(nh + 1) * 512], op=ALU.mult), r=[("psY", tj * 2 + nh), "gt2"], w=["yo"])
                    G(lambda e: e.tensor_tensor(out=yo[:], in0=yo[:], in1=x1t[:, tj, :], op=ALU.add), r=["yo", "x1t"], w=["yo"])
                    A(lambda e: e.activation(out=junk[:], in_=yo[:], func=AF.Square, accum_out=st4[:, 0:1]), r=["yo"], w=["junkF", "sF0"])
                    V(lambda e: e.tensor_scalar(out=st4[:, 1:2], in0=st4[:, 0:1], scalar1=1.0 / D, scalar2=EPS, op0=ALU.mult, op1=ALU.add), r=["sF0"], w=["sF1"])
                    A(lambda e: e.activation(out=st4[:, 2:3], in_=st4[:, 1:2], func=AF.Sqrt), r=["sF1"], w=["sF2"])
                    V(lambda e: e.reciprocal(out=st4[:, 3:4], in_=st4[:, 2:3]), r=["sF2"], w=["sF3"])
                    V(lambda e: e.scalar_tensor_tensor(out=oo[os2][:], in0=yo[:], scalar=st4[:, 3:4], in1=fwb[:], op0=ALU.mult, op1=ALU.mult), r=["yo", "sF3", "fwb"], w=[("oo", os2)])
                    dma(outf[t0 + tj * 128:t0 + (tj + 1) * 128, :], oo[os2][:], r=[("oo", os2)], w=["OUT"])
            k.barrier()
    return nc


_INPUT_ORDER = ["x", "c", "ada_w", "ada_b", "norm1_w", "norm2_w", "w_in", "conv_w", "conv_b", "ml_i_bias", "ml_f_bias",
                "ml_norm_w", "lam_q1", "lam_k1", "lam_q2", "lam_k2", "subln_w", "w_out", "peer_wq", "peer_keys",
                "peer_u", "peer_v", "final_norm_w"]


def make_in_maps(cfg, inputs, n_cores):
    f = lambda a: np.ascontiguousarray(np.asarray(a, dtype=np.float32))
    NB = cfg.NB
    shared = {
        "ada_w": f(inputs["ada_w"][0]), "ada_b": f(inputs["ada_b"][0]).reshape(1, -1),
        "norm1_w": f(inputs["norm1_w"][0]).reshape(1, -1), "norm2_w": f(inputs["norm2_w"][0]).reshape(1, -1),
        "w_in": f(inputs["w_in"][0]), "conv_w": f(inputs["conv_w"][0]), "conv_b": f(inputs["conv_b"][0]).reshape(1, -1),
        "ml_i_bias": f(inputs["ml_i_bias"][0]).reshape(1, -1), "ml_f_bias": f(inputs["ml_f_bias"][0]).reshape(1, -1),
        "ml_norm_w": f(inputs["ml_norm_w"][0]).reshape(1, -1),
        "lam_q1": f(inputs["lam_q1"][0]).reshape(1, -1), "lam_k1": f(inputs["lam_k1"][0]).reshape(1, -1),
        "lam_q2": f(inputs["lam_q2"][0]).reshape(1, -1), "lam_k2": f(inputs["lam_k2"][0]).reshape(1, -1),
        "subln_w": f(inputs["subln_w"][0]).reshape(1, -1), "w_out": f(inputs["w_out"][0]),
        "peer_wq": f(inputs["peer_wq"][0]), "peer_keys": f(inputs["peer_keys"][0]).reshape(16, 128, 128),
        "peer_u": f(inputs["peer_u"][0]), "peer_v": f(inputs["peer_v"][0]),
        "final_norm_w": f(inputs["final_norm_w"]).reshape(1, -1),
        "cst": host_consts(cfg),
    }
    xs = f(inputs["x"])
    cs = f(inputs["c"])
    maps = []
    for i in range(n_cores):
        m = dict(shared)
        m["x"] = np.ascontiguousarray(xs[i * NB:(i + 1) * NB])
        m["c"] = np.ascontiguousarray(cs[i * NB:(i + 1) * NB])
        maps.append(m)
    return maps


def kernel(**inputs):
    n_cores = 8
    cfg = Cfg(S=4096, NB=2)
    nc = build(cfg)
    maps = make_in_maps(cfg, inputs, n_cores)
    res = run_bass_kernel_spmd(nc, maps, core_ids=list(range(n_cores)))
    outs = [np.asarray(r["out"], dtype=np.float32) for r in res.results]
    return np.concatenate(outs, axis=0)
```

```python
import math
from contextlib import ExitStack

import numpy as np
import ml_dtypes
import concourse.bass as bass
import concourse.mybir as mybir
from concourse.bass_utils import run_bass_kernel_spmd

F32 = mybir.dt.float32
BF16 = mybir.dt.bfloat16
U32 = mybir.dt.uint32
ALU = mybir.AluOpType
AF = mybir.ActivationFunctionType
AX = mybir.AxisListType

D = 1024
EPS = 1e-6
IN_W = 8208
NEG = -30000.0


class Tok:
    __slots__ = ("sem", "val", "eng")

    def __init__(self, sem, val, eng):
        self.sem, self.val, self.eng = sem, val, eng


class Eng:
    def __init__(self, kb, name, eng):
        self.kb, self.name, self.eng = kb, name, eng
        self.sem = None
        self.count = 0
        self.seen = {}
        self.last = None
        self.nsem = 0

    def wait(self, tok):
        if tok is None:
            return
        key = id(tok.sem)
        if self.seen.get(key, 0) >= tok.val:
            return
        self.seen[key] = tok.val
        self.eng.wait_ge(tok.sem, tok.val)

    def signal(self, instr):
        if self.sem is None or self.count >= 30000:
            self.sem = self.kb.es.enter_context(self.kb.nc.semaphore(f"s_{self.name}_{self.nsem}"))
            self.nsem += 1
            self.count = 0
        self.count += 1
        instr.then_inc(self.sem, 1)
        t = Tok(self.sem, self.count, self.name)
        self.last = t
        return t


class KB:
    def __init__(self, nc):
        self.nc = nc
        self.es = ExitStack()
        self.e = {
            "pe": Eng(self, "pe", nc.tensor),
            "act": Eng(self, "act", nc.scalar),
            "dve": Eng(self, "dve", nc.vector),
            "pool": Eng(self, "pool", nc.gpsimd),
            "sp": Eng(self, "sp", nc.sync),
        }
        self.res = {}
        self.dsems = []
        self.dvals = []
        self.dnext = 0
        self.ND = 40
        self.all_dma = []

    def _deps(self, r, w):
        deps = []
        for k in r:
            st = self.res.get(k)
            if st is not None and st[0] is not None:
                deps.append(st[0])
        for k in w:
            st = self.res.get(k)
            if st is not None:
                if st[0] is not None:
                    deps.append(st[0])
                deps.extend(st[1])
        return deps

    def _record(self, tok, r, w):
        for k in r:
            st = self.res.setdefault(k, [None, []])
            if tok.eng != "dma":
                st[1] = [t for t in st[1] if t.eng != tok.eng]
            st[1].append(tok)
        for k in w:
            self.res[k] = [tok, []]

    def op(self, en, fn, r=(), w=()):
        E = self.e[en]
        for t in self._deps(r, w):
            if en == "pe" and t.eng == "pe":
                continue
            E.wait(t)
        instr = fn(E.eng)
        tok = E.signal(instr)
        self._record(tok, r, w)
        return tok

    def P(self, fn, r=(), w=()):
        return self.op("pe", fn, r, w)

    def A(self, fn, r=(), w=()):
        return self.op("act", fn, r, w)

    def V(self, fn, r=(), w=()):
        return self.op("dve", fn, r, w)

    def G(self, fn, r=(), w=()):
        return self.op("pool", fn, r, w)

    def dma(self, out, in_, r=(), w=(), q="sp", **kw):
        E = self.e[q]
        for t in self._deps(r, w):
            E.wait(t)
        if len(self.dsems) < self.ND:
            self.dsems.append(self.es.enter_context(self.nc.semaphore(f"s_dma_{len(self.dsems)}")))
            self.dvals.append(0)
        i = self.dnext
        self.dnext = (self.dnext + 1) % self.ND
        sem = self.dsems[i]
        if self.dvals[i] > 0:
            E.wait(Tok(sem, self.dvals[i], "dma"))
        self.dvals[i] += 16
        instr = E.eng.dma_start(out=out, in_=in_, **kw)
        instr.then_inc(sem, 16)
        tok = Tok(sem, self.dvals[i], "dma")
        self._record(tok, r, w)
        return tok

    def barrier(self):
        toks = [E.last for E in self.e.values() if E.last is not None]
        toks += [Tok(s, v, "dma") for s, v in zip(self.dsems, self.dvals) if v > 0]
        for E in self.e.values():
            for t in toks:
                if t.eng == E.name:
                    continue
                E.wait(t)
        self.res = {}


class Cfg:
    def __init__(self, S=4096, NB=2, debug=False):
        self.S, self.NB, self.debug = S, NB, debug
        self.NT = S // 128


def host_consts(cfg):
    NT = cfg.NT
    p = np.arange(128)
    ident = np.eye(128, dtype=np.float32)
    tri = (p[:, None] <= p[None, :]).astype(np.float32)
    negm = np.where(p[:, None] > p[None, :], NEG, 0.0).astype(np.float32)
    cm = (p[:, None] <= p[None, :]).astype(np.float32)
    iota = np.broadcast_to(np.arange(128, dtype=np.float32)[None, :], (128, 128)).copy()
    ones = np.ones((128, 128), np.float32)
    half = 8
    inv = (500000.0 ** (-np.arange(half, dtype=np.float32) * 2.0 / 16)).astype(np.float32)
    pos = (np.arange(NT)[None, :] * 128 + p[:, None]).astype(np.float32)
    ang = pos[:, :, None] * inv[None, None, :]
    cos = np.cos(ang).astype(np.float32).reshape(128, NT * 8)
    sin = np.sin(ang).astype(np.float32).reshape(128, NT * 8)
    a16 = np.broadcast_to((np.arange(16, dtype=np.float32) * 16)[None, :], (128, 16)).copy()
    i16 = np.broadcast_to(np.arange(16, dtype=np.float32)[None, :], (128, 16)).copy()
    return np.concatenate([ident, tri, negm, cm, iota, ones, a16, i16, cos, sin], axis=1).astype(np.float32)


def build(cfg):
    S, NB, NT = cfg.S, cfg.NB, cfg.NT
    NG = S // 512
    T = NB * S
    nc = bass.Bass("TRN2", target_bir_lowering=False)

    def din(name, shape, dt=F32):
        return nc.dram_tensor(name, list(shape), dt, kind="ExternalInput").ap()

    def dscr(name, shape, dt):
        kind = "ExternalOutput" if cfg.debug else "Internal"
        return nc.dram_tensor(name, list(shape), dt, kind=kind).ap()

    x = din("x", [NB, S, D])
    c_in = din("c", [NB, D])
    ada_w = din("ada_w", [D, 6 * D])
    ada_b = din("ada_b", [1, 6 * D])
    norm1_w = din("norm1_w", [1, D])
    norm2_w = din("norm2_w", [1, D])
    w_in = din("w_in", [D, IN_W])
    conv_w = din("conv_w", [4, 1024])
    conv_b = din("conv_b", [1, 1024])
    ml_ib = din("ml_i_bias", [1, 8])
    ml_fb = din("ml_f_bias", [1, 8])
    ml_nw = din("ml_norm_w", [1, D])
    lam_q1 = din("lam_q1", [1, 64])
    lam_k1 = din("lam_k1", [1, 64])
    lam_q2 = din("lam_q2", [1, 64])
    lam_k2 = din("lam_k2", [1, 64])
    subln_w = din("subln_w", [1, 128])
    w_out = din("w_out", [D, D])
    peer_wq = din("peer_wq", [D, 2048])
    peer_keys = din("peer_keys", [16, 128, 128])
    peer_u = din("peer_u", [16384, D])
    peer_v = din("peer_v", [16384, D])
    fin_w = din("final_norm_w", [1, D])
    NCST = 128 * 6 + 32 + 2 * NT * 8
    cst_d = din("cst", [128, NCST])
    out = nc.dram_tensor("out", [NB, S, D], F32, kind="ExternalOutput").ap()

    UT = dscr("UT", [128, 128, 1024], BF16)
    VB = dscr("VB", [128, 128, 1024], BF16)
    MOD = dscr("MOD", [NB, 6 * D], F32)
    QT = dscr("QT", [NB, 8, 128, S], BF16)
    KT = dscr("KT", [NB, 8, 128, S], BF16)
    VA = dscr("VA", [NB, S, D], BF16)
    QMT = dscr("QMT", [NB, 4, 128, S], BF16)
    KMT = dscr("KMT", [NB, 4, 128, S], BF16)
    VM = dscr("VM", [NB, S, D], BF16)
    OMS = dscr("OMS", [NB, S, D], BF16)
    GAS = dscr("GAS", [NB, S, D], BF16)
    GMS = dscr("GMS", [NB, S, D], BF16)
    GATES = dscr("GATES", [NB, S, 16], F32)
    YM = dscr("YM", [NB, S, D], BF16)
    MT = dscr("MT", [NB, 8, 128, S], BF16)
    X1 = dscr("X1", [NB, S, D], F32)
    H2T = dscr("H2T", [NB, 8, 128, S], BF16)
    IG = dscr("IG", [NB, S, 3, 128], F32)

    k = KB(nc)
    P, A, V, G, dma = k.P, k.A, k.V, k.G, k.dma

    with k.es:
        es0 = k.es

        uid = [0]

        def sbt(es, name, shape, dt):
            uid[0] += 1
            return es.enter_context(nc.sbuf_tensor(f"sb{uid[0]}_{name}", list(shape), dt))

        def pst(es, name, shape, dt):
            uid[0] += 1
            return es.enter_context(nc.psum_tensor(f"ps{uid[0]}_{name}", list(shape), dt))

        cst = sbt(es0, "cst", [128, NCST], F32)
        identb = sbt(es0, "identb", [128, 128], BF16)
        cmb = sbt(es0, "cmb", [128, 128], BF16)
        nmx = sbt(es0, "nmx", [128, 32], F32)
        lamt = sbt(es0, "lamt", [128, 4], F32)
        dma(cst[:], cst_d, w=["cst"])
        identf = cst[:, 0:128]
        trif = cst[:, 128:256]
        negmf = cst[:, 256:384]
        cmf = cst[:, 384:512]
        iotaf = cst[:, 512:640]
        onesf = cst[:, 640:768]
        a16f = cst[:, 768:784]
        i16f = cst[:, 784:800]
        cosf = cst[:, 800:800 + NT * 8]
        sinf = cst[:, 800 + NT * 8:800 + 2 * NT * 8]
        V(lambda e: e.tensor_copy(out=identb[:], in_=identf), r=["cst"], w=["identb"])
        V(lambda e: e.tensor_copy(out=cmb[:], in_=cmf), r=["cst"], w=["cmb"])

        with ExitStack() as ph:
            uf = [sbt(ph, f"uf{i}", [128, 1024], F32) for i in range(2)]
            ub = [sbt(ph, f"ub{i}", [128, 1024], BF16) for i in range(2)]
            uts = [sbt(ph, f"uts{i}", [128, 1024], BF16) for i in range(2)]
            vf = [sbt(ph, f"vf{i}", [128, 1024], F32) for i in range(2)]
            vb = [sbt(ph, f"vb{i}", [128, 1024], BF16) for i in range(2)]
            ptb = [pst(ph, f"ptbA{i}", [128, 1024], BF16) for i in range(2)]
            for i1 in range(128):
                s = i1 % 2
                dma(uf[s][:], peer_u[i1 * 128:(i1 + 1) * 128, :], w=[("uf", s)])
                dma(vf[s][:], peer_v[i1 * 128:(i1 + 1) * 128, :], w=[("vf", s)])
                V(lambda e: e.tensor_copy(out=ub[s][:], in_=uf[s][:]), r=[("uf", s)], w=[("ub", s)])
                for kc in range(8):
                    P(lambda e: e.transpose(out=ptb[s][:, kc * 128:(kc + 1) * 128], in_=ub[s][:, kc * 128:(kc + 1) * 128], identity=identb[:]),
                      r=[("ub", s), "identb"], w=[("ptbA", s)])
                A(lambda e: e.activation(out=uts[s][:], in_=ptb[s][:], func=AF.Copy), r=[("ptbA", s)], w=[("uts", s)])
                dma(UT[i1], uts[s][:], r=[("uts", s)], w=["UT"])
                G(lambda e: e.tensor_copy(out=vb[s][:], in_=vf[s][:]), r=[("vf", s)], w=[("vb", s)])
                dma(VB[i1], vb[s][:], r=[("vb", s)], w=["VB"])

            condT = sbt(ph, "condT", [128, 8, NB], F32)
            adab = sbt(ph, "adab", [1, 6 * D], F32)
            modrow = sbt(ph, "modrow", [1, NB, 6 * D], F32)
            wada = [sbt(ph, f"wada{i}", [128, 8, 512], F32) for i in range(2)]
            psm = [pst(ph, f"psm{i}", [128, 512], F32) for i in range(2)]
            for b in range(NB):
                dma(condT[:, :, b], c_in[b].rearrange("(kc p) -> p kc", p=128), w=["condT"], allow_slow_non_contiguous=True)
            dma(adab[:], ada_b, w=["adab"])
            A(lambda e: e.activation(out=condT[:], in_=condT[:], func=AF.Silu), r=["condT"], w=["condT"])
            adaw_v = ada_w.rearrange("(kc p) n -> p kc n", p=128)
            for ncx in range(12):
                s = ncx % 2
                dma(wada[s][:], adaw_v[:, :, ncx * 512:(ncx + 1) * 512], w=[("wada", s)])
                for b in range(NB):
                    pb = (ncx * NB + b) % 2
                    for kc in range(8):
                        P(lambda e: e.matmul(psm[pb][0:1, :], lhsT=condT[:, kc, b:b + 1], rhs=wada[s][:, kc, :], start=(kc == 0), stop=(kc == 7)),
                          r=["condT", ("wada", s)], w=[("psm", pb)])
                    V(lambda e: e.tensor_tensor(out=modrow[0:1, b, ncx * 512:(ncx + 1) * 512], in0=psm[pb][0:1, :], in1=adab[0:1, ncx * 512:(ncx + 1) * 512], op=ALU.add),
                      r=[("psm", pb), "adab"], w=["modrow"])
            for b in range(NB):
                dma(MOD[b:b + 1, :], modrow[0:1, b, :], r=["modrow"], w=["MOD"])

            lq = sbt(ph, "lq", [1, 4, 64], F32)
            lsc = sbt(ph, "lsc", [1, 8], F32)
            dma(lq[0:1, 0, :], lam_q1, w=["lq0"])
            dma(lq[0:1, 1, :], lam_k1, w=["lq1"])
            dma(lq[0:1, 2, :], lam_q2, w=["lq2"])
            dma(lq[0:1, 3, :], lam_k2, w=["lq3"])
            V(lambda e: e.tensor_tensor(out=lq[0:1, 0, :], in0=lq[0:1, 0, :], in1=lq[0:1, 1, :], op=ALU.mult), r=["lq0", "lq1"], w=["lq0"])
            V(lambda e: e.tensor_tensor(out=lq[0:1, 2, :], in0=lq[0:1, 2, :], in1=lq[0:1, 3, :], op=ALU.mult), r=["lq2", "lq3"], w=["lq2"])
            V(lambda e: e.reduce_sum(out=lsc[0:1, 0:1], in_=lq[0:1, 0, :], axis=AX.X), r=["lq0"], w=["lsc0"])
            V(lambda e: e.reduce_sum(out=lsc[0:1, 1:2], in_=lq[0:1, 2, :], axis=AX.X), r=["lq2"], w=["lsc1"])
            A(lambda e: e.activation(out=lsc[0:1, 2:4], in_=lsc[0:1, 0:2], func=AF.Exp), r=["lsc0", "lsc1"], w=["lsc2"])
            lam_init = 0.8 - 0.6 * math.exp(0.0)
            V(lambda e: e.tensor_tensor(out=lsc[0:1, 4:5], in0=lsc[0:1, 3:4], in1=lsc[0:1, 2:3], op=ALU.subtract), r=["lsc2"], w=["lsc4"])
            V(lambda e: e.tensor_scalar(out=lsc[0:1, 5:6], in0=lsc[0:1, 4:5], scalar1=-lam_init, scalar2=None, op0=ALU.add), r=["lsc4"], w=["lsc5"])
            P(lambda e: e.matmul(psm[0][:, 0:1], lhsT=onesf[0:1, :], rhs=lsc[0:1, 5:6], start=True, stop=True), r=["lsc5", "cst", ("psm", 0)], w=[("psm", 0)])
            V(lambda e: e.tensor_copy(out=lamt[:, 0:1], in_=psm[0][:, 0:1]), r=[("psm", 0)], w=["lamt"])
            k.barrier()

        win_v = w_in.rearrange("(kc p) n -> p kc n", p=128)
        for b in range(NB):
            with ExitStack() as ph:
                hT = sbt(ph, "hT", [128, 8, S], BF16)
                A1 = sbt(ph, "A1", [128, D], F32)
                B1 = sbt(ph, "B1", [128, D], F32)
                w1b = sbt(ph, "w1b", [128, D], F32)
                xt = [sbt(ph, f"xt{i}", [128, D], F32) for i in range(2)]
                hxs = [sbt(ph, f"hx{i}", [128, D], F32) for i in range(2)]
                hb = [sbt(ph, f"hb{i}", [128, D], BF16) for i in range(2)]
                junk = sbt(ph, "junkB", [128, D], BF16)
                st4s = [sbt(ph, f"st4{i}", [128, 8], F32) for i in range(2)]
                ptb = [pst(ph, f"ptbB{i}", [128, 1024], BF16) for i in range(2)]
                psb = [pst(ph, f"psB{i}", [128, 512], F32) for i in range(4)]
                dma(A1[:], MOD[b:b + 1, D:2 * D].partition_broadcast(128), r=["MOD"], w=["A1"])
                dma(B1[:], MOD[b:b + 1, 0:D].partition_broadcast(128), r=["MOD"], w=["B1"])
                dma(w1b[:], norm1_w.partition_broadcast(128), w=["w1b"])
                V(lambda e: e.scalar_tensor_tensor(out=A1[:], in0=A1[:], scalar=1.0, in1=w1b[:], op0=ALU.add, op1=ALU.mult), r=["A1", "w1b"], w=["A1"])
                V(lambda e: e.memset(nmx[:], 0.0), w=["nmx"])
                for tt in range(NT):
                    s = tt % 2
                    hx = hxs[s]
                    st4 = st4s[s]
                    dma(xt[s][:], x[b, tt * 128:(tt + 1) * 128, :], w=[("xt", s)])
                    A(lambda e: e.activation(out=junk[:], in_=xt[s][:], func=AF.Square, accum_out=st4[:, 0:1]), r=[("xt", s)], w=["junkB", ("st0", s)])
                    V(lambda e: e.tensor_scalar(out=st4[:, 1:2], in0=st4[:, 0:1], scalar1=1.0 / D, scalar2=EPS, op0=ALU.mult, op1=ALU.add), r=[("st0", s)], w=[("st1", s)])
                    A(lambda e: e.activation(out=st4[:, 2:3], in_=st4[:, 1:2], func=AF.Sqrt), r=[("st1", s)], w=[("st2", s)])
                    V(lambda e: e.reciprocal(out=st4[:, 3:4], in_=st4[:, 2:3]), r=[("st2", s)], w=[("st3", s)])
                    V(lambda e: e.scalar_tensor_tensor(out=hx[:], in0=xt[s][:], scalar=st4[:, 3:4], in1=A1[:], op0=ALU.mult, op1=ALU.mult), r=[("xt", s), ("st3", s), "A1"], w=[("hx", s)])
                    G(lambda e: e.tensor_tensor(out=hb[s][:], in0=hx[:], in1=B1[:], op=ALU.add), r=[("hx", s), "B1"], w=[("hb", s)])
                    for kc in range(8):
                        P(lambda e: e.transpose(out=ptb[s][:, kc * 128:(kc + 1) * 128], in_=hb[s][:, kc * 128:(kc + 1) * 128], identity=identb[:]),
                          r=[("hb", s), "identb"], w=[("ptbB", s)])
                    A(lambda e: e.activation(out=hT[:, :, tt * 128:(tt + 1) * 128], in_=ptb[s][:].rearrange("p (k t) -> p k t", k=8), func=AF.Copy),
                      r=[("ptbB", s)], w=[("hT", tt)])

                wf = [sbt(ph, f"wf{i}", [128, 8, 512], F32) for i in range(2)]
                wb = [sbt(ph, f"wb{i}", [128, 8, 512], BF16) for i in range(2)]
                qfs = [sbt(ph, f"qf{i}", [128, 512], F32) for i in range(2)]
                sqjs = [sbt(ph, f"sqj{i}", [128, 512], F32) for i in range(2)]
                rts = [sbt(ph, f"rt{i}", [128, 4, 8, 8], F32) for i in range(2)]
                nsqs = [sbt(ph, f"nsq{i}", [128, 8], F32) for i in range(2)]
                qb = [sbt(ph, f"qb{i}", [128, 512], BF16) for i in range(2)]
                stage = [sbt(ph, f"stage{i}", [128, 4, 512], BF16) for i in range(2)]
                ob = [sbt(ph, f"ob{i}", [128, 512], BF16) for i in range(3)]
                xbuf = sbt(ph, "xbuf", [128, 4, 515], F32)
                cacc = sbt(ph, "cacc", [128, 512], F32)
                csil = sbt(ph, "csil", [128, 512], F32)
                cstg = [sbt(ph, f"cstg{i}", [128, 512], BF16) for i in range(2)]
                cw = sbt(ph, "cw", [128, 8, 4], F32)
                cbi = sbt(ph, "cbi", [128, 8], F32)
                gbias = sbt(ph, "gbias", [128, 16], F32)
                gz = sbt(ph, "gz", [128, 16], F32)
                gt = [sbt(ph, f"gtB{i}", [128, 16], F32) for i in range(2)]
                for j in range(4):
                    dma(cw[:, :, j], conv_w[j].rearrange("(blk p) -> p blk", p=128), w=["cw"], allow_slow_non_contiguous=True)
                dma(cbi[:], conv_b.rearrange("o (blk p) -> p (o blk)", p=128), w=["cbi"], allow_slow_non_contiguous=True)
                dma(gbias[:, 0:8], ml_ib.partition_broadcast(128), w=["gbias0"])
                dma(gbias[:, 8:16], ml_fb.partition_broadcast(128), w=["gbias1"])

                chunks = []
                for i in range(2):
                    chunks.append((i * 512, 512, "qa", i))
                for i in range(2):
                    chunks.append((1024 + i * 512, 512, "ka", i))
                for i in range(2):
                    chunks.append((2048 + i * 512, 512, "va", i))
                chunks.append((3072, 512, "qm", 0))
                chunks.append((3584, 512, "km", 0))
                for i in range(2):
                    chunks.append((4096 + i * 512, 512, "vm", i))
                for i in range(2):
                    chunks.append((5120 + i * 512, 512, "om", i))
                chunks.append((6144, 16, "gate", 0))
                for i in range(2):
                    chunks.append((6160 + i * 512, 512, "ga", i))
                for i in range(2):
                    chunks.append((7184 + i * 512, 512, "gm", i))

                pcount = 0
                ocount = 0
                qcount = 0
                for ci, (c0, ncol, kind, idx) in enumerate(chunks):
                    s = ci % 2
                    dma(wf[s][:, :, 0:ncol], win_v[:, :, c0:c0 + ncol], w=[("wf", s)])
                    G(lambda e: e.tensor_copy(out=wb[s][:, :, 0:ncol], in_=wf[s][:, :, 0:ncol]), r=[("wf", s)], w=[("wb", s)])
                    if kind in ("qm", "km"):
                        sc_ = 1.0 if kind == "qm" else 0.125
                        dst = QMT if kind == "qm" else KMT
                        boff = 0 if kind == "qm" else 4
                        for cb in range(4):
                            V(lambda e: e.memset(xbuf[:, cb, 0:3], 0.0), w=[("xbuf", cb)])
                        for tg in range(NG):
                            for cb in range(4):
                                pb = pcount % 4
                                pcount += 1
                                for kc in range(8):
                                    P(lambda e: e.matmul(psb[pb][:], lhsT=wb[s][:, kc, cb * 128:(cb + 1) * 128], rhs=hT[:, kc, tg * 512:(tg + 1) * 512], start=(kc == 0), stop=(kc == 7)),
                                      r=[("wb", s)] + [("hT", t_) for t_ in range(tg * 4, tg * 4 + 4)], w=[("psB", pb)])
                                if tg > 0:
                                    V(lambda e: e.tensor_copy(out=xbuf[:, cb, 0:3], in_=xbuf[:, cb, 512:515]), r=[("xbuf", cb)], w=[("xbuf", cb)])
                                A(lambda e: e.activation(out=xbuf[:, cb, 3:515], in_=psb[pb][:], func=AF.Copy), r=[("psB", pb)], w=[("xbuf", cb)])
                                V(lambda e: e.tensor_scalar(out=cacc[:], in0=xbuf[:, cb, 0:512], scalar1=cw[:, boff + cb, 0:1], scalar2=None, op0=ALU.mult), r=[("xbuf", cb), "cw"], w=["cacc"])
                                for j in range(1, 4):
                                    V(lambda e: e.scalar_tensor_tensor(out=cacc[:], in0=xbuf[:, cb, j:j + 512], scalar=cw[:, boff + cb, j:j + 1], in1=cacc[:], op0=ALU.mult, op1=ALU.add),
                                      r=[("xbuf", cb), "cw", "cacc"], w=["cacc"])
                                A(lambda e: e.activation(out=csil[:], in_=cacc[:], func=AF.Silu, bias=cbi[:, boff + cb:boff + cb + 1], scale=1.0), r=["cacc", "cbi"], w=["csil"])
                                cs = qcount % 2
                                qcount += 1
                                G(lambda e: e.tensor_scalar(out=cstg[cs][:], in0=csil[:], scalar1=sc_, scalar2=None, op0=ALU.mult), r=["csil"], w=[("cstg", cs)])
                                dma(dst[b, cb, :, tg * 512:(tg + 1) * 512], cstg[cs][:], r=[("cstg", cs)], w=["QKMT"])
                        continue
                    for tt in range(NT):
                        pb = pcount % 4
                        pcount += 1
                        for kc in range(8):
                            P(lambda e: e.matmul(psb[pb][:, 0:ncol], lhsT=hT[:, kc, tt * 128:(tt + 1) * 128], rhs=wb[s][:, kc, 0:ncol], start=(kc == 0), stop=(kc == 7)),
                              r=[("wb", s), ("hT", tt)], w=[("psB", pb)])
                        if kind in ("qa", "ka"):
                            z = tt % 2
                            qf, sqj, rt, nsq = qfs[z], sqjs[z], rts[z], nsqs[z]
                            kq = ("qf", z)
                            A(lambda e: e.activation(out=qf[:], in_=psb[pb][:], func=AF.Copy), r=[("psB", pb)], w=[kq])
                            qv = qf[:].rearrange("p (g d) -> p g d", g=8)
                            x1 = qv[:, :, 0:8]
                            x2 = qv[:, :, 8:16]
                            cosb = cosf[:, tt * 8:(tt + 1) * 8].unsqueeze(1).to_broadcast([128, 8, 8])
                            sinb = sinf[:, tt * 8:(tt + 1) * 8].unsqueeze(1).to_broadcast([128, 8, 8])
                            V(lambda e: e.tensor_tensor(out=rt[:, 0], in0=x1, in1=cosb, op=ALU.mult), r=[kq, "cst"], w=[("rt0", z)])
                            V(lambda e: e.tensor_tensor(out=rt[:, 1], in0=x2, in1=sinb, op=ALU.mult), r=[kq, "cst"], w=[("rt1", z)])
                            V(lambda e: e.tensor_tensor(out=rt[:, 2], in0=x2, in1=cosb, op=ALU.mult), r=[kq, "cst"], w=[("rt2", z)])
                            V(lambda e: e.tensor_tensor(out=rt[:, 3], in0=x1, in1=sinb, op=ALU.mult), r=[kq, "cst"], w=[("rt3", z)])
                            V(lambda e: e.tensor_tensor(out=x1, in0=rt[:, 0], in1=rt[:, 1], op=ALU.subtract), r=[("rt0", z), ("rt1", z), ("rt2", z), ("rt3", z)], w=[kq])
                            V(lambda e: e.tensor_tensor(out=x2, in0=rt[:, 2], in1=rt[:, 3], op=ALU.add), r=[("rt2", z), ("rt3", z)], w=[kq])
                            G(lambda e: e.tensor_tensor(out=sqj[:], in0=qf[:], in1=qf[:], op=ALU.mult), r=[kq], w=[("sqj", z)])
                            V(lambda e: e.reduce_sum(out=nsq[:], in_=sqj[:].rearrange("p (g d) -> p g d", g=8), axis=AX.X), r=[("sqj", z)], w=[("nsq", z)])
                            ncol0 = (0 if kind == "qa" else 16) + idx * 8
                            V(lambda e: e.tensor_tensor(out=nmx[:, ncol0:ncol0 + 8], in0=nmx[:, ncol0:ncol0 + 8], in1=nsq[:], op=ALU.max), r=[("nsq", z), "nmx"], w=["nmx"])
                            qs = qcount % 2
                            qcount += 1
                            A(lambda e: e.activation(out=qb[qs][:], in_=qf[:], func=AF.Copy, scale=(0.125 if kind == "qa" else 1.0)), r=[kq], w=[("qb", qs)])
                            ps_ = tt % 2
                            for hd in range(4):
                                P(lambda e: e.transpose(out=ptb[ps_][:, hd * 128:(hd + 1) * 128], in_=qb[qs][:, hd * 128:(hd + 1) * 128], identity=identb[:]),
                                  r=[("qb", qs), "identb"], w=[("ptbB", ps_)])
                            sg = (tt // 4) % 2
                            V(lambda e: e.tensor_copy(out=stage[sg][:, :, (tt % 4) * 128:(tt % 4 + 1) * 128], in_=ptb[ps_][:, 0:512].rearrange("p (h t) -> p h t", h=4)),
                              r=[("ptbB", ps_)], w=[("stage", sg)])
                            if tt % 4 == 3:
                                dstT = QT if kind == "qa" else KT
                                t0 = (tt // 4) * 512
                                dma(dstT[b, idx * 4:(idx + 1) * 4, :, t0:t0 + 512].rearrange("h p t -> p h t"), stage[sg][:], r=[("stage", sg)], w=["QKT"])
                        elif kind in ("va", "vm"):
                            os_ = ocount % 3
                            ocount += 1
                            A(lambda e: e.activation(out=ob[os_][:], in_=psb[pb][:], func=AF.Copy), r=[("psB", pb)], w=[("ob", os_)])
                            dstv = VA if kind == "va" else VM
                            dma(dstv[b, tt * 128:(tt + 1) * 128, idx * 512:(idx + 1) * 512], ob[os_][:], r=[("ob", os_)], w=["VAVM"])
                        elif kind in ("om", "ga", "gm"):
                            os_ = ocount % 3
                            ocount += 1
                            A(lambda e: e.activation(out=ob[os_][:], in_=psb[pb][:], func=AF.Sigmoid), r=[("psB", pb)], w=[("ob", os_)])
                            dsts = {"om": OMS, "ga": GAS, "gm": GMS}[kind]
                            dma(dsts[b, tt * 128:(tt + 1) * 128, idx * 512:(idx + 1) * 512], ob[os_][:], r=[("ob", os_)], w=["SIGS"])
                        else:
                            gs = tt % 2
                            V(lambda e: e.tensor_tensor(out=gz[:], in0=psb[pb][:, 0:16], in1=gbias[:], op=ALU.add), r=[("psB", pb), "gbias0", "gbias1"], w=["gz"])
                            A(lambda e: e.activation(out=gz[:, 8:16], in_=gz[:, 8:16], func=AF.Exp, scale=-1.0), r=["gz"], w=["gz"])
                            A(lambda e: e.activation(out=gz[:, 8:16], in_=gz[:, 8:16], func=AF.Ln, bias=1.0, scale=1.0), r=["gz"], w=["gz"])
                            V(lambda e: e.tensor_copy(out=gt[gs][:, 0:8], in_=gz[:, 0:8]), r=["gz"], w=[("gtB", gs)])
                            V(lambda e: e.tensor_scalar(out=gt[gs][:, 8:16], in0=gz[:, 8:16], scalar1=-1.0, scalar2=None, op0=ALU.mult), r=["gz", ("gtB", gs)], w=[("gtB", gs)])
                            dma(GATES[b, tt * 128:(tt + 1) * 128, :], gt[gs][:], r=[("gtB", gs)], w=["GATES"])
                k.barrier()

            with ExitStack() as ph:
                qmt = sbt(ph, "qmt", [128, 4, S], BF16)
                kmt = sbt(ph, "kmt", [128, 4, S], BF16)
                Cst = sbt(ph, "Cst", [128, 4, 129], F32)
                Cbf = sbt(ph, "Cbf", [128, 4, 129], BF16)
                mlw = sbt(ph, "mlw", [128, D], F32)
                gtl = [sbt(ph, f"gtl{i}", [128, 16], F32) for i in range(2)]
                vmt = [sbt(ph, f"vmt{i}", [128, 8, 129], BF16) for i in range(2)]
                omt = [sbt(ph, f"omt{i}", [128, D], BF16) for i in range(2)]
                gmt = [sbt(ph, f"gmt{i}", [128, D], BF16) for i in range(2)]
                sm = sbt(ph, "sm", [128, 8, 8], F32)
                Kw = sbt(ph, "Kw", [128, 4, 128], BF16)
                Bd = [sbt(ph, f"Bd{i}", [128, 128], F32) for i in range(2)]
                WT = [sbt(ph, f"WT{i}", [128, 128], F32) for i in range(2)]
                PT = [sbt(ph, f"PT{i}", [128, 128], BF16) for i in range(2)]
                Isb = [sbt(ph, f"Isb{i}", [128, 129], F32) for i in range(2)]
                tot = sbt(ph, "tot", [128, 8, 129], F32)
                hn = sbt(ph, "hn", [128, 8, 128], F32)
                hj = sbt(ph, "hj", [128, 8, 128], F32)
                n8 = sbt(ph, "n8", [128, 6, 8], F32)
                ymb = [sbt(ph, f"ymb{i}", [128, D], BF16) for i in range(2)]
                psG = pst(ph, "psG", [128, 512], F32)
                psD = [pst(ph, f"psD{i}", [128, 512], F32) for i in range(2)]
                psS = [pst(ph, f"psS{i}", [128, 512], F32) for i in range(2)]
                psI = pst(ph, "psI", [128, 512], F32)
                psJ = pst(ph, "psJ", [128, 512], F32)
                psK = pst(ph, "psK", [128, 1024], BF16)
                dma(qmt[:], QMT[b].rearrange("c p t -> p c t"), w=["qmt"])
                dma(kmt[:], KMT[b].rearrange("c p t -> p c t"), w=["kmt"])
                dma(mlw[:], ml_nw.partition_broadcast(128), w=["mlw"])
                V(lambda e: e.memset(Cst[:], 0.0), w=["Cst"])
                V(lambda e: e.memset(Cbf[:], 0.0), w=["Cbf"])
                for i in range(2):
                    V(lambda e: e.memset(vmt[i][:, :, 128:129], 1.0), w=[("vmt", i)])
                for tt in range(NT):
                    s = tt % 2
                    tsl = slice(tt * 128, (tt + 1) * 128)
                    dma(gtl[s][:], GATES[b, tsl, :], w=[("gtl", s)])
                    dma(vmt[s][:, :, 0:128], VM[b, tsl, :].rearrange("p (h e) -> p h e", h=8), w=[("vmt", s)])
                    dma(omt[s][:], OMS[b, tsl, :], w=[("omt", s)])
                    dma(gmt[s][:], GMS[b, tsl, :], w=[("gmt", s)])
                    lf = gtl[s][:, 8:16]
                    ig = gtl[s][:, 0:8]
                    P(lambda e: e.matmul(psG[:, 0:8], lhsT=trif, rhs=lf, start=True, stop=True), r=[("gtl", s), "cst"], w=["psG"])
                    P(lambda e: e.matmul(psG[:, 8:16], lhsT=onesf, rhs=lf, start=True, stop=True), r=[("gtl", s), "cst"], w=["psG"])
                    V(lambda e: e.tensor_copy(out=sm[:, 0:2, :], in_=psG[:, 0:16].rearrange("p (a h) -> p a h", a=2)), r=["psG"], w=["sm01"])
                    V(lambda e: e.tensor_tensor(out=sm[:, 2, :], in0=ig, in1=sm[:, 0, :], op=ALU.subtract), r=[("gtl", s), "sm01"], w=["sm2"])
                    V(lambda e: e.tensor_tensor(out=sm[:, 6, :], in0=sm[:, 1, :], in1=sm[:, 2, :], op=ALU.add), r=["sm01", "sm2"], w=["sm6"])
                    A(lambda e: e.activation(out=sm[:, 3, :], in_=sm[:, 0, :], func=AF.Exp), r=["sm01"], w=["sm3"])
                    A(lambda e: e.activation(out=sm[:, 4, :], in_=sm[:, 6, :], func=AF.Exp), r=["sm6"], w=["sm4"])
                    A(lambda e: e.activation(out=sm[:, 5, :], in_=sm[:, 1, :], func=AF.Exp), r=["sm01"], w=["sm5"])
                    for cb in range(4):
                        P(lambda e: e.transpose(out=psK[:, cb * 128:(cb + 1) * 128], in_=kmt[:, cb, tsl], identity=identb[:]), r=["kmt", "identb"], w=["psK"])
                    V(lambda e: e.tensor_tensor(out=Kw[:].rearrange("p c (h d) -> p (c h) d", h=2), in0=psK[:, 0:512].rearrange("p (g d) -> p g d", g=8),
                                                in1=sm[:, 4, :].unsqueeze(2).to_broadcast([128, 8, 64]), op=ALU.mult), r=["psK", "sm4"], w=["Kw"])
                    for h in range(8):
                        cb, r0 = h // 2, (h % 2) * 64
                        hs = h % 2
                        G(lambda e: e.tensor_scalar(out=Bd[hs][:], in0=identf, scalar1=sm[:, 0, h:h + 1], scalar2=None, op0=ALU.mult), r=["cst", "sm01"], w=[("Bd", hs)])
                        P(lambda e: e.matmul(psD[hs][:, 0:128], lhsT=onesf, rhs=Bd[hs][:], start=True, stop=False), r=[("Bd", hs), "cst"], w=[("psD", hs)])
                        P(lambda e: e.matmul(psD[hs][:, 0:128], lhsT=identf, rhs=negmf, start=False, stop=True), r=["cst"], w=[("psD", hs)])
                        A(lambda e: e.activation(out=WT[hs][:], in_=psD[hs][:, 0:128], func=AF.Exp, bias=sm[:, 2, h:h + 1], scale=1.0), r=[("psD", hs), "sm2"], w=[("WT", hs)])
                        P(lambda e: e.matmul(psS[hs][:, 0:128], lhsT=kmt[r0:r0 + 64, cb, tsl], rhs=qmt[r0:r0 + 64, cb, tsl], start=True, stop=True), r=["kmt", "qmt"], w=[("psS", hs)])
                        V(lambda e: e.tensor_tensor(out=PT[hs][:], in0=psS[hs][:, 0:128], in1=WT[hs][:], op=ALU.mult), r=[("psS", hs), ("WT", hs)], w=[("PT", hs)])
                        P(lambda e: e.matmul(psI[:, 0:129], lhsT=PT[hs][:], rhs=vmt[s][:, h, :], start=True, stop=True), r=[("PT", hs), ("vmt", s)], w=["psI"])
                        P(lambda e: e.matmul(psJ[:, 0:129], lhsT=qmt[r0:r0 + 64, cb, tsl], rhs=Cbf[r0:r0 + 64, cb, :], start=True, stop=True), r=["qmt", ("Cbf", h)], w=["psJ"])
                        A(lambda e: e.activation(out=Isb[hs][:], in_=psI[:, 0:129], func=AF.Copy), r=["psI"], w=[("Isb", hs)])
                        V(lambda e: e.scalar_tensor_tensor(out=tot[:, h, :], in0=psJ[:, 0:129], scalar=sm[:, 3, h:h + 1], in1=Isb[hs][:], op0=ALU.mult, op1=ALU.add),
                          r=["psJ", "sm3", ("Isb", hs)], w=[("tot", h)])
                        P(lambda e: e.matmul(psJ[:, 256:385], lhsT=Kw[:, cb, :], rhs=vmt[s][:, h, :], start=True, stop=True), r=["Kw", ("vmt", s), "psJ"], w=["psJ"])
                        V(lambda e: e.scalar_tensor_tensor(out=Cst[r0:r0 + 64, cb, :], in0=Cst[r0:r0 + 64, cb, :], scalar=sm[r0:r0 + 64, 5, h:h + 1], in1=psJ[r0:r0 + 64, 256:385], op0=ALU.mult, op1=ALU.add),
                          r=["psJ", "sm5", ("Cst", h)], w=[("Cst", h)])
                        G(lambda e: e.tensor_copy(out=Cbf[r0:r0 + 64, cb, :], in_=Cst[r0:r0 + 64, cb, :]), r=[("Cst", h)], w=[("Cbf", h)])
                    allt = [("tot", h) for h in range(8)]
                    den = tot[:, :, 128]
                    V(lambda e: e.scalar_tensor_tensor(out=n8[:, 0, :], in0=den, scalar=-1.0, in1=den, op0=ALU.mult, op1=ALU.max), r=allt, w=["n80"])
                    V(lambda e: e.tensor_scalar(out=n8[:, 0, :], in0=n8[:, 0, :], scalar1=1.0, scalar2=None, op0=ALU.max), r=["n80"], w=["n80"])
                    V(lambda e: e.reciprocal(out=n8[:, 1, :], in_=n8[:, 0, :]), r=["n80"], w=["n81"])
                    V(lambda e: e.tensor_tensor(out=hn[:], in0=tot[:, :, 0:128], in1=n8[:, 1, :].unsqueeze(2).to_broadcast([128, 8, 128]), op=ALU.mult), r=allt + ["n81"], w=["hn"])
                    G(lambda e: e.tensor_tensor(out=hj[:], in0=hn[:], in1=hn[:], op=ALU.mult), r=["hn"], w=["hj"])
                    V(lambda e: e.reduce_sum(out=n8[:, 2, :], in_=hj[:], axis=AX.X), r=["hj"], w=["n82"])
                    V(lambda e: e.tensor_scalar(out=n8[:, 3, :], in0=n8[:, 2, :], scalar1=1.0 / 128, scalar2=EPS, op0=ALU.mult, op1=ALU.add), r=["n82"], w=["n83"])
                    A(lambda e: e.activation(out=n8[:, 4, :], in_=n8[:, 3, :], func=AF.Sqrt), r=["n83"], w=["n84"])
                    V(lambda e: e.reciprocal(out=n8[:, 5, :], in_=n8[:, 4, :]), r=["n84"], w=["n85"])
                    V(lambda e: e.tensor_tensor(out=hj[:], in0=hn[:], in1=n8[:, 5, :].unsqueeze(2).to_broadcast([128, 8, 128]), op=ALU.mult), r=["hn", "n85", "hj"], w=["hj"])
                    hjf = hj[:].rearrange("p h e -> p (h e)")
                    G(lambda e: e.tensor_tensor(out=hjf, in0=hjf, in1=mlw[:], op=ALU.mult), r=["hj", "mlw"], w=["hj"])
                    V(lambda e: e.tensor_tensor(out=hjf, in0=hjf, in1=omt[s][:], op=ALU.mult), r=["hj", ("omt", s)], w=["hj"])
                    G(lambda e: e.tensor_tensor(out=ymb[s][:], in0=hjf, in1=gmt[s][:], op=ALU.mult), r=["hj", ("gmt", s)], w=[("ymb", s)])
                    dma(YM[b, tsl, :], ymb[s][:], r=[("ymb", s)], w=["YM"])
                k.barrier()

            with ExitStack() as ph:
                qts = [sbt(ph, f"qts{i}", [128, S], BF16) for i in range(2)]
                kts = [sbt(ph, f"kts{i}", [128, S], BF16) for i in range(2)]
                vh = [sbt(ph, f"vh{i}", [128, NT, 129], BF16) for i in range(2)]
                negMb = sbt(ph, "negMb", [128, 16], F32)
                nw = sbt(ph, "nw", [16, 8], F32)
                dg = sbt(ph, "dg", [16, 16], F32)
                swb = sbt(ph, "swb", [128, 128], F32)
                PTa = [[sbt(ph, f"PTa{c}{i}", [128, 512], BF16) for i in range(2)] for c in range(2)]
                gat = [sbt(ph, f"gat{i}", [128, 128], BF16) for i in range(2)]
                ymt = [sbt(ph, f"ymt{i}", [128, 128], BF16) for i in range(2)]
                mb = [sbt(ph, f"mb{i}", [128, 128], BF16) for i in range(2)]
                mstage = [sbt(ph, f"mstage{i}", [128, 512], BF16) for i in range(2)]
                psS = [[pst(ph, f"psSa{c}{i}", [128, 512], F32) for i in range(2)] for c in range(2)]
                psA = [pst(ph, f"psA{i}", [128, 512], F32) for i in range(3)]
                psT = pst(ph, "psT", [128, 1024], BF16)
                P(lambda e: e.transpose(out=psA[0][0:16, 0:128], in_=nmx[:, 0:16], identity=identf), r=["nmx", "cst"], w=[("psA", 0)])
                P(lambda e: e.transpose(out=psA[0][0:16, 128:256], in_=nmx[:, 16:32], identity=identf), r=["nmx", "cst"], w=[("psA", 0)])
                V(lambda e: e.reduce_max(out=nw[:, 0:2], in_=psA[0][0:16, 0:256].rearrange("p (a t) -> p a t", a=2), axis=AX.X), r=[("psA", 0)], w=["nw0"])
                V(lambda e: e.tensor_tensor(out=nw[:, 2:3], in0=nw[:, 0:1], in1=nw[:, 1:2], op=ALU.mult), r=["nw0"], w=["nw2"])
                A(lambda e: e.activation(out=nw[:, 3:4], in_=nw[:, 2:3], func=AF.Sqrt), r=["nw2"], w=["nw3"])
                V(lambda e: e.tensor_scalar(out=nw[:, 4:5], in0=nw[:, 3:4], scalar1=-0.125, scalar2=None, op0=ALU.mult), r=["nw3"], w=["nw4"])
                V(lambda e: e.tensor_scalar(out=dg[:], in0=identf[0:16, 0:16], scalar1=nw[:, 4:5], scalar2=None, op0=ALU.mult), r=["nw4", "cst"], w=["dg"])
                P(lambda e: e.matmul(psA[1][:, 0:16], lhsT=onesf[0:16, :], rhs=dg[:], start=True, stop=True), r=["dg", "cst"], w=[("psA", 1)])
                V(lambda e: e.tensor_copy(out=negMb[:], in_=psA[1][:, 0:16]), r=[("psA", 1)], w=["negMb"])
                dma(swb[:], subln_w.partition_broadcast(128), w=["swb"])
                V(lambda e: e.tensor_scalar(out=swb[:], in0=swb[:], scalar1=(1.0 - lam_init), scalar2=None, op0=ALU.mult), r=["swb"], w=["swb"])
                for i in range(2):
                    V(lambda e: e.memset(vh[i][:, :, 128:129], 1.0), w=[("vh", i)])
                fcount = 0
                accS = [sbt(ph, f"accS{i}", [128, 3, 387], F32) for i in range(2)]
                rrs = [sbt(ph, f"rrs{i}", [128, 8], F32) for i in range(2)]
                o1s = [sbt(ph, f"o1s{i}", [128, 128], F32) for i in range(2)]
                o2s = [sbt(ph, f"o2s{i}", [128, 128], F32) for i in range(2)]
                ojs = [sbt(ph, f"ojs{i}", [128, 128], F32) for i in range(2)]
                gcnt = 0
                for hd in range(8):
                    s = hd % 2
                    dma(qts[s][:], QT[b, hd], w=[("qts", s)])
                    dma(kts[s][:], KT[b, hd], w=[("kts", s)])
                    for t8 in range(0, NT, 8):
                        te = min(NT, t8 + 8)
                        dma(vh[s][:, t8:te, 0:128], VA[b].rearrange("(tt p) c -> p tt c", p=128)[:, t8:te, hd * 128:(hd + 1) * 128], w=[("vh", s)])
                    blocks = [(g, kt) for g in range(NG) for kt in range(4 * g + 4)]

                    def qk_exp(bi):
                        g, kt = blocks[bi]
                        bs = bi % 2
                        for c in range(2):
                            P(lambda e: e.matmul(psS[c][bs][:], lhsT=kts[s][c * 64:(c + 1) * 64, kt * 128:(kt + 1) * 128], rhs=qts[s][c * 64:(c + 1) * 64, g * 512:(g + 1) * 512], start=True, stop=True),
                              r=[("kts", s), ("qts", s)], w=[("psSa", c, bs)])
                            A(lambda e: e.activation(out=PTa[c][bs][:], in_=psS[c][bs][:], func=AF.Exp, bias=negMb[:, hd * 2 + c:hd * 2 + c + 1], scale=1.0),
                              r=[("psSa", c, bs), "negMb"], w=[("PTa", c, bs)])

                    qk_exp(0)
                    started = [False, False, False]
                    for bi, (g, kt) in enumerate(blocks):
                        bs = bi % 2
                        if bi + 1 < len(blocks):
                            qk_exp(bi + 1)
                        if kt == 0:
                            started = [False, False, False]
                        for qi in range(4):
                            qt_ = 4 * g + qi
                            if kt > qt_:
                                continue
                            for c in range(2):
                                if kt == qt_:
                                    eng = V if c == 0 else G
                                    eng(lambda e: e.tensor_tensor(out=PTa[c][bs][:, qi * 128:(qi + 1) * 128], in0=PTa[c][bs][:, qi * 128:(qi + 1) * 128], in1=cmb[:], op=ALU.mult),
                                        r=[("PTa", c, bs), "cmb"], w=[("PTa", c, bs)])
                                ai = c * 4 + qi
                                bank, slot = ai // 3, ai % 3
                                st_ = not started[bank]
                                started[bank] = True
                                P(lambda e: e.matmul(psA[bank][:, slot * 129:(slot + 1) * 129], lhsT=PTa[c][bs][:, qi * 128:(qi + 1) * 128], rhs=vh[s][:, kt, :], start=st_, stop=(kt == qt_), skip_group_check=True),
                                  r=[("PTa", c, bs), ("vh", s)], w=[("psA", bank)])
                        if kt != 4 * g + 3:
                            continue
                        gs_ = gcnt % 2
                        gcnt += 1
                        for bank in range(3):
                            ncol_ = 387 if bank < 2 else 258
                            A(lambda e: e.activation(out=accS[gs_][:, bank, 0:ncol_], in_=psA[bank][:, 0:ncol_], func=AF.Copy), r=[("psA", bank)], w=[("accS", gs_, bank)])
                        ms = g % 2
                        for qi in range(4):
                            qt_ = 4 * g + qi
                            tsl = slice(qt_ * 128, (qt_ + 1) * 128)
                            fs = fcount % 2
                            fcount += 1
                            rr, o1, o2, oj = rrs[fs], o1s[fs], o2s[fs], ojs[fs]
                            dma(gat[fs][:], GAS[b, tsl, hd * 128:(hd + 1) * 128], w=[("gat", fs)])
                            dma(ymt[fs][:], YM[b, tsl, hd * 128:(hd + 1) * 128], w=[("ymt", fs)])
                            a1 = accS[gs_][:, qi // 3, (qi % 3) * 129:(qi % 3 + 1) * 129]
                            a2i = 4 + qi
                            a2 = accS[gs_][:, a2i // 3, (a2i % 3) * 129:(a2i % 3 + 1) * 129]
                            rk = [("accS", gs_, qi // 3), ("accS", gs_, a2i // 3)]
                            V(lambda e: e.reciprocal(out=rr[:, 0:1], in_=a1[:, 128:129]), r=rk, w=[("rr0", fs)])
                            V(lambda e: e.reciprocal(out=rr[:, 1:2], in_=a2[:, 128:129]), r=rk, w=[("rr1", fs)])
                            V(lambda e: e.tensor_tensor(out=rr[:, 2:3], in0=rr[:, 1:2], in1=lamt[:, 0:1], op=ALU.mult), r=[("rr1", fs), "lamt"], w=[("rr2", fs)])
                            A(lambda e: e.activation(out=o1[:], in_=a1[:, 0:128], func=AF.Copy, scale=rr[:, 0:1]), r=rk + [("rr0", fs)], w=[("o1", fs)])
                            V(lambda e: e.scalar_tensor_tensor(out=o2[:], in0=a2[:, 0:128], scalar=rr[:, 2:3], in1=o1[:], op0=ALU.mult, op1=ALU.add), r=rk + [("rr2", fs), ("o1", fs)], w=[("o2", fs)])
                            A(lambda e: e.activation(out=oj[:], in_=o2[:], func=AF.Square, accum_out=rr[:, 3:4]), r=[("o2", fs)], w=[("oj", fs), ("rr3", fs)])
                            V(lambda e: e.tensor_scalar(out=rr[:, 4:5], in0=rr[:, 3:4], scalar1=1.0 / 128, scalar2=EPS, op0=ALU.mult, op1=ALU.add), r=[("rr3", fs)], w=[("rr4", fs)])
                            A(lambda e: e.activation(out=rr[:, 5:6], in_=rr[:, 4:5], func=AF.Sqrt), r=[("rr4", fs)], w=[("rr5", fs)])
                            V(lambda e: e.reciprocal(out=rr[:, 6:7], in_=rr[:, 5:6]), r=[("rr5", fs)], w=[("rr6", fs)])
                            V(lambda e: e.scalar_tensor_tensor(out=oj[:], in0=o2[:], scalar=rr[:, 6:7], in1=swb[:], op0=ALU.mult, op1=ALU.mult), r=[("o2", fs), ("rr6", fs), "swb", ("oj", fs)], w=[("oj", fs)])
                            G(lambda e: e.tensor_tensor(out=oj[:], in0=oj[:], in1=gat[fs][:], op=ALU.mult), r=[("oj", fs), ("gat", fs)], w=[("oj", fs)])
                            G(lambda e: e.tensor_tensor(out=mb[fs][:], in0=oj[:], in1=ymt[fs][:], op=ALU.add), r=[("oj", fs), ("ymt", fs)], w=[("mb", fs)])
                            P(lambda e: e.transpose(out=psT[:, qi * 128:(qi + 1) * 128], in_=mb[fs][:], identity=identb[:]), r=[("mb", fs), "identb"], w=["psT"])
                        V(lambda e: e.tensor_copy(out=mstage[ms][:], in_=psT[:, 0:512]), r=["psT"], w=[("mstage", ms)])
                        dma(MT[b, hd, :, g * 512:(g + 1) * 512], mstage[ms][:], r=[("mstage", ms)], w=["MT"])
                k.barrier()

        wq_v = peer_wq.rearrange("(kc p) n -> p kc n", p=128)
        wo_v = w_out.rearrange("(kc p) n -> p kc n", p=128)
        with ExitStack() as ph:
            wob = sbt(ph, "wob", [128, 8, D], BF16)
            wqb = sbt(ph, "wqb", [128, 8, 2048], BF16)
            keyT = sbt(ph, "keyT", [128, 16, 128], BF16)
            psT = [pst(ph, f"psTE{i}", [128, 1024], BF16) for i in range(2)]
            ph2 = ExitStack()
            wtmp = [sbt(ph2, f"wtmp{i}", [128, 8, 512], F32) for i in range(2)]
            ktmp = sbt(ph2, "ktmp", [128, 16, 128], F32)
            ktb = sbt(ph2, "ktb", [128, 16, 128], BF16)
            wi = 0
            for n0 in range(0, D, 512):
                s = wi % 2
                wi += 1
                dma(wtmp[s][:], wo_v[:, :, n0:n0 + 512], w=[("wtmp", s)])
                V(lambda e: e.tensor_copy(out=wob[:, :, n0:n0 + 512], in_=wtmp[s][:]), r=[("wtmp", s)], w=["wob"])
            for n0 in range(0, 2048, 512):
                s = wi % 2
                wi += 1
                dma(wtmp[s][:], wq_v[:, :, n0:n0 + 512], w=[("wtmp", s)])
                G(lambda e: e.tensor_copy(out=wqb[:, :, n0:n0 + 512], in_=wtmp[s][:]), r=[("wtmp", s)], w=["wqb"])
            dma(ktmp[:], peer_keys.rearrange("g n d -> n g d"), w=["ktmp"])
            V(lambda e: e.tensor_copy(out=ktb[:], in_=ktmp[:]), r=["ktmp"], w=["ktb"])
            for g8 in range(2):
                for j in range(8):
                    gi = g8 * 8 + j
                    P(lambda e: e.transpose(out=psT[g8][:, j * 128:(j + 1) * 128], in_=ktb[:, gi, :], identity=identb[:]), r=["ktb", "identb"], w=[("psTE", g8)])
                V(lambda e: e.tensor_copy(out=keyT[:, g8 * 8:(g8 + 1) * 8, :], in_=psT[g8][:].rearrange("p (g n) -> p g n", g=8)), r=[("psTE", g8)], w=["keyT"])
            k.barrier()
            ph2.close()
            gt1 = sbt(ph, "gt1", [128, D], F32)
            A2 = sbt(ph, "A2", [128, D], F32)
            B2 = sbt(ph, "B2", [128, D], F32)
            w2b = sbt(ph, "w2b", [128, D], F32)
            mt = [sbt(ph, f"mt{i}", [128, 8, 128], BF16) for i in range(2)]
            xt = [sbt(ph, f"xtE{i}", [128, D], F32) for i in range(2)]
            x1 = [sbt(ph, f"x1E{i}", [128, D], F32) for i in range(2)]
            junk = sbt(ph, "junkE", [128, D], BF16)
            hx = sbt(ph, "hxE", [128, D], F32)
            hb = sbt(ph, "hbE", [128, D], BF16)
            h2t = [sbt(ph, f"h2t{i}", [128, 8, 128], BF16) for i in range(2)]
            qpt = sbt(ph, "qpt", [128, 16, 128], BF16)
            scs = [sbt(ph, f"sc{i}", [128, 16, 128], F32) for i in range(2)]
            wk = sbt(ph, "wkE", [128, 16, 128], F32)
            v16 = sbt(ph, "v16", [128, 16, 16], F32)
            i16u = sbt(ph, "i16u", [128, 16, 16], U32)
            i16v = sbt(ph, "i16v", [128, 16, 16], F32)
            cand = sbt(ph, "cand", [128, 8, 256], F32)
            cwk = sbt(ph, "cwk", [128, 8, 256], F32)
            c16 = sbt(ph, "c16", [128, 8, 16], F32)
            p16u = sbt(ph, "p16u", [128, 8, 16], U32)
            p16 = sbt(ph, "p16", [128, 8, 16], F32)
            d1 = sbt(ph, "d1", [128, 8, 16, 16], F32)
            d2 = sbt(ph, "d2", [128, 8, 16, 16], F32)
            ar = sbt(ph, "ar", [128, 8, 16], F32)
            br = sbt(ph, "br", [128, 8, 16], F32)
            igt = [sbt(ph, f"igt{i}", [128, 3, 128], F32) for i in range(2)]
            st4 = sbt(ph, "st4E", [128, 16], F32)
            psO = [pst(ph, f"psO{i}", [128, 512], F32) for i in range(2)]
            psQ = [pst(ph, f"psQ{i}", [128, 512], F32) for i in range(2)]
            psX = [pst(ph, f"psX{i}", [128, 512], F32) for i in range(2)]
            dma(w2b[:], norm2_w.partition_broadcast(128), w=["w2b"])
            for b in range(NB):
                dma(gt1[:], MOD[b:b + 1, 2 * D:3 * D].partition_broadcast(128), r=["MOD"], w=["gt1"])
                dma(B2[:], MOD[b:b + 1, 3 * D:4 * D].partition_broadcast(128), r=["MOD"], w=["B2"])
                dma(A2[:], MOD[b:b + 1, 4 * D:5 * D].partition_broadcast(128), r=["MOD"], w=["A2"])
                V(lambda e: e.scalar_tensor_tensor(out=A2[:], in0=A2[:], scalar=1.0, in1=w2b[:], op0=ALU.add, op1=ALU.mult), r=["A2", "w2b"], w=["A2"])
                for tt in range(NT):
                    s = tt % 2
                    tsl = slice(tt * 128, (tt + 1) * 128)
                    dma(mt[s][:], MT[b, :, :, tsl].rearrange("h p t -> p h t"), r=["MT"], w=[("mt", s)])
                    dma(xt[s][:], x[b, tsl, :], w=[("xtE", s)])
                    for nh in range(2):
                        for kc in range(8):
                            P(lambda e: e.matmul(psO[nh][:], lhsT=mt[s][:, kc, :], rhs=wob[:, kc, nh * 512:(nh + 1) * 512], start=(kc == 0), stop=(kc == 7)),
                              r=[("mt", s), "wob"], w=[("psO", nh)])
                        V(lambda e: e.tensor_tensor(out=x1[s][:, nh * 512:(nh + 1) * 512], in0=psO[nh][:], in1=gt1[:, nh * 512:(nh + 1) * 512], op=ALU.mult), r=[("psO", nh), "gt1"], w=[("x1E", s)])
                    G(lambda e: e.tensor_tensor(out=x1[s][:], in0=x1[s][:], in1=xt[s][:], op=ALU.add), r=[("x1E", s), ("xtE", s)], w=[("x1E", s)])
                    dma(X1[b, tsl, :], x1[s][:], r=[("x1E", s)], w=["X1"])
                    A(lambda e: e.activation(out=junk[:], in_=x1[s][:], func=AF.Square, accum_out=st4[:, 0:1]), r=[("x1E", s)], w=["junkE", "sE0"])
                    V(lambda e: e.tensor_scalar(out=st4[:, 1:2], in0=st4[:, 0:1], scalar1=1.0 / D, scalar2=EPS, op0=ALU.mult, op1=ALU.add), r=["sE0"], w=["sE1"])
                    A(lambda e: e.activation(out=st4[:, 2:3], in_=st4[:, 1:2], func=AF.Sqrt), r=["sE1"], w=["sE2"])
                    V(lambda e: e.reciprocal(out=st4[:, 3:4], in_=st4[:, 2:3]), r=["sE2"], w=["sE3"])
                    V(lambda e: e.scalar_tensor_tensor(out=hx[:], in0=x1[s][:], scalar=st4[:, 3:4], in1=A2[:], op0=ALU.mult, op1=ALU.mult), r=[("x1E", s), "sE3", "A2"], w=["hxE"])
                    G(lambda e: e.tensor_tensor(out=hb[:], in0=hx[:], in1=B2[:], op=ALU.add), r=["hxE", "B2"], w=["hbE"])
                    for kc in range(8):
                        P(lambda e: e.transpose(out=psT[0][:, kc * 128:(kc + 1) * 128], in_=hb[:, kc * 128:(kc + 1) * 128], identity=identb[:]), r=["hbE", "identb"], w=[("psTE", 0)])
                    A(lambda e: e.activation(out=h2t[s][:], in_=psT[0][:].rearrange("p (k t) -> p k t", k=8), func=AF.Copy), r=[("psTE", 0)], w=[("h2t", s)])
                    dma(H2T[b, :, :, tsl].rearrange("k p t -> p k t"), h2t[s][:], r=[("h2t", s)], w=["H2T"])
                    for gq in range(4):
                        pq = gq % 2
                        for j in range(4):
                            gi = gq * 4 + j
                            for kc in range(8):
                                P(lambda e: e.matmul(psQ[pq][:, j * 128:(j + 1) * 128], lhsT=wqb[:, kc, gi * 128:(gi + 1) * 128], rhs=h2t[s][:, kc, :], start=(kc == 0), stop=(kc == 7)),
                                  r=["wqb", ("h2t", s)], w=[("psQ", pq)])
                        A(lambda e: e.activation(out=qpt[:, gq * 4:(gq + 1) * 4, :], in_=psQ[pq][:].rearrange("p (g t) -> p g t", g=4), func=AF.Copy), r=[("psQ", pq)], w=["qpt"])
                    sc = scs[s]
                    for gq in range(4):
                        px = gq % 2
                        for j in range(4):
                            gi = gq * 4 + j
                            P(lambda e: e.matmul(psX[px][:, j * 128:(j + 1) * 128], lhsT=qpt[:, gi, :], rhs=keyT[:, gi, :], start=True, stop=True), r=["qpt", "keyT"], w=[("psX", px)])
                        A(lambda e: e.activation(out=sc[:, gq * 4:(gq + 1) * 4, :], in_=psX[px][:].rearrange("p (g n) -> p g n", g=4), func=AF.Copy), r=[("psX", px)], w=[("sc", s, gq)])
                    for gi in range(16):
                        V(lambda e: e.max(out=v16[:, gi, 0:8], in_=sc[:, gi, :]), r=[("sc", s, gi // 4)], w=[("v16a", gi)])
                    for gi in range(16):
                        V(lambda e: e.max_index(out=i16u[:, gi, 0:8], in_max=v16[:, gi, 0:8], in_values=sc[:, gi, :]), r=[("sc", s, gi // 4), ("v16a", gi)], w=[("i16a", gi)])
                    for gi in range(16):
                        V(lambda e: e.match_replace(out=wk[:, gi, :], in_to_replace=v16[:, gi, 0:8], in_values=sc[:, gi, :], imm_value=-1e30), r=[("sc", s, gi // 4), ("v16a", gi)], w=[("wkE", gi)])
                    for gi in range(16):
                        V(lambda e: e.max(out=v16[:, gi, 8:16], in_=wk[:, gi, :]), r=[("wkE", gi)], w=[("v16b", gi)])
                    for gi in range(16):
                        V(lambda e: e.max_index(out=i16u[:, gi, 8:16], in_max=v16[:, gi, 8:16], in_values=wk[:, gi, :]), r=[("wkE", gi), ("v16b", gi)], w=[("i16b", gi)])
                    allv = [("v16a", gi) for gi in range(16)] + [("v16b", gi) for gi in range(16)]
                    alli = [("i16a", gi) for gi in range(16)] + [("i16b", gi) for gi in range(16)]
                    V(lambda e: e.tensor_copy(out=i16v[:], in_=i16u[:]), r=alli, w=["i16v"])
                    v16v = v16[:].rearrange("p (h q) k -> p h q k", q=2)
                    i16vv = i16v[:].rearrange("p (h q) k -> p h q k", q=2)
                    V(lambda e: e.tensor_tensor(out=cand[:].rearrange("p h (a b) -> p h a b", a=16), in0=v16v[:, :, 0, :].unsqueeze(3).to_broadcast([128, 8, 16, 16]),
                                                in1=v16v[:, :, 1, :].unsqueeze(2).to_broadcast([128, 8, 16, 16]), op=ALU.add), r=allv, w=["cand"])
                    for h in range(8):
                        V(lambda e: e.max(out=c16[:, h, 0:8], in_=cand[:, h, :]), r=["cand"], w=[("c16a", h)])
                    for h in range(8):
                        V(lambda e: e.max_index(out=p16u[:, h, 0:8], in_max=c16[:, h, 0:8], in_values=cand[:, h, :]), r=["cand", ("c16a", h)], w=[("p16a", h)])
                    for h in range(8):
                        V(lambda e: e.match_replace(out=cwk[:, h, :], in_to_replace=c16[:, h, 0:8], in_values=cand[:, h, :], imm_value=-1e30), r=["cand", ("c16a", h)], w=[("cwk", h)])
                    for h in range(8):
                        V(lambda e: e.max(out=c16[:, h, 8:16], in_=cwk[:, h, :]), r=[("cwk", h)], w=[("c16b", h)])
                    for h in range(8):
                        V(lambda e: e.max_index(out=p16u[:, h, 8:16], in_max=c16[:, h, 8:16], in_values=cwk[:, h, :]), r=[("cwk", h), ("c16b", h)], w=[("p16b", h)])
                    allc = [("c16a", h) for h in range(8)] + [("c16b", h) for h in range(8)]
                    allp = [("p16a", h) for h in range(8)] + [("p16b", h) for h in range(8)]
                    V(lambda e: e.tensor_copy(out=p16[:], in_=p16u[:]), r=allp, w=["p16"])
                    p16b = p16[:].unsqueeze(3).to_broadcast([128, 8, 16, 16])
                    a16b = a16f.unsqueeze(1).unsqueeze(1).to_broadcast([128, 8, 16, 16])
                    i16b = i16f.unsqueeze(1).unsqueeze(1).to_broadcast([128, 8, 16, 16])
                    V(lambda e: e.tensor_tensor(out=d1[:], in0=p16b, in1=a16b, op=ALU.subtract), r=["p16", "cst"], w=["d1"])
                    V(lambda e: e.tensor_scalar(out=d2[:], in0=d1[:], scalar1=0.0, scalar2=None, op0=ALU.is_ge), r=["d1"], w=["d2"])
                    V(lambda e: e.tensor_scalar(out=d1[:], in0=d1[:], scalar1=15.5, scalar2=None, op0=ALU.is_lt), r=["d1", "d2"], w=["d1"])
                    V(lambda e: e.tensor_tensor(out=d1[:], in0=d1[:], in1=d2[:], op=ALU.mult), r=["d1", "d2"], w=["d1"])
                    G(lambda e: e.tensor_tensor(out=d2[:], in0=d1[:], in1=a16b, op=ALU.mult), r=["d1", "cst"], w=["d2"])
                    V(lambda e: e.reduce_sum(out=ar[:], in_=d2[:], axis=AX.X), r=["d2"], w=["ar"])
                    G(lambda e: e.tensor_tensor(out=d2[:], in0=d1[:], in1=i16vv[:, :, 0, :].unsqueeze(2).to_broadcast([128, 8, 16, 16]), op=ALU.mult), r=["d1", "i16v", "ar"], w=["d2"])
                    V(lambda e: e.reduce_sum(out=igt[s][:, 0, :].rearrange("p (h r) -> p h r", h=8), in_=d2[:], axis=AX.X), r=["d2"], w=[("igt", s)])
                    V(lambda e: e.tensor_tensor(out=br[:], in0=p16[:], in1=ar[:], op=ALU.subtract), r=["p16", "ar"], w=["br"])
                    V(lambda e: e.tensor_tensor(out=d1[:], in0=br[:].unsqueeze(3).to_broadcast([128, 8, 16, 16]), in1=i16b, op=ALU.is_equal), r=["br", "cst", "d2"], w=["d1"])
                    G(lambda e: e.tensor_tensor(out=d2[:], in0=d1[:], in1=i16vv[:, :, 1, :].unsqueeze(2).to_broadcast([128, 8, 16, 16]), op=ALU.mult), r=["d1", "i16v"], w=["d2"])
                    V(lambda e: e.reduce_sum(out=igt[s][:, 1, :].rearrange("p (h r) -> p h r", h=8), in_=d2[:], axis=AX.X), r=["d2", ("igt", s)], w=[("igt", s)])
                    V(lambda e: e.tensor_tensor(out=ar[:], in0=c16[:], in1=c16[:, :, 0:1].to_broadcast([128, 8, 16]), op=ALU.subtract), r=allc + ["br"], w=["ar"])
                    A(lambda e: e.activation(out=ar[:], in_=ar[:], func=AF.Exp), r=["ar"], w=["ar"])
                    V(lambda e: e.reduce_sum(out=st4[:, 8:16], in_=ar[:], axis=AX.X), r=["ar"], w=["sE8"])
                    V(lambda e: e.reciprocal(out=st4[:, 8:16], in_=st4[:, 8:16]), r=["sE8"], w=["sE8"])
                    V(lambda e: e.tensor_tensor(out=igt[s][:, 2, :].rearrange("p (h r) -> p h r", h=8), in0=ar[:], in1=st4[:, 8:16].unsqueeze(2).to_broadcast([128, 8, 16]), op=ALU.mult),
                      r=["ar", "sE8", ("igt", s)], w=[("igt", s)])
                    dma(IG[b, tsl, :, :], igt[s][:], r=[("igt", s)], w=["IG"])
            k.barrier()

        TG = 256
        NGR = T // TG
        X1f = X1.rearrange("b s d -> (b s) d")
        outf = out.rearrange("b s d -> (b s) d")
        IGf = IG.rearrange("b s a r -> (b s) a r")
        with ExitStack() as ph:
            Gm = [sbt(ph, f"Gm{i}", [128, TG, 128], BF16) for i in range(2)]
            ust = [sbt(ph, f"ust{i}", [128, 2, 1024], BF16) for i in range(3)]
            vst = [sbt(ph, f"vst{i}", [128, 2, 1024], BF16) for i in range(3)]
            h2s = [sbt(ph, f"h2F{i}", [128, 8, TG], BF16) for i in range(2)]
            igl = sbt(ph, "igl", [128, 2, 3, 128], F32)
            igT = sbt(ph, "igT", [128, 3, TG], F32)
            ohA = [sbt(ph, f"ohA{i}", [128, 8, 128], BF16) for i in range(2)]
            ohB = [sbt(ph, f"ohB{i}", [128, 8, 128], BF16) for i in range(2)]
            ohT = sbt(ph, "ohT", [128, 8, 128], BF16)
            act = [sbt(ph, f"actF{i}", [128, TG], BF16) for i in range(4)]
            ga = [sbt(ph, f"gaF{i}", [128, TG], BF16) for i in range(4)]
            gt2 = sbt(ph, "gt2", [128, D], F32)
            fwb = sbt(ph, "fwb", [128, D], F32)
            x1l = sbt(ph, "x1l", [128, D], F32)
            yo = sbt(ph, "yo", [128, D], F32)
            oo = sbt(ph, "oo", [128, D], F32)
            st4 = sbt(ph, "st4F", [128, 8], F32)
            psY = [pst(ph, f"psY{i}", [128, 512], F32) for i in range(4)]
            psSc = [pst(ph, f"psSc{i}", [128, 512], F32) for i in range(2)]
            psGb = [pst(ph, f"psGb{i}", [128, 512], F32) for i in range(2)]
            dma(fwb[:], fin_w.partition_broadcast(128), w=["fwb"])
            UTv = UT.rearrange("i p f -> p i f")
            VBv = VB.rearrange("i p f -> p i f")
            ldc = [0]
            gcount = [0]
            slots = {}
            iob = iotaf.unsqueeze(1).to_broadcast([128, 8, 128])

            def prep_group(gr):
                t0 = gr * TG
                b = t0 // S
                s0 = t0 % S
                hs = gr % 2
                dma(h2s[hs][:], H2T[b, :, :, s0:s0 + TG].rearrange("k p t -> p k t"), w=[("h2F", hs)])
                dma(igl[:].rearrange("p j a r -> p j (a r)"), IGf[t0:t0 + TG].rearrange("(j p) a r -> p j (a r)", p=128), w=["igl"])
                for j in range(2):
                    for a in range(3):
                        P(lambda e: e.transpose(out=psGb[0][:, 0:128], in_=igl[:, j, a, :], identity=identf), r=["igl", "cst"], w=[("psGb", 0)])
                        V(lambda e: e.tensor_copy(out=igT[:, a, j * 128:(j + 1) * 128], in_=psGb[0][:, 0:128]), r=[("psGb", 0)], w=["igT"])

            def gb_onehots(gr, sub):
                os_ = sub % 2
                tq = slice(sub * 8, (sub + 1) * 8)
                V(lambda e: e.tensor_tensor(out=ohA[os_][:], in0=iob, in1=igT[:, 0, tq].unsqueeze(2).to_broadcast([128, 8, 128]), op=ALU.is_equal), r=["cst", "igT"], w=[("ohA", os_)])
                V(lambda e: e.tensor_tensor(out=ohT[:], in0=iob, in1=igT[:, 1, tq].unsqueeze(2).to_broadcast([128, 8, 128]), op=ALU.is_equal), r=["cst", "igT"], w=["ohT"])
                G(lambda e: e.tensor_tensor(out=ohB[os_][:], in0=ohT[:], in1=igT[:, 2, tq].unsqueeze(2).to_broadcast([128, 8, 128]), op=ALU.mult), r=["ohT", "igT"], w=[("ohB", os_)])

            def gb_mm(gr, sub):
                os_ = sub % 2
                gb = gr % 2
                for q4 in range(2):
                    pg = gcount[0] % 2
                    gcount[0] += 1
                    for j in range(4):
                        tl = q4 * 4 + j
                        P(lambda e: e.matmul(psGb[pg][:, j * 128:(j + 1) * 128], lhsT=ohB[os_][:, tl, :], rhs=ohA[os_][:, tl, :], start=True, stop=True),
                          r=[("ohA", os_), ("ohB", os_)], w=[("psGb", pg)])
                    tb = sub * 8 + q4 * 4
                    if pg == 0:
                        A(lambda e: e.activation(out=Gm[gb][:, tb:tb + 4, :], in_=psGb[pg][:].rearrange("p (t i) -> p t i", t=4), func=AF.Copy), r=[("psGb", pg)], w=[("Gm", gb)])
                    else:
                        V(lambda e: e.tensor_copy(out=Gm[gb][:, tb:tb + 4, :], in_=psGb[pg][:].rearrange("p (t i) -> p t i", t=4)), r=[("psGb", pg)], w=[("Gm", gb)])

            def load_blk(i2b):
                ldc[0] += 1
                ss_ = ldc[0] % 3
                slots[i2b] = ss_
                dma(ust[ss_][:], UTv[:, i2b * 2:(i2b + 1) * 2, :], r=["UT"], w=[("ust", ss_)])
                dma(vst[ss_][:], VBv[:, i2b * 2:(i2b + 1) * 2, :], r=["VB"], w=[("vst", ss_)])

            def scores(gr, i1):
                ss_ = slots[i1 // 2]
                j = i1 % 2
                q = i1 % 2
                h2 = h2s[gr % 2]
                for kc in range(8):
                    P(lambda e: e.matmul(psSc[q][:, 0:TG], lhsT=ust[ss_][:, j, kc * 128:(kc + 1) * 128], rhs=h2[:, kc, :], start=(kc == 0), stop=(kc == 7)),
                      r=[("ust", ss_), ("h2F", gr % 2)], w=[("psSc", q)])

            prep_group(0)
            for sub in range(TG // 8):
                gb_onehots(0, sub)
                gb_mm(0, sub)
            if NGR > 1:
                prep_group(1)
            for gr in range(NGR):
                t0 = gr * TG
                b = t0 // S
                s0 = t0 % S
                gb = gr % 2
                if s0 == 0:
                    dma(gt2[:], MOD[b:b + 1, 5 * D:6 * D].partition_broadcast(128), r=["MOD"], w=["gt2"])
                nxt = gr + 1 < NGR
                load_blk(0)
                load_blk(1)
                scores(gr, 0)
                for i1 in range(128):
                    ss_ = slots[i1 // 2]
                    j = i1 % 2
                    q = i1 % 2
                    if i1 % 2 == 0 and i1 // 2 + 2 < 64:
                        load_blk(i1 // 2 + 2)
                    if i1 + 1 < 128:
                        scores(gr, i1 + 1)
                    if nxt and i1 % 4 == 0:
                        sub = i1 // 4
                        gb_onehots(gr + 1, sub)
                        if sub >= 1:
                            gb_mm(gr + 1, sub - 1)
                    A(lambda e: e.activation(out=act[q][:], in_=psSc[q][:, 0:TG], func=AF.Gelu), r=[("psSc", q)], w=[("actF", q)])
                    V(lambda e: e.tensor_tensor(out=ga[q][:], in0=act[q][:], in1=Gm[gb][:, :, i1], op=ALU.mult), r=[("actF", q), ("Gm", gb)], w=[("gaF", q)])
                    for tj in range(2):
                        for nh in range(2):
                            P(lambda e: e.matmul(psY[tj * 2 + nh][:], lhsT=ga[q][:, tj * 128:(tj + 1) * 128], rhs=vst[ss_][:, j, nh * 512:(nh + 1) * 512], start=(i1 == 0), stop=(i1 == 127)),
                              r=[("gaF", q), ("vst", ss_)], w=[("psY", tj * 2 + nh)])
                if nxt:
                    gb_mm(gr + 1, 31)
                if gr + 2 < NGR:
                    prep_group(gr + 2)
                for tj in range(2):
                    dma(x1l[:], X1f[t0 + tj * 128:t0 + (tj + 1) * 128, :], w=["x1l"])
                    for nh in range(2):
                        V(lambda e: e.tensor_tensor(out=yo[:, nh * 512:(nh + 1) * 512], in0=psY[tj * 2 + nh][:], in1=gt2[:, nh * 512:(nh + 1) * 512], op=ALU.mult), r=[("psY", tj * 2 + nh), "gt2"], w=["yo"])
                    G(lambda e: e.tensor_tensor(out=yo[:], in0=yo[:], in1=x1l[:], op=ALU.add), r=["yo", "x1l"], w=["yo"])
                    A(lambda e: e.activation(out=x1l[:], in_=yo[:], func=AF.Square, accum_out=st4[:, 0:1]), r=["yo", "x1l"], w=["x1l", "sF0"])
                    V(lambda e: e.tensor_scalar(out=st4[:, 1:2], in0=st4[:, 0:1], scalar1=1.0 / D, scalar2=EPS, op0=ALU.mult, op1=ALU.add), r=["sF0"], w=["sF1"])
                    A(lambda e: e.activation(out=st4[:, 2:3], in_=st4[:, 1:2], func=AF.Sqrt), r=["sF1"], w=["sF2"])
                    V(lambda e: e.reciprocal(out=st4[:, 3:4], in_=st4[:, 2:3]), r=["sF2"], w=["sF3"])
                    V(lambda e: e.scalar_tensor_tensor(out=oo[:], in0=yo[:], scalar=st4[:, 3:4], in1=fwb[:], op0=ALU.mult, op1=ALU.mult), r=["yo", "sF3", "fwb", "oo"], w=["oo"])
                    dma(outf[t0 + tj * 128:t0 + (tj + 1) * 128, :], oo[:], r=["oo"], w=["OUT", "oo"])
            k.barrier()
    return nc


_INPUT_ORDER = ["x", "c", "ada_w", "ada_b", "norm1_w", "norm2_w", "w_in", "conv_w", "conv_b", "ml_i_bias", "ml_f_bias",
                "ml_norm_w", "lam_q1", "lam_k1", "lam_q2", "lam_k2", "subln_w", "w_out", "peer_wq", "peer_keys",
                "peer_u", "peer_v", "final_norm_w"]


def make_in_maps(cfg, inputs, n_cores):
    f = lambda a: np.ascontiguousarray(np.asarray(a, dtype=np.float32))
    NB = cfg.NB
    shared = {
        "ada_w": f(inputs["ada_w"][0]), "ada_b": f(inputs["ada_b"][0]).reshape(1, -1),
        "norm1_w": f(inputs["norm1_w"][0]).reshape(1, -1), "norm2_w": f(inputs["norm2_w"][0]).reshape(1, -1),
        "w_in": f(inputs["w_in"][0]), "conv_w": f(inputs["conv_w"][0]), "conv_b": f(inputs["conv_b"][0]).reshape(1, -1),
        "ml_i_bias": f(inputs["ml_i_bias"][0]).reshape(1, -1), "ml_f_bias": f(inputs["ml_f_bias"][0]).reshape(1, -1),
        "ml_norm_w": f(inputs["ml_norm_w"][0]).reshape(1, -1),
        "lam_q1": f(inputs["lam_q1"][0]).reshape(1, -1), "lam_k1": f(inputs["lam_k1"][0]).reshape(1, -1),
        "lam_q2": f(inputs["lam_q2"][0]).reshape(1, -1), "lam_k2": f(inputs["lam_k2"][0]).reshape(1, -1),
        "subln_w": f(inputs["subln_w"][0]).reshape(1, -1), "w_out": f(inputs["w_out"][0]),
        "peer_wq": f(inputs["peer_wq"][0]), "peer_keys": f(inputs["peer_keys"][0]).reshape(16, 128, 128),
        "peer_u": f(inputs["peer_u"][0]), "peer_v": f(inputs["peer_v"][0]),
        "final_norm_w": f(inputs["final_norm_w"]).reshape(1, -1),
        "cst": host_consts(cfg),
    }
    xs = f(inputs["x"])
    cs = f(inputs["c"])
    maps = []
    for i in range(n_cores):
        m = dict(shared)
        m["x"] = np.ascontiguousarray(xs[i * NB:(i + 1) * NB])
        m["c"] = np.ascontiguousarray(cs[i * NB:(i + 1) * NB])
        maps.append(m)
    return maps


def kernel(**inputs):
    n_cores = 8
    cfg = Cfg(S=4096, NB=2)
    nc = build(cfg)
    maps = make_in_maps(cfg, inputs, n_cores)
    res = run_bass_kernel_spmd(nc, maps, core_ids=list(range(n_cores)))
    outs = [np.asarray(r["out"], dtype=np.float32) for r in res.results]
    return np.concatenate(outs, axis=0)
```

```python
import math
from contextlib import ExitStack

import numpy as np
import ml_dtypes
import concourse.bass as bass
import concourse.mybir as mybir
from concourse.bass_utils import run_bass_kernel_spmd

F32 = mybir.dt.float32
BF16 = mybir.dt.bfloat16
U32 = mybir.dt.uint32
ALU = mybir.AluOpType
AF = mybir.ActivationFunctionType
AX = mybir.AxisListType

D = 1024
EPS = 1e-6
IN_W = 8208
NEG = -30000.0


class Tok:
    __slots__ = ("sem", "val", "eng")

    def __init__(self, sem, val, eng):
        self.sem, self.val, self.eng = sem, val, eng


class Eng:
    def __init__(self, kb, name, eng):
        self.kb, self.name, self.eng = kb, name, eng
        self.sem = None
        self.count = 0
        self.seen = {}
        self.last = None
        self.nsem = 0

    def wait(self, tok):
        if tok is None:
            return
        key = id(tok.sem)
        if self.seen.get(key, 0) >= tok.val:
            return
        self.seen[key] = tok.val
        self.eng.wait_ge(tok.sem, tok.val)

    def signal(self, instr):
        if self.sem is None or self.count >= 30000:
            self.sem = self.kb.es.enter_context(self.kb.nc.semaphore(f"s_{self.name}_{self.nsem}"))
            self.nsem += 1
            self.count = 0
        self.count += 1
        instr.then_inc(self.sem, 1)
        t = Tok(self.sem, self.count, self.name)
        self.last = t
        return t


class KB:
    def __init__(self, nc):
        self.nc = nc
        self.es = ExitStack()
        self.e = {
            "pe": Eng(self, "pe", nc.tensor),
            "act": Eng(self, "act", nc.scalar),
            "dve": Eng(self, "dve", nc.vector),
            "pool": Eng(self, "pool", nc.gpsimd),
            "sp": Eng(self, "sp", nc.sync),
        }
        self.res = {}
        self.dsems = []
        self.dvals = []
        self.dnext = 0
        self.ND = 40
        self.all_dma = []

    def _deps(self, r, w):
        deps = []
        for k in r:
            st = self.res.get(k)
            if st is not None and st[0] is not None:
                deps.append(st[0])
        for k in w:
            st = self.res.get(k)
            if st is not None:
                if st[0] is not None:
                    deps.append(st[0])
                deps.extend(st[1])
        return deps

    def _record(self, tok, r, w):
        for k in r:
            st = self.res.setdefault(k, [None, []])
            if tok.eng != "dma":
                st[1] = [t for t in st[1] if t.eng != tok.eng]
            st[1].append(tok)
        for k in w:
            self.res[k] = [tok, []]

    def op(self, en, fn, r=(), w=()):
        E = self.e[en]
        for t in self._deps(r, w):
            if en == "pe" and t.eng == "pe":
                continue
            E.wait(t)
        instr = fn(E.eng)
        tok = E.signal(instr)
        self._record(tok, r, w)
        return tok

    def P(self, fn, r=(), w=()):
        return self.op("pe", fn, r, w)

    def A(self, fn, r=(), w=()):
        return self.op("act", fn, r, w)

    def V(self, fn, r=(), w=()):
        return self.op("dve", fn, r, w)

    def G(self, fn, r=(), w=()):
        return self.op("pool", fn, r, w)

    def dma(self, out, in_, r=(), w=(), q="sp", **kw):
        E = self.e[q]
        for t in self._deps(r, w):
            E.wait(t)
        if len(self.dsems) < self.ND:
            self.dsems.append(self.es.enter_context(self.nc.semaphore(f"s_dma_{len(self.dsems)}")))
            self.dvals.append(0)
        i = self.dnext
        self.dnext = (self.dnext + 1) % self.ND
        sem = self.dsems[i]
        if self.dvals[i] > 0:
            E.wait(Tok(sem, self.dvals[i], "dma"))
        self.dvals[i] += 16
        instr = E.eng.dma_start(out=out, in_=in_, **kw)
        instr.then_inc(sem, 16)
        tok = Tok(sem, self.dvals[i], "dma")
        self._record(tok, r, w)
        return tok

    def barrier(self):
        toks = [E.last for E in self.e.values() if E.last is not None]
        toks += [Tok(s, v, "dma") for s, v in zip(self.dsems, self.dvals) if v > 0]
        for E in self.e.values():
            for t in toks:
                if t.eng == E.name:
                    continue
                E.wait(t)
        self.res = {}


class Cfg:
    def __init__(self, S=4096, NB=2, debug=False):
        self.S, self.NB, self.debug = S, NB, debug
        self.NT = S // 128


def host_consts(cfg):
    NT = cfg.NT
    p = np.arange(128)
    ident = np.eye(128, dtype=np.float32)
    tri = (p[:, None] <= p[None, :]).astype(np.float32)
    negm = np.where(p[:, None] > p[None, :], NEG, 0.0).astype(np.float32)
    cm = (p[:, None] <= p[None, :]).astype(np.float32)
    iota = np.broadcast_to(np.arange(128, dtype=np.float32)[None, :], (128, 128)).copy()
    ones = np.ones((128, 128), np.float32)
    half = 8
    inv = (500000.0 ** (-np.arange(half, dtype=np.float32) * 2.0 / 16)).astype(np.float32)
    pos = (np.arange(NT)[None, :] * 128 + p[:, None]).astype(np.float32)
    ang = pos[:, :, None] * inv[None, None, :]
    cos = np.cos(ang).astype(np.float32).reshape(128, NT * 8)
    sin = np.sin(ang).astype(np.float32).reshape(128, NT * 8)
    a16 = np.broadcast_to((np.arange(16, dtype=np.float32) * 16)[None, :], (128, 16)).copy()
    i16 = np.broadcast_to(np.arange(16, dtype=np.float32)[None, :], (128, 16)).copy()
    return np.concatenate([ident, tri, negm, cm, iota, ones, a16, i16, cos, sin], axis=1).astype(np.float32)


def build(cfg):
    S, NB, NT = cfg.S, cfg.NB, cfg.NT
    NG = S // 512
    T = NB * S
    nc = bass.Bass("TRN2", target_bir_lowering=False)

    def din(name, shape, dt=F32):
        return nc.dram_tensor(name, list(shape), dt, kind="ExternalInput").ap()

    def dscr(name, shape, dt):
        kind = "ExternalOutput" if cfg.debug else "Internal"
        return nc.dram_tensor(name, list(shape), dt, kind=kind).ap()

    x = din("x", [NB, S, D])
    c_in = din("c", [NB, D])
    ada_w = din("ada_w", [D, 6 * D])
    ada_b = din("ada_b", [1, 6 * D])
    norm1_w = din("norm1_w", [1, D])
    norm2_w = din("norm2_w", [1, D])
    w_in = din("w_in", [D, IN_W])
    conv_w = din("conv_w", [4, 1024])
    conv_b = din("conv_b", [1, 1024])
    ml_ib = din("ml_i_bias", [1, 8])
    ml_fb = din("ml_f_bias", [1, 8])
    ml_nw = din("ml_norm_w", [1, D])
    lam_q1 = din("lam_q1", [1, 64])
    lam_k1 = din("lam_k1", [1, 64])
    lam_q2 = din("lam_q2", [1, 64])
    lam_k2 = din("lam_k2", [1, 64])
    subln_w = din("subln_w", [1, 128])
    w_out = din("w_out", [D, D])
    peer_wq = din("peer_wq", [D, 2048])
    peer_keys = din("peer_keys", [16, 128, 128])
    peer_u = din("peer_u", [16384, D])
    peer_v = din("peer_v", [16384, D])
    fin_w = din("final_norm_w", [1, D])
    NCST = 128 * 6 + 32 + 2 * NT * 8
    cst_d = din("cst", [128, NCST])
    out = nc.dram_tensor("out", [NB, S, D], F32, kind="ExternalOutput").ap()

    UT = dscr("UT", [128, 128, 1024], BF16)
    VB = dscr("VB", [128, 128, 1024], BF16)
    MOD = dscr("MOD", [NB, 6 * D], F32)
    QT = dscr("QT", [NB, 8, 128, S], BF16)
    KT = dscr("KT", [NB, 8, 128, S], BF16)
    VA = dscr("VA", [NB, S, D], BF16)
    QMT = dscr("QMT", [NB, 4, 128, S], BF16)
    KMT = dscr("KMT", [NB, 4, 128, S], BF16)
    VM = dscr("VM", [NB, S, D], BF16)
    OMS = dscr("OMS", [NB, S, D], BF16)
    GAS = dscr("GAS", [NB, S, D], BF16)
    GMS = dscr("GMS", [NB, S, D], BF16)
    GATES = dscr("GATES", [NB, S, 16], F32)
    YM = dscr("YM", [NB, S, D], BF16)
    MT = dscr("MT", [NB, 8, 128, S], BF16)
    X1 = dscr("X1", [NB, S, D], F32)
    H2T = dscr("H2T", [NB, 8, 128, S], BF16)
    IG = dscr("IG", [NB, S, 3, 128], F32)

    k = KB(nc)
    P, A, V, G, dma = k.P, k.A, k.V, k.G, k.dma

    with k.es:
        es0 = k.es

        uid = [0]

        def sbt(es, name, shape, dt):
            uid[0] += 1
            return es.enter_context(nc.sbuf_tensor(f"sb{uid[0]}_{name}", list(shape), dt))

        def pst(es, name, shape, dt):
            uid[0] += 1
            return es.enter_context(nc.psum_tensor(f"ps{uid[0]}_{name}", list(shape), dt))

        cst = sbt(es0, "cst", [128, NCST], F32)
        identb = sbt(es0, "identb", [128, 128], BF16)
        cmb = sbt(es0, "cmb", [128, 128], BF16)
        nmx = sbt(es0, "nmx", [128, 32], F32)
        lamt = sbt(es0, "lamt", [128, 4], F32)
        dma(cst[:], cst_d, w=["cst"])
        identf = cst[:, 0:128]
        trif = cst[:, 128:256]
        negmf = cst[:, 256:384]
        cmf = cst[:, 384:512]
        iotaf = cst[:, 512:640]
        onesf = cst[:, 640:768]
        a16f = cst[:, 768:784]
        i16f = cst[:, 784:800]
        cosf = cst[:, 800:800 + NT * 8]
        sinf = cst[:, 800 + NT * 8:800 + 2 * NT * 8]
        V(lambda e: e.tensor_copy(out=identb[:], in_=identf), r=["cst"], w=["identb"])
        V(lambda e: e.tensor_copy(out=cmb[:], in_=cmf), r=["cst"], w=["cmb"])

        with ExitStack() as ph:
            uf = [sbt(ph, f"uf{i}", [128, 1024], F32) for i in range(2)]
            ub = [sbt(ph, f"ub{i}", [128, 1024], BF16) for i in range(2)]
            uts = [sbt(ph, f"uts{i}", [128, 1024], BF16) for i in range(2)]
            vf = [sbt(ph, f"vf{i}", [128, 1024], F32) for i in range(2)]
            vb = [sbt(ph, f"vb{i}", [128, 1024], BF16) for i in range(2)]
            ptb = [pst(ph, f"ptbA{i}", [128, 1024], BF16) for i in range(2)]
            for i1 in range(128):
                s = i1 % 2
                dma(uf[s][:], peer_u[i1 * 128:(i1 + 1) * 128, :], w=[("uf", s)])
                dma(vf[s][:], peer_v[i1 * 128:(i1 + 1) * 128, :], w=[("vf", s)])
                V(lambda e: e.tensor_copy(out=ub[s][:], in_=uf[s][:]), r=[("uf", s)], w=[("ub", s)])
                for kc in range(8):
                    P(lambda e: e.transpose(out=ptb[s][:, kc * 128:(kc + 1) * 128], in_=ub[s][:, kc * 128:(kc + 1) * 128], identity=identb[:]),
                      r=[("ub", s), "identb"], w=[("ptbA", s)])
                A(lambda e: e.activation(out=uts[s][:], in_=ptb[s][:], func=AF.Copy), r=[("ptbA", s)], w=[("uts", s)])
                dma(UT[i1], uts[s][:], r=[("uts", s)], w=["UT"])
                G(lambda e: e.tensor_copy(out=vb[s][:], in_=vf[s][:]), r=[("vf", s)], w=[("vb", s)])
                dma(VB[i1], vb[s][:], r=[("vb", s)], w=["VB"])

            condT = sbt(ph, "condT", [128, 8, NB], F32)
            adab = sbt(ph, "adab", [1, 6 * D], F32)
            modrow = sbt(ph, "modrow", [1, NB, 6 * D], F32)
            wada = [sbt(ph, f"wada{i}", [128, 8, 512], F32) for i in range(2)]
            psm = [pst(ph, f"psm{i}", [128, 512], F32) for i in range(2)]
            for b in range(NB):
                dma(condT[:, :, b], c_in[b].rearrange("(kc p) -> p kc", p=128), w=["condT"], allow_slow_non_contiguous=True)
            dma(adab[:], ada_b, w=["adab"])
            A(lambda e: e.activation(out=condT[:], in_=condT[:], func=AF.Silu), r=["condT"], w=["condT"])
            adaw_v = ada_w.rearrange("(kc p) n -> p kc n", p=128)
            for ncx in range(12):
                s = ncx % 2
                dma(wada[s][:], adaw_v[:, :, ncx * 512:(ncx + 1) * 512], w=[("wada", s)])
                for b in range(NB):
                    pb = (ncx * NB + b) % 2
                    for kc in range(8):
                        P(lambda e: e.matmul(psm[pb][0:1, :], lhsT=condT[:, kc, b:b + 1], rhs=wada[s][:, kc, :], start=(kc == 0), stop=(kc == 7)),
                          r=["condT", ("wada", s)], w=[("psm", pb)])
                    V(lambda e: e.tensor_tensor(out=modrow[0:1, b, ncx * 512:(ncx + 1) * 512], in0=psm[pb][0:1, :], in1=adab[0:1, ncx * 512:(ncx + 1) * 512], op=ALU.add),
                      r=[("psm", pb), "adab"], w=["modrow"])
            for b in range(NB):
                dma(MOD[b:b + 1, :], modrow[0:1, b, :], r=["modrow"], w=["MOD"])

            lq = sbt(ph, "lq", [1, 4, 64], F32)
            lsc = sbt(ph, "lsc", [1, 8], F32)
            dma(lq[0:1, 0, :], lam_q1, w=["lq0"])
            dma(lq[0:1, 1, :], lam_k1, w=["lq1"])
            dma(lq[0:1, 2, :], lam_q2, w=["lq2"])
            dma(lq[0:1, 3, :], lam_k2, w=["lq3"])
            V(lambda e: e.tensor_tensor(out=lq[0:1, 0, :], in0=lq[0:1, 0, :], in1=lq[0:1, 1, :], op=ALU.mult), r=["lq0", "lq1"], w=["lq0"])
            V(lambda e: e.tensor_tensor(out=lq[0:1, 2, :], in0=lq[0:1, 2, :], in1=lq[0:1, 3, :], op=ALU.mult), r=["lq2", "lq3"], w=["lq2"])
            V(lambda e: e.reduce_sum(out=lsc[0:1, 0:1], in_=lq[0:1, 0, :], axis=AX.X), r=["lq0"], w=["lsc0"])
            V(lambda e: e.reduce_sum(out=lsc[0:1, 1:2], in_=lq[0:1, 2, :], axis=AX.X), r=["lq2"], w=["lsc1"])
            A(lambda e: e.activation(out=lsc[0:1, 2:4], in_=lsc[0:1, 0:2], func=AF.Exp), r=["lsc0", "lsc1"], w=["lsc2"])
            lam_init = 0.8 - 0.6 * math.exp(0.0)
            V(lambda e: e.tensor_tensor(out=lsc[0:1, 4:5], in0=lsc[0:1, 3:4], in1=lsc[0:1, 2:3], op=ALU.subtract), r=["lsc2"], w=["lsc4"])
            V(lambda e: e.tensor_scalar(out=lsc[0:1, 5:6], in0=lsc[0:1, 4:5], scalar1=-lam_init, scalar2=None, op0=ALU.add), r=["lsc4"], w=["lsc5"])
            P(lambda e: e.matmul(psm[0][:, 0:1], lhsT=onesf[0:1, :], rhs=lsc[0:1, 5:6], start=True, stop=True), r=["lsc5", "cst", ("psm", 0)], w=[("psm", 0)])
            V(lambda e: e.tensor_copy(out=lamt[:, 0:1], in_=psm[0][:, 0:1]), r=[("psm", 0)], w=["lamt"])
            k.barrier()

        win_v = w_in.rearrange("(kc p) n -> p kc n", p=128)
        for b in range(NB):
            with ExitStack() as ph:
                hT = sbt(ph, "hT", [128, 8, S], BF16)
                A1 = sbt(ph, "A1", [128, D], F32)
                B1 = sbt(ph, "B1", [128, D], F32)
                w1b = sbt(ph, "w1b", [128, D], F32)
                xt = [sbt(ph, f"xt{i}", [128, D], F32) for i in range(2)]
                hxs = [sbt(ph, f"hx{i}", [128, D], F32) for i in range(2)]
                hb = [sbt(ph, f"hb{i}", [128, D], BF16) for i in range(2)]
                junk = sbt(ph, "junkB", [128, D], BF16)
                st4s = [sbt(ph, f"st4{i}", [128, 8], F32) for i in range(2)]
                ptb = [pst(ph, f"ptbB{i}", [128, 1024], BF16) for i in range(2)]
                psb = [pst(ph, f"psB{i}", [128, 512], F32) for i in range(4)]
                dma(A1[:], MOD[b:b + 1, D:2 * D].partition_broadcast(128), r=["MOD"], w=["A1"])
                dma(B1[:], MOD[b:b + 1, 0:D].partition_broadcast(128), r=["MOD"], w=["B1"])
                dma(w1b[:], norm1_w.partition_broadcast(128), w=["w1b"])
                V(lambda e: e.scalar_tensor_tensor(out=A1[:], in0=A1[:], scalar=1.0, in1=w1b[:], op0=ALU.add, op1=ALU.mult), r=["A1", "w1b"], w=["A1"])
                V(lambda e: e.memset(nmx[:], 0.0), w=["nmx"])
                for tt in range(NT):
                    s = tt % 2
                    hx = hxs[s]
                    st4 = st4s[s]
                    dma(xt[s][:], x[b, tt * 128:(tt + 1) * 128, :], w=[("xt", s)])
                    A(lambda e: e.activation(out=junk[:], in_=xt[s][:], func=AF.Square, accum_out=st4[:, 0:1]), r=[("xt", s)], w=["junkB", ("st0", s)])
                    V(lambda e: e.tensor_scalar(out=st4[:, 1:2], in0=st4[:, 0:1], scalar1=1.0 / D, scalar2=EPS, op0=ALU.mult, op1=ALU.add), r=[("st0", s)], w=[("st1", s)])
                    A(lambda e: e.activation(out=st4[:, 2:3], in_=st4[:, 1:2], func=AF.Sqrt), r=[("st1", s)], w=[("st2", s)])
                    V(lambda e: e.reciprocal(out=st4[:, 3:4], in_=st4[:, 2:3]), r=[("st2", s)], w=[("st3", s)])
                    V(lambda e: e.scalar_tensor_tensor(out=hx[:], in0=xt[s][:], scalar=st4[:, 3:4], in1=A1[:], op0=ALU.mult, op1=ALU.mult), r=[("xt", s), ("st3", s), "A1"], w=[("hx", s)])
                    G(lambda e: e.tensor_tensor(out=hb[s][:], in0=hx[:], in1=B1[:], op=ALU.add), r=[("hx", s), "B1"], w=[("hb", s)])
                    for kc in range(8):
                        P(lambda e: e.transpose(out=ptb[s][:, kc * 128:(kc + 1) * 128], in_=hb[s][:, kc * 128:(kc + 1) * 128], identity=identb[:]),
                          r=[("hb", s), "identb"], w=[("ptbB", s)])
                    A(lambda e: e.activation(out=hT[:, :, tt * 128:(tt + 1) * 128], in_=ptb[s][:].rearrange("p (k t) -> p k t", k=8), func=AF.Copy),
                      r=[("ptbB", s)], w=[("hT", tt)])

                wf = [sbt(ph, f"wf{i}", [128, 8, 512], F32) for i in range(2)]
                wb = [sbt(ph, f"wb{i}", [128, 8, 512], BF16) for i in range(2)]
                qfs = [sbt(ph, f"qf{i}", [128, 512], F32) for i in range(2)]
                sqjs = [sbt(ph, f"sqj{i}", [128, 512], F32) for i in range(2)]
                rts = [sbt(ph, f"rt{i}", [128, 4, 8, 8], F32) for i in range(2)]
                nsqs = [sbt(ph, f"nsq{i}", [128, 8], F32) for i in range(2)]
                qb = [sbt(ph, f"qb{i}", [128, 512], BF16) for i in range(2)]
                stage = [sbt(ph, f"stage{i}", [128, 4, 512], BF16) for i in range(2)]
                ob = [sbt(ph, f"ob{i}", [128, 512], BF16) for i in range(3)]
                xbuf = sbt(ph, "xbuf", [128, 4, 515], F32)
                cacc = sbt(ph, "cacc", [128, 512], F32)
                csil = sbt(ph, "csil", [128, 512], F32)
                cstg = [sbt(ph, f"cstg{i}", [128, 512], BF16) for i in range(2)]
                cw = sbt(ph, "cw", [128, 8, 4], F32)
                cbi = sbt(ph, "cbi", [128, 8], F32)
                gbias = sbt(ph, "gbias", [128, 16], F32)
                gz = sbt(ph, "gz", [128, 16], F32)
                gt = [sbt(ph, f"gtB{i}", [128, 16], F32) for i in range(2)]
                for j in range(4):
                    dma(cw[:, :, j], conv_w[j].rearrange("(blk p) -> p blk", p=128), w=["cw"], allow_slow_non_contiguous=True)
                dma(cbi[:], conv_b.rearrange("o (blk p) -> p (o blk)", p=128), w=["cbi"], allow_slow_non_contiguous=True)
                dma(gbias[:, 0:8], ml_ib.partition_broadcast(128), w=["gbias0"])
                dma(gbias[:, 8:16], ml_fb.partition_broadcast(128), w=["gbias1"])

                chunks = []
                for i in range(2):
                    chunks.append((i * 512, 512, "qa", i))
                for i in range(2):
                    chunks.append((1024 + i * 512, 512, "ka", i))
                for i in range(2):
                    chunks.append((2048 + i * 512, 512, "va", i))
                chunks.append((3072, 512, "qm", 0))
                chunks.append((3584, 512, "km", 0))
                for i in range(2):
                    chunks.append((4096 + i * 512, 512, "vm", i))
                for i in range(2):
                    chunks.append((5120 + i * 512, 512, "om", i))
                chunks.append((6144, 16, "gate", 0))
                for i in range(2):
                    chunks.append((6160 + i * 512, 512, "ga", i))
                for i in range(2):
                    chunks.append((7184 + i * 512, 512, "gm", i))

                pcount = 0
                ocount = 0
                qcount = 0
                for ci, (c0, ncol, kind, idx) in enumerate(chunks):
                    s = ci % 2
                    dma(wf[s][:, :, 0:ncol], win_v[:, :, c0:c0 + ncol], w=[("wf", s)])
                    G(lambda e: e.tensor_copy(out=wb[s][:, :, 0:ncol], in_=wf[s][:, :, 0:ncol]), r=[("wf", s)], w=[("wb", s)])
                    if kind in ("qm", "km"):
                        sc_ = 1.0 if kind == "qm" else 0.125
                        dst = QMT if kind == "qm" else KMT
                        boff = 0 if kind == "qm" else 4
                        for cb in range(4):
                            V(lambda e: e.memset(xbuf[:, cb, 0:3], 0.0), w=[("xbuf", cb)])
                        for tg in range(NG):
                            for cb in range(4):
                                pb = pcount % 4
                                pcount += 1
                                for kc in range(8):
                                    P(lambda e: e.matmul(psb[pb][:], lhsT=wb[s][:, kc, cb * 128:(cb + 1) * 128], rhs=hT[:, kc, tg * 512:(tg + 1) * 512], start=(kc == 0), stop=(kc == 7)),
                                      r=[("wb", s)] + [("hT", t_) for t_ in range(tg * 4, tg * 4 + 4)], w=[("psB", pb)])
                                if tg > 0:
                                    V(lambda e: e.tensor_copy(out=xbuf[:, cb, 0:3], in_=xbuf[:, cb, 512:515]), r=[("xbuf", cb)], w=[("xbuf", cb)])
                                A(lambda e: e.activation(out=xbuf[:, cb, 3:515], in_=psb[pb][:], func=AF.Copy), r=[("psB", pb)], w=[("xbuf", cb)])
                                V(lambda e: e.tensor_scalar(out=cacc[:], in0=xbuf[:, cb, 0:512], scalar1=cw[:, boff + cb, 0:1], scalar2=None, op0=ALU.mult), r=[("xbuf", cb), "cw"], w=["cacc"])
                                for j in range(1, 4):
                                    V(lambda e: e.scalar_tensor_tensor(out=cacc[:], in0=xbuf[:, cb, j:j + 512], scalar=cw[:, boff + cb, j:j + 1], in1=cacc[:], op0=ALU.mult, op1=ALU.add),
                                      r=[("xbuf", cb), "cw", "cacc"], w=["cacc"])
                                A(lambda e: e.activation(out=csil[:], in_=cacc[:], func=AF.Silu, bias=cbi[:, boff + cb:boff + cb + 1], scale=1.0), r=["cacc", "cbi"], w=["csil"])
                                cs = qcount % 2
                                qcount += 1
                                G(lambda e: e.tensor_scalar(out=cstg[cs][:], in0=csil[:], scalar1=sc_, scalar2=None, op0=ALU.mult), r=["csil"], w=[("cstg", cs)])
                                dma(dst[b, cb, :, tg * 512:(tg + 1) * 512], cstg[cs][:], r=[("cstg", cs)], w=["QKMT"])
                        continue
                    for tt in range(NT):
                        pb = pcount % 4
                        pcount += 1
                        for kc in range(8):
                            P(lambda e: e.matmul(psb[pb][:, 0:ncol], lhsT=hT[:, kc, tt * 128:(tt + 1) * 128], rhs=wb[s][:, kc, 0:ncol], start=(kc == 0), stop=(kc == 7)),
                              r=[("wb", s), ("hT", tt)], w=[("psB", pb)])
                        if kind in ("qa", "ka"):
                            z = tt % 2
                            qf, sqj, rt, nsq = qfs[z], sqjs[z], rts[z], nsqs[z]
                            kq = ("qf", z)
                            A(lambda e: e.activation(out=qf[:], in_=psb[pb][:], func=AF.Copy), r=[("psB", pb)], w=[kq])
                            qv = qf[:].rearrange("p (g d) -> p g d", g=8)
                            x1 = qv[:, :, 0:8]
                            x2 = qv[:, :, 8:16]
                            cosb = cosf[:, tt * 8:(tt + 1) * 8].unsqueeze(1).to_broadcast([128, 8, 8])
                            sinb = sinf[:, tt * 8:(tt + 1) * 8].unsqueeze(1).to_broadcast([128, 8, 8])
                            V(lambda e: e.tensor_tensor(out=rt[:, 0], in0=x1, in1=cosb, op=ALU.mult), r=[kq, "cst"], w=[("rt0", z)])
                            V(lambda e: e.tensor_tensor(out=rt[:, 1], in0=x2, in1=sinb, op=ALU.mult), r=[kq, "cst"], w=[("rt1", z)])
                            V(lambda e: e.tensor_tensor(out=rt[:, 2], in0=x2, in1=cosb, op=ALU.mult), r=[kq, "cst"], w=[("rt2", z)])
                            V(lambda e: e.tensor_tensor(out=rt[:, 3], in0=x1, in1=sinb, op=ALU.mult), r=[kq, "cst"], w=[("rt3", z)])
                            V(lambda e: e.tensor_tensor(out=x1, in0=rt[:, 0], in1=rt[:, 1], op=ALU.subtract), r=[("rt0", z), ("rt1", z), ("rt2", z), ("rt3", z)], w=[kq])
                            V(lambda e: e.tensor_tensor(out=x2, in0=rt[:, 2], in1=rt[:, 3], op=ALU.add), r=[("rt2", z), ("rt3", z)], w=[kq])
                            G(lambda e: e.tensor_tensor(out=sqj[:], in0=qf[:], in1=qf[:], op=ALU.mult), r=[kq], w=[("sqj", z)])
                            V(lambda e: e.reduce_sum(out=nsq[:], in_=sqj[:].rearrange("p (g d) -> p g d", g=8), axis=AX.X), r=[("sqj", z)], w=[("nsq", z)])
                            ncol0 = (0 if kind == "qa" else 16) + idx * 8
                            V(lambda e: e.tensor_tensor(out=nmx[:, ncol0:ncol0 + 8], in0=nmx[:, ncol0:ncol0 + 8], in1=nsq[:], op=ALU.max), r=[("nsq", z), "nmx"], w=["nmx"])
                            qs = qcount % 2
                            qcount += 1
                            A(lambda e: e.activation(out=qb[qs][:], in_=qf[:], func=AF.Copy, scale=(0.125 if kind == "qa" else 1.0)), r=[kq], w=[("qb", qs)])
                            ps_ = tt % 2
                            for hd in range(4):
                                P(lambda e: e.transpose(out=ptb[ps_][:, hd * 128:(hd + 1) * 128], in_=qb[qs][:, hd * 128:(hd + 1) * 128], identity=identb[:]),
                                  r=[("qb", qs), "identb"], w=[("ptbB", ps_)])
                            sg = (tt // 4) % 2
                            V(lambda e: e.tensor_copy(out=stage[sg][:, :, (tt % 4) * 128:(tt % 4 + 1) * 128], in_=ptb[ps_][:, 0:512].rearrange("p (h t) -> p h t", h=4)),
                              r=[("ptbB", ps_)], w=[("stage", sg)])
                            if tt % 4 == 3:
                                dstT = QT if kind == "qa" else KT
                                t0 = (tt // 4) * 512
                                dma(dstT[b, idx * 4:(idx + 1) * 4, :, t0:t0 + 512].rearrange("h p t -> p h t"), stage[sg][:], r=[("stage", sg)], w=["QKT"])
                        elif kind in ("va", "vm"):
                            os_ = ocount % 3
                            ocount += 1
                            A(lambda e: e.activation(out=ob[os_][:], in_=psb[pb][:], func=AF.Copy), r=[("psB", pb)], w=[("ob", os_)])
                            dstv = VA if kind == "va" else VM
                            dma(dstv[b, tt * 128:(tt + 1) * 128, idx * 512:(idx + 1) * 512], ob[os_][:], r=[("ob", os_)], w=["VAVM"])
                        elif kind in ("om", "ga", "gm"):
                            os_ = ocount % 3
                            ocount += 1
                            A(lambda e: e.activation(out=ob[os_][:], in_=psb[pb][:], func=AF.Sigmoid), r=[("psB", pb)], w=[("ob", os_)])
                            dsts = {"om": OMS, "ga": GAS, "gm": GMS}[kind]
                            dma(dsts[b, tt * 128:(tt + 1) * 128, idx * 512:(idx + 1) * 512], ob[os_][:], r=[("ob", os_)], w=["SIGS"])
                        else:
                            gs = tt % 2
                            V(lambda e: e.tensor_tensor(out=gz[:], in0=psb[pb][:, 0:16], in1=gbias[:], op=ALU.add), r=[("psB", pb), "gbias0", "gbias1"], w=["gz"])
                            A(lambda e: e.activation(out=gz[:, 8:16], in_=gz[:, 8:16], func=AF.Exp, scale=-1.0), r=["gz"], w=["gz"])
                            A(lambda e: e.activation(out=gz[:, 8:16], in_=gz[:, 8:16], func=AF.Ln, bias=1.0, scale=1.0), r=["gz"], w=["gz"])
                            V(lambda e: e.tensor_copy(out=gt[gs][:, 0:8], in_=gz[:, 0:8]), r=["gz"], w=[("gtB", gs)])
                            V(lambda e: e.tensor_scalar(out=gt[gs][:, 8:16], in0=gz[:, 8:16], scalar1=-1.0, scalar2=None, op0=ALU.mult), r=["gz", ("gtB", gs)], w=[("gtB", gs)])
                            dma(GATES[b, tt * 128:(tt + 1) * 128, :], gt[gs][:], r=[("gtB", gs)], w=["GATES"])
                k.barrier()

            with ExitStack() as ph:
                qmt = sbt(ph, "qmt", [128, 4, S], BF16)
                kmt = sbt(ph, "kmt", [128, 4, S], BF16)
                Cst = sbt(ph, "Cst", [128, 4, 129], F32)
                Cbf = sbt(ph, "Cbf", [128, 4, 129], BF16)
                mlw = sbt(ph, "mlw", [128, D], F32)
                gtl = [sbt(ph, f"gtl{i}", [128, 16], F32) for i in range(2)]
                vmt = [sbt(ph, f"vmt{i}", [128, 8, 129], BF16) for i in range(2)]
                omt = [sbt(ph, f"omt{i}", [128, D], BF16) for i in range(2)]
                gmt = [sbt(ph, f"gmt{i}", [128, D], BF16) for i in range(2)]
                sms = [sbt(ph, f"sm{i}", [128, 8, 8], F32) for i in range(2)]
                Kws = [sbt(ph, f"Kw{i}", [128, 4, 128], BF16) for i in range(2)]
                PT = [sbt(ph, f"PT{i}", [128, 128], BF16) for i in range(2)]
                hn = sbt(ph, "hn", [128, 8, 128], F32)
                hj = sbt(ph, "hj", [128, 8, 128], F32)
                n8 = sbt(ph, "n8", [128, 8, 8], F32)
                ymb = [sbt(ph, f"ymb{i}", [128, D], BF16) for i in range(2)]
                psGK = pst(ph, "psGK", [128, 1024], BF16)
                psGf = psGK[:, 512:1024].bitcast(F32)
                psS = [pst(ph, f"psS{i}", [128, 512], F32) for i in range(2)]
                psTot = [pst(ph, f"psTot{i}", [128, 512], F32) for i in range(3)]
                psC = [pst(ph, f"psC{i}", [128, 512], F32) for i in range(2)]
                dma(qmt[:], QMT[b].rearrange("c p t -> p c t"), w=["qmt"])
                dma(kmt[:], KMT[b].rearrange("c p t -> p c t"), w=["kmt"])
                dma(mlw[:], ml_nw.partition_broadcast(128), w=["mlw"])
                V(lambda e: e.memset(Cst[:], 0.0), w=[("Cst", h) for h in range(8)])
                V(lambda e: e.memset(Cbf[:], 0.0), w=[("Cbf", h) for h in range(8)])
                for i in range(2):
                    V(lambda e: e.memset(vmt[i][:, :, 128:129], 1.0), w=[("vmt", i)])
                for tt in range(NT):
                    s = tt % 2
                    sm = sms[s]
                    Kw = Kws[s]
                    tsl = slice(tt * 128, (tt + 1) * 128)
                    dma(gtl[s][:], GATES[b, tsl, :], w=[("gtl", s)])
                    dma(vmt[s][:, :, 0:128], VM[b, tsl, :].rearrange("p (h e) -> p h e", h=8), w=[("vmt", s)])
                    dma(omt[s][:], OMS[b, tsl, :], w=[("omt", s)])
                    dma(gmt[s][:], GMS[b, tsl, :], w=[("gmt", s)])
                    lf = gtl[s][:, 8:16]
                    ig = gtl[s][:, 0:8]
                    P(lambda e: e.matmul(psGf[:, 0:8], lhsT=trif, rhs=lf, start=True, stop=True), r=[("gtl", s), "cst"], w=["psGK"])
                    P(lambda e: e.matmul(psGf[:, 8:16], lhsT=onesf, rhs=lf, start=True, stop=True), r=[("gtl", s), "cst"], w=["psGK"])
                    V(lambda e: e.tensor_copy(out=sm[:, 0:2, :], in_=psGf[:, 0:16].rearrange("p (a h) -> p a h", a=2)), r=["psGK"], w=[("sm01", s)])
                    V(lambda e: e.tensor_tensor(out=sm[:, 2, :], in0=ig, in1=sm[:, 0, :], op=ALU.subtract), r=[("gtl", s), ("sm01", s)], w=[("sm2", s)])
                    V(lambda e: e.tensor_tensor(out=sm[:, 6, :], in0=sm[:, 1, :], in1=sm[:, 2, :], op=ALU.add), r=[("sm01", s), ("sm2", s)], w=[("sm6", s)])
                    A(lambda e: e.activation(out=sm[:, 3, :], in_=sm[:, 2, :], func=AF.Exp), r=[("sm2", s)], w=[("sm3", s)])
                    A(lambda e: e.activation(out=sm[:, 4, :], in_=sm[:, 6, :], func=AF.Exp), r=[("sm6", s)], w=[("sm4", s)])
                    A(lambda e: e.activation(out=sm[:, 5, :], in_=sm[:, 1, :], func=AF.Exp), r=[("sm01", s)], w=[("sm5", s)])
                    A(lambda e: e.activation(out=sm[:, 7, :], in_=sm[:, 0, :], func=AF.Exp, scale=-1.0), r=[("sm01", s)], w=[("sm7", s)])
                    for cb in range(4):
                        P(lambda e: e.transpose(out=psGK[:, cb * 128:(cb + 1) * 128], in_=kmt[:, cb, tsl], identity=identb[:]), r=["kmt", "identb"], w=["psGK"])
                    V(lambda e: e.tensor_tensor(out=Kw[:].rearrange("p c (h d) -> p (c h) d", h=2), in0=psGK[:, 0:512].rearrange("p (g d) -> p g d", g=8),
                                                in1=sm[:, 4, :].unsqueeze(2).to_broadcast([128, 8, 64]), op=ALU.mult), r=["psGK", ("sm4", s)], w=[("Kw", s)])
                    started = [False, False, False]
                    for h in range(8):
                        cb, r0 = h // 2, (h % 2) * 64
                        hs = h % 2
                        bank, slot = h // 3, h % 3
                        tcol = slice(slot * 129, (slot + 1) * 129)
                        P(lambda e: e.matmul(psS[hs][:, 0:128], lhsT=kmt[r0:r0 + 64, cb, tsl], rhs=qmt[r0:r0 + 64, cb, tsl], start=True, stop=True), r=["kmt", "qmt"], w=[("psS", hs)])
                        V(lambda e: e.scalar_tensor_tensor(out=PT[hs][:], in0=psS[hs][:, 0:128], scalar=sm[:, 3, h:h + 1], in1=cmf, op0=ALU.mult, op1=ALU.mult),
                          r=[("psS", hs), ("sm3", s), "cst"], w=[("PT", hs)])
                        st_ = not started[bank]
                        started[bank] = True
                        P(lambda e: e.matmul(psTot[bank][:, tcol], lhsT=PT[hs][:], rhs=vmt[s][:, h, :], start=st_, stop=False, skip_group_check=True), r=[("PT", hs), ("vmt", s)], w=[("psTot", bank)])
                        P(lambda e: e.matmul(psTot[bank][:, tcol], lhsT=qmt[r0:r0 + 64, cb, tsl], rhs=Cbf[r0:r0 + 64, cb, :], start=False, stop=True, skip_group_check=True), r=["qmt", ("Cbf", h)], w=[("psTot", bank)])
                        P(lambda e: e.matmul(psC[hs][:, 0:129], lhsT=Kw[:, cb, :], rhs=vmt[s][:, h, :], start=True, stop=True), r=[("Kw", s), ("vmt", s)], w=[("psC", hs)])
                        V(lambda e: e.scalar_tensor_tensor(out=Cst[r0:r0 + 64, cb, :], in0=Cst[r0:r0 + 64, cb, :], scalar=sm[r0:r0 + 64, 5, h:h + 1], in1=psC[hs][r0:r0 + 64, 0:129], op0=ALU.mult, op1=ALU.add),
                          r=[("psC", hs), ("sm5", s), ("Cst", h)], w=[("Cst", h)])
                        G(lambda e: e.tensor_copy(out=Cbf[r0:r0 + 64, cb, :], in_=Cst[r0:r0 + 64, cb, :]), r=[("Cst", h)], w=[("Cbf", h)])
                    for bank in range(3):
                        nh_ = 3 if bank < 2 else 2
                        V(lambda e: e.tensor_copy(out=n8[:, 0, bank * 3:bank * 3 + nh_], in_=psTot[bank][:, 0:nh_ * 129].rearrange("p (s e) -> p s e", e=129)[:, :, 128]), r=[("psTot", bank)], w=[("n80", bank)])
                    k80 = [("n80", i) for i in range(3)]
                    V(lambda e: e.scalar_tensor_tensor(out=n8[:, 1, :], in0=n8[:, 0, :], scalar=-1.0, in1=n8[:, 0, :], op0=ALU.mult, op1=ALU.max), r=k80, w=["n81"])
                    V(lambda e: e.tensor_tensor(out=n8[:, 2, :], in0=n8[:, 1, :], in1=sm[:, 7, :], op=ALU.max), r=["n81", ("sm7", s)], w=["n82"])
                    V(lambda e: e.reciprocal(out=n8[:, 3, :], in_=n8[:, 2, :]), r=["n82"], w=["n83"])
                    for bank in range(3):
                        nh_ = 3 if bank < 2 else 2
                        V(lambda e: e.tensor_tensor(out=hn[:, bank * 3:bank * 3 + nh_, :], in0=psTot[bank][:, 0:nh_ * 129].rearrange("p (s e) -> p s e", e=129)[:, :, 0:128],
                                                    in1=n8[:, 3, bank * 3:bank * 3 + nh_].unsqueeze(2).to_broadcast([128, nh_, 128]), op=ALU.mult), r=[("psTot", bank), "n83"], w=[("hn", bank)])
                    khn = [("hn", i) for i in range(3)]
                    G(lambda e: e.tensor_tensor(out=hj[:], in0=hn[:], in1=hn[:], op=ALU.mult), r=khn, w=["hj"])
                    V(lambda e: e.reduce_sum(out=n8[:, 4, :], in_=hj[:], axis=AX.X), r=["hj"], w=["n84"])
                    V(lambda e: e.tensor_scalar(out=n8[:, 5, :], in0=n8[:, 4, :], scalar1=1.0 / 128, scalar2=EPS, op0=ALU.mult, op1=ALU.add), r=["n84"], w=["n85"])
                    A(lambda e: e.activation(out=n8[:, 6, :], in_=n8[:, 5, :], func=AF.Sqrt), r=["n85"], w=["n86"])
                    V(lambda e: e.reciprocal(out=n8[:, 7, :], in_=n8[:, 6, :]), r=["n86"], w=["n87"])
                    V(lambda e: e.tensor_tensor(out=hj[:], in0=hn[:], in1=n8[:, 7, :].unsqueeze(2).to_broadcast([128, 8, 128]), op=ALU.mult), r=khn + ["n87", "hj"], w=["hj"])
                    hjf = hj[:].rearrange("p h e -> p (h e)")
                    G(lambda e: e.tensor_tensor(out=hjf, in0=hjf, in1=mlw[:], op=ALU.mult), r=["hj", "mlw"], w=["hj"])
                    V(lambda e: e.tensor_tensor(out=hjf, in0=hjf, in1=omt[s][:], op=ALU.mult), r=["hj", ("omt", s)], w=["hj"])
                    G(lambda e: e.tensor_tensor(out=ymb[s][:], in0=hjf, in1=gmt[s][:], op=ALU.mult), r=["hj", ("gmt", s)], w=[("ymb", s)])
                    dma(YM[b, tsl, :], ymb[s][:], r=[("ymb", s)], w=["YM"])
                k.barrier()

            with ExitStack() as ph:
                qts = [sbt(ph, f"qts{i}", [128, S], BF16) for i in range(2)]
                kts = [sbt(ph, f"kts{i}", [128, S], BF16) for i in range(2)]
                vh = [sbt(ph, f"vh{i}", [128, NT, 129], BF16) for i in range(2)]
                negMb = sbt(ph, "negMb", [128, 16], F32)
                nw = sbt(ph, "nw", [16, 8], F32)
                dg = sbt(ph, "dg", [16, 16], F32)
                swb = sbt(ph, "swb", [128, 128], F32)
                PTa = [[sbt(ph, f"PTa{c}{i}", [128, 512], BF16) for i in range(2)] for c in range(2)]
                gat = [sbt(ph, f"gat{i}", [128, 128], BF16) for i in range(2)]
                ymt = [sbt(ph, f"ymt{i}", [128, 128], BF16) for i in range(2)]
                mb = [sbt(ph, f"mb{i}", [128, 128], BF16) for i in range(2)]
                mstage = [sbt(ph, f"mstage{i}", [128, 512], BF16) for i in range(2)]
                psS = [[pst(ph, f"psSa{c}{i}", [128, 512], F32) for i in range(2)] for c in range(2)]
                psA = [pst(ph, f"psA{i}", [128, 512], F32) for i in range(3)]
                psT = pst(ph, "psT", [128, 1024], BF16)
                P(lambda e: e.transpose(out=psA[0][0:16, 0:128], in_=nmx[:, 0:16], identity=identf), r=["nmx", "cst"], w=[("psA", 0)])
                P(lambda e: e.transpose(out=psA[0][0:16, 128:256], in_=nmx[:, 16:32], identity=identf), r=["nmx", "cst"], w=[("psA", 0)])
                V(lambda e: e.reduce_max(out=nw[:, 0:2], in_=psA[0][0:16, 0:256].rearrange("p (a t) -> p a t", a=2), axis=AX.X), r=[("psA", 0)], w=["nw0"])
                V(lambda e: e.tensor_tensor(out=nw[:, 2:3], in0=nw[:, 0:1], in1=nw[:, 1:2], op=ALU.mult), r=["nw0"], w=["nw2"])
                A(lambda e: e.activation(out=nw[:, 3:4], in_=nw[:, 2:3], func=AF.Sqrt), r=["nw2"], w=["nw3"])
                V(lambda e: e.tensor_scalar(out=nw[:, 4:5], in0=nw[:, 3:4], scalar1=-0.125, scalar2=None, op0=ALU.mult), r=["nw3"], w=["nw4"])
                V(lambda e: e.tensor_scalar(out=dg[:], in0=identf[0:16, 0:16], scalar1=nw[:, 4:5], scalar2=None, op0=ALU.mult), r=["nw4", "cst"], w=["dg"])
                P(lambda e: e.matmul(psA[1][:, 0:16], lhsT=onesf[0:16, :], rhs=dg[:], start=True, stop=True), r=["dg", "cst"], w=[("psA", 1)])
                V(lambda e: e.tensor_copy(out=negMb[:], in_=psA[1][:, 0:16]), r=[("psA", 1)], w=["negMb"])
                dma(swb[:], subln_w.partition_broadcast(128), w=["swb"])
                V(lambda e: e.tensor_scalar(out=swb[:], in0=swb[:], scalar1=(1.0 - lam_init), scalar2=None, op0=ALU.mult), r=["swb"], w=["swb"])
                for i in range(2):
                    V(lambda e: e.memset(vh[i][:, :, 128:129], 1.0), w=[("vh", i)])
                fcount = 0
                accS = [sbt(ph, f"accS{i}", [128, 3, 387], F32) for i in range(2)]
                rrs = [sbt(ph, f"rrs{i}", [128, 8], F32) for i in range(2)]
                o1s = [sbt(ph, f"o1s{i}", [128, 128], F32) for i in range(2)]
                o2s = [sbt(ph, f"o2s{i}", [128, 128], F32) for i in range(2)]
                ojs = [sbt(ph, f"ojs{i}", [128, 128], F32) for i in range(2)]
                gcnt = 0
                for hd in range(8):
                    s = hd % 2
                    dma(qts[s][:], QT[b, hd], w=[("qts", s)])
                    dma(kts[s][:], KT[b, hd], w=[("kts", s)])
                    for t8 in range(0, NT, 8):
                        te = min(NT, t8 + 8)
                        dma(vh[s][:, t8:te, 0:128], VA[b].rearrange("(tt p) c -> p tt c", p=128)[:, t8:te, hd * 128:(hd + 1) * 128], w=[("vh", s)])
                    blocks = [(g, kt) for g in range(NG) for kt in range(4 * g + 4)]

                    def qk_exp(bi):
                        g, kt = blocks[bi]
                        bs = bi % 2
                        for c in range(2):
                            P(lambda e: e.matmul(psS[c][bs][:], lhsT=kts[s][c * 64:(c + 1) * 64, kt * 128:(kt + 1) * 128], rhs=qts[s][c * 64:(c + 1) * 64, g * 512:(g + 1) * 512], start=True, stop=True),
                              r=[("kts", s), ("qts", s)], w=[("psSa", c, bs)])
                            A(lambda e: e.activation(out=PTa[c][bs][:], in_=psS[c][bs][:], func=AF.Exp, bias=negMb[:, hd * 2 + c:hd * 2 + c + 1], scale=1.0),
                              r=[("psSa", c, bs), "negMb"], w=[("PTa", c, bs)])

                    qk_exp(0)
                    started = [False, False, False]
                    for bi, (g, kt) in enumerate(blocks):
                        bs = bi % 2
                        if bi + 1 < len(blocks):
                            qk_exp(bi + 1)
                        if kt == 0:
                            started = [False, False, False]
                        for qi in range(4):
                            qt_ = 4 * g + qi
                            if kt > qt_:
                                continue
                            for c in range(2):
                                if kt == qt_:
                                    eng = V if c == 0 else G
                                    eng(lambda e: e.tensor_tensor(out=PTa[c][bs][:, qi * 128:(qi + 1) * 128], in0=PTa[c][bs][:, qi * 128:(qi + 1) * 128], in1=cmb[:], op=ALU.mult),
                                        r=[("PTa", c, bs), "cmb"], w=[("PTa", c, bs)])
                                ai = c * 4 + qi
                                bank, slot = ai // 3, ai % 3
                                st_ = not started[bank]
                                started[bank] = True
                                P(lambda e: e.matmul(psA[bank][:, slot * 129:(slot + 1) * 129], lhsT=PTa[c][bs][:, qi * 128:(qi + 1) * 128], rhs=vh[s][:, kt, :], start=st_, stop=(kt == qt_), skip_group_check=True),
                                  r=[("PTa", c, bs), ("vh", s)], w=[("psA", bank)])
                        if kt != 4 * g + 3:
                            continue
                        gs_ = gcnt % 2
                        gcnt += 1
                        for bank in range(3):
                            ncol_ = 387 if bank < 2 else 258
                            A(lambda e: e.activation(out=accS[gs_][:, bank, 0:ncol_], in_=psA[bank][:, 0:ncol_], func=AF.Copy), r=[("psA", bank)], w=[("accS", gs_, bank)])
                        ms = g % 2
                        for qi in range(4):
                            qt_ = 4 * g + qi
                            tsl = slice(qt_ * 128, (qt_ + 1) * 128)
                            fs = fcount % 2
                            fcount += 1
                            rr, o1, o2, oj = rrs[fs], o1s[fs], o2s[fs], ojs[fs]
                            dma(gat[fs][:], GAS[b, tsl, hd * 128:(hd + 1) * 128], w=[("gat", fs)])
                            dma(ymt[fs][:], YM[b, tsl, hd * 128:(hd + 1) * 128], w=[("ymt", fs)])
                            a1 = accS[gs_][:, qi // 3, (qi % 3) * 129:(qi % 3 + 1) * 129]
                            a2i = 4 + qi
                            a2 = accS[gs_][:, a2i // 3, (a2i % 3) * 129:(a2i % 3 + 1) * 129]
                            rk = [("accS", gs_, qi // 3), ("accS", gs_, a2i // 3)]
                            V(lambda e: e.reciprocal(out=rr[:, 0:1], in_=a1[:, 128:129]), r=rk, w=[("rr0", fs)])
                            V(lambda e: e.reciprocal(out=rr[:, 1:2], in_=a2[:, 128:129]), r=rk, w=[("rr1", fs)])
                            V(lambda e: e.tensor_tensor(out=rr[:, 2:3], in0=rr[:, 1:2], in1=lamt[:, 0:1], op=ALU.mult), r=[("rr1", fs), "lamt"], w=[("rr2", fs)])
                            A(lambda e: e.activation(out=o1[:], in_=a1[:, 0:128], func=AF.Copy, scale=rr[:, 0:1]), r=rk + [("rr0", fs)], w=[("o1", fs)])
                            V(lambda e: e.scalar_tensor_tensor(out=o2[:], in0=a2[:, 0:128], scalar=rr[:, 2:3], in1=o1[:], op0=ALU.mult, op1=ALU.add), r=rk + [("rr2", fs), ("o1", fs)], w=[("o2", fs)])
                            A(lambda e: e.activation(out=oj[:], in_=o2[:], func=AF.Square, accum_out=rr[:, 3:4]), r=[("o2", fs)], w=[("oj", fs), ("rr3", fs)])
                            V(lambda e: e.tensor_scalar(out=rr[:, 4:5], in0=rr[:, 3:4], scalar1=1.0 / 128, scalar2=EPS, op0=ALU.mult, op1=ALU.add), r=[("rr3", fs)], w=[("rr4", fs)])
                            A(lambda e: e.activation(out=rr[:, 5:6], in_=rr[:, 4:5], func=AF.Sqrt), r=[("rr4", fs)], w=[("rr5", fs)])
                            V(lambda e: e.reciprocal(out=rr[:, 6:7], in_=rr[:, 5:6]), r=[("rr5", fs)], w=[("rr6", fs)])
                            V(lambda e: e.scalar_tensor_tensor(out=oj[:], in0=o2[:], scalar=rr[:, 6:7], in1=swb[:], op0=ALU.mult, op1=ALU.mult), r=[("o2", fs), ("rr6", fs), "swb", ("oj", fs)], w=[("oj", fs)])
                            G(lambda e: e.tensor_tensor(out=oj[:], in0=oj[:], in1=gat[fs][:], op=ALU.mult), r=[("oj", fs), ("gat", fs)], w=[("oj", fs)])
                            G(lambda e: e.tensor_tensor(out=mb[fs][:], in0=oj[:], in1=ymt[fs][:], op=ALU.add), r=[("oj", fs), ("ymt", fs)], w=[("mb", fs)])
                            P(lambda e: e.transpose(out=psT[:, qi * 128:(qi + 1) * 128], in_=mb[fs][:], identity=identb[:]), r=[("mb", fs), "identb"], w=["psT"])
                        V(lambda e: e.tensor_copy(out=mstage[ms][:], in_=psT[:, 0:512]), r=["psT"], w=[("mstage", ms)])
                        dma(MT[b, hd, :, g * 512:(g + 1) * 512], mstage[ms][:], r=[("mstage", ms)], w=["MT"])
                k.barrier()

        wq_v = peer_wq.rearrange("(kc p) n -> p kc n", p=128)
        wo_v = w_out.rearrange("(kc p) n -> p kc n", p=128)
        with ExitStack() as ph:
            wob = sbt(ph, "wob", [128, 8, D], BF16)
            wqb = sbt(ph, "wqb", [128, 8, 2048], BF16)
            keyT = sbt(ph, "keyT", [128, 16, 128], BF16)
            psT = [pst(ph, f"psTE{i}", [128, 1024], BF16) for i in range(2)]
            ph2 = ExitStack()
            wtmp = [sbt(ph2, f"wtmp{i}", [128, 8, 512], F32) for i in range(2)]
            ktmp = sbt(ph2, "ktmp", [128, 16, 128], F32)
            ktb = sbt(ph2, "ktb", [128, 16, 128], BF16)
            wi = 0
            for n0 in range(0, D, 512):
                s = wi % 2
                wi += 1
                dma(wtmp[s][:], wo_v[:, :, n0:n0 + 512], w=[("wtmp", s)])
                V(lambda e: e.tensor_copy(out=wob[:, :, n0:n0 + 512], in_=wtmp[s][:]), r=[("wtmp", s)], w=["wob"])
            for n0 in range(0, 2048, 512):
                s = wi % 2
                wi += 1
                dma(wtmp[s][:], wq_v[:, :, n0:n0 + 512], w=[("wtmp", s)])
                G(lambda e: e.tensor_copy(out=wqb[:, :, n0:n0 + 512], in_=wtmp[s][:]), r=[("wtmp", s)], w=["wqb"])
            dma(ktmp[:], peer_keys.rearrange("g n d -> n g d"), w=["ktmp"])
            V(lambda e: e.tensor_copy(out=ktb[:], in_=ktmp[:]), r=["ktmp"], w=["ktb"])
            for g8 in range(2):
                for j in range(8):
                    gi = g8 * 8 + j
                    P(lambda e: e.transpose(out=psT[g8][:, j * 128:(j + 1) * 128], in_=ktb[:, gi, :], identity=identb[:]), r=["ktb", "identb"], w=[("psTE", g8)])
                V(lambda e: e.tensor_copy(out=keyT[:, g8 * 8:(g8 + 1) * 8, :], in_=psT[g8][:].rearrange("p (g n) -> p g n", g=8)), r=[("psTE", g8)], w=["keyT"])
            k.barrier()
            ph2.close()
            gt1 = sbt(ph, "gt1", [128, D], F32)
            A2 = sbt(ph, "A2", [128, D], F32)
            B2 = sbt(ph, "B2", [128, D], F32)
            w2b = sbt(ph, "w2b", [128, D], F32)
            mt = [sbt(ph, f"mt{i}", [128, 8, 128], BF16) for i in range(2)]
            xt = [sbt(ph, f"xtE{i}", [128, D], F32) for i in range(2)]
            x1 = [sbt(ph, f"x1E{i}", [128, D], F32) for i in range(2)]
            junk = sbt(ph, "junkE", [128, D], BF16)
            hx = sbt(ph, "hxE", [128, D], F32)
            hb = sbt(ph, "hbE", [128, D], BF16)
            h2t = [sbt(ph, f"h2t{i}", [128, 8, 128], BF16) for i in range(2)]
            qpt = sbt(ph, "qpt", [128, 16, 128], BF16)
            scs = [sbt(ph, f"sc{i}", [128, 16, 128], F32) for i in range(2)]
            wk = sbt(ph, "wkE", [128, 16, 128], F32)
            v16 = sbt(ph, "v16", [128, 16, 16], F32)
            i16u = sbt(ph, "i16u", [128, 16, 16], U32)
            i16v = sbt(ph, "i16v", [128, 16, 16], F32)
            cand = sbt(ph, "cand", [128, 8, 256], F32)
            cwk = sbt(ph, "cwk", [128, 8, 256], F32)
            c16 = sbt(ph, "c16", [128, 8, 16], F32)
            p16u = sbt(ph, "p16u", [128, 8, 16], U32)
            p16 = sbt(ph, "p16", [128, 8, 16], F32)
            d1 = sbt(ph, "d1", [128, 8, 16, 16], F32)
            d2 = sbt(ph, "d2", [128, 8, 16, 16], F32)
            ar = sbt(ph, "ar", [128, 8, 16], F32)
            br = sbt(ph, "br", [128, 8, 16], F32)
            igt = [sbt(ph, f"igt{i}", [128, 3, 128], F32) for i in range(2)]
            st4 = sbt(ph, "st4E", [128, 16], F32)
            psO = [pst(ph, f"psO{i}", [128, 512], F32) for i in range(2)]
            psQ = [pst(ph, f"psQ{i}", [128, 512], F32) for i in range(2)]
            psX = [pst(ph, f"psX{i}", [128, 512], F32) for i in range(2)]
            dma(w2b[:], norm2_w.partition_broadcast(128), w=["w2b"])
            for b in range(NB):
                dma(gt1[:], MOD[b:b + 1, 2 * D:3 * D].partition_broadcast(128), r=["MOD"], w=["gt1"])
                dma(B2[:], MOD[b:b + 1, 3 * D:4 * D].partition_broadcast(128), r=["MOD"], w=["B2"])
                dma(A2[:], MOD[b:b + 1, 4 * D:5 * D].partition_broadcast(128), r=["MOD"], w=["A2"])
                V(lambda e: e.scalar_tensor_tensor(out=A2[:], in0=A2[:], scalar=1.0, in1=w2b[:], op0=ALU.add, op1=ALU.mult), r=["A2", "w2b"], w=["A2"])
                for tt in range(NT):
                    s = tt % 2
                    tsl = slice(tt * 128, (tt + 1) * 128)
                    dma(mt[s][:], MT[b, :, :, tsl].rearrange("h p t -> p h t"), r=["MT"], w=[("mt", s)])
                    dma(xt[s][:], x[b, tsl, :], w=[("xtE", s)])
                    for nh in range(2):
                        for kc in range(8):
                            P(lambda e: e.matmul(psO[nh][:], lhsT=mt[s][:, kc, :], rhs=wob[:, kc, nh * 512:(nh + 1) * 512], start=(kc == 0), stop=(kc == 7)),
                              r=[("mt", s), "wob"], w=[("psO", nh)])
                        V(lambda e: e.tensor_tensor(out=x1[s][:, nh * 512:(nh + 1) * 512], in0=psO[nh][:], in1=gt1[:, nh * 512:(nh + 1) * 512], op=ALU.mult), r=[("psO", nh), "gt1"], w=[("x1E", s)])
                    G(lambda e: e.tensor_tensor(out=x1[s][:], in0=x1[s][:], in1=xt[s][:], op=ALU.add), r=[("x1E", s), ("xtE", s)], w=[("x1E", s)])
                    dma(X1[b, tsl, :], x1[s][:], r=[("x1E", s)], w=["X1"])
                    A(lambda e: e.activation(out=junk[:], in_=x1[s][:], func=AF.Square, accum_out=st4[:, 0:1]), r=[("x1E", s)], w=["junkE", "sE0"])
                    V(lambda e: e.tensor_scalar(out=st4[:, 1:2], in0=st4[:, 0:1], scalar1=1.0 / D, scalar2=EPS, op0=ALU.mult, op1=ALU.add), r=["sE0"], w=["sE1"])
                    A(lambda e: e.activation(out=st4[:, 2:3], in_=st4[:, 1:2], func=AF.Sqrt), r=["sE1"], w=["sE2"])
                    V(lambda e: e.reciprocal(out=st4[:, 3:4], in_=st4[:, 2:3]), r=["sE2"], w=["sE3"])
                    V(lambda e: e.scalar_tensor_tensor(out=hx[:], in0=x1[s][:], scalar=st4[:, 3:4], in1=A2[:], op0=ALU.mult, op1=ALU.mult), r=[("x1E", s), "sE3", "A2"], w=["hxE"])
                    G(lambda e: e.tensor_tensor(out=hb[:], in0=hx[:], in1=B2[:], op=ALU.add), r=["hxE", "B2"], w=["hbE"])
                    for kc in range(8):
                        P(lambda e: e.transpose(out=psT[0][:, kc * 128:(kc + 1) * 128], in_=hb[:, kc * 128:(kc + 1) * 128], identity=identb[:]), r=["hbE", "identb"], w=[("psTE", 0)])
                    A(lambda e: e.activation(out=h2t[s][:], in_=psT[0][:].rearrange("p (k t) -> p k t", k=8), func=AF.Copy), r=[("psTE", 0)], w=[("h2t", s)])
                    dma(H2T[b, :, :, tsl].rearrange("k p t -> p k t"), h2t[s][:], r=[("h2t", s)], w=["H2T"])
                    for gq in range(4):
                        pq = gq % 2
                        for j in range(4):
                            gi = gq * 4 + j
                            for kc in range(8):
                                P(lambda e: e.matmul(psQ[pq][:, j * 128:(j + 1) * 128], lhsT=wqb[:, kc, gi * 128:(gi + 1) * 128], rhs=h2t[s][:, kc, :], start=(kc == 0), stop=(kc == 7)),
                                  r=["wqb", ("h2t", s)], w=[("psQ", pq)])
                        A(lambda e: e.activation(out=qpt[:, gq * 4:(gq + 1) * 4, :], in_=psQ[pq][:].rearrange("p (g t) -> p g t", g=4), func=AF.Copy), r=[("psQ", pq)], w=["qpt"])
                    sc = scs[s]
                    for gq in range(4):
                        px = gq % 2
                        for j in range(4):
                            gi = gq * 4 + j
                            P(lambda e: e.matmul(psX[px][:, j * 128:(j + 1) * 128], lhsT=qpt[:, gi, :], rhs=keyT[:, gi, :], start=True, stop=True), r=["qpt", "keyT"], w=[("psX", px)])
                        A(lambda e: e.activation(out=sc[:, gq * 4:(gq + 1) * 4, :], in_=psX[px][:].rearrange("p (g n) -> p g n", g=4), func=AF.Copy), r=[("psX", px)], w=[("sc", s, gq)])
                    for gi in range(16):
                        V(lambda e: e.max(out=v16[:, gi, 0:8], in_=sc[:, gi, :]), r=[("sc", s, gi // 4)], w=[("v16a", gi)])
                    for gi in range(16):
                        V(lambda e: e.max_index(out=i16u[:, gi, 0:8], in_max=v16[:, gi, 0:8], in_values=sc[:, gi, :]), r=[("sc", s, gi // 4), ("v16a", gi)], w=[("i16a", gi)])
                    for gi in range(16):
                        V(lambda e: e.match_replace(out=wk[:, gi, :], in_to_replace=v16[:, gi, 0:8], in_values=sc[:, gi, :], imm_value=-1e30), r=[("sc", s, gi // 4), ("v16a", gi)], w=[("wkE", gi)])
                    for gi in range(16):
                        V(lambda e: e.max(out=v16[:, gi, 8:16], in_=wk[:, gi, :]), r=[("wkE", gi)], w=[("v16b", gi)])
                    for gi in range(16):
                        V(lambda e: e.max_index(out=i16u[:, gi, 8:16], in_max=v16[:, gi, 8:16], in_values=wk[:, gi, :]), r=[("wkE", gi), ("v16b", gi)], w=[("i16b", gi)])
                    allv = [("v16a", gi) for gi in range(16)] + [("v16b", gi) for gi in range(16)]
                    alli = [("i16a", gi) for gi in range(16)] + [("i16b", gi) for gi in range(16)]
                    V(lambda e: e.tensor_copy(out=i16v[:], in_=i16u[:]), r=alli, w=["i16v"])
                    v16v = v16[:].rearrange("p (h q) k -> p h q k", q=2)
                    i16vv = i16v[:].rearrange("p (h q) k -> p h q k", q=2)
                    V(lambda e: e.tensor_tensor(out=cand[:].rearrange("p h (a b) -> p h a b", a=16), in0=v16v[:, :, 0, :].unsqueeze(3).to_broadcast([128, 8, 16, 16]),
                                                in1=v16v[:, :, 1, :].unsqueeze(2).to_broadcast([128, 8, 16, 16]), op=ALU.add), r=allv, w=["cand"])
                    for h in range(8):
                        V(lambda e: e.max(out=c16[:, h, 0:8], in_=cand[:, h, :]), r=["cand"], w=[("c16a", h)])
                    for h in range(8):
                        V(lambda e: e.max_index(out=p16u[:, h, 0:8], in_max=c16[:, h, 0:8], in_values=cand[:, h, :]), r=["cand", ("c16a", h)], w=[("p16a", h)])
                    for h in range(8):
                        V(lambda e: e.match_replace(out=cwk[:, h, :], in_to_replace=c16[:, h, 0:8], in_values=cand[:, h, :], imm_value=-1e30), r=["cand", ("c16a", h)], w=[("cwk", h)])
                    for h in range(8):
                        V(lambda e: e.max(out=c16[:, h, 8:16], in_=cwk[:, h, :]), r=[("cwk", h)], w=[("c16b", h)])
                    for h in range(8):
                        V(lambda e: e.max_index(out=p16u[:, h, 8:16], in_max=c16[:, h, 8:16], in_values=cwk[:, h, :]), r=[("cwk", h), ("c16b", h)], w=[("p16b", h)])
                    allc = [("c16a", h) for h in range(8)] + [("c16b", h) for h in range(8)]
                    allp = [("p16a", h) for h in range(8)] + [("p16b", h) for h in range(8)]
                    V(lambda e: e.tensor_copy(out=p16[:], in_=p16u[:]), r=allp, w=["p16"])
                    ig0 = igt[s][:, 0, :].rearrange("p (h r) -> p h r", h=8)
                    ig1 = igt[s][:, 1, :].rearrange("p (h r) -> p h r", h=8)
                    HS = [slice(0, 4), slice(4, 8)]

                    def bc4(ap3, axis):
                        return ap3.unsqueeze(axis).to_broadcast([128, 4, 16, 16])

                    a16b = a16f.unsqueeze(1).unsqueeze(1).to_broadcast([128, 4, 16, 16])
                    i16b = i16f.unsqueeze(1).unsqueeze(1).to_broadcast([128, 4, 16, 16])
                    for z, hsl in enumerate(HS):
                        V(lambda e: e.tensor_tensor(out=d1[:, hsl], in0=bc4(p16[:, hsl, :], 3), in1=a16b, op=ALU.subtract), r=["p16", "cst"], w=[("d1", z)])
                    for z, hsl in enumerate(HS):
                        V(lambda e: e.tensor_scalar(out=d2[:, hsl], in0=d1[:, hsl], scalar1=0.0, scalar2=None, op0=ALU.is_ge), r=[("d1", z)], w=[("d2", z)])
                    for z, hsl in enumerate(HS):
                        V(lambda e: e.tensor_scalar(out=d1[:, hsl], in0=d1[:, hsl], scalar1=15.5, scalar2=None, op0=ALU.is_lt), r=[("d1", z), ("d2", z)], w=[("d1", z)])
                    for z, hsl in enumerate(HS):
                        V(lambda e: e.tensor_tensor(out=d1[:, hsl], in0=d1[:, hsl], in1=d2[:, hsl], op=ALU.mult), r=[("d1", z), ("d2", z)], w=[("d1", z)])
                    for z, hsl in enumerate(HS):
                        G(lambda e: e.tensor_tensor(out=d2[:, hsl], in0=d1[:, hsl], in1=a16b, op=ALU.mult), r=[("d1", z), "cst"], w=[("d2", z)])
                    for z, hsl in enumerate(HS):
                        V(lambda e: e.reduce_sum(out=ar[:, hsl, :], in_=d2[:, hsl], axis=AX.X), r=[("d2", z)], w=[("ar", z)])
                    for z, hsl in enumerate(HS):
                        G(lambda e: e.tensor_tensor(out=d2[:, hsl], in0=d1[:, hsl], in1=bc4(i16vv[:, hsl, 0, :], 2), op=ALU.mult), r=[("d1", z), "i16v", ("ar", z)], w=[("d2", z)])
                    for z, hsl in enumerate(HS):
                        V(lambda e: e.reduce_sum(out=ig0[:, hsl, :], in_=d2[:, hsl], axis=AX.X), r=[("d2", z)], w=[("igt", s, z)])
                    for z, hsl in enumerate(HS):
                        V(lambda e: e.tensor_tensor(out=br[:, hsl, :], in0=p16[:, hsl, :], in1=ar[:, hsl, :], op=ALU.subtract), r=["p16", ("ar", z)], w=[("br", z)])
                    for z, hsl in enumerate(HS):
                        V(lambda e: e.tensor_tensor(out=d1[:, hsl], in0=bc4(br[:, hsl, :], 3), in1=i16b, op=ALU.is_equal), r=[("br", z), "cst", ("d2", z)], w=[("d1", z)])
                    for z, hsl in enumerate(HS):
                        G(lambda e: e.tensor_tensor(out=d2[:, hsl], in0=d1[:, hsl], in1=bc4(i16vv[:, hsl, 1, :], 2), op=ALU.mult), r=[("d1", z), "i16v"], w=[("d2", z)])
                    for z, hsl in enumerate(HS):
                        V(lambda e: e.reduce_sum(out=ig1[:, hsl, :], in_=d2[:, hsl], axis=AX.X), r=[("d2", z), ("igt", s, z)], w=[("igt", s, z)])
                    V(lambda e: e.tensor_tensor(out=ar[:], in0=c16[:], in1=c16[:, :, 0:1].to_broadcast([128, 8, 16]), op=ALU.subtract), r=allc + [("br", 0), ("br", 1), ("ar", 0), ("ar", 1)], w=[("ar", 0), ("ar", 1)])
                    A(lambda e: e.activation(out=ar[:], in_=ar[:], func=AF.Exp), r=[("ar", 0), ("ar", 1)], w=[("ar", 0), ("ar", 1)])
                    V(lambda e: e.reduce_sum(out=st4[:, 8:16], in_=ar[:], axis=AX.X), r=[("ar", 0), ("ar", 1)], w=["sE8"])
                    V(lambda e: e.reciprocal(out=st4[:, 8:16], in_=st4[:, 8:16]), r=["sE8"], w=["sE8"])
                    V(lambda e: e.tensor_tensor(out=igt[s][:, 2, :].rearrange("p (h r) -> p h r", h=8), in0=ar[:], in1=st4[:, 8:16].unsqueeze(2).to_broadcast([128, 8, 16]), op=ALU.mult),
                      r=[("ar", 0), ("ar", 1), "sE8", ("igt", s, 0), ("igt", s, 1)], w=[("igt", s, 0), ("igt", s, 1)])
                    dma(IG[b, tsl, :, :], igt[s][:], r=[("igt", s, 0), ("igt", s, 1)], w=["IG"])
            k.barrier()

        TG = 256
        NGR = T // TG
        X1f = X1.rearrange("b s d -> (b s) d")
        outf = out.rearrange("b s d -> (b s) d")
        IGf = IG.rearrange("b s a r -> (b s) a r")
        with ExitStack() as ph:
            Gm = [sbt(ph, f"Gm{i}", [128, TG, 128], BF16) for i in range(2)]
            ust = [sbt(ph, f"ust{i}", [128, 2, 1024], BF16) for i in range(3)]
            vst = [sbt(ph, f"vst{i}", [128, 2, 1024], BF16) for i in range(3)]
            h2s = [sbt(ph, f"h2F{i}", [128, 8, TG], BF16) for i in range(2)]
            igl = sbt(ph, "igl", [128, 2, 3, 128], F32)
            igT = sbt(ph, "igT", [128, 3, TG], F32)
            ohA = [sbt(ph, f"ohA{i}", [128, 8, 128], BF16) for i in range(2)]
            ohB = [sbt(ph, f"ohB{i}", [128, 8, 128], BF16) for i in range(2)]
            ohT = sbt(ph, "ohT", [128, 8, 128], BF16)
            act = [sbt(ph, f"actF{i}", [128, TG], BF16) for i in range(4)]
            ga = [sbt(ph, f"gaF{i}", [128, TG], BF16) for i in range(4)]
            gt2 = sbt(ph, "gt2", [128, D], F32)
            fwb = sbt(ph, "fwb", [128, D], F32)
            x1l = sbt(ph, "x1l", [128, D], F32)
            yo = sbt(ph, "yo", [128, D], F32)
            oo = sbt(ph, "oo", [128, D], F32)
            st4 = sbt(ph, "st4F", [128, 8], F32)
            psY = [pst(ph, f"psY{i}", [128, 512], F32) for i in range(4)]
            psSc = [pst(ph, f"psSc{i}", [128, 512], F32) for i in range(2)]
            psGb = [pst(ph, f"psGb{i}", [128, 512], F32) for i in range(2)]
            dma(fwb[:], fin_w.partition_broadcast(128), w=["fwb"])
            UTv = UT.rearrange("i p f -> p i f")
            VBv = VB.rearrange("i p f -> p i f")
            ldc = [0]
            gcount = [0]
            slots = {}
            iob = iotaf.unsqueeze(1).to_broadcast([128, 8, 128])

            def prep_group(gr):
                t0 = gr * TG
                b = t0 // S
                s0 = t0 % S
                hs = gr % 2
                dma(h2s[hs][:], H2T[b, :, :, s0:s0 + TG].rearrange("k p t -> p k t"), w=[("h2F", hs)])
                dma(igl[:].rearrange("p j a r -> p j (a r)"), IGf[t0:t0 + TG].rearrange("(j p) a r -> p j (a r)", p=128), w=["igl"])
                for j in range(2):
                    for a in range(3):
                        P(lambda e: e.transpose(out=psGb[0][:, 0:128], in_=igl[:, j, a, :], identity=identf), r=["igl", "cst"], w=[("psGb", 0)])
                        V(lambda e: e.tensor_copy(out=igT[:, a, j * 128:(j + 1) * 128], in_=psGb[0][:, 0:128]), r=[("psGb", 0)], w=["igT"])

            def gb_onehots(gr, sub):
                os_ = sub % 2
                tq = slice(sub * 8, (sub + 1) * 8)
                V(lambda e: e.tensor_tensor(out=ohA[os_][:], in0=iob, in1=igT[:, 0, tq].unsqueeze(2).to_broadcast([128, 8, 128]), op=ALU.is_equal), r=["cst", "igT"], w=[("ohA", os_)])
                V(lambda e: e.tensor_tensor(out=ohT[:], in0=iob, in1=igT[:, 1, tq].unsqueeze(2).to_broadcast([128, 8, 128]), op=ALU.is_equal), r=["cst", "igT"], w=["ohT"])
                G(lambda e: e.tensor_tensor(out=ohB[os_][:], in0=ohT[:], in1=igT[:, 2, tq].unsqueeze(2).to_broadcast([128, 8, 128]), op=ALU.mult), r=["ohT", "igT"], w=[("ohB", os_)])

            def gb_mm(gr, sub):
                os_ = sub % 2
                gb = gr % 2
                for q4 in range(2):
                    pg = gcount[0] % 2
                    gcount[0] += 1
                    for j in range(4):
                        tl = q4 * 4 + j
                        P(lambda e: e.matmul(psGb[pg][:, j * 128:(j + 1) * 128], lhsT=ohB[os_][:, tl, :], rhs=ohA[os_][:, tl, :], start=True, stop=True),
                          r=[("ohA", os_), ("ohB", os_)], w=[("psGb", pg)])
                    tb = sub * 8 + q4 * 4
                    A(lambda e: e.activation(out=Gm[gb][:, tb:tb + 4, :], in_=psGb[pg][:].rearrange("p (t i) -> p t i", t=4), func=AF.Copy), r=[("psGb", pg)], w=[("Gm", gb)])

            def load_blk(i2b):
                ldc[0] += 1
                ss_ = ldc[0] % 3
                slots[i2b] = ss_
                dma(ust[ss_][:], UTv[:, i2b * 2:(i2b + 1) * 2, :], r=["UT"], w=[("ust", ss_)])
                dma(vst[ss_][:], VBv[:, i2b * 2:(i2b + 1) * 2, :], r=["VB"], w=[("vst", ss_)])

            def scores(gr, i1):
                ss_ = slots[i1 // 2]
                j = i1 % 2
                q = i1 % 2
                h2 = h2s[gr % 2]
                for kc in range(8):
                    P(lambda e: e.matmul(psSc[q][:, 0:TG], lhsT=ust[ss_][:, j, kc * 128:(kc + 1) * 128], rhs=h2[:, kc, :], start=(kc == 0), stop=(kc == 7)),
                      r=[("ust", ss_), ("h2F", gr % 2)], w=[("psSc", q)])

            prep_group(0)
            for sub in range(TG // 8):
                gb_onehots(0, sub)
                gb_mm(0, sub)
            if NGR > 1:
                prep_group(1)
            for gr in range(NGR):
                t0 = gr * TG
                b = t0 // S
                s0 = t0 % S
                gb = gr % 2
                if s0 == 0:
                    dma(gt2[:], MOD[b:b + 1, 5 * D:6 * D].partition_broadcast(128), r=["MOD"], w=["gt2"])
                nxt = gr + 1 < NGR
                load_blk(0)
                load_blk(1)
                scores(gr, 0)
                for i1 in range(128):
                    ss_ = slots[i1 // 2]
                    j = i1 % 2
                    q = i1 % 2
                    if i1 % 2 == 0 and i1 // 2 + 2 < 64:
                        load_blk(i1 // 2 + 2)
                    if i1 + 1 < 128:
                        scores(gr, i1 + 1)
                    if nxt and i1 % 4 == 0:
                        sub = i1 // 4
                        gb_onehots(gr + 1, sub)
                        if sub >= 1:
                            gb_mm(gr + 1, sub - 1)
                    A(lambda e: e.activation(out=act[q][:], in_=psSc[q][:, 0:TG], func=AF.Gelu), r=[("psSc", q)], w=[("actF", q)])
                    eng = G if (i1 % 4 != 3) else V
                    eng(lambda e: e.tensor_tensor(out=ga[q][:], in0=act[q][:], in1=Gm[gb][:, :, i1], op=ALU.mult), r=[("actF", q), ("Gm", gb)], w=[("gaF", q)])
                    for tj in range(2):
                        for nh in range(2):
                            P(lambda e: e.matmul(psY[tj * 2 + nh][:], lhsT=ga[q][:, tj * 128:(tj + 1) * 128], rhs=vst[ss_][:, j, nh * 512:(nh + 1) * 512], start=(i1 == 0), stop=(i1 == 127)),
                              r=[("gaF", q), ("vst", ss_)], w=[("psY", tj * 2 + nh)])
                if nxt:
                    gb_mm(gr + 1, 31)
                if gr + 2 < NGR:
                    prep_group(gr + 2)
                for tj in range(2):
                    dma(x1l[:], X1f[t0 + tj * 128:t0 + (tj + 1) * 128, :], w=["x1l"])
                    for nh in range(2):
                        V(lambda e: e.tensor_tensor(out=yo[:, nh * 512:(nh + 1) * 512], in0=psY[tj * 2 + nh][:], in1=gt2[:, nh * 512:(nh + 1) * 512], op=ALU.mult), r=[("psY", tj * 2 + nh), "gt2"], w=["yo"])
                    G(lambda e: e.tensor_tensor(out=yo[:], in0=yo[:], in1=x1l[:], op=ALU.add), r=["yo", "x1l"], w=["yo"])
                    A(lambda e: e.activation(out=x1l[:], in_=yo[:], func=AF.Square, accum_out=st4[:, 0:1]), r=["yo", "x1l"], w=["x1l", "sF0"])
                    V(lambda e: e.tensor_scalar(out=st4[:, 1:2], in0=st4[:, 0:1], scalar1=1.0 / D, scalar2=EPS, op0=ALU.mult, op1=ALU.add), r=["sF0"], w=["sF1"])
                    A(lambda e: e.activation(out=st4[:, 2:3], in_=st4[:, 1:2], func=AF.Sqrt), r=["sF1"], w=["sF2"])
                    V(lambda e: e.reciprocal(out=st4[:, 3:4], in_=st4[:, 2:3]), r=["sF2"], w=["sF3"])
                    V(lambda e: e.scalar_tensor_tensor(out=oo[:], in0=yo[:], scalar=st4[:, 3:4], in1=fwb[:], op0=ALU.mult, op1=ALU.mult), r=["yo", "sF3", "fwb", "oo"], w=["oo"])
                    dma(outf[t0 + tj * 128:t0 + (tj + 1) * 128, :], oo[:], r=["oo"], w=["OUT", "oo"])
            k.barrier()
    return nc


_INPUT_ORDER = ["x", "c", "ada_w", "ada_b", "norm1_w", "norm2_w", "w_in", "conv_w", "conv_b", "ml_i_bias", "ml_f_bias",
                "ml_norm_w", "lam_q1", "lam_k1", "lam_q2", "lam_k2", "subln_w", "w_out", "peer_wq", "peer_keys",
                "peer_u", "peer_v", "final_norm_w"]


def make_in_maps(cfg, inputs, n_cores):
    f = lambda a: np.ascontiguousarray(np.asarray(a, dtype=np.float32))
    NB = cfg.NB
    shared = {
        "ada_w": f(inputs["ada_w"][0]), "ada_b": f(inputs["ada_b"][0]).reshape(1, -1),
        "norm1_w": f(inputs["norm1_w"][0]).reshape(1, -1), "norm2_w": f(inputs["norm2_w"][0]).reshape(1, -1),
        "w_in": f(inputs["w_in"][0]), "conv_w": f(inputs["conv_w"][0]), "conv_b": f(inputs["conv_b"][0]).reshape(1, -1),
        "ml_i_bias": f(inputs["ml_i_bias"][0]).reshape(1, -1), "ml_f_bias": f(inputs["ml_f_bias"][0]).reshape(1, -1),
        "ml_norm_w": f(inputs["ml_norm_w"][0]).reshape(1, -1),
        "lam_q1": f(inputs["lam_q1"][0]).reshape(1, -1), "lam_k1": f(inputs["lam_k1"][0]).reshape(1, -1),
        "lam_q2": f(inputs["lam_q2"][0]).reshape(1, -1), "lam_k2": f(inputs["lam_k2"][0]).reshape(1, -1),
        "subln_w": f(inputs["subln_w"][0]).reshape(1, -1), "w_out": f(inputs["w_out"][0]),
        "peer_wq": f(inputs["peer_wq"][0]), "peer_keys": f(inputs["peer_keys"][0]).reshape(16, 128, 128),
        "peer_u": f(inputs["peer_u"][0]), "peer_v": f(inputs["peer_v"][0]),
        "final_norm_w": f(inputs["final_norm_w"]).reshape(1, -1),
        "cst": host_consts(cfg),
    }
    xs = f(inputs["x"])
    cs = f(inputs["c"])
    maps = []
    for i in range(n_cores):
        m = dict(shared)
        m["x"] = np.ascontiguousarray(xs[i * NB:(i + 1) * NB])
        m["c"] = np.ascontiguousarray(cs[i * NB:(i + 1) * NB])
        maps.append(m)
    return maps


def kernel(**inputs):
    n_cores = 8
    cfg = Cfg(S=4096, NB=2)
    nc = build(cfg)
    maps = make_in_maps(cfg, inputs, n_cores)
    res = run_bass_kernel_spmd(nc, maps, core_ids=list(range(n_cores)))
    outs = [np.asarray(r["out"], dtype=np.float32) for r in res.results]
    return np.concatenate(outs, axis=0)
```

```python
import math
from contextlib import ExitStack

import numpy as np
import ml_dtypes
import concourse.bass as bass
import concourse.mybir as mybir
from concourse.bass_utils import run_bass_kernel_spmd

F32 = mybir.dt.float32
BF16 = mybir.dt.bfloat16
U32 = mybir.dt.uint32
ALU = mybir.AluOpType
AF = mybir.ActivationFunctionType
AX = mybir.AxisListType

D = 1024
EPS = 1e-6
IN_W = 8208
NEG = -30000.0


class Tok:
    __slots__ = ("sem", "val", "eng")

    def __init__(self, sem, val, eng):
        self.sem, self.val, self.eng = sem, val, eng


class Eng:
    def __init__(self, kb, name, eng):
        self.kb, self.name, self.eng = kb, name, eng
        self.sem = None
        self.count = 0
        self.seen = {}
        self.last = None
        self.nsem = 0

    def wait(self, tok):
        if tok is None:
            return
        key = id(tok.sem)
        if self.seen.get(key, 0) >= tok.val:
            return
        self.seen[key] = tok.val
        self.eng.wait_ge(tok.sem, tok.val)

    def signal(self, instr):
        if self.sem is None or self.count >= 30000:
            self.sem = self.kb.es.enter_context(self.kb.nc.semaphore(f"s_{self.name}_{self.nsem}"))
            self.nsem += 1
            self.count = 0
        self.count += 1
        instr.then_inc(self.sem, 1)
        t = Tok(self.sem, self.count, self.name)
        self.last = t
        return t


class KB:
    def __init__(self, nc):
        self.nc = nc
        self.es = ExitStack()
        self.e = {
            "pe": Eng(self, "pe", nc.tensor),
            "act": Eng(self, "act", nc.scalar),
            "dve": Eng(self, "dve", nc.vector),
            "pool": Eng(self, "pool", nc.gpsimd),
            "sp": Eng(self, "sp", nc.sync),
        }
        self.res = {}
        self.dsems = []
        self.dvals = []
        self.dnext = 0
        self.ND = 40
        self.all_dma = []

    def _deps(self, r, w):
        deps = []
        for k in r:
            st = self.res.get(k)
            if st is not None and st[0] is not None:
                deps.append(st[0])
        for k in w:
            st = self.res.get(k)
            if st is not None:
                if st[0] is not None:
                    deps.append(st[0])
                deps.extend(st[1])
        return deps

    def _record(self, tok, r, w):
        for k in r:
            st = self.res.setdefault(k, [None, []])
            if tok.eng != "dma":
                st[1] = [t for t in st[1] if t.eng != tok.eng]
            st[1].append(tok)
        for k in w:
            self.res[k] = [tok, []]

    def op(self, en, fn, r=(), w=()):
        E = self.e[en]
        for t in self._deps(r, w):
            if en == "pe" and t.eng == "pe":
                continue
            E.wait(t)
        instr = fn(E.eng)
        tok = E.signal(instr)
        self._record(tok, r, w)
        return tok

    def P(self, fn, r=(), w=()):
        return self.op("pe", fn, r, w)

    def A(self, fn, r=(), w=()):
        return self.op("act", fn, r, w)

    def V(self, fn, r=(), w=()):
        return self.op("dve", fn, r, w)

    def G(self, fn, r=(), w=()):
        return self.op("pool", fn, r, w)

    def dma(self, out, in_, r=(), w=(), q="sp", **kw):
        E = self.e[q]
        for t in self._deps(r, w):
            E.wait(t)
        if len(self.dsems) < self.ND:
            self.dsems.append(self.es.enter_context(self.nc.semaphore(f"s_dma_{len(self.dsems)}")))
            self.dvals.append(0)
        i = self.dnext
        self.dnext = (self.dnext + 1) % self.ND
        sem = self.dsems[i]
        if self.dvals[i] > 0:
            E.wait(Tok(sem, self.dvals[i], "dma"))
        self.dvals[i] += 16
        instr = E.eng.dma_start(out=out, in_=in_, **kw)
        instr.then_inc(sem, 16)
        tok = Tok(sem, self.dvals[i], "dma")
        self._record(tok, r, w)
        return tok

    def barrier(self):
        toks = [E.last for E in self.e.values() if E.last is not None]
        toks += [Tok(s, v, "dma") for s, v in zip(self.dsems, self.dvals) if v > 0]
        for E in self.e.values():
            for t in toks:
                if t.eng == E.name:
                    continue
                E.wait(t)
        self.res = {}


class Cfg:
    def __init__(self, S=4096, NB=2, debug=False):
        self.S, self.NB, self.debug = S, NB, debug
        self.NT = S // 128


def host_consts(cfg):
    NT = cfg.NT
    p = np.arange(128)
    ident = np.eye(128, dtype=np.float32)
    tri = (p[:, None] <= p[None, :]).astype(np.float32)
    negm = np.where(p[:, None] > p[None, :], NEG, 0.0).astype(np.float32)
    cm = (p[:, None] <= p[None, :]).astype(np.float32)
    iota = np.broadcast_to(np.arange(128, dtype=np.float32)[None, :], (128, 128)).copy()
    ones = np.ones((128, 128), np.float32)
    half = 8
    inv = (500000.0 ** (-np.arange(half, dtype=np.float32) * 2.0 / 16)).astype(np.float32)
    pos = (np.arange(NT)[None, :] * 128 + p[:, None]).astype(np.float32)
    ang = pos[:, :, None] * inv[None, None, :]
    cos = np.cos(ang).astype(np.float32).reshape(128, NT * 8)
    sin = np.sin(ang).astype(np.float32).reshape(128, NT * 8)
    a16 = np.broadcast_to((np.arange(16, dtype=np.float32) * 16)[None, :], (128, 16)).copy()
    i16 = np.broadcast_to(np.arange(16, dtype=np.float32)[None, :], (128, 16)).copy()
    return np.concatenate([ident, tri, negm, cm, iota, ones, a16, i16, cos, sin], axis=1).astype(np.float32)


def build(cfg):
    S, NB, NT = cfg.S, cfg.NB, cfg.NT
    NG = S // 512
    T = NB * S
    nc = bass.Bass("TRN2", target_bir_lowering=False)

    def din(name, shape, dt=F32):
        return nc.dram_tensor(name, list(shape), dt, kind="ExternalInput").ap()

    def dscr(name, shape, dt):
        kind = "ExternalOutput" if cfg.debug else "Internal"
        return nc.dram_tensor(name, list(shape), dt, kind=kind).ap()

    x = din("x", [NB, S, D])
    c_in = din("c", [NB, D])
    ada_w = din("ada_w", [D, 6 * D])
    ada_b = din("ada_b", [1, 6 * D])
    norm1_w = din("norm1_w", [1, D])
    norm2_w = din("norm2_w", [1, D])
    w_in = din("w_in", [D, IN_W])
    conv_w = din("conv_w", [4, 1024])
    conv_b = din("conv_b", [1, 1024])
    ml_ib = din("ml_i_bias", [1, 8])
    ml_fb = din("ml_f_bias", [1, 8])
    ml_nw = din("ml_norm_w", [1, D])
    lam_q1 = din("lam_q1", [1, 64])
    lam_k1 = din("lam_k1", [1, 64])
    lam_q2 = din("lam_q2", [1, 64])
    lam_k2 = din("lam_k2", [1, 64])
    subln_w = din("subln_w", [1, 128])
    w_out = din("w_out", [D, D])
    peer_wq = din("peer_wq", [D, 2048])
    peer_keys = din("peer_keys", [16, 128, 128])
    peer_u = din("peer_u", [16384, D])
    peer_v = din("peer_v", [16384, D])
    fin_w = din("final_norm_w", [1, D])
    NCST = 128 * 6 + 32 + 2 * NT * 8
    cst_d = din("cst", [128, NCST])
    out = nc.dram_tensor("out", [NB, S, D], F32, kind="ExternalOutput").ap()

    UT = dscr("UT", [128, 128, 1024], BF16)
    VB = dscr("VB", [128, 128, 1024], BF16)
    MOD = dscr("MOD", [NB, 6 * D], F32)
    QT = dscr("QT", [NB, 8, 128, S], BF16)
    KT = dscr("KT", [NB, 8, 128, S], BF16)
    VA = dscr("VA", [NB, S, D], BF16)
    QMT = dscr("QMT", [NB, 4, 128, S], BF16)
    KMT = dscr("KMT", [NB, 4, 128, S], BF16)
    VM = dscr("VM", [NB, S, D], BF16)
    OMS = dscr("OMS", [NB, S, D], BF16)
    GAS = dscr("GAS", [NB, S, D], BF16)
    GMS = dscr("GMS", [NB, S, D], BF16)
    GATES = dscr("GATES", [NB, S, 16], F32)
    YM = dscr("YM", [NB, S, D], BF16)
    MT = dscr("MT", [NB, 8, 128, S], BF16)
    X1 = dscr("X1", [NB, S, D], F32)
    H2T = dscr("H2T", [NB, 8, 128, S], BF16)
    IG = dscr("IG", [NB, S, 3, 128], F32)

    k = KB(nc)
    P, A, V, G, dma = k.P, k.A, k.V, k.G, k.dma

    with k.es:
        es0 = k.es

        uid = [0]

        def sbt(es, name, shape, dt):
            uid[0] += 1
            return es.enter_context(nc.sbuf_tensor(f"sb{uid[0]}_{name}", list(shape), dt))

        def pst(es, name, shape, dt):
            uid[0] += 1
            return es.enter_context(nc.psum_tensor(f"ps{uid[0]}_{name}", list(shape), dt))

        cst = sbt(es0, "cst", [128, NCST], F32)
        identb = sbt(es0, "identb", [128, 128], BF16)
        cmb = sbt(es0, "cmb", [128, 128], BF16)
        nmx = sbt(es0, "nmx", [128, 32], F32)
        lamt = sbt(es0, "lamt", [128, 4], F32)
        dma(cst[:], cst_d, w=["cst"])
        identf = cst[:, 0:128]
        trif = cst[:, 128:256]
        negmf = cst[:, 256:384]
        cmf = cst[:, 384:512]
        iotaf = cst[:, 512:640]
        onesf = cst[:, 640:768]
        a16f = cst[:, 768:784]
        i16f = cst[:, 784:800]
        cosf = cst[:, 800:800 + NT * 8]
        sinf = cst[:, 800 + NT * 8:800 + 2 * NT * 8]
        V(lambda e: e.tensor_copy(out=identb[:], in_=identf), r=["cst"], w=["identb"])
        V(lambda e: e.tensor_copy(out=cmb[:], in_=cmf), r=["cst"], w=["cmb"])

        with ExitStack() as ph:
            uf = [sbt(ph, f"uf{i}", [128, 1024], F32) for i in range(2)]
            ub = [sbt(ph, f"ub{i}", [128, 1024], BF16) for i in range(2)]
            uts = [sbt(ph, f"uts{i}", [128, 1024], BF16) for i in range(2)]
            vf = [sbt(ph, f"vf{i}", [128, 1024], F32) for i in range(2)]
            vb = [sbt(ph, f"vb{i}", [128, 1024], BF16) for i in range(2)]
            ptb = [pst(ph, f"ptbA{i}", [128, 1024], BF16) for i in range(2)]
            for i1 in range(128):
                s = i1 % 2
                dma(uf[s][:], peer_u[i1 * 128:(i1 + 1) * 128, :], w=[("uf", s)])
                dma(vf[s][:], peer_v[i1 * 128:(i1 + 1) * 128, :], w=[("vf", s)])
                V(lambda e: e.tensor_copy(out=ub[s][:], in_=uf[s][:]), r=[("uf", s)], w=[("ub", s)])
                for kc in range(8):
                    P(lambda e: e.transpose(out=ptb[s][:, kc * 128:(kc + 1) * 128], in_=ub[s][:, kc * 128:(kc + 1) * 128], identity=identb[:]),
                      r=[("ub", s), "identb"], w=[("ptbA", s)])
                A(lambda e: e.activation(out=uts[s][:], in_=ptb[s][:], func=AF.Copy), r=[("ptbA", s)], w=[("uts", s)])
                dma(UT[i1], uts[s][:], r=[("uts", s)], w=["UT"])
                G(lambda e: e.tensor_copy(out=vb[s][:], in_=vf[s][:]), r=[("vf", s)], w=[("vb", s)])
                dma(VB[i1], vb[s][:], r=[("vb", s)], w=["VB"])

            condT = sbt(ph, "condT", [128, 8, NB], F32)
            adab = sbt(ph, "adab", [1, 6 * D], F32)
            modrow = sbt(ph, "modrow", [1, NB, 6 * D], F32)
            wada = [sbt(ph, f"wada{i}", [128, 8, 512], F32) for i in range(2)]
            psm = [pst(ph, f"psm{i}", [128, 512], F32) for i in range(2)]
            for b in range(NB):
                dma(condT[:, :, b], c_in[b].rearrange("(kc p) -> p kc", p=128), w=["condT"], allow_slow_non_contiguous=True)
            dma(adab[:], ada_b, w=["adab"])
            A(lambda e: e.activation(out=condT[:], in_=condT[:], func=AF.Silu), r=["condT"], w=["condT"])
            adaw_v = ada_w.rearrange("(kc p) n -> p kc n", p=128)
            for ncx in range(12):
                s = ncx % 2
                dma(wada[s][:], adaw_v[:, :, ncx * 512:(ncx + 1) * 512], w=[("wada", s)])
                for b in range(NB):
                    pb = (ncx * NB + b) % 2
                    for kc in range(8):
                        P(lambda e: e.matmul(psm[pb][0:1, :], lhsT=condT[:, kc, b:b + 1], rhs=wada[s][:, kc, :], start=(kc == 0), stop=(kc == 7)),
                          r=["condT", ("wada", s)], w=[("psm", pb)])
                    V(lambda e: e.tensor_tensor(out=modrow[0:1, b, ncx * 512:(ncx + 1) * 512], in0=psm[pb][0:1, :], in1=adab[0:1, ncx * 512:(ncx + 1) * 512], op=ALU.add),
                      r=[("psm", pb), "adab"], w=["modrow"])
            for b in range(NB):
                dma(MOD[b:b + 1, :], modrow[0:1, b, :], r=["modrow"], w=["MOD"])

            lq = sbt(ph, "lq", [1, 4, 64], F32)
            lsc = sbt(ph, "lsc", [1, 8], F32)
            dma(lq[0:1, 0, :], lam_q1, w=["lq0"])
            dma(lq[0:1, 1, :], lam_k1, w=["lq1"])
            dma(lq[0:1, 2, :], lam_q2, w=["lq2"])
            dma(lq[0:1, 3, :], lam_k2, w=["lq3"])
            V(lambda e: e.tensor_tensor(out=lq[0:1, 0, :], in0=lq[0:1, 0, :], in1=lq[0:1, 1, :], op=ALU.mult), r=["lq0", "lq1"], w=["lq0"])
            V(lambda e: e.tensor_tensor(out=lq[0:1, 2, :], in0=lq[0:1, 2, :], in1=lq[0:1, 3, :], op=ALU.mult), r=["lq2", "lq3"], w=["lq2"])
            V(lambda e: e.reduce_sum(out=lsc[0:1, 0:1], in_=lq[0:1, 0, :], axis=AX.X), r=["lq0"], w=["lsc0"])
            V(lambda e: e.reduce_sum(out=lsc[0:1, 1:2], in_=lq[0:1, 2, :], axis=AX.X), r=["lq2"], w=["lsc1"])
            A(lambda e: e.activation(out=lsc[0:1, 2:4], in_=lsc[0:1, 0:2], func=AF.Exp), r=["lsc0", "lsc1"], w=["lsc2"])
            lam_init = 0.8 - 0.6 * math.exp(0.0)
            V(lambda e: e.tensor_tensor(out=lsc[0:1, 4:5], in0=lsc[0:1, 3:4], in1=lsc[0:1, 2:3], op=ALU.subtract), r=["lsc2"], w=["lsc4"])
            V(lambda e: e.tensor_scalar(out=lsc[0:1, 5:6], in0=lsc[0:1, 4:5], scalar1=-lam_init, scalar2=None, op0=ALU.add), r=["lsc4"], w=["lsc5"])
            P(lambda e: e.matmul(psm[0][:, 0:1], lhsT=onesf[0:1, :], rhs=lsc[0:1, 5:6], start=True, stop=True), r=["lsc5", "cst", ("psm", 0)], w=[("psm", 0)])
            V(lambda e: e.tensor_copy(out=lamt[:, 0:1], in_=psm[0][:, 0:1]), r=[("psm", 0)], w=["lamt"])
            k.barrier()

        win_v = w_in.rearrange("(kc p) n -> p kc n", p=128)
        for b in range(NB):
            with ExitStack() as ph:
                hT = sbt(ph, "hT", [128, 8, S], BF16)
                A1 = sbt(ph, "A1", [128, D], F32)
                B1 = sbt(ph, "B1", [128, D], F32)
                w1b = sbt(ph, "w1b", [128, D], F32)
                xt = [sbt(ph, f"xt{i}", [128, D], F32) for i in range(2)]
                hxs = [sbt(ph, f"hx{i}", [128, D], F32) for i in range(2)]
                hb = [sbt(ph, f"hb{i}", [128, D], BF16) for i in range(2)]
                junk = sbt(ph, "junkB", [128, D], BF16)
                st4s = [sbt(ph, f"st4{i}", [128, 8], F32) for i in range(2)]
                ptb = [pst(ph, f"ptbB{i}", [128, 1024], BF16) for i in range(2)]
                psb = [pst(ph, f"psB{i}", [128, 512], F32) for i in range(4)]
                dma(A1[:], MOD[b:b + 1, D:2 * D].partition_broadcast(128), r=["MOD"], w=["A1"])
                dma(B1[:], MOD[b:b + 1, 0:D].partition_broadcast(128), r=["MOD"], w=["B1"])
                dma(w1b[:], norm1_w.partition_broadcast(128), w=["w1b"])
                V(lambda e: e.scalar_tensor_tensor(out=A1[:], in0=A1[:], scalar=1.0, in1=w1b[:], op0=ALU.add, op1=ALU.mult), r=["A1", "w1b"], w=["A1"])
                V(lambda e: e.memset(nmx[:], 0.0), w=["nmx"])
                for tt in range(NT):
                    s = tt % 2
                    hx = hxs[s]
                    st4 = st4s[s]
                    dma(xt[s][:], x[b, tt * 128:(tt + 1) * 128, :], w=[("xt", s)])
                    A(lambda e: e.activation(out=junk[:], in_=xt[s][:], func=AF.Square, accum_out=st4[:, 0:1]), r=[("xt", s)], w=["junkB", ("st0", s)])
                    V(lambda e: e.tensor_scalar(out=st4[:, 1:2], in0=st4[:, 0:1], scalar1=1.0 / D, scalar2=EPS, op0=ALU.mult, op1=ALU.add), r=[("st0", s)], w=[("st1", s)])
                    A(lambda e: e.activation(out=st4[:, 2:3], in_=st4[:, 1:2], func=AF.Sqrt), r=[("st1", s)], w=[("st2", s)])
                    V(lambda e: e.reciprocal(out=st4[:, 3:4], in_=st4[:, 2:3]), r=[("st2", s)], w=[("st3", s)])
                    V(lambda e: e.scalar_tensor_tensor(out=hx[:], in0=xt[s][:], scalar=st4[:, 3:4], in1=A1[:], op0=ALU.mult, op1=ALU.mult), r=[("xt", s), ("st3", s), "A1"], w=[("hx", s)])
                    G(lambda e: e.tensor_tensor(out=hb[s][:], in0=hx[:], in1=B1[:], op=ALU.add), r=[("hx", s), "B1"], w=[("hb", s)])
                    for kc in range(8):
                        P(lambda e: e.transpose(out=ptb[s][:, kc * 128:(kc + 1) * 128], in_=hb[s][:, kc * 128:(kc + 1) * 128], identity=identb[:]),
                          r=[("hb", s), "identb"], w=[("ptbB", s)])
                    A(lambda e: e.activation(out=hT[:, :, tt * 128:(tt + 1) * 128], in_=ptb[s][:].rearrange("p (k t) -> p k t", k=8), func=AF.Copy),
                      r=[("ptbB", s)], w=[("hT", tt)])

                wf = [sbt(ph, f"wf{i}", [128, 8, 512], F32) for i in range(2)]
                wb = [sbt(ph, f"wb{i}", [128, 8, 512], BF16) for i in range(2)]
                qfs = [sbt(ph, f"qf{i}", [128, 512], F32) for i in range(4)]
                sqjs = [sbt(ph, f"sqj{i}", [128, 512], F32) for i in range(2)]
                rts = [sbt(ph, f"rt{i}", [128, 4, 8, 8], F32) for i in range(4)]
                nsqs = [sbt(ph, f"nsq{i}", [128, 8], F32) for i in range(4)]
                qb = [sbt(ph, f"qb{i}", [128, 512], BF16) for i in range(4)]
                stage = [sbt(ph, f"stage{i}", [128, 4, 512], BF16) for i in range(2)]
                ob = [sbt(ph, f"ob{i}", [128, 512], BF16) for i in range(3)]
                xbuf = sbt(ph, "xbuf", [128, 4, 515], F32)
                cacc = sbt(ph, "cacc", [128, 512], F32)
                csil = sbt(ph, "csil", [128, 512], F32)
                cstg = [sbt(ph, f"cstg{i}", [128, 512], BF16) for i in range(2)]
                cw = sbt(ph, "cw", [128, 8, 4], F32)
                cbi = sbt(ph, "cbi", [128, 8], F32)
                gbias = sbt(ph, "gbias", [128, 16], F32)
                gz = sbt(ph, "gz", [128, 16], F32)
                gt = [sbt(ph, f"gtB{i}", [128, 16], F32) for i in range(2)]
                for j in range(4):
                    dma(cw[:, :, j], conv_w[j].rearrange("(blk p) -> p blk", p=128), w=["cw"], allow_slow_non_contiguous=True)
                dma(cbi[:], conv_b.rearrange("o (blk p) -> p (o blk)", p=128), w=["cbi"], allow_slow_non_contiguous=True)
                dma(gbias[:, 0:8], ml_ib.partition_broadcast(128), w=["gbias0"])
                dma(gbias[:, 8:16], ml_fb.partition_broadcast(128), w=["gbias1"])

                chunks = []
                for i in range(2):
                    chunks.append((i * 512, 512, "qa", i))
                for i in range(2):
                    chunks.append((1024 + i * 512, 512, "ka", i))
                for i in range(2):
                    chunks.append((2048 + i * 512, 512, "va", i))
                chunks.append((3072, 512, "qm", 0))
                chunks.append((3584, 512, "km", 0))
                for i in range(2):
                    chunks.append((4096 + i * 512, 512, "vm", i))
                for i in range(2):
                    chunks.append((5120 + i * 512, 512, "om", i))
                chunks.append((6144, 16, "gate", 0))
                for i in range(2):
                    chunks.append((6160 + i * 512, 512, "ga", i))
                for i in range(2):
                    chunks.append((7184 + i * 512, 512, "gm", i))

                pcount = 0
                ocount = 0
                qcount = 0
                for ci, (c0, ncol, kind, idx) in enumerate(chunks):
                    s = ci % 2
                    dma(wf[s][:, :, 0:ncol], win_v[:, :, c0:c0 + ncol], w=[("wf", s)])
                    G(lambda e: e.tensor_copy(out=wb[s][:, :, 0:ncol], in_=wf[s][:, :, 0:ncol]), r=[("wf", s)], w=[("wb", s)])
                    if kind in ("qm", "km"):
                        sc_ = 1.0 if kind == "qm" else 0.125
                        dst = QMT if kind == "qm" else KMT
                        boff = 0 if kind == "qm" else 4
                        for cb in range(4):
                            V(lambda e: e.memset(xbuf[:, cb, 0:3], 0.0), w=[("xbuf", cb)])
                        for tg in range(NG):
                            for cb in range(4):
                                pb = pcount % 4
                                pcount += 1
                                for kc in range(8):
                                    P(lambda e: e.matmul(psb[pb][:], lhsT=wb[s][:, kc, cb * 128:(cb + 1) * 128], rhs=hT[:, kc, tg * 512:(tg + 1) * 512], start=(kc == 0), stop=(kc == 7)),
                                      r=[("wb", s)] + [("hT", t_) for t_ in range(tg * 4, tg * 4 + 4)], w=[("psB", pb)])
                                if tg > 0:
                                    V(lambda e: e.tensor_copy(out=xbuf[:, cb, 0:3], in_=xbuf[:, cb, 512:515]), r=[("xbuf", cb)], w=[("xbuf", cb)])
                                A(lambda e: e.activation(out=xbuf[:, cb, 3:515], in_=psb[pb][:], func=AF.Copy), r=[("psB", pb)], w=[("xbuf", cb)])
                                V(lambda e: e.tensor_scalar(out=cacc[:], in0=xbuf[:, cb, 0:512], scalar1=cw[:, boff + cb, 0:1], scalar2=None, op0=ALU.mult), r=[("xbuf", cb), "cw"], w=["cacc"])
                                for j in range(1, 4):
                                    V(lambda e: e.scalar_tensor_tensor(out=cacc[:], in0=xbuf[:, cb, j:j + 512], scalar=cw[:, boff + cb, j:j + 1], in1=cacc[:], op0=ALU.mult, op1=ALU.add),
                                      r=[("xbuf", cb), "cw", "cacc"], w=["cacc"])
                                A(lambda e: e.activation(out=csil[:], in_=cacc[:], func=AF.Silu, bias=cbi[:, boff + cb:boff + cb + 1], scale=1.0), r=["cacc", "cbi"], w=["csil"])
                                cs = qcount % 2
                                qcount += 1
                                G(lambda e: e.tensor_scalar(out=cstg[cs][:], in0=csil[:], scalar1=sc_, scalar2=None, op0=ALU.mult), r=["csil"], w=[("cstg", cs)])
                                dma(dst[b, cb, :, tg * 512:(tg + 1) * 512], cstg[cs][:], r=[("cstg", cs)], w=["QKMT"])
                        continue
                    for tt in range(NT):
                        pb = pcount % 4
                        pcount += 1
                        for kc in range(8):
                            P(lambda e: e.matmul(psb[pb][:, 0:ncol], lhsT=hT[:, kc, tt * 128:(tt + 1) * 128], rhs=wb[s][:, kc, 0:ncol], start=(kc == 0), stop=(kc == 7)),
                              r=[("wb", s), ("hT", tt)], w=[("psB", pb)])
                        if kind in ("qa", "ka"):
                            z = tt % 4
                            z2 = tt % 2
                            qf, sqj, rt, nsq = qfs[z], sqjs[z2], rts[z], nsqs[z]
                            kq = ("qf", z)
                            A(lambda e: e.activation(out=qf[:], in_=psb[pb][:], func=AF.Copy), r=[("psB", pb)], w=[kq])
                            qv = qf[:].rearrange("p (g d) -> p g d", g=8)
                            x1 = qv[:, :, 0:8]
                            x2 = qv[:, :, 8:16]
                            cosb = cosf[:, tt * 8:(tt + 1) * 8].unsqueeze(1).to_broadcast([128, 8, 8])
                            sinb = sinf[:, tt * 8:(tt + 1) * 8].unsqueeze(1).to_broadcast([128, 8, 8])
                            V(lambda e: e.tensor_tensor(out=rt[:, 0], in0=x1, in1=cosb, op=ALU.mult), r=[kq, "cst"], w=[("rt0", z)])
                            V(lambda e: e.tensor_tensor(out=rt[:, 1], in0=x2, in1=sinb, op=ALU.mult), r=[kq, "cst"], w=[("rt1", z)])
                            V(lambda e: e.tensor_tensor(out=rt[:, 2], in0=x2, in1=cosb, op=ALU.mult), r=[kq, "cst"], w=[("rt2", z)])
                            V(lambda e: e.tensor_tensor(out=rt[:, 3], in0=x1, in1=sinb, op=ALU.mult), r=[kq, "cst"], w=[("rt3", z)])
                            V(lambda e: e.tensor_tensor(out=x1, in0=rt[:, 0], in1=rt[:, 1], op=ALU.subtract), r=[("rt0", z), ("rt1", z), ("rt2", z), ("rt3", z)], w=[kq])
                            V(lambda e: e.tensor_tensor(out=x2, in0=rt[:, 2], in1=rt[:, 3], op=ALU.add), r=[("rt2", z), ("rt3", z)], w=[kq])
                            G(lambda e: e.tensor_tensor(out=sqj[:], in0=qf[:], in1=qf[:], op=ALU.mult), r=[kq], w=[("sqj", z2)])
                            V(lambda e: e.reduce_sum(out=nsq[:], in_=sqj[:].rearrange("p (g d) -> p g d", g=8), axis=AX.X), r=[("sqj", z2)], w=[("nsq", z)])
                            ncol0 = (0 if kind == "qa" else 16) + idx * 8
                            V(lambda e: e.tensor_tensor(out=nmx[:, ncol0:ncol0 + 8], in0=nmx[:, ncol0:ncol0 + 8], in1=nsq[:], op=ALU.max), r=[("nsq", z), "nmx"], w=["nmx"])
                            qs = qcount % 4
                            qcount += 1
                            A(lambda e: e.activation(out=qb[qs][:], in_=qf[:], func=AF.Copy, scale=(0.125 if kind == "qa" else 1.0)), r=[kq], w=[("qb", qs)])
                            ps_ = tt % 2
                            for hd in range(4):
                                P(lambda e: e.transpose(out=ptb[ps_][:, hd * 128:(hd + 1) * 128], in_=qb[qs][:, hd * 128:(hd + 1) * 128], identity=identb[:]),
                                  r=[("qb", qs), "identb"], w=[("ptbB", ps_)])
                            sg = (tt // 4) % 2
                            V(lambda e: e.tensor_copy(out=stage[sg][:, :, (tt % 4) * 128:(tt % 4 + 1) * 128], in_=ptb[ps_][:, 0:512].rearrange("p (h t) -> p h t", h=4)),
                              r=[("ptbB", ps_)], w=[("stage", sg)])
                            if tt % 4 == 3:
                                dstT = QT if kind == "qa" else KT
                                t0 = (tt // 4) * 512
                                dma(dstT[b, idx * 4:(idx + 1) * 4, :, t0:t0 + 512].rearrange("h p t -> p h t"), stage[sg][:], r=[("stage", sg)], w=["QKT"])
                        elif kind in ("va", "vm"):
                            os_ = ocount % 3
                            ocount += 1
                            A(lambda e: e.activation(out=ob[os_][:], in_=psb[pb][:], func=AF.Copy), r=[("psB", pb)], w=[("ob", os_)])
                            dstv = VA if kind == "va" else VM
                            dma(dstv[b, tt * 128:(tt + 1) * 128, idx * 512:(idx + 1) * 512], ob[os_][:], r=[("ob", os_)], w=["VAVM"])
                        elif kind in ("om", "ga", "gm"):
                            os_ = ocount % 3
                            ocount += 1
                            A(lambda e: e.activation(out=ob[os_][:], in_=psb[pb][:], func=AF.Sigmoid), r=[("psB", pb)], w=[("ob", os_)])
                            dsts = {"om": OMS, "ga": GAS, "gm": GMS}[kind]
                            dma(dsts[b, tt * 128:(tt + 1) * 128, idx * 512:(idx + 1) * 512], ob[os_][:], r=[("ob", os_)], w=["SIGS"])
                        else:
                            gs = tt % 2
                            V(lambda e: e.tensor_tensor(out=gz[:], in0=psb[pb][:, 0:16], in1=gbias[:], op=ALU.add), r=[("psB", pb), "gbias0", "gbias1"], w=["gz"])
                            A(lambda e: e.activation(out=gz[:, 8:16], in_=gz[:, 8:16], func=AF.Exp, scale=-1.0), r=["gz"], w=["gz"])
                            A(lambda e: e.activation(out=gz[:, 8:16], in_=gz[:, 8:16], func=AF.Ln, bias=1.0, scale=1.0), r=["gz"], w=["gz"])
                            V(lambda e: e.tensor_copy(out=gt[gs][:, 0:8], in_=gz[:, 0:8]), r=["gz"], w=[("gtB", gs)])
                            V(lambda e: e.tensor_scalar(out=gt[gs][:, 8:16], in0=gz[:, 8:16], scalar1=-1.0, scalar2=None, op0=ALU.mult), r=["gz", ("gtB", gs)], w=[("gtB", gs)])
                            dma(GATES[b, tt * 128:(tt + 1) * 128, :], gt[gs][:], r=[("gtB", gs)], w=["GATES"])
                k.barrier()

            with ExitStack() as ph:
                qmt = sbt(ph, "qmt", [128, 4, S], BF16)
                kmt = sbt(ph, "kmt", [128, 4, S], BF16)
                Cst = sbt(ph, "Cst", [128, 4, 129], F32)
                Cbf = sbt(ph, "Cbf", [128, 4, 129], BF16)
                mlw = sbt(ph, "mlw", [128, D], F32)
                gtl = [sbt(ph, f"gtl{i}", [128, 16], F32) for i in range(2)]
                vmt = [sbt(ph, f"vmt{i}", [128, 8, 129], BF16) for i in range(2)]
                omt = [sbt(ph, f"omt{i}", [128, D], BF16) for i in range(2)]
                gmt = [sbt(ph, f"gmt{i}", [128, D], BF16) for i in range(2)]
                sms = [sbt(ph, f"sm{i}", [128, 8, 8], F32) for i in range(2)]
                Kws = [sbt(ph, f"Kw{i}", [128, 4, 128], BF16) for i in range(2)]
                PT = [sbt(ph, f"PT{i}", [128, 128], BF16) for i in range(2)]
                hn = sbt(ph, "hn", [128, 8, 128], F32)
                hj = sbt(ph, "hj", [128, 8, 128], F32)
                n8 = sbt(ph, "n8", [128, 8, 8], F32)
                ymb = [sbt(ph, f"ymb{i}", [128, D], BF16) for i in range(2)]
                psGK = pst(ph, "psGK", [128, 1024], BF16)
                psGf = psGK[:, 512:1024].bitcast(F32)
                psS = [pst(ph, f"psS{i}", [128, 512], F32) for i in range(2)]
                psTot = [pst(ph, f"psTot{i}", [128, 512], F32) for i in range(3)]
                psC = [pst(ph, f"psC{i}", [128, 512], F32) for i in range(2)]
                dma(qmt[:], QMT[b].rearrange("c p t -> p c t"), w=["qmt"])
                dma(kmt[:], KMT[b].rearrange("c p t -> p c t"), w=["kmt"])
                dma(mlw[:], ml_nw.partition_broadcast(128), w=["mlw"])
                V(lambda e: e.memset(Cst[:], 0.0), w=[("Cst", h) for h in range(8)])
                V(lambda e: e.memset(Cbf[:], 0.0), w=[("Cbf", h) for h in range(8)])
                for i in range(2):
                    V(lambda e: e.memset(vmt[i][:, :, 128:129], 1.0), w=[("vmt", i)])
                for tt in range(NT):
                    s = tt % 2
                    sm = sms[s]
                    Kw = Kws[s]
                    tsl = slice(tt * 128, (tt + 1) * 128)
                    dma(gtl[s][:], GATES[b, tsl, :], w=[("gtl", s)])
                    dma(vmt[s][:, :, 0:128], VM[b, tsl, :].rearrange("p (h e) -> p h e", h=8), w=[("vmt", s)])
                    dma(omt[s][:], OMS[b, tsl, :], w=[("omt", s)])
                    dma(gmt[s][:], GMS[b, tsl, :], w=[("gmt", s)])
                    lf = gtl[s][:, 8:16]
                    ig = gtl[s][:, 0:8]
                    P(lambda e: e.matmul(psGf[:, 0:8], lhsT=trif, rhs=lf, start=True, stop=True), r=[("gtl", s), "cst"], w=["psGK"])
                    P(lambda e: e.matmul(psGf[:, 8:16], lhsT=onesf, rhs=lf, start=True, stop=True), r=[("gtl", s), "cst"], w=["psGK"])
                    V(lambda e: e.tensor_copy(out=sm[:, 0:2, :], in_=psGf[:, 0:16].rearrange("p (a h) -> p a h", a=2)), r=["psGK"], w=[("sm01", s)])
                    V(lambda e: e.tensor_tensor(out=sm[:, 2, :], in0=ig, in1=sm[:, 0, :], op=ALU.subtract), r=[("gtl", s), ("sm01", s)], w=[("sm2", s)])
                    V(lambda e: e.tensor_tensor(out=sm[:, 6, :], in0=sm[:, 1, :], in1=sm[:, 2, :], op=ALU.add), r=[("sm01", s), ("sm2", s)], w=[("sm6", s)])
                    A(lambda e: e.activation(out=sm[:, 3, :], in_=sm[:, 2, :], func=AF.Exp), r=[("sm2", s)], w=[("sm3", s)])
                    A(lambda e: e.activation(out=sm[:, 4, :], in_=sm[:, 6, :], func=AF.Exp), r=[("sm6", s)], w=[("sm4", s)])
                    A(lambda e: e.activation(out=sm[:, 5, :], in_=sm[:, 1, :], func=AF.Exp), r=[("sm01", s)], w=[("sm5", s)])
                    A(lambda e: e.activation(out=sm[:, 7, :], in_=sm[:, 0, :], func=AF.Exp, scale=-1.0), r=[("sm01", s)], w=[("sm7", s)])
                    for cb in range(4):
                        P(lambda e: e.transpose(out=psGK[:, cb * 128:(cb + 1) * 128], in_=kmt[:, cb, tsl], identity=identb[:]), r=["kmt", "identb"], w=["psGK"])
                    V(lambda e: e.tensor_tensor(out=Kw[:].rearrange("p c (h d) -> p (c h) d", h=2), in0=psGK[:, 0:512].rearrange("p (g d) -> p g d", g=8),
                                                in1=sm[:, 4, :].unsqueeze(2).to_broadcast([128, 8, 64]), op=ALU.mult), r=["psGK", ("sm4", s)], w=[("Kw", s)])
                    started = [False, False, False]
                    for h in range(8):
                        cb, r0 = h // 2, (h % 2) * 64
                        hs = h % 2
                        bank, slot = h // 3, h % 3
                        tcol = slice(slot * 129, (slot + 1) * 129)
                        P(lambda e: e.matmul(psS[hs][:, 0:128], lhsT=kmt[r0:r0 + 64, cb, tsl], rhs=qmt[r0:r0 + 64, cb, tsl], start=True, stop=True), r=["kmt", "qmt"], w=[("psS", hs)])
                        V(lambda e: e.scalar_tensor_tensor(out=PT[hs][:], in0=psS[hs][:, 0:128], scalar=sm[:, 3, h:h + 1], in1=cmf, op0=ALU.mult, op1=ALU.mult),
                          r=[("psS", hs), ("sm3", s), "cst"], w=[("PT", hs)])
                        st_ = not started[bank]
                        started[bank] = True
                        P(lambda e: e.matmul(psTot[bank][:, tcol], lhsT=PT[hs][:], rhs=vmt[s][:, h, :], start=st_, stop=False, skip_group_check=True), r=[("PT", hs), ("vmt", s)], w=[("psTot", bank)])
                        P(lambda e: e.matmul(psTot[bank][:, tcol], lhsT=qmt[r0:r0 + 64, cb, tsl], rhs=Cbf[r0:r0 + 64, cb, :], start=False, stop=True, skip_group_check=True), r=["qmt", ("Cbf", h)], w=[("psTot", bank)])
                        P(lambda e: e.matmul(psC[hs][:, 0:129], lhsT=Kw[:, cb, :], rhs=vmt[s][:, h, :], start=True, stop=True), r=[("Kw", s), ("vmt", s)], w=[("psC", hs)])
                        V(lambda e: e.scalar_tensor_tensor(out=Cst[r0:r0 + 64, cb, :], in0=Cst[r0:r0 + 64, cb, :], scalar=sm[r0:r0 + 64, 5, h:h + 1], in1=psC[hs][r0:r0 + 64, 0:129], op0=ALU.mult, op1=ALU.add),
                          r=[("psC", hs), ("sm5", s), ("Cst", h)], w=[("Cst", h)])
                        G(lambda e: e.tensor_copy(out=Cbf[r0:r0 + 64, cb, :], in_=Cst[r0:r0 + 64, cb, :]), r=[("Cst", h)], w=[("Cbf", h)])
                    for bank in range(3):
                        nh_ = 3 if bank < 2 else 2
                        V(lambda e: e.tensor_copy(out=n8[:, 0, bank * 3:bank * 3 + nh_], in_=psTot[bank][:, 0:nh_ * 129].rearrange("p (s e) -> p s e", e=129)[:, :, 128]), r=[("psTot", bank)], w=[("n80", bank)])
                    k80 = [("n80", i) for i in range(3)]
                    V(lambda e: e.scalar_tensor_tensor(out=n8[:, 1, :], in0=n8[:, 0, :], scalar=-1.0, in1=n8[:, 0, :], op0=ALU.mult, op1=ALU.max), r=k80, w=["n81"])
                    V(lambda e: e.tensor_tensor(out=n8[:, 2, :], in0=n8[:, 1, :], in1=sm[:, 7, :], op=ALU.max), r=["n81", ("sm7", s)], w=["n82"])
                    V(lambda e: e.reciprocal(out=n8[:, 3, :], in_=n8[:, 2, :]), r=["n82"], w=["n83"])
                    for bank in range(3):
                        nh_ = 3 if bank < 2 else 2
                        V(lambda e: e.tensor_tensor(out=hn[:, bank * 3:bank * 3 + nh_, :], in0=psTot[bank][:, 0:nh_ * 129].rearrange("p (s e) -> p s e", e=129)[:, :, 0:128],
                                                    in1=n8[:, 3, bank * 3:bank * 3 + nh_].unsqueeze(2).to_broadcast([128, nh_, 128]), op=ALU.mult), r=[("psTot", bank), "n83"], w=[("hn", bank)])
                    khn = [("hn", i) for i in range(3)]
                    G(lambda e: e.tensor_tensor(out=hj[:], in0=hn[:], in1=hn[:], op=ALU.mult), r=khn, w=["hj"])
                    V(lambda e: e.reduce_sum(out=n8[:, 4, :], in_=hj[:], axis=AX.X), r=["hj"], w=["n84"])
                    V(lambda e: e.tensor_scalar(out=n8[:, 5, :], in0=n8[:, 4, :], scalar1=1.0 / 128, scalar2=EPS, op0=ALU.mult, op1=ALU.add), r=["n84"], w=["n85"])
                    A(lambda e: e.activation(out=n8[:, 6, :], in_=n8[:, 5, :], func=AF.Sqrt), r=["n85"], w=["n86"])
                    V(lambda e: e.reciprocal(out=n8[:, 7, :], in_=n8[:, 6, :]), r=["n86"], w=["n87"])
                    V(lambda e: e.tensor_tensor(out=hj[:], in0=hn[:], in1=n8[:, 7, :].unsqueeze(2).to_broadcast([128, 8, 128]), op=ALU.mult), r=khn + ["n87", "hj"], w=["hj"])
                    hjf = hj[:].rearrange("p h e -> p (h e)")
                    G(lambda e: e.tensor_tensor(out=hjf, in0=hjf, in1=mlw[:], op=ALU.mult), r=["hj", "mlw"], w=["hj"])
                    V(lambda e: e.tensor_tensor(out=hjf, in0=hjf, in1=omt[s][:], op=ALU.mult), r=["hj", ("omt", s)], w=["hj"])
                    G(lambda e: e.tensor_tensor(out=ymb[s][:], in0=hjf, in1=gmt[s][:], op=ALU.mult), r=["hj", ("gmt", s)], w=[("ymb", s)])
                    dma(YM[b, tsl, :], ymb[s][:], r=[("ymb", s)], w=["YM"])
                k.barrier()

            with ExitStack() as ph:
                qts = [sbt(ph, f"qts{i}", [128, S], BF16) for i in range(2)]
                kts = [sbt(ph, f"kts{i}", [128, S], BF16) for i in range(2)]
                vh = [sbt(ph, f"vh{i}", [128, NT, 129], BF16) for i in range(2)]
                negMb = sbt(ph, "negMb", [128, 16], F32)
                nw = sbt(ph, "nw", [16, 8], F32)
                dg = sbt(ph, "dg", [16, 16], F32)
                swb = sbt(ph, "swb", [128, 128], F32)
                PTa = [[sbt(ph, f"PTa{c}{i}", [128, 512], BF16) for i in range(2)] for c in range(2)]
                gat = [sbt(ph, f"gat{i}", [128, 128], BF16) for i in range(2)]
                ymt = [sbt(ph, f"ymt{i}", [128, 128], BF16) for i in range(2)]
                mb = [sbt(ph, f"mb{i}", [128, 128], BF16) for i in range(2)]
                mstage = [sbt(ph, f"mstage{i}", [128, 512], BF16) for i in range(2)]
                psS = [[pst(ph, f"psSa{c}{i}", [128, 512], F32) for i in range(2)] for c in range(2)]
                psA = [pst(ph, f"psA{i}", [128, 512], F32) for i in range(3)]
                psT = pst(ph, "psT", [128, 1024], BF16)
                P(lambda e: e.transpose(out=psA[0][0:16, 0:128], in_=nmx[:, 0:16], identity=identf), r=["nmx", "cst"], w=[("psA", 0)])
                P(lambda e: e.transpose(out=psA[0][0:16, 128:256], in_=nmx[:, 16:32], identity=identf), r=["nmx", "cst"], w=[("psA", 0)])
                V(lambda e: e.reduce_max(out=nw[:, 0:2], in_=psA[0][0:16, 0:256].rearrange("p (a t) -> p a t", a=2), axis=AX.X), r=[("psA", 0)], w=["nw0"])
                V(lambda e: e.tensor_tensor(out=nw[:, 2:3], in0=nw[:, 0:1], in1=nw[:, 1:2], op=ALU.mult), r=["nw0"], w=["nw2"])
                A(lambda e: e.activation(out=nw[:, 3:4], in_=nw[:, 2:3], func=AF.Sqrt), r=["nw2"], w=["nw3"])
                V(lambda e: e.tensor_scalar(out=nw[:, 4:5], in0=nw[:, 3:4], scalar1=-0.125, scalar2=None, op0=ALU.mult), r=["nw3"], w=["nw4"])
                V(lambda e: e.tensor_scalar(out=dg[:], in0=identf[0:16, 0:16], scalar1=nw[:, 4:5], scalar2=None, op0=ALU.mult), r=["nw4", "cst"], w=["dg"])
                P(lambda e: e.matmul(psA[1][:, 0:16], lhsT=onesf[0:16, :], rhs=dg[:], start=True, stop=True), r=["dg", "cst"], w=[("psA", 1)])
                V(lambda e: e.tensor_copy(out=negMb[:], in_=psA[1][:, 0:16]), r=[("psA", 1)], w=["negMb"])
                dma(swb[:], subln_w.partition_broadcast(128), w=["swb"])
                V(lambda e: e.tensor_scalar(out=swb[:], in0=swb[:], scalar1=(1.0 - lam_init), scalar2=None, op0=ALU.mult), r=["swb"], w=["swb"])
                for i in range(2):
                    V(lambda e: e.memset(vh[i][:, :, 128:129], 1.0), w=[("vh", i)])
                fcount = 0
                accS = [sbt(ph, f"accS{i}", [128, 3, 387], F32) for i in range(2)]
                rrs = [sbt(ph, f"rrs{i}", [128, 8], F32) for i in range(2)]
                o1s = [sbt(ph, f"o1s{i}", [128, 128], F32) for i in range(2)]
                o2s = [sbt(ph, f"o2s{i}", [128, 128], F32) for i in range(2)]
                ojs = [sbt(ph, f"ojs{i}", [128, 128], F32) for i in range(2)]
                gcnt = 0
                def load_head(hd_):
                    s_ = hd_ % 2
                    dma(qts[s_][:], QT[b, hd_], w=[("qts", s_)])
                    dma(kts[s_][:], KT[b, hd_], w=[("kts", s_)])
                    for t8 in range(0, NT, 8):
                        te = min(NT, t8 + 8)
                        dma(vh[s_][:, t8:te, 0:128], VA[b].rearrange("(tt p) c -> p tt c", p=128)[:, t8:te, hd_ * 128:(hd_ + 1) * 128], w=[("vh", s_)])

                load_head(0)
                for hd in range(8):
                    s = hd % 2
                    if hd + 1 < 8:
                        load_head(hd + 1)
                    blocks = [(g, kt) for g in range(NG) for kt in range(4 * g + 4)]

                    def qk_exp(bi):
                        g, kt = blocks[bi]
                        bs = bi % 2
                        for c in range(2):
                            P(lambda e: e.matmul(psS[c][bs][:], lhsT=kts[s][c * 64:(c + 1) * 64, kt * 128:(kt + 1) * 128], rhs=qts[s][c * 64:(c + 1) * 64, g * 512:(g + 1) * 512], start=True, stop=True),
                              r=[("kts", s), ("qts", s)], w=[("psSa", c, bs)])
                            A(lambda e: e.activation(out=PTa[c][bs][:], in_=psS[c][bs][:], func=AF.Exp, bias=negMb[:, hd * 2 + c:hd * 2 + c + 1], scale=1.0),
                              r=[("psSa", c, bs), "negMb"], w=[("PTa", c, bs)])

                    qk_exp(0)
                    started = [False, False, False]
                    for bi, (g, kt) in enumerate(blocks):
                        bs = bi % 2
                        if bi + 1 < len(blocks):
                            qk_exp(bi + 1)
                        if kt == 0:
                            started = [False, False, False]
                        for qi in range(4):
                            qt_ = 4 * g + qi
                            if kt > qt_:
                                continue
                            for c in range(2):
                                if kt == qt_:
                                    eng = V if c == 0 else G
                                    eng(lambda e: e.tensor_tensor(out=PTa[c][bs][:, qi * 128:(qi + 1) * 128], in0=PTa[c][bs][:, qi * 128:(qi + 1) * 128], in1=cmb[:], op=ALU.mult),
                                        r=[("PTa", c, bs), "cmb"], w=[("PTa", c, bs)])
                                ai = c * 4 + qi
                                bank, slot = ai // 3, ai % 3
                                st_ = not started[bank]
                                started[bank] = True
                                P(lambda e: e.matmul(psA[bank][:, slot * 129:(slot + 1) * 129], lhsT=PTa[c][bs][:, qi * 128:(qi + 1) * 128], rhs=vh[s][:, kt, :], start=st_, stop=(kt == qt_), skip_group_check=True),
                                  r=[("PTa", c, bs), ("vh", s)], w=[("psA", bank)])
                        if kt != 4 * g + 3:
                            continue
                        gs_ = gcnt % 2
                        gcnt += 1
                        for bank in range(3):
                            ncol_ = 387 if bank < 2 else 258
                            A(lambda e: e.activation(out=accS[gs_][:, bank, 0:ncol_], in_=psA[bank][:, 0:ncol_], func=AF.Copy), r=[("psA", bank)], w=[("accS", gs_, bank)])
                        ms = g % 2
                        for qi in range(4):
                            qt_ = 4 * g + qi
                            tsl = slice(qt_ * 128, (qt_ + 1) * 128)
                            fs = fcount % 2
                            fcount += 1
                            rr, o1, o2, oj = rrs[fs], o1s[fs], o2s[fs], ojs[fs]
                            dma(gat[fs][:], GAS[b, tsl, hd * 128:(hd + 1) * 128], w=[("gat", fs)])
                            dma(ymt[fs][:], YM[b, tsl, hd * 128:(hd + 1) * 128], w=[("ymt", fs)])
                            a1 = accS[gs_][:, qi // 3, (qi % 3) * 129:(qi % 3 + 1) * 129]
                            a2i = 4 + qi
                            a2 = accS[gs_][:, a2i // 3, (a2i % 3) * 129:(a2i % 3 + 1) * 129]
                            rk = [("accS", gs_, qi // 3), ("accS", gs_, a2i // 3)]
                            V(lambda e: e.reciprocal(out=rr[:, 0:1], in_=a1[:, 128:129]), r=rk, w=[("rr0", fs)])
                            V(lambda e: e.reciprocal(out=rr[:, 1:2], in_=a2[:, 128:129]), r=rk, w=[("rr1", fs)])
                            V(lambda e: e.tensor_tensor(out=rr[:, 2:3], in0=rr[:, 1:2], in1=lamt[:, 0:1], op=ALU.mult), r=[("rr1", fs), "lamt"], w=[("rr2", fs)])
                            A(lambda e: e.activation(out=o1[:], in_=a1[:, 0:128], func=AF.Copy, scale=rr[:, 0:1]), r=rk + [("rr0", fs)], w=[("o1", fs)])
                            V(lambda e: e.scalar_tensor_tensor(out=o2[:], in0=a2[:, 0:128], scalar=rr[:, 2:3], in1=o1[:], op0=ALU.mult, op1=ALU.add), r=rk + [("rr2", fs), ("o1", fs)], w=[("o2", fs)])
                            A(lambda e: e.activation(out=oj[:], in_=o2[:], func=AF.Square, accum_out=rr[:, 3:4]), r=[("o2", fs)], w=[("oj", fs), ("rr3", fs)])
                            V(lambda e: e.tensor_scalar(out=rr[:, 4:5], in0=rr[:, 3:4], scalar1=1.0 / 128, scalar2=EPS, op0=ALU.mult, op1=ALU.add), r=[("rr3", fs)], w=[("rr4", fs)])
                            A(lambda e: e.activation(out=rr[:, 5:6], in_=rr[:, 4:5], func=AF.Sqrt), r=[("rr4", fs)], w=[("rr5", fs)])
                            V(lambda e: e.reciprocal(out=rr[:, 6:7], in_=rr[:, 5:6]), r=[("rr5", fs)], w=[("rr6", fs)])
                            V(lambda e: e.scalar_tensor_tensor(out=oj[:], in0=o2[:], scalar=rr[:, 6:7], in1=swb[:], op0=ALU.mult, op1=ALU.mult), r=[("o2", fs), ("rr6", fs), "swb", ("oj", fs)], w=[("oj", fs)])
                            G(lambda e: e.tensor_tensor(out=oj[:], in0=oj[:], in1=gat[fs][:], op=ALU.mult), r=[("oj", fs), ("gat", fs)], w=[("oj", fs)])
                            G(lambda e: e.tensor_tensor(out=mb[fs][:], in0=oj[:], in1=ymt[fs][:], op=ALU.add), r=[("oj", fs), ("ymt", fs)], w=[("mb", fs)])
                            P(lambda e: e.transpose(out=psT[:, qi * 128:(qi + 1) * 128], in_=mb[fs][:], identity=identb[:]), r=[("mb", fs), "identb"], w=["psT"])
                        V(lambda e: e.tensor_copy(out=mstage[ms][:], in_=psT[:, 0:512]), r=["psT"], w=[("mstage", ms)])
                        dma(MT[b, hd, :, g * 512:(g + 1) * 512], mstage[ms][:], r=[("mstage", ms)], w=["MT"])
                k.barrier()

        wq_v = peer_wq.rearrange("(kc p) n -> p kc n", p=128)
        wo_v = w_out.rearrange("(kc p) n -> p kc n", p=128)
        with ExitStack() as ph:
            wob = sbt(ph, "wob", [128, 8, D], BF16)
            wqb = sbt(ph, "wqb", [128, 8, 2048], BF16)
            keyT = sbt(ph, "keyT", [128, 16, 128], BF16)
            psT = [pst(ph, f"psTE{i}", [128, 1024], BF16) for i in range(2)]
            ph2 = ExitStack()
            wtmp = [sbt(ph2, f"wtmp{i}", [128, 8, 512], F32) for i in range(2)]
            ktmp = sbt(ph2, "ktmp", [128, 16, 128], F32)
            ktb = sbt(ph2, "ktb", [128, 16, 128], BF16)
            wi = 0
            for n0 in range(0, D, 512):
                s = wi % 2
                wi += 1
                dma(wtmp[s][:], wo_v[:, :, n0:n0 + 512], w=[("wtmp", s)])
                V(lambda e: e.tensor_copy(out=wob[:, :, n0:n0 + 512], in_=wtmp[s][:]), r=[("wtmp", s)], w=["wob"])
            for n0 in range(0, 2048, 512):
                s = wi % 2
                wi += 1
                dma(wtmp[s][:], wq_v[:, :, n0:n0 + 512], w=[("wtmp", s)])
                G(lambda e: e.tensor_copy(out=wqb[:, :, n0:n0 + 512], in_=wtmp[s][:]), r=[("wtmp", s)], w=["wqb"])
            dma(ktmp[:], peer_keys.rearrange("g n d -> n g d"), w=["ktmp"])
            V(lambda e: e.tensor_copy(out=ktb[:], in_=ktmp[:]), r=["ktmp"], w=["ktb"])
            for g8 in range(2):
                for j in range(8):
                    gi = g8 * 8 + j
                    P(lambda e: e.transpose(out=psT[g8][:, j * 128:(j + 1) * 128], in_=ktb[:, gi, :], identity=identb[:]), r=["ktb", "identb"], w=[("psTE", g8)])
                V(lambda e: e.tensor_copy(out=keyT[:, g8 * 8:(g8 + 1) * 8, :], in_=psT[g8][:].rearrange("p (g n) -> p g n", g=8)), r=[("psTE", g8)], w=["keyT"])
            k.barrier()
            ph2.close()
            gt1 = sbt(ph, "gt1", [128, D], F32)
            A2 = sbt(ph, "A2", [128, D], F32)
            B2 = sbt(ph, "B2", [128, D], F32)
            w2b = sbt(ph, "w2b", [128, D], F32)
            mt = [sbt(ph, f"mt{i}", [128, 8, 128], BF16) for i in range(2)]
            xt = [sbt(ph, f"xtE{i}", [128, D], F32) for i in range(2)]
            x1 = [sbt(ph, f"x1E{i}", [128, D], F32) for i in range(2)]
            junk = sbt(ph, "junkE", [128, D], BF16)
            hx = sbt(ph, "hxE", [128, D], F32)
            hb = sbt(ph, "hbE", [128, D], BF16)
            h2t = [sbt(ph, f"h2t{i}", [128, 8, 128], BF16) for i in range(2)]
            qpt = sbt(ph, "qpt", [128, 16, 128], BF16)
            scs = [sbt(ph, f"sc{i}", [128, 16, 128], F32) for i in range(2)]
            wk = sbt(ph, "wkE", [128, 16, 128], F32)
            v16 = sbt(ph, "v16", [128, 16, 16], F32)
            i16u = sbt(ph, "i16u", [128, 16, 16], U32)
            i16v = sbt(ph, "i16v", [128, 16, 16], F32)
            cand = sbt(ph, "cand", [128, 8, 256], F32)
            cwk = sbt(ph, "cwk", [128, 8, 256], F32)
            c16 = sbt(ph, "c16", [128, 8, 16], F32)
            p16u = sbt(ph, "p16u", [128, 8, 16], U32)
            p16 = sbt(ph, "p16", [128, 8, 16], F32)
            d1 = sbt(ph, "d1", [128, 8, 16, 16], F32)
            d2 = sbt(ph, "d2", [128, 8, 16, 16], F32)
            ar = sbt(ph, "ar", [128, 8, 16], F32)
            br = sbt(ph, "br", [128, 8, 16], F32)
            igt = [sbt(ph, f"igt{i}", [128, 3, 128], F32) for i in range(2)]
            st4 = sbt(ph, "st4E", [128, 16], F32)
            psO = [pst(ph, f"psO{i}", [128, 512], F32) for i in range(2)]
            psQ = [pst(ph, f"psQ{i}", [128, 512], F32) for i in range(2)]
            psX = [pst(ph, f"psX{i}", [128, 512], F32) for i in range(2)]
            dma(w2b[:], norm2_w.partition_broadcast(128), w=["w2b"])
            def e_front(b, tt):
                s = tt % 2
                tsl = slice(tt * 128, (tt + 1) * 128)
                if tt == 0:
                    dma(gt1[:], MOD[b:b + 1, 2 * D:3 * D].partition_broadcast(128), r=["MOD"], w=["gt1"])
                    dma(B2[:], MOD[b:b + 1, 3 * D:4 * D].partition_broadcast(128), r=["MOD"], w=["B2"])
                    dma(A2[:], MOD[b:b + 1, 4 * D:5 * D].partition_broadcast(128), r=["MOD"], w=["A2"])
                    V(lambda e: e.scalar_tensor_tensor(out=A2[:], in0=A2[:], scalar=1.0, in1=w2b[:], op0=ALU.add, op1=ALU.mult), r=["A2", "w2b"], w=["A2"])
                dma(mt[s][:], MT[b, :, :, tsl].rearrange("h p t -> p h t"), r=["MT"], w=[("mt", s)])
                dma(xt[s][:], x[b, tsl, :], w=[("xtE", s)])
                for nh in range(2):
                    for kc in range(8):
                        P(lambda e: e.matmul(psO[nh][:], lhsT=mt[s][:, kc, :], rhs=wob[:, kc, nh * 512:(nh + 1) * 512], start=(kc == 0), stop=(kc == 7)),
                          r=[("mt", s), "wob"], w=[("psO", nh)])
                    V(lambda e: e.tensor_tensor(out=x1[s][:, nh * 512:(nh + 1) * 512], in0=psO[nh][:], in1=gt1[:, nh * 512:(nh + 1) * 512], op=ALU.mult), r=[("psO", nh), "gt1"], w=[("x1E", s)])
                G(lambda e: e.tensor_tensor(out=x1[s][:], in0=x1[s][:], in1=xt[s][:], op=ALU.add), r=[("x1E", s), ("xtE", s)], w=[("x1E", s)])
                dma(X1[b, tsl, :], x1[s][:], r=[("x1E", s)], w=["X1"])
                A(lambda e: e.activation(out=junk[:], in_=x1[s][:], func=AF.Square, accum_out=st4[:, 0:1]), r=[("x1E", s)], w=["junkE", "sE0"])
                V(lambda e: e.tensor_scalar(out=st4[:, 1:2], in0=st4[:, 0:1], scalar1=1.0 / D, scalar2=EPS, op0=ALU.mult, op1=ALU.add), r=["sE0"], w=["sE1"])
                A(lambda e: e.activation(out=st4[:, 2:3], in_=st4[:, 1:2], func=AF.Sqrt), r=["sE1"], w=["sE2"])
                V(lambda e: e.reciprocal(out=st4[:, 3:4], in_=st4[:, 2:3]), r=["sE2"], w=["sE3"])
                V(lambda e: e.scalar_tensor_tensor(out=hx[:], in0=x1[s][:], scalar=st4[:, 3:4], in1=A2[:], op0=ALU.mult, op1=ALU.mult), r=[("x1E", s), "sE3", "A2"], w=["hxE"])
                G(lambda e: e.tensor_tensor(out=hb[:], in0=hx[:], in1=B2[:], op=ALU.add), r=["hxE", "B2"], w=["hbE"])
                for kc in range(8):
                    P(lambda e: e.transpose(out=psT[0][:, kc * 128:(kc + 1) * 128], in_=hb[:, kc * 128:(kc + 1) * 128], identity=identb[:]), r=["hbE", "identb"], w=[("psTE", 0)])
                A(lambda e: e.activation(out=h2t[s][:], in_=psT[0][:].rearrange("p (k t) -> p k t", k=8), func=AF.Copy), r=[("psTE", 0)], w=[("h2t", s)])
                dma(H2T[b, :, :, tsl].rearrange("k p t -> p k t"), h2t[s][:], r=[("h2t", s)], w=["H2T"])
                for gq in range(4):
                    pq = gq % 2
                    for j in range(4):
                        gi = gq * 4 + j
                        for kc in range(8):
                            P(lambda e: e.matmul(psQ[pq][:, j * 128:(j + 1) * 128], lhsT=wqb[:, kc, gi * 128:(gi + 1) * 128], rhs=h2t[s][:, kc, :], start=(kc == 0), stop=(kc == 7)),
                              r=["wqb", ("h2t", s)], w=[("psQ", pq)])
                    A(lambda e: e.activation(out=qpt[:, gq * 4:(gq + 1) * 4, :], in_=psQ[pq][:].rearrange("p (g t) -> p g t", g=4), func=AF.Copy), r=[("psQ", pq)], w=["qpt"])
                sc = scs[s]
                for gq in range(4):
                    px = gq % 2
                    for j in range(4):
                        gi = gq * 4 + j
                        P(lambda e: e.matmul(psX[px][:, j * 128:(j + 1) * 128], lhsT=qpt[:, gi, :], rhs=keyT[:, gi, :], start=True, stop=True), r=["qpt", "keyT"], w=[("psX", px)])
                    A(lambda e: e.activation(out=sc[:, gq * 4:(gq + 1) * 4, :], in_=psX[px][:].rearrange("p (g n) -> p g n", g=4), func=AF.Copy), r=[("psX", px)], w=[("sc", s, gq)])

            def e_back(b, tt):
                s = tt % 2
                tsl = slice(tt * 128, (tt + 1) * 128)
                sc = scs[s]
                for gi in range(16):
                    V(lambda e: e.max(out=v16[:, gi, 0:8], in_=sc[:, gi, :]), r=[("sc", s, gi // 4)], w=[("v16a", gi)])
                for gi in range(16):
                    V(lambda e: e.max_index(out=i16u[:, gi, 0:8], in_max=v16[:, gi, 0:8], in_values=sc[:, gi, :]), r=[("sc", s, gi // 4), ("v16a", gi)], w=[("i16a", gi)])
                for gi in range(16):
                    V(lambda e: e.match_replace(out=wk[:, gi, :], in_to_replace=v16[:, gi, 0:8], in_values=sc[:, gi, :], imm_value=-1e30), r=[("sc", s, gi // 4), ("v16a", gi)], w=[("wkE", gi)])
                for gi in range(16):
                    V(lambda e: e.max(out=v16[:, gi, 8:16], in_=wk[:, gi, :]), r=[("wkE", gi)], w=[("v16b", gi)])
                for gi in range(16):
                    V(lambda e: e.max_index(out=i16u[:, gi, 8:16], in_max=v16[:, gi, 8:16], in_values=wk[:, gi, :]), r=[("wkE", gi), ("v16b", gi)], w=[("i16b", gi)])
                allv = [("v16a", gi) for gi in range(16)] + [("v16b", gi) for gi in range(16)]
                alli = [("i16a", gi) for gi in range(16)] + [("i16b", gi) for gi in range(16)]
                V(lambda e: e.tensor_copy(out=i16v[:], in_=i16u[:]), r=alli, w=["i16v"])
                v16v = v16[:].rearrange("p (h q) k -> p h q k", q=2)
                i16vv = i16v[:].rearrange("p (h q) k -> p h q k", q=2)
                V(lambda e: e.tensor_tensor(out=cand[:].rearrange("p h (a b) -> p h a b", a=16), in0=v16v[:, :, 0, :].unsqueeze(3).to_broadcast([128, 8, 16, 16]),
                                            in1=v16v[:, :, 1, :].unsqueeze(2).to_broadcast([128, 8, 16, 16]), op=ALU.add), r=allv, w=["cand"])
                for h in range(8):
                    V(lambda e: e.max(out=c16[:, h, 0:8], in_=cand[:, h, :]), r=["cand"], w=[("c16a", h)])
                for h in range(8):
                    V(lambda e: e.max_index(out=p16u[:, h, 0:8], in_max=c16[:, h, 0:8], in_values=cand[:, h, :]), r=["cand", ("c16a", h)], w=[("p16a", h)])
                for h in range(8):
                    V(lambda e: e.match_replace(out=cwk[:, h, :], in_to_replace=c16[:, h, 0:8], in_values=cand[:, h, :], imm_value=-1e30), r=["cand", ("c16a", h)], w=[("cwk", h)])
                for h in range(8):
                    V(lambda e: e.max(out=c16[:, h, 8:16], in_=cwk[:, h, :]), r=[("cwk", h)], w=[("c16b", h)])
                for h in range(8):
                    V(lambda e: e.max_index(out=p16u[:, h, 8:16], in_max=c16[:, h, 8:16], in_values=cwk[:, h, :]), r=[("cwk", h), ("c16b", h)], w=[("p16b", h)])
                allc = [("c16a", h) for h in range(8)] + [("c16b", h) for h in range(8)]
                allp = [("p16a", h) for h in range(8)] + [("p16b", h) for h in range(8)]
                V(lambda e: e.tensor_copy(out=p16[:], in_=p16u[:]), r=allp, w=["p16"])
                ig0 = igt[s][:, 0, :].rearrange("p (h r) -> p h r", h=8)
                ig1 = igt[s][:, 1, :].rearrange("p (h r) -> p h r", h=8)
                HS = [slice(0, 4), slice(4, 8)]

                def bc4(ap3, axis):
                    return ap3.unsqueeze(axis).to_broadcast([128, 4, 16, 16])

                a16b = a16f.unsqueeze(1).unsqueeze(1).to_broadcast([128, 4, 16, 16])
                i16b = i16f.unsqueeze(1).unsqueeze(1).to_broadcast([128, 4, 16, 16])
                for z, hsl in enumerate(HS):
                    V(lambda e: e.tensor_tensor(out=d1[:, hsl], in0=bc4(p16[:, hsl, :], 3), in1=a16b, op=ALU.subtract), r=["p16", "cst"], w=[("d1", z)])
                for z, hsl in enumerate(HS):
                    V(lambda e: e.tensor_scalar(out=d2[:, hsl], in0=d1[:, hsl], scalar1=0.0, scalar2=None, op0=ALU.is_ge), r=[("d1", z)], w=[("d2", z)])
                for z, hsl in enumerate(HS):
                    V(lambda e: e.tensor_scalar(out=d1[:, hsl], in0=d1[:, hsl], scalar1=15.5, scalar2=None, op0=ALU.is_lt), r=[("d1", z), ("d2", z)], w=[("d1", z)])
                for z, hsl in enumerate(HS):
                    V(lambda e: e.tensor_tensor(out=d1[:, hsl], in0=d1[:, hsl], in1=d2[:, hsl], op=ALU.mult), r=[("d1", z), ("d2", z)], w=[("d1", z)])
                for z, hsl in enumerate(HS):
                    G(lambda e: e.tensor_tensor(out=d2[:, hsl], in0=d1[:, hsl], in1=a16b, op=ALU.mult), r=[("d1", z), "cst"], w=[("d2", z)])
                for z, hsl in enumerate(HS):
                    V(lambda e: e.reduce_sum(out=ar[:, hsl, :], in_=d2[:, hsl], axis=AX.X), r=[("d2", z)], w=[("ar", z)])
                for z, hsl in enumerate(HS):
                    G(lambda e: e.tensor_tensor(out=d2[:, hsl], in0=d1[:, hsl], in1=bc4(i16vv[:, hsl, 0, :], 2), op=ALU.mult), r=[("d1", z), "i16v", ("ar", z)], w=[("d2", z)])
                for z, hsl in enumerate(HS):
                    V(lambda e: e.reduce_sum(out=ig0[:, hsl, :], in_=d2[:, hsl], axis=AX.X), r=[("d2", z)], w=[("igt", s, z)])
                for z, hsl in enumerate(HS):
                    V(lambda e: e.tensor_tensor(out=br[:, hsl, :], in0=p16[:, hsl, :], in1=ar[:, hsl, :], op=ALU.subtract), r=["p16", ("ar", z)], w=[("br", z)])
                for z, hsl in enumerate(HS):
                    V(lambda e: e.tensor_tensor(out=d1[:, hsl], in0=bc4(br[:, hsl, :], 3), in1=i16b, op=ALU.is_equal), r=[("br", z), "cst", ("d2", z)], w=[("d1", z)])
                for z, hsl in enumerate(HS):
                    G(lambda e: e.tensor_tensor(out=d2[:, hsl], in0=d1[:, hsl], in1=bc4(i16vv[:, hsl, 1, :], 2), op=ALU.mult), r=[("d1", z), "i16v"], w=[("d2", z)])
                for z, hsl in enumerate(HS):
                    V(lambda e: e.reduce_sum(out=ig1[:, hsl, :], in_=d2[:, hsl], axis=AX.X), r=[("d2", z), ("igt", s, z)], w=[("igt", s, z)])
                V(lambda e: e.tensor_tensor(out=ar[:], in0=c16[:], in1=c16[:, :, 0:1].to_broadcast([128, 8, 16]), op=ALU.subtract), r=allc + [("br", 0), ("br", 1), ("ar", 0), ("ar", 1)], w=[("ar", 0), ("ar", 1)])
                A(lambda e: e.activation(out=ar[:], in_=ar[:], func=AF.Exp), r=[("ar", 0), ("ar", 1)], w=[("ar", 0), ("ar", 1)])
                V(lambda e: e.reduce_sum(out=st4[:, 8:16], in_=ar[:], axis=AX.X), r=[("ar", 0), ("ar", 1)], w=["sE8"])
                V(lambda e: e.reciprocal(out=st4[:, 8:16], in_=st4[:, 8:16]), r=["sE8"], w=["sE8"])
                V(lambda e: e.tensor_tensor(out=igt[s][:, 2, :].rearrange("p (h r) -> p h r", h=8), in0=ar[:], in1=st4[:, 8:16].unsqueeze(2).to_broadcast([128, 8, 16]), op=ALU.mult),
                  r=[("ar", 0), ("ar", 1), "sE8", ("igt", s, 0), ("igt", s, 1)], w=[("igt", s, 0), ("igt", s, 1)])
                dma(IG[b, tsl, :, :], igt[s][:], r=[("igt", s, 0), ("igt", s, 1)], w=["IG"])

            tiles = [(b, tt) for b in range(NB) for tt in range(NT)]
            e_front(*tiles[0])
            for ti, (b, tt) in enumerate(tiles):
                if ti + 1 < len(tiles):
                    e_front(*tiles[ti + 1])
                e_back(b, tt)
            k.barrier()

        TG = 256
        NGR = T // TG
        X1f = X1.rearrange("b s d -> (b s) d")
        outf = out.rearrange("b s d -> (b s) d")
        IGf = IG.rearrange("b s a r -> (b s) a r")
        with ExitStack() as ph:
            Gm = [sbt(ph, f"Gm{i}", [128, TG, 128], BF16) for i in range(2)]
            ust = [sbt(ph, f"ust{i}", [128, 2, 1024], BF16) for i in range(3)]
            vst = [sbt(ph, f"vst{i}", [128, 2, 1024], BF16) for i in range(3)]
            h2s = [sbt(ph, f"h2F{i}", [128, 8, TG], BF16) for i in range(2)]
            igl = sbt(ph, "igl", [128, 2, 3, 128], F32)
            igT = sbt(ph, "igT", [128, 3, TG], F32)
            ohA = [sbt(ph, f"ohA{i}", [128, 8, 128], BF16) for i in range(2)]
            ohB = [sbt(ph, f"ohB{i}", [128, 8, 128], BF16) for i in range(2)]
            ohT = sbt(ph, "ohT", [128, 8, 128], BF16)
            act = [sbt(ph, f"actF{i}", [128, TG], BF16) for i in range(4)]
            ga = [sbt(ph, f"gaF{i}", [128, TG], BF16) for i in range(4)]
            gt2 = sbt(ph, "gt2", [128, D], F32)
            fwb = sbt(ph, "fwb", [128, D], F32)
            x1l = sbt(ph, "x1l", [128, D], F32)
            yo = sbt(ph, "yo", [128, D], F32)
            oo = sbt(ph, "oo", [128, D], F32)
            st4 = sbt(ph, "st4F", [128, 8], F32)
            psY = [pst(ph, f"psY{i}", [128, 512], F32) for i in range(4)]
            psSc = [pst(ph, f"psSc{i}", [128, 512], F32) for i in range(2)]
            psGb = [pst(ph, f"psGb{i}", [128, 512], F32) for i in range(2)]
            dma(fwb[:], fin_w.partition_broadcast(128), w=["fwb"])
            UTv = UT.rearrange("i p f -> p i f")
            VBv = VB.rearrange("i p f -> p i f")
            ldc = [0]
            gcount = [0]
            slots = {}
            iob = iotaf.unsqueeze(1).to_broadcast([128, 8, 128])

            def prep_group(gr):
                t0 = gr * TG
                b = t0 // S
                s0 = t0 % S
                hs = gr % 2
                dma(h2s[hs][:], H2T[b, :, :, s0:s0 + TG].rearrange("k p t -> p k t"), w=[("h2F", hs)])
                dma(igl[:].rearrange("p j a r -> p j (a r)"), IGf[t0:t0 + TG].rearrange("(j p) a r -> p j (a r)", p=128), w=["igl"])
                for j in range(2):
                    for a in range(3):
                        P(lambda e: e.transpose(out=psGb[0][:, 0:128], in_=igl[:, j, a, :], identity=identf), r=["igl", "cst"], w=[("psGb", 0)])
                        V(lambda e: e.tensor_copy(out=igT[:, a, j * 128:(j + 1) * 128], in_=psGb[0][:, 0:128]), r=[("psGb", 0)], w=["igT"])

            def gb_onehots(gr, sub):
                os_ = sub % 2
                tq = slice(sub * 8, (sub + 1) * 8)
                V(lambda e: e.tensor_tensor(out=ohA[os_][:], in0=iob, in1=igT[:, 0, tq].unsqueeze(2).to_broadcast([128, 8, 128]), op=ALU.is_equal), r=["cst", "igT"], w=[("ohA", os_)])
                V(lambda e: e.tensor_tensor(out=ohT[:], in0=iob, in1=igT[:, 1, tq].unsqueeze(2).to_broadcast([128, 8, 128]), op=ALU.is_equal), r=["cst", "igT"], w=["ohT"])
                G(lambda e: e.tensor_tensor(out=ohB[os_][:], in0=ohT[:], in1=igT[:, 2, tq].unsqueeze(2).to_broadcast([128, 8, 128]), op=ALU.mult), r=["ohT", "igT"], w=[("ohB", os_)])

            def gb_mm(gr, sub):
                os_ = sub % 2
                gb = gr % 2
                for q4 in range(2):
                    pg = gcount[0] % 2
                    gcount[0] += 1
                    for j in range(4):
                        tl = q4 * 4 + j
                        P(lambda e: e.matmul(psGb[pg][:, j * 128:(j + 1) * 128], lhsT=ohB[os_][:, tl, :], rhs=ohA[os_][:, tl, :], start=True, stop=True),
                          r=[("ohA", os_), ("ohB", os_)], w=[("psGb", pg)])
                    tb = sub * 8 + q4 * 4
                    A(lambda e: e.activation(out=Gm[gb][:, tb:tb + 4, :], in_=psGb[pg][:].rearrange("p (t i) -> p t i", t=4), func=AF.Copy), r=[("psGb", pg)], w=[("Gm", gb)])

            def load_blk(i2b):
                ldc[0] += 1
                ss_ = ldc[0] % 3
                slots[i2b] = ss_
                dma(ust[ss_][:], UTv[:, i2b * 2:(i2b + 1) * 2, :], r=["UT"], w=[("ust", ss_)])
                dma(vst[ss_][:], VBv[:, i2b * 2:(i2b + 1) * 2, :], r=["VB"], w=[("vst", ss_)])

            def scores(gr, i1):
                ss_ = slots[i1 // 2]
                j = i1 % 2
                q = i1 % 2
                h2 = h2s[gr % 2]
                for kc in range(8):
                    P(lambda e: e.matmul(psSc[q][:, 0:TG], lhsT=ust[ss_][:, j, kc * 128:(kc + 1) * 128], rhs=h2[:, kc, :], start=(kc == 0), stop=(kc == 7)),
                      r=[("ust", ss_), ("h2F", gr % 2)], w=[("psSc", q)])

            prep_group(0)
            for sub in range(TG // 8):
                gb_onehots(0, sub)
                gb_mm(0, sub)
            if NGR > 1:
                prep_group(1)
            for gr in range(NGR):
                t0 = gr * TG
                b = t0 // S
                s0 = t0 % S
                gb = gr % 2
                if s0 == 0:
                    dma(gt2[:], MOD[b:b + 1, 5 * D:6 * D].partition_broadcast(128), r=["MOD"], w=["gt2"])
                nxt = gr + 1 < NGR
                load_blk(0)
                load_blk(1)
                scores(gr, 0)

                def ymm(i1):
                    ss_y = slots_y[i1]
                    j_ = i1 % 2
                    q_ = i1 % 4
                    for tj in range(2):
                        for nh in range(2):
                            P(lambda e: e.matmul(psY[tj * 2 + nh][:], lhsT=ga[q_][:, tj * 128:(tj + 1) * 128], rhs=vst[ss_y][:, j_, nh * 512:(nh + 1) * 512], start=(i1 == 0), stop=(i1 == 127)),
                              r=[("gaF", q_), ("vst", ss_y)], w=[("psY", tj * 2 + nh)])

                slots_y = {}
                for i1 in range(128):
                    ss_ = slots[i1 // 2]
                    slots_y[i1] = ss_
                    q = i1 % 2
                    q4 = i1 % 4
                    if i1 + 1 < 128:
                        scores(gr, i1 + 1)
                    if i1 >= 1:
                        ymm(i1 - 1)
                    if i1 % 2 == 1 and (i1 + 1) // 2 + 1 < 64:
                        load_blk((i1 + 1) // 2 + 1)
                    if nxt and i1 % 4 == 0:
                        sub = i1 // 4
                        gb_onehots(gr + 1, sub)
                        if sub >= 1:
                            gb_mm(gr + 1, sub - 1)
                    A(lambda e: e.activation(out=act[q][:], in_=psSc[q][:, 0:TG], func=AF.Gelu), r=[("psSc", q)], w=[("actF", q)])
                    eng = V if (i1 % 2 == 0) else G
                    eng(lambda e: e.tensor_tensor(out=ga[q4][:], in0=act[q][:], in1=Gm[gb][:, :, i1], op=ALU.mult), r=[("actF", q), ("Gm", gb)], w=[("gaF", q4)])
                ymm(127)
                if nxt:
                    gb_mm(gr + 1, 31)
                if gr + 2 < NGR:
                    prep_group(gr + 2)
                for tj in range(2):
                    dma(x1l[:], X1f[t0 + tj * 128:t0 + (tj + 1) * 128, :], w=["x1l"])
                    for nh in range(2):
                        V(lambda e: e.tensor_tensor(out=yo[:, nh * 512:(nh + 1) * 512], in0=psY[tj * 2 + nh][:], in1=gt2[:, nh * 512:(nh + 1) * 512], op=ALU.mult), r=[("psY", tj * 2 + nh), "gt2"], w=["yo"])
                    G(lambda e: e.tensor_tensor(out=yo[:], in0=yo[:], in1=x1l[:], op=ALU.add), r=["yo", "x1l"], w=["yo"])
                    A(lambda e: e.activation(out=x1l[:], in_=yo[:], func=AF.Square, accum_out=st4[:, 0:1]), r=["yo", "x1l"], w=["x1l", "sF0"])
                    V(lambda e: e.tensor_scalar(out=st4[:, 1:2], in0=st4[:, 0:1], scalar1=1.0 / D, scalar2=EPS, op0=ALU.mult, op1=ALU.add), r=["sF0"], w=["sF1"])
                    A(lambda e: e.activation(out=st4[:, 2:3], in_=st4[:, 1:2], func=AF.Sqrt), r=["sF1"], w=["sF2"])
                    V(lambda e: e.reciprocal(out=st4[:, 3:4], in_=st4[:, 2:3]), r=["sF2"], w=["sF3"])
                    V(lambda e: e.scalar_tensor_tensor(out=oo[:], in0=yo[:], scalar=st4[:, 3:4], in1=fwb[:], op0=ALU.mult, op1=ALU.mult), r=["yo", "sF3", "fwb", "oo"], w=["oo"])
                    dma(outf[t0 + tj * 128:t0 + (tj + 1) * 128, :], oo[:], r=["oo"], w=["OUT", "oo"])
            k.barrier()
    return nc


_INPUT_ORDER = ["x", "c", "ada_w", "ada_b", "norm1_w", "norm2_w", "w_in", "conv_w", "conv_b", "ml_i_bias", "ml_f_bias",
                "ml_norm_w", "lam_q1", "lam_k1", "lam_q2", "lam_k2", "subln_w", "w_out", "peer_wq", "peer_keys",
                "peer_u", "peer_v", "final_norm_w"]


def make_in_maps(cfg, inputs, n_cores):
    f = lambda a: np.ascontiguousarray(np.asarray(a, dtype=np.float32))
    NB = cfg.NB
    shared = {
        "ada_w": f(inputs["ada_w"][0]), "ada_b": f(inputs["ada_b"][0]).reshape(1, -1),
        "norm1_w": f(inputs["norm1_w"][0]).reshape(1, -1), "norm2_w": f(inputs["norm2_w"][0]).reshape(1, -1),
        "w_in": f(inputs["w_in"][0]), "conv_w": f(inputs["conv_w"][0]), "conv_b": f(inputs["conv_b"][0]).reshape(1, -1),
        "ml_i_bias": f(inputs["ml_i_bias"][0]).reshape(1, -1), "ml_f_bias": f(inputs["ml_f_bias"][0]).reshape(1, -1),
        "ml_norm_w": f(inputs["ml_norm_w"][0]).reshape(1, -1),
        "lam_q1": f(inputs["lam_q1"][0]).reshape(1, -1), "lam_k1": f(inputs["lam_k1"][0]).reshape(1, -1),
        "lam_q2": f(inputs["lam_q2"][0]).reshape(1, -1), "lam_k2": f(inputs["lam_k2"][0]).reshape(1, -1),
        "subln_w": f(inputs["subln_w"][0]).reshape(1, -1), "w_out": f(inputs["w_out"][0]),
        "peer_wq": f(inputs["peer_wq"][0]), "peer_keys": f(inputs["peer_keys"][0]).reshape(16, 128, 128),
        "peer_u": f(inputs["peer_u"][0]), "peer_v": f(inputs["peer_v"][0]),
        "final_norm_w": f(inputs["final_norm_w"]).reshape(1, -1),
        "cst": host_consts(cfg),
    }
    xs = f(inputs["x"])
    cs = f(inputs["c"])
    maps = []
    for i in range(n_cores):
        m = dict(shared)
        m["x"] = np.ascontiguousarray(xs[i * NB:(i + 1) * NB])
        m["c"] = np.ascontiguousarray(cs[i * NB:(i + 1) * NB])
        maps.append(m)
    return maps


def kernel(**inputs):
    n_cores = 8
    cfg = Cfg(S=4096, NB=2)
    nc = build(cfg)
    maps = make_in_maps(cfg, inputs, n_cores)
    res = run_bass_kernel_spmd(nc, maps, core_ids=list(range(n_cores)))
    outs = [np.asarray(r["out"], dtype=np.float32) for r in res.results]
    return np.concatenate(outs, axis=0)
```

```python
import math
from contextlib import ExitStack

import numpy as np
import ml_dtypes
import concourse.bass as bass
import concourse.mybir as mybir
from concourse.bass_utils import run_bass_kernel_spmd

F32 = mybir.dt.float32
BF16 = mybir.dt.bfloat16
U32 = mybir.dt.uint32
ALU = mybir.AluOpType
AF = mybir.ActivationFunctionType
AX = mybir.AxisListType

D = 1024
EPS = 1e-6
IN_W = 8208
NEG = -30000.0


class Tok:
    __slots__ = ("sem", "val", "eng")

    def __init__(self, sem, val, eng):
        self.sem, self.val, self.eng = sem, val, eng


class Eng:
    def __init__(self, kb, name, eng):
        self.kb, self.name, self.eng = kb, name, eng
        self.sem = None
        self.count = 0
        self.seen = {}
        self.last = None
        self.nsem = 0

    def wait(self, tok):
        if tok is None:
            return
        key = id(tok.sem)
        if self.seen.get(key, 0) >= tok.val:
            return
        self.seen[key] = tok.val
        self.eng.wait_ge(tok.sem, tok.val)

    def signal(self, instr):
        if self.sem is None or self.count >= 30000:
            self.sem = self.kb.es.enter_context(self.kb.nc.semaphore(f"s_{self.name}_{self.nsem}"))
            self.nsem += 1
            self.count = 0
        self.count += 1
        instr.then_inc(self.sem, 1)
        t = Tok(self.sem, self.count, self.name)
        self.last = t
        return t


class KB:
    def __init__(self, nc):
        self.nc = nc
        self.es = ExitStack()
        self.e = {
            "pe": Eng(self, "pe", nc.tensor),
            "act": Eng(self, "act", nc.scalar),
            "dve": Eng(self, "dve", nc.vector),
            "pool": Eng(self, "pool", nc.gpsimd),
            "sp": Eng(self, "sp", nc.sync),
        }
        self.res = {}
        self.dsems = []
        self.dvals = []
        self.dnext = 0
        self.ND = 40
        self.all_dma = []

    def _deps(self, r, w):
        deps = []
        for k in r:
            st = self.res.get(k)
            if st is not None and st[0] is not None:
                deps.append(st[0])
        for k in w:
            st = self.res.get(k)
            if st is not None:
                if st[0] is not None:
                    deps.append(st[0])
                deps.extend(st[1])
        return deps

    def _record(self, tok, r, w):
        for k in r:
            st = self.res.setdefault(k, [None, []])
            if tok.eng != "dma":
                st[1] = [t for t in st[1] if t.eng != tok.eng]
            st[1].append(tok)
        for k in w:
            self.res[k] = [tok, []]

    def op(self, en, fn, r=(), w=()):
        E = self.e[en]
        for t in self._deps(r, w):
            if en == "pe" and t.eng == "pe":
                continue
            E.wait(t)
        instr = fn(E.eng)
        tok = E.signal(instr)
        self._record(tok, r, w)
        return tok

    def P(self, fn, r=(), w=()):
        return self.op("pe", fn, r, w)

    def A(self, fn, r=(), w=()):
        return self.op("act", fn, r, w)

    def V(self, fn, r=(), w=()):
        return self.op("dve", fn, r, w)

    def G(self, fn, r=(), w=()):
        return self.op("pool", fn, r, w)

    def dma(self, out, in_, r=(), w=(), q="sp", **kw):
        E = self.e[q]
        for t in self._deps(r, w):
            E.wait(t)
        if len(self.dsems) < self.ND:
            self.dsems.append(self.es.enter_context(self.nc.semaphore(f"s_dma_{len(self.dsems)}")))
            self.dvals.append(0)
        i = self.dnext
        self.dnext = (self.dnext + 1) % self.ND
        sem = self.dsems[i]
        if self.dvals[i] > 0:
            E.wait(Tok(sem, self.dvals[i], "dma"))
        self.dvals[i] += 16
        instr = E.eng.dma_start(out=out, in_=in_, **kw)
        instr.then_inc(sem, 16)
        tok = Tok(sem, self.dvals[i], "dma")
        self._record(tok, r, w)
        return tok

    def barrier(self):
        toks = [E.last for E in self.e.values() if E.last is not None]
        toks += [Tok(s, v, "dma") for s, v in zip(self.dsems, self.dvals) if v > 0]
        for E in self.e.values():
            for t in toks:
                if t.eng == E.name:
                    continue
                E.wait(t)
        self.res = {}


class Cfg:
    def __init__(self, S=4096, NB=2, debug=False):
        self.S, self.NB, self.debug = S, NB, debug
        self.NT = S // 128


def host_consts(cfg):
    NT = cfg.NT
    p = np.arange(128)
    ident = np.eye(128, dtype=np.float32)
    tri = (p[:, None] <= p[None, :]).astype(np.float32)
    negm = np.where(p[:, None] > p[None, :], NEG, 0.0).astype(np.float32)
    cm = (p[:, None] <= p[None, :]).astype(np.float32)
    iota = np.broadcast_to(np.arange(128, dtype=np.float32)[None, :], (128, 128)).copy()
    ones = np.ones((128, 128), np.float32)
    half = 8
    inv = (500000.0 ** (-np.arange(half, dtype=np.float32) * 2.0 / 16)).astype(np.float32)
    pos = (np.arange(NT)[None, :] * 128 + p[:, None]).astype(np.float32)
    ang = pos[:, :, None] * inv[None, None, :]
    cos = np.cos(ang).astype(np.float32).reshape(128, NT * 8)
    sin = np.sin(ang).astype(np.float32).reshape(128, NT * 8)
    a16 = np.broadcast_to((np.arange(16, dtype=np.float32) * 16)[None, :], (128, 16)).copy()
    i16 = np.broadcast_to(np.arange(16, dtype=np.float32)[None, :], (128, 16)).copy()
    return np.concatenate([ident, tri, negm, cm, iota, ones, a16, i16, cos, sin], axis=1).astype(np.float32)


def build(cfg):
    S, NB, NT = cfg.S, cfg.NB, cfg.NT
    NG = S // 512
    T = NB * S
    nc = bass.Bass("TRN2", target_bir_lowering=False)

    def din(name, shape, dt=F32):
        return nc.dram_tensor(name, list(shape), dt, kind="ExternalInput").ap()

    def dscr(name, shape, dt):
        kind = "ExternalOutput" if cfg.debug else "Internal"
        return nc.dram_tensor(name, list(shape), dt, kind=kind).ap()

    x = din("x", [NB, S, D])
    c_in = din("c", [NB, D])
    ada_w = din("ada_w", [D, 6 * D])
    ada_b = din("ada_b", [1, 6 * D])
    norm1_w = din("norm1_w", [1, D])
    norm2_w = din("norm2_w", [1, D])
    w_in = din("w_in", [D, IN_W])
    conv_w = din("conv_w", [4, 1024])
    conv_b = din("conv_b", [1, 1024])
    ml_ib = din("ml_i_bias", [1, 8])
    ml_fb = din("ml_f_bias", [1, 8])
    ml_nw = din("ml_norm_w", [1, D])
    lam_q1 = din("lam_q1", [1, 64])
    lam_k1 = din("lam_k1", [1, 64])
    lam_q2 = din("lam_q2", [1, 64])
    lam_k2 = din("lam_k2", [1, 64])
    subln_w = din("subln_w", [1, 128])
    w_out = din("w_out", [D, D])
    peer_wq = din("peer_wq", [D, 2048])
    peer_keys = din("peer_keys", [16, 128, 128])
    peer_u = din("peer_u", [16384, D])
    peer_v = din("peer_v", [16384, D])
    fin_w = din("final_norm_w", [1, D])
    NCST = 128 * 6 + 32 + 2 * NT * 8
    cst_d = din("cst", [128, NCST])
    out = nc.dram_tensor("out", [NB, S, D], F32, kind="ExternalOutput").ap()

    UT = dscr("UT", [64, 128, 2048], BF16)
    VB = dscr("VB", [64, 128, 2048], BF16)
    MOD = dscr("MOD", [NB, 6 * D], F32)
    QT = dscr("QT", [NB, 8, 128, S], BF16)
    KT = dscr("KT", [NB, 8, 128, S], BF16)
    VA = dscr("VA", [NB, S, D], BF16)
    QMT = dscr("QMT", [NB, 4, 128, S], BF16)
    KMT = dscr("KMT", [NB, 4, 128, S], BF16)
    VM = dscr("VM", [NB, S, D], BF16)
    OMS = dscr("OMS", [NB, S, D], BF16)
    GAS = dscr("GAS", [NB, S, D], BF16)
    GMS = dscr("GMS", [NB, S, D], BF16)
    GATES = dscr("GATES", [NB, S, 16], F32)
    YM = dscr("YM", [NB, S, D], BF16)
    MT = dscr("MT", [NB, 8, 128, S], BF16)
    X1 = dscr("X1", [NB, S, D], F32)
    H2T = dscr("H2T", [NB, 8, 128, S], BF16)
    IG = dscr("IG", [NB, S, 3, 128], F32)

    k = KB(nc)
    P, A, V, G, dma = k.P, k.A, k.V, k.G, k.dma

    with k.es:
        es0 = k.es

        uid = [0]

        def sbt(es, name, shape, dt):
            uid[0] += 1
            return es.enter_context(nc.sbuf_tensor(f"sb{uid[0]}_{name}", list(shape), dt))

        def pst(es, name, shape, dt):
            uid[0] += 1
            return es.enter_context(nc.psum_tensor(f"ps{uid[0]}_{name}", list(shape), dt))

        cst = sbt(es0, "cst", [128, NCST], F32)
        identb = sbt(es0, "identb", [128, 128], BF16)
        cmb = sbt(es0, "cmb", [128, 128], BF16)
        nmx = sbt(es0, "nmx", [128, 32], F32)
        lamt = sbt(es0, "lamt", [128, 4], F32)
        dma(cst[:], cst_d, w=["cst"])
        identf = cst[:, 0:128]
        trif = cst[:, 128:256]
        negmf = cst[:, 256:384]
        cmf = cst[:, 384:512]
        iotaf = cst[:, 512:640]
        onesf = cst[:, 640:768]
        a16f = cst[:, 768:784]
        i16f = cst[:, 784:800]
        cosf = cst[:, 800:800 + NT * 8]
        sinf = cst[:, 800 + NT * 8:800 + 2 * NT * 8]
        V(lambda e: e.tensor_copy(out=identb[:], in_=identf), r=["cst"], w=["identb"])
        V(lambda e: e.tensor_copy(out=cmb[:], in_=cmf), r=["cst"], w=["cmb"])

        with ExitStack() as ph:
            uf = [sbt(ph, f"uf{i}", [128, 1024], F32) for i in range(2)]
            ub = [sbt(ph, f"ub{i}", [128, 1024], BF16) for i in range(2)]
            uts = [sbt(ph, f"uts{i}", [128, 1024], BF16) for i in range(2)]
            vf = [sbt(ph, f"vf{i}", [128, 1024], F32) for i in range(2)]
            vb = [sbt(ph, f"vb{i}", [128, 1024], BF16) for i in range(2)]
            ptb = [pst(ph, f"ptbA{i}", [128, 1024], BF16) for i in range(2)]
            for i1 in range(128):
                s = i1 % 2
                dma(uf[s][:], peer_u[i1 * 128:(i1 + 1) * 128, :], w=[("uf", s)])
                dma(vf[s][:], peer_v[i1 * 128:(i1 + 1) * 128, :], w=[("vf", s)])
                V(lambda e: e.tensor_copy(out=ub[s][:], in_=uf[s][:]), r=[("uf", s)], w=[("ub", s)])
                for kc in range(8):
                    P(lambda e: e.transpose(out=ptb[s][:, kc * 128:(kc + 1) * 128], in_=ub[s][:, kc * 128:(kc + 1) * 128], identity=identb[:]),
                      r=[("ub", s), "identb"], w=[("ptbA", s)])
                A(lambda e: e.activation(out=uts[s][:], in_=ptb[s][:], func=AF.Copy), r=[("ptbA", s)], w=[("uts", s)])
                dma(UT[i1 // 2][:, (i1 % 2) * 1024:(i1 % 2 + 1) * 1024], uts[s][:], r=[("uts", s)], w=["UT"])
                G(lambda e: e.tensor_copy(out=vb[s][:], in_=vf[s][:]), r=[("vf", s)], w=[("vb", s)])
                dma(VB[i1 // 2][:, (i1 % 2) * 1024:(i1 % 2 + 1) * 1024], vb[s][:], r=[("vb", s)], w=["VB"])

            condT = sbt(ph, "condT", [128, 8, NB], F32)
            adab = sbt(ph, "adab", [1, 6 * D], F32)
            modrow = sbt(ph, "modrow", [1, NB, 6 * D], F32)
            wada = [sbt(ph, f"wada{i}", [128, 8, 512], F32) for i in range(2)]
            psm = [pst(ph, f"psm{i}", [128, 512], F32) for i in range(2)]
            for b in range(NB):
                dma(condT[:, :, b], c_in[b].rearrange("(kc p) -> p kc", p=128), w=["condT"], allow_slow_non_contiguous=True)
            dma(adab[:], ada_b, w=["adab"])
            A(lambda e: e.activation(out=condT[:], in_=condT[:], func=AF.Silu), r=["condT"], w=["condT"])
            adaw_v = ada_w.rearrange("(kc p) n -> p kc n", p=128)
            for ncx in range(12):
                s = ncx % 2
                dma(wada[s][:], adaw_v[:, :, ncx * 512:(ncx + 1) * 512], w=[("wada", s)])
                for b in range(NB):
                    pb = (ncx * NB + b) % 2
                    for kc in range(8):
                        P(lambda e: e.matmul(psm[pb][0:1, :], lhsT=condT[:, kc, b:b + 1], rhs=wada[s][:, kc, :], start=(kc == 0), stop=(kc == 7)),
                          r=["condT", ("wada", s)], w=[("psm", pb)])
                    V(lambda e: e.tensor_tensor(out=modrow[0:1, b, ncx * 512:(ncx + 1) * 512], in0=psm[pb][0:1, :], in1=adab[0:1, ncx * 512:(ncx + 1) * 512], op=ALU.add),
                      r=[("psm", pb), "adab"], w=["modrow"])
            for b in range(NB):
                dma(MOD[b:b + 1, :], modrow[0:1, b, :], r=["modrow"], w=["MOD"])

            lq = sbt(ph, "lq", [1, 4, 64], F32)
            lsc = sbt(ph, "lsc", [1, 8], F32)
            dma(lq[0:1, 0, :], lam_q1, w=["lq0"])
            dma(lq[0:1, 1, :], lam_k1, w=["lq1"])
            dma(lq[0:1, 2, :], lam_q2, w=["lq2"])
            dma(lq[0:1, 3, :], lam_k2, w=["lq3"])
            V(lambda e: e.tensor_tensor(out=lq[0:1, 0, :], in0=lq[0:1, 0, :], in1=lq[0:1, 1, :], op=ALU.mult), r=["lq0", "lq1"], w=["lq0"])
            V(lambda e: e.tensor_tensor(out=lq[0:1, 2, :], in0=lq[0:1, 2, :], in1=lq[0:1, 3, :], op=ALU.mult), r=["lq2", "lq3"], w=["lq2"])
            V(lambda e: e.reduce_sum(out=lsc[0:1, 0:1], in_=lq[0:1, 0, :], axis=AX.X), r=["lq0"], w=["lsc0"])
            V(lambda e: e.reduce_sum(out=lsc[0:1, 1:2], in_=lq[0:1, 2, :], axis=AX.X), r=["lq2"], w=["lsc1"])
            A(lambda e: e.activation(out=lsc[0:1, 2:4], in_=lsc[0:1, 0:2], func=AF.Exp), r=["lsc0", "lsc1"], w=["lsc2"])
            lam_init = 0.8 - 0.6 * math.exp(0.0)
            V(lambda e: e.tensor_tensor(out=lsc[0:1, 4:5], in0=lsc[0:1, 3:4], in1=lsc[0:1, 2:3], op=ALU.subtract), r=["lsc2"], w=["lsc4"])
            V(lambda e: e.tensor_scalar(out=lsc[0:1, 5:6], in0=lsc[0:1, 4:5], scalar1=-lam_init, scalar2=None, op0=ALU.add), r=["lsc4"], w=["lsc5"])
            P(lambda e: e.matmul(psm[0][:, 0:1], lhsT=onesf[0:1, :], rhs=lsc[0:1, 5:6], start=True, stop=True), r=["lsc5", "cst", ("psm", 0)], w=[("psm", 0)])
            V(lambda e: e.tensor_copy(out=lamt[:, 0:1], in_=psm[0][:, 0:1]), r=[("psm", 0)], w=["lamt"])
            k.barrier()

        win_v = w_in.rearrange("(kc p) n -> p kc n", p=128)
        for b in range(NB):
            with ExitStack() as ph:
                hT = sbt(ph, "hT", [128, 8, S], BF16)
                A1 = sbt(ph, "A1", [128, D], F32)
                B1 = sbt(ph, "B1", [128, D], F32)
                w1b = sbt(ph, "w1b", [128, D], F32)
                xt = [sbt(ph, f"xt{i}", [128, D], F32) for i in range(2)]
                hxs = [sbt(ph, f"hx{i}", [128, D], F32) for i in range(2)]
                hb = [sbt(ph, f"hb{i}", [128, D], BF16) for i in range(2)]
                junk = sbt(ph, "junkB", [128, D], BF16)
                st4s = [sbt(ph, f"st4{i}", [128, 8], F32) for i in range(2)]
                ptb = [pst(ph, f"ptbB{i}", [128, 1024], BF16) for i in range(2)]
                psb = [pst(ph, f"psB{i}", [128, 512], F32) for i in range(4)]
                dma(A1[:], MOD[b:b + 1, D:2 * D].partition_broadcast(128), r=["MOD"], w=["A1"])
                dma(B1[:], MOD[b:b + 1, 0:D].partition_broadcast(128), r=["MOD"], w=["B1"])
                dma(w1b[:], norm1_w.partition_broadcast(128), w=["w1b"])
                V(lambda e: e.scalar_tensor_tensor(out=A1[:], in0=A1[:], scalar=1.0, in1=w1b[:], op0=ALU.add, op1=ALU.mult), r=["A1", "w1b"], w=["A1"])
                V(lambda e: e.memset(nmx[:], 0.0), w=["nmx"])
                for tt in range(NT):
                    s = tt % 2
                    hx = hxs[s]
                    st4 = st4s[s]
                    dma(xt[s][:], x[b, tt * 128:(tt + 1) * 128, :], w=[("xt", s)])
                    A(lambda e: e.activation(out=junk[:], in_=xt[s][:], func=AF.Square, accum_out=st4[:, 0:1]), r=[("xt", s)], w=["junkB", ("st0", s)])
                    V(lambda e: e.tensor_scalar(out=st4[:, 1:2], in0=st4[:, 0:1], scalar1=1.0 / D, scalar2=EPS, op0=ALU.mult, op1=ALU.add), r=[("st0", s)], w=[("st1", s)])
                    A(lambda e: e.activation(out=st4[:, 2:3], in_=st4[:, 1:2], func=AF.Sqrt), r=[("st1", s)], w=[("st2", s)])
                    V(lambda e: e.reciprocal(out=st4[:, 3:4], in_=st4[:, 2:3]), r=[("st2", s)], w=[("st3", s)])
                    V(lambda e: e.scalar_tensor_tensor(out=hx[:], in0=xt[s][:], scalar=st4[:, 3:4], in1=A1[:], op0=ALU.mult, op1=ALU.mult), r=[("xt", s), ("st3", s), "A1"], w=[("hx", s)])
                    G(lambda e: e.tensor_tensor(out=hb[s][:], in0=hx[:], in1=B1[:], op=ALU.add), r=[("hx", s), "B1"], w=[("hb", s)])
                    for kc in range(8):
                        P(lambda e: e.transpose(out=ptb[s][:, kc * 128:(kc + 1) * 128], in_=hb[s][:, kc * 128:(kc + 1) * 128], identity=identb[:]),
                          r=[("hb", s), "identb"], w=[("ptbB", s)])
                    A(lambda e: e.activation(out=hT[:, :, tt * 128:(tt + 1) * 128], in_=ptb[s][:].rearrange("p (k t) -> p k t", k=8), func=AF.Copy),
                      r=[("ptbB", s)], w=[("hT", tt)])

                wf = [sbt(ph, f"wf{i}", [128, 8, 512], F32) for i in range(2)]
                wb = [sbt(ph, f"wb{i}", [128, 8, 512], BF16) for i in range(2)]
                qfs = [sbt(ph, f"qf{i}", [128, 512], F32) for i in range(4)]
                sqjs = [sbt(ph, f"sqj{i}", [128, 512], F32) for i in range(2)]
                rts = [sbt(ph, f"rt{i}", [128, 4, 8, 8], F32) for i in range(4)]
                nsqs = [sbt(ph, f"nsq{i}", [128, 8], F32) for i in range(4)]
                qb = [sbt(ph, f"qb{i}", [128, 512], BF16) for i in range(4)]
                stage = [sbt(ph, f"stage{i}", [128, 4, 512], BF16) for i in range(2)]
                ob = [sbt(ph, f"ob{i}", [128, 512], BF16) for i in range(3)]
                xbuf = sbt(ph, "xbuf", [128, 4, 515], F32)
                caccs = [sbt(ph, f"cacc{i}", [128, 512], F32) for i in range(3)]
                csil = sbt(ph, "csil", [128, 512], F32)
                cstg = [sbt(ph, f"cstg{i}", [128, 512], BF16) for i in range(2)]
                cw = sbt(ph, "cw", [128, 8, 4], F32)
                cbi = sbt(ph, "cbi", [128, 8], F32)
                gbias = sbt(ph, "gbias", [128, 16], F32)
                gz = sbt(ph, "gz", [128, 16], F32)
                gt = [sbt(ph, f"gtB{i}", [128, 16], F32) for i in range(2)]
                for j in range(4):
                    dma(cw[:, :, j], conv_w[j].rearrange("(blk p) -> p blk", p=128), w=["cw"], allow_slow_non_contiguous=True)
                dma(cbi[:], conv_b.rearrange("o (blk p) -> p (o blk)", p=128), w=["cbi"], allow_slow_non_contiguous=True)
                dma(gbias[:, 0:8], ml_ib.partition_broadcast(128), w=["gbias0"])
                dma(gbias[:, 8:16], ml_fb.partition_broadcast(128), w=["gbias1"])

                chunks = []
                for i in range(2):
                    chunks.append((i * 512, 512, "qa", i))
                for i in range(2):
                    chunks.append((1024 + i * 512, 512, "ka", i))
                for i in range(2):
                    chunks.append((2048 + i * 512, 512, "va", i))
                chunks.append((3072, 512, "qm", 0))
                chunks.append((3584, 512, "km", 0))
                for i in range(2):
                    chunks.append((4096 + i * 512, 512, "vm", i))
                for i in range(2):
                    chunks.append((5120 + i * 512, 512, "om", i))
                chunks.append((6144, 16, "gate", 0))
                for i in range(2):
                    chunks.append((6160 + i * 512, 512, "ga", i))
                for i in range(2):
                    chunks.append((7184 + i * 512, 512, "gm", i))

                pcount = 0
                ocount = 0
                qcount = 0
                for ci, (c0, ncol, kind, idx) in enumerate(chunks):
                    s = ci % 2
                    dma(wf[s][:, :, 0:ncol], win_v[:, :, c0:c0 + ncol], w=[("wf", s)])
                    G(lambda e: e.tensor_copy(out=wb[s][:, :, 0:ncol], in_=wf[s][:, :, 0:ncol]), r=[("wf", s)], w=[("wb", s)])
                    if kind in ("qm", "km"):
                        sc_ = 1.0 if kind == "qm" else 0.125
                        dst = QMT if kind == "qm" else KMT
                        boff = 0 if kind == "qm" else 4
                        for cb in range(4):
                            V(lambda e: e.memset(xbuf[:, cb, 0:3], 0.0), w=[("xbuf", cb)])
                        for tg in range(NG):
                            for cb in range(4):
                                pb = pcount % 4
                                pcount += 1
                                for kc in range(8):
                                    P(lambda e: e.matmul(psb[pb][:], lhsT=wb[s][:, kc, cb * 128:(cb + 1) * 128], rhs=hT[:, kc, tg * 512:(tg + 1) * 512], start=(kc == 0), stop=(kc == 7)),
                                      r=[("wb", s)] + [("hT", t_) for t_ in range(tg * 4, tg * 4 + 4)], w=[("psB", pb)])
                                if tg > 0:
                                    V(lambda e: e.tensor_copy(out=xbuf[:, cb, 0:3], in_=xbuf[:, cb, 512:515]), r=[("xbuf", cb)], w=[("xbuf", cb)])
                                A(lambda e: e.activation(out=xbuf[:, cb, 3:515], in_=psb[pb][:], func=AF.Copy), r=[("psB", pb)], w=[("xbuf", cb)])
                                ca = caccs[qcount % 3]
                                kca = ("cacc", qcount % 3)
                                A(lambda e: e.activation(out=ca[:], in_=xbuf[:, cb, 0:512], func=AF.Identity, scale=cw[:, boff + cb, 0:1], bias=cbi[:, boff + cb:boff + cb + 1]), r=[("xbuf", cb), "cw", "cbi"], w=[kca])
                                V(lambda e: e.scalar_tensor_tensor(out=ca[:], in0=xbuf[:, cb, 1:513], scalar=cw[:, boff + cb, 1:2], in1=ca[:], op0=ALU.mult, op1=ALU.add), r=[("xbuf", cb), "cw", kca], w=[kca])
                                V(lambda e: e.scalar_tensor_tensor(out=ca[:], in0=xbuf[:, cb, 2:514], scalar=cw[:, boff + cb, 2:3], in1=ca[:], op0=ALU.mult, op1=ALU.add), r=[("xbuf", cb), "cw", kca], w=[kca])
                                V(lambda e: e.scalar_tensor_tensor(out=ca[:], in0=xbuf[:, cb, 3:515], scalar=cw[:, boff + cb, 3:4], in1=ca[:], op0=ALU.mult, op1=ALU.add), r=[("xbuf", cb), "cw", kca], w=[kca])
                                cs = qcount % 2
                                qcount += 1
                                if kind == "qm":
                                    A(lambda e: e.activation(out=cstg[cs][:], in_=ca[:], func=AF.Silu), r=[kca], w=[("cstg", cs)])
                                else:
                                    A(lambda e: e.activation(out=csil[:], in_=ca[:], func=AF.Silu), r=[kca], w=["csil"])
                                    G(lambda e: e.tensor_scalar(out=cstg[cs][:], in0=csil[:], scalar1=sc_, scalar2=None, op0=ALU.mult), r=["csil"], w=[("cstg", cs)])
                                dma(dst[b, cb, :, tg * 512:(tg + 1) * 512], cstg[cs][:], r=[("cstg", cs)], w=["QKMT"])
                        continue
                    for tt in range(NT):
                        pb = pcount % 4
                        pcount += 1
                        for kc in range(8):
                            P(lambda e: e.matmul(psb[pb][:, 0:ncol], lhsT=hT[:, kc, tt * 128:(tt + 1) * 128], rhs=wb[s][:, kc, 0:ncol], start=(kc == 0), stop=(kc == 7)),
                              r=[("wb", s), ("hT", tt)], w=[("psB", pb)])
                        if kind in ("qa", "ka"):
                            z = tt % 4
                            z2 = tt % 2
                            qf, sqj, rt, nsq = qfs[z], sqjs[z2], rts[z], nsqs[z]
                            kq = ("qf", z)
                            A(lambda e: e.activation(out=qf[:], in_=psb[pb][:], func=AF.Copy), r=[("psB", pb)], w=[kq])
                            qv = qf[:].rearrange("p (g d) -> p g d", g=8)
                            x1 = qv[:, :, 0:8]
                            x2 = qv[:, :, 8:16]
                            cosb = cosf[:, tt * 8:(tt + 1) * 8].unsqueeze(1).to_broadcast([128, 8, 8])
                            sinb = sinf[:, tt * 8:(tt + 1) * 8].unsqueeze(1).to_broadcast([128, 8, 8])
                            V(lambda e: e.tensor_tensor(out=rt[:, 0], in0=x1, in1=cosb, op=ALU.mult), r=[kq, "cst"], w=[("rt0", z)])
                            V(lambda e: e.tensor_tensor(out=rt[:, 1], in0=x2, in1=sinb, op=ALU.mult), r=[kq, "cst"], w=[("rt1", z)])
                            V(lambda e: e.tensor_tensor(out=rt[:, 2], in0=x2, in1=cosb, op=ALU.mult), r=[kq, "cst"], w=[("rt2", z)])
                            V(lambda e: e.tensor_tensor(out=rt[:, 3], in0=x1, in1=sinb, op=ALU.mult), r=[kq, "cst"], w=[("rt3", z)])
                            V(lambda e: e.tensor_tensor(out=x1, in0=rt[:, 0], in1=rt[:, 1], op=ALU.subtract), r=[("rt0", z), ("rt1", z), ("rt2", z), ("rt3", z)], w=[kq])
                            V(lambda e: e.tensor_tensor(out=x2, in0=rt[:, 2], in1=rt[:, 3], op=ALU.add), r=[("rt2", z), ("rt3", z)], w=[kq])
                            G(lambda e: e.tensor_tensor(out=sqj[:], in0=qf[:], in1=qf[:], op=ALU.mult), r=[kq], w=[("sqj", z2)])
                            V(lambda e: e.reduce_sum(out=nsq[:], in_=sqj[:].rearrange("p (g d) -> p g d", g=8), axis=AX.X), r=[("sqj", z2)], w=[("nsq", z)])
                            ncol0 = (0 if kind == "qa" else 16) + idx * 8
                            V(lambda e: e.tensor_tensor(out=nmx[:, ncol0:ncol0 + 8], in0=nmx[:, ncol0:ncol0 + 8], in1=nsq[:], op=ALU.max), r=[("nsq", z), "nmx"], w=["nmx"])
                            qs = qcount % 4
                            qcount += 1
                            A(lambda e: e.activation(out=qb[qs][:], in_=qf[:], func=AF.Copy, scale=(0.125 if kind == "qa" else 1.0)), r=[kq], w=[("qb", qs)])
                            ps_ = tt % 2
                            for hd in range(4):
                                P(lambda e: e.transpose(out=ptb[ps_][:, hd * 128:(hd + 1) * 128], in_=qb[qs][:, hd * 128:(hd + 1) * 128], identity=identb[:]),
                                  r=[("qb", qs), "identb"], w=[("ptbB", ps_)])
                            sg = (tt // 4) % 2
                            V(lambda e: e.tensor_copy(out=stage[sg][:, :, (tt % 4) * 128:(tt % 4 + 1) * 128], in_=ptb[ps_][:, 0:512].rearrange("p (h t) -> p h t", h=4)),
                              r=[("ptbB", ps_)], w=[("stage", sg)])
                            if tt % 4 == 3:
                                dstT = QT if kind == "qa" else KT
                                t0 = (tt // 4) * 512
                                dma(dstT[b, idx * 4:(idx + 1) * 4, :, t0:t0 + 512].rearrange("h p t -> p h t"), stage[sg][:], r=[("stage", sg)], w=["QKT"])
                        elif kind in ("va", "vm"):
                            os_ = ocount % 3
                            ocount += 1
                            A(lambda e: e.activation(out=ob[os_][:], in_=psb[pb][:], func=AF.Copy), r=[("psB", pb)], w=[("ob", os_)])
                            dstv = VA if kind == "va" else VM
                            dma(dstv[b, tt * 128:(tt + 1) * 128, idx * 512:(idx + 1) * 512], ob[os_][:], r=[("ob", os_)], w=["VAVM"])
                        elif kind in ("om", "ga", "gm"):
                            os_ = ocount % 3
                            ocount += 1
                            A(lambda e: e.activation(out=ob[os_][:], in_=psb[pb][:], func=AF.Sigmoid), r=[("psB", pb)], w=[("ob", os_)])
                            dsts = {"om": OMS, "ga": GAS, "gm": GMS}[kind]
                            dma(dsts[b, tt * 128:(tt + 1) * 128, idx * 512:(idx + 1) * 512], ob[os_][:], r=[("ob", os_)], w=["SIGS"])
                        else:
                            gs = tt % 2
                            V(lambda e: e.tensor_tensor(out=gz[:], in0=psb[pb][:, 0:16], in1=gbias[:], op=ALU.add), r=[("psB", pb), "gbias0", "gbias1"], w=["gz"])
                            A(lambda e: e.activation(out=gz[:, 8:16], in_=gz[:, 8:16], func=AF.Exp, scale=-1.0), r=["gz"], w=["gz"])
                            A(lambda e: e.activation(out=gz[:, 8:16], in_=gz[:, 8:16], func=AF.Ln, bias=1.0, scale=1.0), r=["gz"], w=["gz"])
                            V(lambda e: e.tensor_copy(out=gt[gs][:, 0:8], in_=gz[:, 0:8]), r=["gz"], w=[("gtB", gs)])
                            V(lambda e: e.tensor_scalar(out=gt[gs][:, 8:16], in0=gz[:, 8:16], scalar1=-1.0, scalar2=None, op0=ALU.mult), r=["gz", ("gtB", gs)], w=[("gtB", gs)])
                            dma(GATES[b, tt * 128:(tt + 1) * 128, :], gt[gs][:], r=[("gtB", gs)], w=["GATES"])
                k.barrier()

            with ExitStack() as ph:
                qmt = sbt(ph, "qmt", [128, 4, S], BF16)
                kmt = sbt(ph, "kmt", [128, 4, S], BF16)
                Cst = sbt(ph, "Cst", [128, 4, 129], F32)
                Cbf = sbt(ph, "Cbf", [128, 4, 129], BF16)
                mlw = sbt(ph, "mlw", [128, D], F32)
                gtl = [sbt(ph, f"gtl{i}", [128, 16], F32) for i in range(2)]
                vmt = [sbt(ph, f"vmt{i}", [128, 8, 129], BF16) for i in range(2)]
                omt = [sbt(ph, f"omt{i}", [128, D], BF16) for i in range(2)]
                gmt = [sbt(ph, f"gmt{i}", [128, D], BF16) for i in range(2)]
                sms = [sbt(ph, f"sm{i}", [128, 8, 8], F32) for i in range(2)]
                Kws = [sbt(ph, f"Kw{i}", [128, 4, 128], BF16) for i in range(2)]
                PT = [sbt(ph, f"PT{i}", [128, 128], BF16) for i in range(2)]
                hn = sbt(ph, "hn", [128, 8, 128], F32)
                hj = sbt(ph, "hj", [128, 8, 128], F32)
                n8 = sbt(ph, "n8", [128, 8, 8], F32)
                ymb = [sbt(ph, f"ymb{i}", [128, D], BF16) for i in range(2)]
                psGK = pst(ph, "psGK", [128, 1024], BF16)
                psGf = psGK[:, 512:1024].bitcast(F32)
                psS = [pst(ph, f"psS{i}", [128, 512], F32) for i in range(2)]
                psTot = [pst(ph, f"psTot{i}", [128, 512], F32) for i in range(3)]
                psC = [pst(ph, f"psC{i}", [128, 512], F32) for i in range(2)]
                dma(qmt[:], QMT[b].rearrange("c p t -> p c t"), w=["qmt"])
                dma(kmt[:], KMT[b].rearrange("c p t -> p c t"), w=["kmt"])
                dma(mlw[:], ml_nw.partition_broadcast(128), w=["mlw"])
                V(lambda e: e.memset(Cst[:], 0.0), w=[("Cst", h) for h in range(8)])
                V(lambda e: e.memset(Cbf[:], 0.0), w=[("Cbf", h) for h in range(8)])
                for i in range(2):
                    V(lambda e: e.memset(vmt[i][:, :, 128:129], 1.0), w=[("vmt", i)])
                for tt in range(NT):
                    s = tt % 2
                    sm = sms[s]
                    Kw = Kws[s]
                    tsl = slice(tt * 128, (tt + 1) * 128)
                    dma(gtl[s][:], GATES[b, tsl, :], w=[("gtl", s)])
                    dma(vmt[s][:, :, 0:128], VM[b, tsl, :].rearrange("p (h e) -> p h e", h=8), w=[("vmt", s)])
                    dma(omt[s][:], OMS[b, tsl, :], w=[("omt", s)])
                    dma(gmt[s][:], GMS[b, tsl, :], w=[("gmt", s)])
                    lf = gtl[s][:, 8:16]
                    ig = gtl[s][:, 0:8]
                    P(lambda e: e.matmul(psGf[:, 0:8], lhsT=trif, rhs=lf, start=True, stop=True), r=[("gtl", s), "cst"], w=["psGK"])
                    P(lambda e: e.matmul(psGf[:, 8:16], lhsT=onesf, rhs=lf, start=True, stop=True), r=[("gtl", s), "cst"], w=["psGK"])
                    V(lambda e: e.tensor_copy(out=sm[:, 0:2, :], in_=psGf[:, 0:16].rearrange("p (a h) -> p a h", a=2)), r=["psGK"], w=[("sm01", s)])
                    V(lambda e: e.tensor_tensor(out=sm[:, 2, :], in0=ig, in1=sm[:, 0, :], op=ALU.subtract), r=[("gtl", s), ("sm01", s)], w=[("sm2", s)])
                    V(lambda e: e.tensor_tensor(out=sm[:, 6, :], in0=sm[:, 1, :], in1=sm[:, 2, :], op=ALU.add), r=[("sm01", s), ("sm2", s)], w=[("sm6", s)])
                    A(lambda e: e.activation(out=sm[:, 3, :], in_=sm[:, 2, :], func=AF.Exp), r=[("sm2", s)], w=[("sm3", s)])
                    A(lambda e: e.activation(out=sm[:, 4, :], in_=sm[:, 6, :], func=AF.Exp), r=[("sm6", s)], w=[("sm4", s)])
                    A(lambda e: e.activation(out=sm[:, 5, :], in_=sm[:, 1, :], func=AF.Exp), r=[("sm01", s)], w=[("sm5", s)])
                    A(lambda e: e.activation(out=sm[:, 7, :], in_=sm[:, 0, :], func=AF.Exp, scale=-1.0), r=[("sm01", s)], w=[("sm7", s)])
                    for cb in range(4):
                        P(lambda e: e.transpose(out=psGK[:, cb * 128:(cb + 1) * 128], in_=kmt[:, cb, tsl], identity=identb[:]), r=["kmt", "identb"], w=["psGK"])
                    V(lambda e: e.tensor_tensor(out=Kw[:].rearrange("p c (h d) -> p (c h) d", h=2), in0=psGK[:, 0:512].rearrange("p (g d) -> p g d", g=8),
                                                in1=sm[:, 4, :].unsqueeze(2).to_broadcast([128, 8, 64]), op=ALU.mult), r=["psGK", ("sm4", s)], w=[("Kw", s)])
                    started = [False, False, False]
                    for h in range(8):
                        cb, r0 = h // 2, (h % 2) * 64
                        hs = h % 2
                        bank, slot = h // 3, h % 3
                        tcol = slice(slot * 129, (slot + 1) * 129)
                        P(lambda e: e.matmul(psS[hs][:, 0:128], lhsT=kmt[r0:r0 + 64, cb, tsl], rhs=qmt[r0:r0 + 64, cb, tsl], start=True, stop=True), r=["kmt", "qmt"], w=[("psS", hs)])
                        V(lambda e: e.scalar_tensor_tensor(out=PT[hs][:], in0=psS[hs][:, 0:128], scalar=sm[:, 3, h:h + 1], in1=cmf, op0=ALU.mult, op1=ALU.mult),
                          r=[("psS", hs), ("sm3", s), "cst"], w=[("PT", hs)])
                        st_ = not started[bank]
                        started[bank] = True
                        P(lambda e: e.matmul(psTot[bank][:, tcol], lhsT=PT[hs][:], rhs=vmt[s][:, h, :], start=st_, stop=False, skip_group_check=True), r=[("PT", hs), ("vmt", s)], w=[("psTot", bank)])
                        P(lambda e: e.matmul(psTot[bank][:, tcol], lhsT=qmt[r0:r0 + 64, cb, tsl], rhs=Cbf[r0:r0 + 64, cb, :], start=False, stop=True, skip_group_check=True), r=["qmt", ("Cbf", h)], w=[("psTot", bank)])
                        P(lambda e: e.matmul(psC[hs][:, 0:129], lhsT=Kw[:, cb, :], rhs=vmt[s][:, h, :], start=True, stop=True), r=[("Kw", s), ("vmt", s)], w=[("psC", hs)])
                        V(lambda e: e.scalar_tensor_tensor(out=Cst[r0:r0 + 64, cb, :], in0=Cst[r0:r0 + 64, cb, :], scalar=sm[r0:r0 + 64, 5, h:h + 1], in1=psC[hs][r0:r0 + 64, 0:129], op0=ALU.mult, op1=ALU.add),
                          r=[("psC", hs), ("sm5", s), ("Cst", h)], w=[("Cst", h)])
                        G(lambda e: e.tensor_copy(out=Cbf[r0:r0 + 64, cb, :], in_=Cst[r0:r0 + 64, cb, :]), r=[("Cst", h)], w=[("Cbf", h)])
                    for bank in range(3):
                        nh_ = 3 if bank < 2 else 2
                        V(lambda e: e.tensor_copy(out=n8[:, 0, bank * 3:bank * 3 + nh_], in_=psTot[bank][:, 0:nh_ * 129].rearrange("p (s e) -> p s e", e=129)[:, :, 128]), r=[("psTot", bank)], w=[("n80", bank)])
                    k80 = [("n80", i) for i in range(3)]
                    V(lambda e: e.scalar_tensor_tensor(out=n8[:, 1, :], in0=n8[:, 0, :], scalar=-1.0, in1=n8[:, 0, :], op0=ALU.mult, op1=ALU.max), r=k80, w=["n81"])
                    V(lambda e: e.tensor_tensor(out=n8[:, 2, :], in0=n8[:, 1, :], in1=sm[:, 7, :], op=ALU.max), r=["n81", ("sm7", s)], w=["n82"])
                    V(lambda e: e.reciprocal(out=n8[:, 3, :], in_=n8[:, 2, :]), r=["n82"], w=["n83"])
                    for bank in range(3):
                        nh_ = 3 if bank < 2 else 2
                        V(lambda e: e.tensor_tensor(out=hn[:, bank * 3:bank * 3 + nh_, :], in0=psTot[bank][:, 0:nh_ * 129].rearrange("p (s e) -> p s e", e=129)[:, :, 0:128],
                                                    in1=n8[:, 3, bank * 3:bank * 3 + nh_].unsqueeze(2).to_broadcast([128, nh_, 128]), op=ALU.mult), r=[("psTot", bank), "n83"], w=[("hn", bank)])
                    khn = [("hn", i) for i in range(3)]
                    G(lambda e: e.tensor_tensor(out=hj[:], in0=hn[:], in1=hn[:], op=ALU.mult), r=khn, w=["hj"])
                    V(lambda e: e.reduce_sum(out=n8[:, 4, :], in_=hj[:], axis=AX.X), r=["hj"], w=["n84"])
                    V(lambda e: e.tensor_scalar(out=n8[:, 5, :], in0=n8[:, 4, :], scalar1=1.0 / 128, scalar2=EPS, op0=ALU.mult, op1=ALU.add), r=["n84"], w=["n85"])
                    A(lambda e: e.activation(out=n8[:, 6, :], in_=n8[:, 5, :], func=AF.Sqrt), r=["n85"], w=["n86"])
                    V(lambda e: e.reciprocal(out=n8[:, 7, :], in_=n8[:, 6, :]), r=["n86"], w=["n87"])
                    V(lambda e: e.tensor_tensor(out=hj[:], in0=hn[:], in1=n8[:, 7, :].unsqueeze(2).to_broadcast([128, 8, 128]), op=ALU.mult), r=khn + ["n87", "hj"], w=["hj"])
                    hjf = hj[:].rearrange("p h e -> p (h e)")
                    G(lambda e: e.tensor_tensor(out=hjf, in0=hjf, in1=mlw[:], op=ALU.mult), r=["hj", "mlw"], w=["hj"])
                    V(lambda e: e.tensor_tensor(out=hjf, in0=hjf, in1=omt[s][:], op=ALU.mult), r=["hj", ("omt", s)], w=["hj"])
                    G(lambda e: e.tensor_tensor(out=ymb[s][:], in0=hjf, in1=gmt[s][:], op=ALU.mult), r=["hj", ("gmt", s)], w=[("ymb", s)])
                    dma(YM[b, tsl, :], ymb[s][:], r=[("ymb", s)], w=["YM"])
                k.barrier()

            with ExitStack() as ph:
                qts = [sbt(ph, f"qts{i}", [128, S], BF16) for i in range(2)]
                kts = [sbt(ph, f"kts{i}", [128, S], BF16) for i in range(2)]
                vh = [sbt(ph, f"vh{i}", [128, NT, 129], BF16) for i in range(2)]
                negMb = sbt(ph, "negMb", [128, 16], F32)
                nw = sbt(ph, "nw", [16, 8], F32)
                dg = sbt(ph, "dg", [16, 16], F32)
                swb = sbt(ph, "swb", [128, 128], F32)
                PTa = [sbt(ph, f"PTa{i}", [128, 2, 512], BF16) for i in range(2)]
                negMh = sbt(ph, "negMh", [128, 8], F32)
                gat = [sbt(ph, f"gat{i}", [128, 128], BF16) for i in range(2)]
                ymt = [sbt(ph, f"ymt{i}", [128, 128], BF16) for i in range(2)]
                mb = [sbt(ph, f"mb{i}", [128, 128], BF16) for i in range(2)]
                mstage = [sbt(ph, f"mstage{i}", [128, 512], BF16) for i in range(2)]
                psS = [pst(ph, f"psSa{i}", [128, 1024], F32) for i in range(2)]
                psA = [pst(ph, f"psA{i}", [128, 512], F32) for i in range(3)]
                psT = pst(ph, "psT", [128, 1024], BF16)
                P(lambda e: e.transpose(out=psA[0][0:16, 0:128], in_=nmx[:, 0:16], identity=identf), r=["nmx", "cst"], w=[("psA", 0)])
                P(lambda e: e.transpose(out=psA[0][0:16, 128:256], in_=nmx[:, 16:32], identity=identf), r=["nmx", "cst"], w=[("psA", 0)])
                V(lambda e: e.reduce_max(out=nw[:, 0:2], in_=psA[0][0:16, 0:256].rearrange("p (a t) -> p a t", a=2), axis=AX.X), r=[("psA", 0)], w=["nw0"])
                V(lambda e: e.tensor_tensor(out=nw[:, 2:3], in0=nw[:, 0:1], in1=nw[:, 1:2], op=ALU.mult), r=["nw0"], w=["nw2"])
                A(lambda e: e.activation(out=nw[:, 3:4], in_=nw[:, 2:3], func=AF.Sqrt), r=["nw2"], w=["nw3"])
                V(lambda e: e.tensor_scalar(out=nw[:, 4:5], in0=nw[:, 3:4], scalar1=-0.125, scalar2=None, op0=ALU.mult), r=["nw3"], w=["nw4"])
                V(lambda e: e.tensor_scalar(out=dg[:], in0=identf[0:16, 0:16], scalar1=nw[:, 4:5], scalar2=None, op0=ALU.mult), r=["nw4", "cst"], w=["dg"])
                P(lambda e: e.matmul(psA[1][:, 0:16], lhsT=onesf[0:16, :], rhs=dg[:], start=True, stop=True), r=["dg", "cst"], w=[("psA", 1)])
                V(lambda e: e.tensor_copy(out=negMb[:], in_=psA[1][:, 0:16]), r=[("psA", 1)], w=["negMb"])
                nv2 = negMb[:].rearrange("p (h c) -> p h c", c=2)
                V(lambda e: e.tensor_tensor(out=negMh[:], in0=nv2[:, :, 0], in1=nv2[:, :, 1], op=ALU.min), r=["negMb"], w=["negMh"])
                dma(swb[:], subln_w.partition_broadcast(128), w=["swb"])
                V(lambda e: e.tensor_scalar(out=swb[:], in0=swb[:], scalar1=(1.0 - lam_init), scalar2=None, op0=ALU.mult), r=["swb"], w=["swb"])
                for i in range(2):
                    V(lambda e: e.memset(vh[i][:, :, 128:129], 1.0), w=[("vh", i)])
                fcount = 0
                accS = [sbt(ph, f"accS{i}", [128, 3, 387], F32) for i in range(2)]
                rrs = [sbt(ph, f"rrs{i}", [128, 8], F32) for i in range(2)]
                o1s = [sbt(ph, f"o1s{i}", [128, 128], F32) for i in range(2)]
                o2s = [sbt(ph, f"o2s{i}", [128, 128], F32) for i in range(2)]
                ojs = [sbt(ph, f"ojs{i}", [128, 128], F32) for i in range(2)]
                gcnt = 0
                def load_head(hd_):
                    s_ = hd_ % 2
                    dma(qts[s_][:], QT[b, hd_], w=[("qts", s_)])
                    dma(kts[s_][:], KT[b, hd_], w=[("kts", s_)])
                    for t8 in range(0, NT, 8):
                        te = min(NT, t8 + 8)
                        dma(vh[s_][:, t8:te, 0:128], VA[b].rearrange("(tt p) c -> p tt c", p=128)[:, t8:te, hd_ * 128:(hd_ + 1) * 128], w=[("vh", s_)])

                load_head(0)
                for hd in range(8):
                    s = hd % 2
                    if hd + 1 < 8:
                        load_head(hd + 1)
                    blocks = [(g, kt) for g in range(NG) for kt in range(4 * g + 4)]

                    def qk_exp(bi):
                        g, kt = blocks[bi]
                        bs = bi % 2
                        for c in range(2):
                            P(lambda e: e.matmul(psS[bs][:, c * 512:(c + 1) * 512], lhsT=kts[s][c * 64:(c + 1) * 64, kt * 128:(kt + 1) * 128], rhs=qts[s][c * 64:(c + 1) * 64, g * 512:(g + 1) * 512], start=True, stop=True),
                              r=[("kts", s), ("qts", s)], w=[("psSa", bs)])
                        A(lambda e: e.activation(out=PTa[bs][:].rearrange("p c q -> p (c q)"), in_=psS[bs][:], func=AF.Exp, bias=negMh[:, hd:hd + 1], scale=1.0),
                          r=[("psSa", bs), "negMh"], w=[("PTa", bs)])

                    qk_exp(0)
                    started = [False, False, False]
                    for bi, (g, kt) in enumerate(blocks):
                        bs = bi % 2
                        if bi + 1 < len(blocks):
                            qk_exp(bi + 1)
                        if kt == 0:
                            started = [False, False, False]
                        for qi in range(4):
                            qt_ = 4 * g + qi
                            if kt > qt_:
                                continue
                            for c in range(2):
                                if kt == qt_ and c == 0:
                                    V(lambda e: e.tensor_tensor(out=PTa[bs][:, :, qi * 128:(qi + 1) * 128], in0=PTa[bs][:, :, qi * 128:(qi + 1) * 128], in1=cmb[:].unsqueeze(1).to_broadcast([128, 2, 128]), op=ALU.mult),
                                      r=[("PTa", bs), "cmb"], w=[("PTa", bs)])
                                ai = c * 4 + qi
                                bank, slot = ai // 3, ai % 3
                                st_ = not started[bank]
                                started[bank] = True
                                P(lambda e: e.matmul(psA[bank][:, slot * 129:(slot + 1) * 129], lhsT=PTa[bs][:, c, qi * 128:(qi + 1) * 128], rhs=vh[s][:, kt, :], start=st_, stop=(kt == qt_), skip_group_check=True),
                                  r=[("PTa", bs), ("vh", s)], w=[("psA", bank)])
                        if kt != 4 * g + 3:
                            continue
                        gs_ = gcnt % 2
                        gcnt += 1
                        for bank in range(3):
                            ncol_ = 387 if bank < 2 else 258
                            A(lambda e: e.activation(out=accS[gs_][:, bank, 0:ncol_], in_=psA[bank][:, 0:ncol_], func=AF.Copy), r=[("psA", bank)], w=[("accS", gs_, bank)])
                        ms = g % 2
                        for qi in range(4):
                            qt_ = 4 * g + qi
                            tsl = slice(qt_ * 128, (qt_ + 1) * 128)
                            fs = fcount % 2
                            fcount += 1
                            rr, o1, o2, oj = rrs[fs], o1s[fs], o2s[fs], ojs[fs]
                            dma(gat[fs][:], GAS[b, tsl, hd * 128:(hd + 1) * 128], w=[("gat", fs)])
                            dma(ymt[fs][:], YM[b, tsl, hd * 128:(hd + 1) * 128], w=[("ymt", fs)])
                            a1 = accS[gs_][:, qi // 3, (qi % 3) * 129:(qi % 3 + 1) * 129]
                            a2i = 4 + qi
                            a2 = accS[gs_][:, a2i // 3, (a2i % 3) * 129:(a2i % 3 + 1) * 129]
                            rk = [("accS", gs_, qi // 3), ("accS", gs_, a2i // 3)]
                            V(lambda e: e.reciprocal(out=rr[:, 0:1], in_=a1[:, 128:129]), r=rk, w=[("rr0", fs)])
                            V(lambda e: e.reciprocal(out=rr[:, 1:2], in_=a2[:, 128:129]), r=rk, w=[("rr1", fs)])
                            V(lambda e: e.tensor_tensor(out=rr[:, 2:3], in0=rr[:, 1:2], in1=lamt[:, 0:1], op=ALU.mult), r=[("rr1", fs), "lamt"], w=[("rr2", fs)])
                            A(lambda e: e.activation(out=o1[:], in_=a1[:, 0:128], func=AF.Copy, scale=rr[:, 0:1]), r=rk + [("rr0", fs)], w=[("o1", fs)])
                            V(lambda e: e.scalar_tensor_tensor(out=o2[:], in0=a2[:, 0:128], scalar=rr[:, 2:3], in1=o1[:], op0=ALU.mult, op1=ALU.add), r=rk + [("rr2", fs), ("o1", fs)], w=[("o2", fs)])
                            A(lambda e: e.activation(out=oj[:], in_=o2[:], func=AF.Square, accum_out=rr[:, 3:4]), r=[("o2", fs)], w=[("oj", fs), ("rr3", fs)])
                            V(lambda e: e.tensor_scalar(out=rr[:, 4:5], in0=rr[:, 3:4], scalar1=1.0 / 128, scalar2=EPS, op0=ALU.mult, op1=ALU.add), r=[("rr3", fs)], w=[("rr4", fs)])
                            A(lambda e: e.activation(out=rr[:, 5:6], in_=rr[:, 4:5], func=AF.Sqrt), r=[("rr4", fs)], w=[("rr5", fs)])
                            V(lambda e: e.reciprocal(out=rr[:, 6:7], in_=rr[:, 5:6]), r=[("rr5", fs)], w=[("rr6", fs)])
                            V(lambda e: e.scalar_tensor_tensor(out=oj[:], in0=o2[:], scalar=rr[:, 6:7], in1=swb[:], op0=ALU.mult, op1=ALU.mult), r=[("o2", fs), ("rr6", fs), "swb", ("oj", fs)], w=[("oj", fs)])
                            G(lambda e: e.tensor_tensor(out=oj[:], in0=oj[:], in1=gat[fs][:], op=ALU.mult), r=[("oj", fs), ("gat", fs)], w=[("oj", fs)])
                            G(lambda e: e.tensor_tensor(out=mb[fs][:], in0=oj[:], in1=ymt[fs][:], op=ALU.add), r=[("oj", fs), ("ymt", fs)], w=[("mb", fs)])
                            P(lambda e: e.transpose(out=psT[:, qi * 128:(qi + 1) * 128], in_=mb[fs][:], identity=identb[:]), r=[("mb", fs), "identb"], w=["psT"])
                        V(lambda e: e.tensor_copy(out=mstage[ms][:], in_=psT[:, 0:512]), r=["psT"], w=[("mstage", ms)])
                        dma(MT[b, hd, :, g * 512:(g + 1) * 512], mstage[ms][:], r=[("mstage", ms)], w=["MT"])
                k.barrier()

        wq_v = peer_wq.rearrange("(kc p) n -> p kc n", p=128)
        wo_v = w_out.rearrange("(kc p) n -> p kc n", p=128)
        with ExitStack() as ph:
            wob = sbt(ph, "wob", [128, 8, D], BF16)
            wqb = sbt(ph, "wqb", [128, 8, 2048], BF16)
            keyT = sbt(ph, "keyT", [128, 16, 128], BF16)
            psT = [pst(ph, f"psTE{i}", [128, 1024], BF16) for i in range(2)]
            ph2 = ExitStack()
            wtmp = [sbt(ph2, f"wtmp{i}", [128, 8, 512], F32) for i in range(2)]
            ktmp = sbt(ph2, "ktmp", [128, 16, 128], F32)
            ktb = sbt(ph2, "ktb", [128, 16, 128], BF16)
            wi = 0
            for n0 in range(0, D, 512):
                s = wi % 2
                wi += 1
                dma(wtmp[s][:], wo_v[:, :, n0:n0 + 512], w=[("wtmp", s)])
                V(lambda e: e.tensor_copy(out=wob[:, :, n0:n0 + 512], in_=wtmp[s][:]), r=[("wtmp", s)], w=["wob"])
            for n0 in range(0, 2048, 512):
                s = wi % 2
                wi += 1
                dma(wtmp[s][:], wq_v[:, :, n0:n0 + 512], w=[("wtmp", s)])
                G(lambda e: e.tensor_copy(out=wqb[:, :, n0:n0 + 512], in_=wtmp[s][:]), r=[("wtmp", s)], w=["wqb"])
            dma(ktmp[:], peer_keys.rearrange("g n d -> n g d"), w=["ktmp"])
            V(lambda e: e.tensor_copy(out=ktb[:], in_=ktmp[:]), r=["ktmp"], w=["ktb"])
            for g8 in range(2):
                for j in range(8):
                    gi = g8 * 8 + j
                    P(lambda e: e.transpose(out=psT[g8][:, j * 128:(j + 1) * 128], in_=ktb[:, gi, :], identity=identb[:]), r=["ktb", "identb"], w=[("psTE", g8)])
                V(lambda e: e.tensor_copy(out=keyT[:, g8 * 8:(g8 + 1) * 8, :], in_=psT[g8][:].rearrange("p (g n) -> p g n", g=8)), r=[("psTE", g8)], w=["keyT"])
            k.barrier()
            ph2.close()
            gt1 = sbt(ph, "gt1", [128, D], F32)
            A2 = sbt(ph, "A2", [128, D], F32)
            B2 = sbt(ph, "B2", [128, D], F32)
            w2b = sbt(ph, "w2b", [128, D], F32)
            mt = [sbt(ph, f"mt{i}", [128, 8, 128], BF16) for i in range(2)]
            xt = [sbt(ph, f"xtE{i}", [128, D], F32) for i in range(2)]
            x1 = [sbt(ph, f"x1E{i}", [128, D], F32) for i in range(2)]
            junk = sbt(ph, "junkE", [128, D], BF16)
            hx = sbt(ph, "hxE", [128, D], F32)
            hb = sbt(ph, "hbE", [128, D], BF16)
            h2t = [sbt(ph, f"h2t{i}", [128, 8, 128], BF16) for i in range(2)]
            qpt = sbt(ph, "qpt", [128, 16, 128], BF16)
            scs = [sbt(ph, f"sc{i}", [128, 16, 128], F32) for i in range(2)]
            wk = sbt(ph, "wkE", [128, 16, 128], F32)
            v16 = sbt(ph, "v16", [128, 16, 16], F32)
            i16u = sbt(ph, "i16u", [128, 16, 16], U32)
            i16v = sbt(ph, "i16v", [128, 16, 16], F32)
            cand = sbt(ph, "cand", [128, 8, 256], F32)
            cwk = sbt(ph, "cwk", [128, 8, 256], F32)
            c16 = sbt(ph, "c16", [128, 8, 16], F32)
            p16u = sbt(ph, "p16u", [128, 8, 16], U32)
            p16 = sbt(ph, "p16", [128, 8, 16], F32)
            d1 = sbt(ph, "d1", [128, 8, 16, 16], F32)
            d2 = sbt(ph, "d2", [128, 8, 16, 16], F32)
            ar = sbt(ph, "ar", [128, 8, 16], F32)
            br = sbt(ph, "br", [128, 8, 16], F32)
            igt = [sbt(ph, f"igt{i}", [128, 3, 128], F32) for i in range(2)]
            st4 = sbt(ph, "st4E", [128, 16], F32)
            psO = [pst(ph, f"psO{i}", [128, 512], F32) for i in range(2)]
            psQ = [pst(ph, f"psQ{i}", [128, 512], F32) for i in range(2)]
            psX = [pst(ph, f"psX{i}", [128, 512], F32) for i in range(2)]
            dma(w2b[:], norm2_w.partition_broadcast(128), w=["w2b"])
            def e_front(b, tt):
                s = tt % 2
                tsl = slice(tt * 128, (tt + 1) * 128)
                if tt == 0:
                    dma(gt1[:], MOD[b:b + 1, 2 * D:3 * D].partition_broadcast(128), r=["MOD"], w=["gt1"])
                    dma(B2[:], MOD[b:b + 1, 3 * D:4 * D].partition_broadcast(128), r=["MOD"], w=["B2"])
                    dma(A2[:], MOD[b:b + 1, 4 * D:5 * D].partition_broadcast(128), r=["MOD"], w=["A2"])
                    V(lambda e: e.scalar_tensor_tensor(out=A2[:], in0=A2[:], scalar=1.0, in1=w2b[:], op0=ALU.add, op1=ALU.mult), r=["A2", "w2b"], w=["A2"])
                dma(mt[s][:], MT[b, :, :, tsl].rearrange("h p t -> p h t"), r=["MT"], w=[("mt", s)])
                dma(xt[s][:], x[b, tsl, :], w=[("xtE", s)])
                for nh in range(2):
                    for kc in range(8):
                        P(lambda e: e.matmul(psO[nh][:], lhsT=mt[s][:, kc, :], rhs=wob[:, kc, nh * 512:(nh + 1) * 512], start=(kc == 0), stop=(kc == 7)),
                          r=[("mt", s), "wob"], w=[("psO", nh)])
                    V(lambda e: e.tensor_tensor(out=x1[s][:, nh * 512:(nh + 1) * 512], in0=psO[nh][:], in1=gt1[:, nh * 512:(nh + 1) * 512], op=ALU.mult), r=[("psO", nh), "gt1"], w=[("x1E", s)])
                G(lambda e: e.tensor_tensor(out=x1[s][:], in0=x1[s][:], in1=xt[s][:], op=ALU.add), r=[("x1E", s), ("xtE", s)], w=[("x1E", s)])
                dma(X1[b, tsl, :], x1[s][:], r=[("x1E", s)], w=["X1"])
                A(lambda e: e.activation(out=junk[:], in_=x1[s][:], func=AF.Square, accum_out=st4[:, 0:1]), r=[("x1E", s)], w=["junkE", "sE0"])
                V(lambda e: e.tensor_scalar(out=st4[:, 1:2], in0=st4[:, 0:1], scalar1=1.0 / D, scalar2=EPS, op0=ALU.mult, op1=ALU.add), r=["sE0"], w=["sE1"])
                A(lambda e: e.activation(out=st4[:, 2:3], in_=st4[:, 1:2], func=AF.Sqrt), r=["sE1"], w=["sE2"])
                V(lambda e: e.reciprocal(out=st4[:, 3:4], in_=st4[:, 2:3]), r=["sE2"], w=["sE3"])
                V(lambda e: e.scalar_tensor_tensor(out=hx[:], in0=x1[s][:], scalar=st4[:, 3:4], in1=A2[:], op0=ALU.mult, op1=ALU.mult), r=[("x1E", s), "sE3", "A2"], w=["hxE"])
                G(lambda e: e.tensor_tensor(out=hb[:], in0=hx[:], in1=B2[:], op=ALU.add), r=["hxE", "B2"], w=["hbE"])
                for kc in range(8):
                    P(lambda e: e.transpose(out=psT[0][:, kc * 128:(kc + 1) * 128], in_=hb[:, kc * 128:(kc + 1) * 128], identity=identb[:]), r=["hbE", "identb"], w=[("psTE", 0)])
                A(lambda e: e.activation(out=h2t[s][:], in_=psT[0][:].rearrange("p (k t) -> p k t", k=8), func=AF.Copy), r=[("psTE", 0)], w=[("h2t", s)])
                dma(H2T[b, :, :, tsl].rearrange("k p t -> p k t"), h2t[s][:], r=[("h2t", s)], w=["H2T"])
                for gq in range(4):
                    pq = gq % 2
                    for j in range(4):
                        gi = gq * 4 + j
                        for kc in range(8):
                            P(lambda e: e.matmul(psQ[pq][:, j * 128:(j + 1) * 128], lhsT=wqb[:, kc, gi * 128:(gi + 1) * 128], rhs=h2t[s][:, kc, :], start=(kc == 0), stop=(kc == 7)),
                              r=["wqb", ("h2t", s)], w=[("psQ", pq)])
                    A(lambda e: e.activation(out=qpt[:, gq * 4:(gq + 1) * 4, :], in_=psQ[pq][:].rearrange("p (g t) -> p g t", g=4), func=AF.Copy), r=[("psQ", pq)], w=["qpt"])
                sc = scs[s]
                for gq in range(4):
                    px = gq % 2
                    for j in range(4):
                        gi = gq * 4 + j
                        P(lambda e: e.matmul(psX[px][:, j * 128:(j + 1) * 128], lhsT=qpt[:, gi, :], rhs=keyT[:, gi, :], start=True, stop=True), r=["qpt", "keyT"], w=[("psX", px)])
                    A(lambda e: e.activation(out=sc[:, gq * 4:(gq + 1) * 4, :], in_=psX[px][:].rearrange("p (g n) -> p g n", g=4), func=AF.Copy), r=[("psX", px)], w=[("sc", s, gq)])

            def e_back(b, tt):
                s = tt % 2
                tsl = slice(tt * 128, (tt + 1) * 128)
                sc = scs[s]
                for gi in range(16):
                    V(lambda e: e.max(out=v16[:, gi, 0:8], in_=sc[:, gi, :]), r=[("sc", s, gi // 4)], w=[("v16a", gi)])
                for gi in range(16):
                    V(lambda e: e.max_index(out=i16u[:, gi, 0:8], in_max=v16[:, gi, 0:8], in_values=sc[:, gi, :]), r=[("sc", s, gi // 4), ("v16a", gi)], w=[("i16a", gi)])
                for gi in range(16):
                    V(lambda e: e.match_replace(out=wk[:, gi, :], in_to_replace=v16[:, gi, 0:8], in_values=sc[:, gi, :], imm_value=-1e30), r=[("sc", s, gi // 4), ("v16a", gi)], w=[("wkE", gi)])
                for gi in range(16):
                    V(lambda e: e.max(out=v16[:, gi, 8:16], in_=wk[:, gi, :]), r=[("wkE", gi)], w=[("v16b", gi)])
                for gi in range(16):
                    V(lambda e: e.max_index(out=i16u[:, gi, 8:16], in_max=v16[:, gi, 8:16], in_values=wk[:, gi, :]), r=[("wkE", gi), ("v16b", gi)], w=[("i16b", gi)])
                allv = [("v16a", gi) for gi in range(16)] + [("v16b", gi) for gi in range(16)]
                alli = [("i16a", gi) for gi in range(16)] + [("i16b", gi) for gi in range(16)]
                V(lambda e: e.tensor_copy(out=i16v[:], in_=i16u[:]), r=alli, w=["i16v"])
                v16v = v16[:].rearrange("p (h q) k -> p h q k", q=2)
                i16vv = i16v[:].rearrange("p (h q) k -> p h q k", q=2)
                V(lambda e: e.tensor_tensor(out=cand[:].rearrange("p h (a b) -> p h a b", a=16), in0=v16v[:, :, 0, :].unsqueeze(3).to_broadcast([128, 8, 16, 16]),
                                            in1=v16v[:, :, 1, :].unsqueeze(2).to_broadcast([128, 8, 16, 16]), op=ALU.add), r=allv, w=["cand"])
                for h in range(8):
                    V(lambda e: e.max(out=c16[:, h, 0:8], in_=cand[:, h, :]), r=["cand"], w=[("c16a", h)])
                for h in range(8):
                    V(lambda e: e.max_index(out=p16u[:, h, 0:8], in_max=c16[:, h, 0:8], in_values=cand[:, h, :]), r=["cand", ("c16a", h)], w=[("p16a", h)])
                for h in range(8):
                    V(lambda e: e.match_replace(out=cwk[:, h, :], in_to_replace=c16[:, h, 0:8], in_values=cand[:, h, :], imm_value=-1e30), r=["cand", ("c16a", h)], w=[("cwk", h)])
                for h in range(8):
                    V(lambda e: e.max(out=c16[:, h, 8:16], in_=cwk[:, h, :]), r=[("cwk", h)], w=[("c16b", h)])
                for h in range(8):
                    V(lambda e: e.max_index(out=p16u[:, h, 8:16], in_max=c16[:, h, 8:16], in_values=cwk[:, h, :]), r=[("cwk", h), ("c16b", h)], w=[("p16b", h)])
                allc = [("c16a", h) for h in range(8)] + [("c16b", h) for h in range(8)]
                allp = [("p16a", h) for h in range(8)] + [("p16b", h) for h in range(8)]
                V(lambda e: e.tensor_copy(out=p16[:], in_=p16u[:]), r=allp, w=["p16"])
                ig0 = igt[s][:, 0, :].rearrange("p (h r) -> p h r", h=8)
                ig1 = igt[s][:, 1, :].rearrange("p (h r) -> p h r", h=8)
                HS = [slice(0, 4), slice(4, 8)]

                def bc4(ap3, axis):
                    return ap3.unsqueeze(axis).to_broadcast([128, 4, 16, 16])

                a16b = a16f.unsqueeze(1).unsqueeze(1).to_broadcast([128, 4, 16, 16])
                i16b = i16f.unsqueeze(1).unsqueeze(1).to_broadcast([128, 4, 16, 16])
                for z, hsl in enumerate(HS):
                    V(lambda e: e.tensor_tensor(out=d1[:, hsl], in0=bc4(p16[:, hsl, :], 3), in1=a16b, op=ALU.subtract), r=["p16", "cst"], w=[("d1", z)])
                for z, hsl in enumerate(HS):
                    V(lambda e: e.tensor_scalar(out=d2[:, hsl], in0=d1[:, hsl], scalar1=0.0, scalar2=None, op0=ALU.is_ge), r=[("d1", z)], w=[("d2", z)])
                for z, hsl in enumerate(HS):
                    V(lambda e: e.tensor_scalar(out=d1[:, hsl], in0=d1[:, hsl], scalar1=15.5, scalar2=None, op0=ALU.is_lt), r=[("d1", z), ("d2", z)], w=[("d1", z)])
                for z, hsl in enumerate(HS):
                    V(lambda e: e.tensor_tensor(out=d1[:, hsl], in0=d1[:, hsl], in1=d2[:, hsl], op=ALU.mult), r=[("d1", z), ("d2", z)], w=[("d1", z)])
                for z, hsl in enumerate(HS):
                    G(lambda e: e.tensor_tensor(out=d2[:, hsl], in0=d1[:, hsl], in1=a16b, op=ALU.mult), r=[("d1", z), "cst"], w=[("d2", z)])
                for z, hsl in enumerate(HS):
                    V(lambda e: e.reduce_sum(out=ar[:, hsl, :], in_=d2[:, hsl], axis=AX.X), r=[("d2", z)], w=[("ar", z)])
                for z, hsl in enumerate(HS):
                    G(lambda e: e.tensor_tensor(out=d2[:, hsl], in0=d1[:, hsl], in1=bc4(i16vv[:, hsl, 0, :], 2), op=ALU.mult), r=[("d1", z), "i16v", ("ar", z)], w=[("d2", z)])
                for z, hsl in enumerate(HS):
                    V(lambda e: e.reduce_sum(out=ig0[:, hsl, :], in_=d2[:, hsl], axis=AX.X), r=[("d2", z)], w=[("igt", s, z)])
                for z, hsl in enumerate(HS):
                    V(lambda e: e.tensor_tensor(out=br[:, hsl, :], in0=p16[:, hsl, :], in1=ar[:, hsl, :], op=ALU.subtract), r=["p16", ("ar", z)], w=[("br", z)])
                for z, hsl in enumerate(HS):
                    V(lambda e: e.tensor_tensor(out=d1[:, hsl], in0=bc4(br[:, hsl, :], 3), in1=i16b, op=ALU.is_equal), r=[("br", z), "cst", ("d2", z)], w=[("d1", z)])
                for z, hsl in enumerate(HS):
                    G(lambda e: e.tensor_tensor(out=d2[:, hsl], in0=d1[:, hsl], in1=bc4(i16vv[:, hsl, 1, :], 2), op=ALU.mult), r=[("d1", z), "i16v"], w=[("d2", z)])
                for z, hsl in enumerate(HS):
                    V(lambda e: e.reduce_sum(out=ig1[:, hsl, :], in_=d2[:, hsl], axis=AX.X), r=[("d2", z), ("igt", s, z)], w=[("igt", s, z)])
                V(lambda e: e.tensor_tensor(out=ar[:], in0=c16[:], in1=c16[:, :, 0:1].to_broadcast([128, 8, 16]), op=ALU.subtract), r=allc + [("br", 0), ("br", 1), ("ar", 0), ("ar", 1)], w=[("ar", 0), ("ar", 1)])
                A(lambda e: e.activation(out=ar[:], in_=ar[:], func=AF.Exp), r=[("ar", 0), ("ar", 1)], w=[("ar", 0), ("ar", 1)])
                V(lambda e: e.reduce_sum(out=st4[:, 8:16], in_=ar[:], axis=AX.X), r=[("ar", 0), ("ar", 1)], w=["sE8"])
                V(lambda e: e.reciprocal(out=st4[:, 8:16], in_=st4[:, 8:16]), r=["sE8"], w=["sE8"])
                V(lambda e: e.tensor_tensor(out=igt[s][:, 2, :].rearrange("p (h r) -> p h r", h=8), in0=ar[:], in1=st4[:, 8:16].unsqueeze(2).to_broadcast([128, 8, 16]), op=ALU.mult),
                  r=[("ar", 0), ("ar", 1), "sE8", ("igt", s, 0), ("igt", s, 1)], w=[("igt", s, 0), ("igt", s, 1)])
                dma(IG[b, tsl, :, :], igt[s][:], r=[("igt", s, 0), ("igt", s, 1)], w=["IG"])

            tiles = [(b, tt) for b in range(NB) for tt in range(NT)]
            e_front(*tiles[0])
            for ti, (b, tt) in enumerate(tiles):
                if ti + 1 < len(tiles):
                    e_front(*tiles[ti + 1])
                e_back(b, tt)
            k.barrier()

        TG = 256
        NGR = T // TG
        X1f = X1.rearrange("b s d -> (b s) d")
        outf = out.rearrange("b s d -> (b s) d")
        IGf = IG.rearrange("b s a r -> (b s) a r")
        with ExitStack() as ph:
            Gm = [sbt(ph, f"Gm{i}", [128, TG, 128], BF16) for i in range(2)]
            ust = [sbt(ph, f"ust{i}", [128, 2, 1024], BF16) for i in range(3)]
            vst = [sbt(ph, f"vst{i}", [128, 2, 1024], BF16) for i in range(3)]
            h2s = [sbt(ph, f"h2F{i}", [128, 8, TG], BF16) for i in range(2)]
            igl = sbt(ph, "igl", [128, 2, 3, 128], F32)
            igT = sbt(ph, "igT", [128, 3, TG], F32)
            ohA = [sbt(ph, f"ohA{i}", [128, 8, 128], BF16) for i in range(2)]
            ohB = [sbt(ph, f"ohB{i}", [128, 8, 128], BF16) for i in range(2)]
            ohT = sbt(ph, "ohT", [128, 8, 128], BF16)
            act = [sbt(ph, f"actF{i}", [128, TG], BF16) for i in range(4)]
            ga = [sbt(ph, f"gaF{i}", [128, TG], BF16) for i in range(4)]
            gt2 = sbt(ph, "gt2", [128, D], F32)
            fwb = sbt(ph, "fwb", [128, D], F32)
            x1l = sbt(ph, "x1l", [128, D], F32)
            yo = sbt(ph, "yo", [128, D], F32)
            oo = sbt(ph, "oo", [128, D], F32)
            st4 = sbt(ph, "st4F", [128, 8], F32)
            psY = [pst(ph, f"psY{i}", [128, 512], F32) for i in range(4)]
            psSc = [pst(ph, f"psSc{i}", [128, 512], F32) for i in range(2)]
            psGb = [pst(ph, f"psGb{i}", [128, 512], F32) for i in range(2)]
            dma(fwb[:], fin_w.partition_broadcast(128), w=["fwb"])
            UTv = UT.rearrange("i p f -> p i f")
            VBv = VB.rearrange("i p f -> p i f")
            ldc = [0]
            gcount = [0]
            slots = {}
            iob = iotaf.unsqueeze(1).to_broadcast([128, 8, 128])

            def prep_group(gr):
                t0 = gr * TG
                b = t0 // S
                s0 = t0 % S
                hs = gr % 2
                dma(h2s[hs][:], H2T[b, :, :, s0:s0 + TG].rearrange("k p t -> p k t"), w=[("h2F", hs)])
                dma(igl[:].rearrange("p j a r -> p j (a r)"), IGf[t0:t0 + TG].rearrange("(j p) a r -> p j (a r)", p=128), w=["igl"])
                for j in range(2):
                    for a in range(3):
                        P(lambda e: e.transpose(out=psGb[0][:, 0:128], in_=igl[:, j, a, :], identity=identf), r=["igl", "cst"], w=[("psGb", 0)])
                        V(lambda e: e.tensor_copy(out=igT[:, a, j * 128:(j + 1) * 128], in_=psGb[0][:, 0:128]), r=[("psGb", 0)], w=["igT"])

            def gb_onehots(gr, sub):
                os_ = sub % 2
                tq = slice(sub * 8, (sub + 1) * 8)
                V(lambda e: e.tensor_tensor(out=ohA[os_][:], in0=iob, in1=igT[:, 0, tq].unsqueeze(2).to_broadcast([128, 8, 128]), op=ALU.is_equal), r=["cst", "igT"], w=[("ohA", os_)])
                V(lambda e: e.tensor_tensor(out=ohT[:], in0=iob, in1=igT[:, 1, tq].unsqueeze(2).to_broadcast([128, 8, 128]), op=ALU.is_equal), r=["cst", "igT"], w=["ohT"])
                G(lambda e: e.tensor_tensor(out=ohB[os_][:], in0=ohT[:], in1=igT[:, 2, tq].unsqueeze(2).to_broadcast([128, 8, 128]), op=ALU.mult), r=["ohT", "igT"], w=[("ohB", os_)])

            def gb_mm(gr, sub):
                os_ = sub % 2
                gb = gr % 2
                for q4 in range(2):
                    pg = gcount[0] % 2
                    gcount[0] += 1
                    for j in range(4):
                        tl = q4 * 4 + j
                        P(lambda e: e.matmul(psGb[pg][:, j * 128:(j + 1) * 128], lhsT=ohB[os_][:, tl, :], rhs=ohA[os_][:, tl, :], start=True, stop=True),
                          r=[("ohA", os_), ("ohB", os_)], w=[("psGb", pg)])
                    tb = sub * 8 + q4 * 4
                    A(lambda e: e.activation(out=Gm[gb][:, tb:tb + 4, :], in_=psGb[pg][:].rearrange("p (t i) -> p t i", t=4), func=AF.Copy), r=[("psGb", pg)], w=[("Gm", gb)])

            def load_blk(i2b):
                ldc[0] += 1
                ss_ = ldc[0] % 3
                slots[i2b] = ss_
                dma(ust[ss_][:].rearrange("p j f -> p (j f)"), UT[i2b], r=["UT"], w=[("ust", ss_)])
                dma(vst[ss_][:].rearrange("p j f -> p (j f)"), VB[i2b], r=["VB"], w=[("vst", ss_)])

            def scores(gr, i1):
                ss_ = slots[i1 // 2]
                j = i1 % 2
                q = i1 % 2
                h2 = h2s[gr % 2]
                for kc in range(8):
                    P(lambda e: e.matmul(psSc[q][:, 0:TG], lhsT=ust[ss_][:, j, kc * 128:(kc + 1) * 128], rhs=h2[:, kc, :], start=(kc == 0), stop=(kc == 7)),
                      r=[("ust", ss_), ("h2F", gr % 2)], w=[("psSc", q)])

            prep_group(0)
            for sub in range(TG // 8):
                gb_onehots(0, sub)
                gb_mm(0, sub)
            if NGR > 1:
                prep_group(1)
            for gr in range(NGR):
                t0 = gr * TG
                b = t0 // S
                s0 = t0 % S
                gb = gr % 2
                if s0 == 0:
                    dma(gt2[:], MOD[b:b + 1, 5 * D:6 * D].partition_broadcast(128), r=["MOD"], w=["gt2"])
                nxt = gr + 1 < NGR
                load_blk(0)
                load_blk(1)
                scores(gr, 0)

                def ymm(i1):
                    ss_y = slots_y[i1]
                    j_ = i1 % 2
                    q_ = i1 % 4
                    for tj in range(2):
                        for nh in range(2):
                            P(lambda e: e.matmul(psY[tj * 2 + nh][:], lhsT=ga[q_][:, tj * 128:(tj + 1) * 128], rhs=vst[ss_y][:, j_, nh * 512:(nh + 1) * 512], start=(i1 == 0), stop=(i1 == 127)),
                              r=[("gaF", q_), ("vst", ss_y)], w=[("psY", tj * 2 + nh)])

                slots_y = {}
                for i1 in range(128):
                    ss_ = slots[i1 // 2]
                    slots_y[i1] = ss_
                    q = i1 % 2
                    q4 = i1 % 4
                    if i1 + 1 < 128:
                        scores(gr, i1 + 1)
                    if i1 >= 1:
                        ymm(i1 - 1)
                    if i1 % 2 == 1 and (i1 + 1) // 2 + 1 < 64:
                        load_blk((i1 + 1) // 2 + 1)
                    if nxt and i1 % 4 == 0:
                        sub = i1 // 4
                        gb_onehots(gr + 1, sub)
                        if sub >= 1:
                            gb_mm(gr + 1, sub - 1)
                    A(lambda e: e.activation(out=act[q][:], in_=psSc[q][:, 0:TG], func=AF.Gelu), r=[("psSc", q)], w=[("actF", q)])
                    eng = V if (i1 % 2 == 0) else G
                    eng(lambda e: e.tensor_tensor(out=ga[q4][:], in0=act[q][:], in1=Gm[gb][:, :, i1], op=ALU.mult), r=[("actF", q), ("Gm", gb)], w=[("gaF", q4)])
                ymm(127)
                if nxt:
                    gb_mm(gr + 1, 31)
                if gr + 2 < NGR:
                    prep_group(gr + 2)
                for tj in range(2):
                    dma(x1l[:], X1f[t0 + tj * 128:t0 + (tj + 1) * 128, :], w=["x1l"])
                    for nh in range(2):
                        V(lambda e: e.tensor_tensor(out=yo[:, nh * 512:(nh + 1) * 512], in0=psY[tj * 2 + nh][:], in1=gt2[:, nh * 512:(nh + 1) * 512], op=ALU.mult), r=[("psY", tj * 2 + nh), "gt2"], w=["yo"])
                    G(lambda e: e.tensor_tensor(out=yo[:], in0=yo[:], in1=x1l[:], op=ALU.add), r=["yo", "x1l"], w=["yo"])
                    A(lambda e: e.activation(out=x1l[:], in_=yo[:], func=AF.Square, accum_out=st4[:, 0:1]), r=["yo", "x1l"], w=["x1l", "sF0"])
                    V(lambda e: e.tensor_scalar(out=st4[:, 1:2], in0=st4[:, 0:1], scalar1=1.0 / D, scalar2=EPS, op0=ALU.mult, op1=ALU.add), r=["sF0"], w=["sF1"])
                    A(lambda e: e.activation(out=st4[:, 2:3], in_=st4[:, 1:2], func=AF.Sqrt), r=["sF1"], w=["sF2"])
                    V(lambda e: e.reciprocal(out=st4[:, 3:4], in_=st4[:, 2:3]), r=["sF2"], w=["sF3"])
                    V(lambda e: e.scalar_tensor_tensor(out=oo[:], in0=yo[:], scalar=st4[:, 3:4], in1=fwb[:], op0=ALU.mult, op1=ALU.mult), r=["yo", "sF3", "fwb", "oo"], w=["oo"])
                    dma(outf[t0 + tj * 128:t0 + (tj + 1) * 128, :], oo[:], r=["oo"], w=["OUT", "oo"])
            k.barrier()
    return nc


_INPUT_ORDER = ["x", "c", "ada_w", "ada_b", "norm1_w", "norm2_w", "w_in", "conv_w", "conv_b", "ml_i_bias", "ml_f_bias",
                "ml_norm_w", "lam_q1", "lam_k1", "lam_q2", "lam_k2", "subln_w", "w_out", "peer_wq", "peer_keys",
                "peer_u", "peer_v", "final_norm_w"]


def make_in_maps(cfg, inputs, n_cores):
    f = lambda a: np.ascontiguousarray(np.asarray(a, dtype=np.float32))
    NB = cfg.NB
    shared = {
        "ada_w": f(inputs["ada_w"][0]), "ada_b": f(inputs["ada_b"][0]).reshape(1, -1),
        "norm1_w": f(inputs["norm1_w"][0]).reshape(1, -1), "norm2_w": f(inputs["norm2_w"][0]).reshape(1, -1),
        "w_in": f(inputs["w_in"][0]), "conv_w": f(inputs["conv_w"][0]), "conv_b": f(inputs["conv_b"][0]).reshape(1, -1),
        "ml_i_bias": f(inputs["ml_i_bias"][0]).reshape(1, -1), "ml_f_bias": f(inputs["ml_f_bias"][0]).reshape(1, -1),
        "ml_norm_w": f(inputs["ml_norm_w"][0]).reshape(1, -1),
        "lam_q1": f(inputs["lam_q1"][0]).reshape(1, -1), "lam_k1": f(inputs["lam_k1"][0]).reshape(1, -1),
        "lam_q2": f(inputs["lam_q2"][0]).reshape(1, -1), "lam_k2": f(inputs["lam_k2"][0]).reshape(1, -1),
        "subln_w": f(inputs["subln_w"][0]).reshape(1, -1), "w_out": f(inputs["w_out"][0]),
        "peer_wq": f(inputs["peer_wq"][0]), "peer_keys": f(inputs["peer_keys"][0]).reshape(16, 128, 128),
        "peer_u": f(inputs["peer_u"][0]), "peer_v": f(inputs["peer_v"][0]),
        "final_norm_w": f(inputs["final_norm_w"]).reshape(1, -1),
        "cst": host_consts(cfg),
    }
    xs = f(inputs["x"])
    cs = f(inputs["c"])
    maps = []
    for i in range(n_cores):
        m = dict(shared)
        m["x"] = np.ascontiguousarray(xs[i * NB:(i + 1) * NB])
        m["c"] = np.ascontiguousarray(cs[i * NB:(i + 1) * NB])
        maps.append(m)
    return maps


def kernel(**inputs):
    n_cores = 8
    cfg = Cfg(S=4096, NB=2)
    nc = build(cfg)
    maps = make_in_maps(cfg, inputs, n_cores)
    res = run_bass_kernel_spmd(nc, maps, core_ids=list(range(n_cores)))
    outs = [np.asarray(r["out"], dtype=np.float32) for r in res.results]
    return np.concatenate(outs, axis=0)
```

```python
import math
from contextlib import ExitStack

import numpy as np
import ml_dtypes
import concourse.bass as bass
import concourse.mybir as mybir
from concourse.bass_utils import run_bass_kernel_spmd

F32 = mybir.dt.float32
BF16 = mybir.dt.bfloat16
U32 = mybir.dt.uint32
ALU = mybir.AluOpType
AF = mybir.ActivationFunctionType
AX = mybir.AxisListType

D = 1024
EPS = 1e-6
IN_W = 8208
NEG = -30000.0


class Tok:
    __slots__ = ("sem", "val", "eng")

    def __init__(self, sem, val, eng):
        self.sem, self.val, self.eng = sem, val, eng


class Eng:
    def __init__(self, kb, name, eng):
        self.kb, self.name, self.eng = kb, name, eng
        self.sem = None
        self.count = 0
        self.seen = {}
        self.last = None
        self.nsem = 0

    def wait(self, tok):
        if tok is None:
            return
        key = id(tok.sem)
        if self.seen.get(key, 0) >= tok.val:
            return
        self.seen[key] = tok.val
        self.eng.wait_ge(tok.sem, tok.val)

    def signal(self, instr):
        if self.sem is None or self.count >= 30000:
            self.sem = self.kb.es.enter_context(self.kb.nc.semaphore(f"s_{self.name}_{self.nsem}"))
            self.nsem += 1
            self.count = 0
        self.count += 1
        instr.then_inc(self.sem, 1)
        t = Tok(self.sem, self.count, self.name)
        self.last = t
        return t


class KB:
    def __init__(self, nc):
        self.nc = nc
        self.es = ExitStack()
        self.e = {
            "pe": Eng(self, "pe", nc.tensor),
            "act": Eng(self, "act", nc.scalar),
            "dve": Eng(self, "dve", nc.vector),
            "pool": Eng(self, "pool", nc.gpsimd),
            "sp": Eng(self, "sp", nc.sync),
        }
        self.res = {}
        self.dsems = []
        self.dvals = []
        self.dnext = 0
        self.ND = 40
        self.all_dma = []

    def _deps(self, r, w):
        deps = []
        for k in r:
            st = self.res.get(k)
            if st is not None and st[0] is not None:
                deps.append(st[0])
        for k in w:
            st = self.res.get(k)
            if st is not None:
                if st[0] is not None:
                    deps.append(st[0])
                deps.extend(st[1])
        return deps

    def _record(self, tok, r, w):
        for k in r:
            st = self.res.setdefault(k, [None, []])
            if tok.eng != "dma":
                st[1] = [t for t in st[1] if t.eng != tok.eng]
            st[1].append(tok)
        for k in w:
            self.res[k] = [tok, []]

    def op(self, en, fn, r=(), w=()):
        E = self.e[en]
        for t in self._deps(r, w):
            if en == "pe" and t.eng == "pe":
                continue
            E.wait(t)
        instr = fn(E.eng)
        tok = E.signal(instr)
        self._record(tok, r, w)
        return tok

    def P(self, fn, r=(), w=()):
        return self.op("pe", fn, r, w)

    def A(self, fn, r=(), w=()):
        return self.op("act", fn, r, w)

    def V(self, fn, r=(), w=()):
        return self.op("dve", fn, r, w)

    def G(self, fn, r=(), w=()):
        return self.op("pool", fn, r, w)

    def dma(self, out, in_, r=(), w=(), q="sp", **kw):
        E = self.e[q]
        for t in self._deps(r, w):
            E.wait(t)
        if len(self.dsems) < self.ND:
            self.dsems.append(self.es.enter_context(self.nc.semaphore(f"s_dma_{len(self.dsems)}")))
            self.dvals.append(0)
        i = self.dnext
        self.dnext = (self.dnext + 1) % self.ND
        sem = self.dsems[i]
        if self.dvals[i] > 0:
            E.wait(Tok(sem, self.dvals[i], "dma"))
        self.dvals[i] += 16
        instr = E.eng.dma_start(out=out, in_=in_, **kw)
        instr.then_inc(sem, 16)
        tok = Tok(sem, self.dvals[i], "dma")
        self._record(tok, r, w)
        return tok

    def barrier(self):
        toks = [E.last for E in self.e.values() if E.last is not None]
        toks += [Tok(s, v, "dma") for s, v in zip(self.dsems, self.dvals) if v > 0]
        for E in self.e.values():
            for t in toks:
                if t.eng == E.name:
                    continue
                E.wait(t)
        self.res = {}


class Cfg:
    def __init__(self, S=4096, NB=2, debug=False):
        self.S, self.NB, self.debug = S, NB, debug
        self.NT = S // 128


def host_consts(cfg):
    NT = cfg.NT
    p = np.arange(128)
    ident = np.eye(128, dtype=np.float32)
    tri = (p[:, None] <= p[None, :]).astype(np.float32)
    negm = np.where(p[:, None] > p[None, :], NEG, 0.0).astype(np.float32)
    cm = (p[:, None] <= p[None, :]).astype(np.float32)
    iota = np.broadcast_to(np.arange(128, dtype=np.float32)[None, :], (128, 128)).copy()
    ones = np.ones((128, 128), np.float32)
    half = 8
    inv = (500000.0 ** (-np.arange(half, dtype=np.float32) * 2.0 / 16)).astype(np.float32)
    pos = (np.arange(NT)[None, :] * 128 + p[:, None]).astype(np.float32)
    ang = pos[:, :, None] * inv[None, None, :]
    cos = np.cos(ang).astype(np.float32).reshape(128, NT * 8)
    sin = np.sin(ang).astype(np.float32).reshape(128, NT * 8)
    a16 = np.broadcast_to((np.arange(16, dtype=np.float32) * 16)[None, :], (128, 16)).copy()
    i16 = np.broadcast_to(np.arange(16, dtype=np.float32)[None, :], (128, 16)).copy()
    return np.concatenate([ident, tri, negm, cm, iota, ones, a16, i16, cos, sin], axis=1).astype(np.float32)


def build(cfg):
    S, NB, NT = cfg.S, cfg.NB, cfg.NT
    NG = S // 512
    T = NB * S
    nc = bass.Bass("TRN2", target_bir_lowering=False)

    def din(name, shape, dt=F32):
        return nc.dram_tensor(name, list(shape), dt, kind="ExternalInput").ap()

    def dscr(name, shape, dt):
        kind = "ExternalOutput" if cfg.debug else "Internal"
        return nc.dram_tensor(name, list(shape), dt, kind=kind).ap()

    x = din("x", [NB, S, D])
    c_in = din("c", [NB, D])
    ada_w = din("ada_w", [D, 6 * D])
    ada_b = din("ada_b", [1, 6 * D])
    norm1_w = din("norm1_w", [1, D])
    norm2_w = din("norm2_w", [1, D])
    w_in = din("w_in", [D, IN_W])
    conv_w = din("conv_w", [4, 1024])
    conv_b = din("conv_b", [1, 1024])
    ml_ib = din("ml_i_bias", [1, 8])
    ml_fb = din("ml_f_bias", [1, 8])
    ml_nw = din("ml_norm_w", [1, D])
    lam_q1 = din("lam_q1", [1, 64])
    lam_k1 = din("lam_k1", [1, 64])
    lam_q2 = din("lam_q2", [1, 64])
    lam_k2 = din("lam_k2", [1, 64])
    subln_w = din("subln_w", [1, 128])
    w_out = din("w_out", [D, D])
    peer_wq = din("peer_wq", [D, 2048])
    peer_keys = din("peer_keys", [16, 128, 128])
    peer_u = din("peer_u", [16384, D])
    peer_v = din("peer_v", [16384, D])
    fin_w = din("final_norm_w", [1, D])
    NCST = 128 * 6 + 32 + 2 * NT * 8
    cst_d = din("cst", [128, NCST])
    out = nc.dram_tensor("out", [NB, S, D], F32, kind="ExternalOutput").ap()

    UT = dscr("UT", [64, 128, 2048], BF16)
    VB = dscr("VB", [64, 128, 2048], BF16)
    MOD = dscr("MOD", [NB, 6 * D], F32)
    QT = dscr("QT", [NB, 8, 128, S], BF16)
    KT = dscr("KT", [NB, 8, 128, S], BF16)
    VA = dscr("VA", [NB, S, D], BF16)
    QMT = dscr("QMT", [NB, 4, 128, S], BF16)
    KMT = dscr("KMT", [NB, 4, 128, S], BF16)
    VM = dscr("VM", [NB, S, D], BF16)
    OMS = dscr("OMS", [NB, S, D], BF16)
    GAS = dscr("GAS", [NB, S, D], BF16)
    GMS = dscr("GMS", [NB, S, D], BF16)
    GATES = dscr("GATES", [NB, S, 16], F32)
    YM = dscr("YM", [NB, S, D], BF16)
    MT = dscr("MT", [NB, 8, 128, S], BF16)
    X1 = dscr("X1", [NB, S, D], F32)
    H2T = dscr("H2T", [NB, 8, 128, S], BF16)
    IG = dscr("IG", [NB, S, 3, 128], F32)

    k = KB(nc)
    P, A, V, G, dma = k.P, k.A, k.V, k.G, k.dma

    with k.es:
        es0 = k.es

        uid = [0]

        def sbt(es, name, shape, dt):
            uid[0] += 1
            return es.enter_context(nc.sbuf_tensor(f"sb{uid[0]}_{name}", list(shape), dt))

        def pst(es, name, shape, dt):
            uid[0] += 1
            return es.enter_context(nc.psum_tensor(f"ps{uid[0]}_{name}", list(shape), dt))

        cst = sbt(es0, "cst", [128, NCST], F32)
        identb = sbt(es0, "identb", [128, 128], BF16)
        cmb = sbt(es0, "cmb", [128, 128], BF16)
        nmx = sbt(es0, "nmx", [128, 32], F32)
        lamt = sbt(es0, "lamt", [128, 4], F32)
        dma(cst[:], cst_d, w=["cst"])
        identf = cst[:, 0:128]
        trif = cst[:, 128:256]
        negmf = cst[:, 256:384]
        cmf = cst[:, 384:512]
        iotaf = cst[:, 512:640]
        onesf = cst[:, 640:768]
        a16f = cst[:, 768:784]
        i16f = cst[:, 784:800]
        cosf = cst[:, 800:800 + NT * 8]
        sinf = cst[:, 800 + NT * 8:800 + 2 * NT * 8]
        V(lambda e: e.tensor_copy(out=identb[:], in_=identf), r=["cst"], w=["identb"])
        V(lambda e: e.tensor_copy(out=cmb[:], in_=cmf), r=["cst"], w=["cmb"])

        with ExitStack() as ph:
            uf = [sbt(ph, f"uf{i}", [128, 1024], F32) for i in range(2)]
            ub = [sbt(ph, f"ub{i}", [128, 1024], BF16) for i in range(2)]
            uts = [sbt(ph, f"uts{i}", [128, 1024], BF16) for i in range(2)]
            vf = [sbt(ph, f"vf{i}", [128, 1024], F32) for i in range(2)]
            vb = [sbt(ph, f"vb{i}", [128, 1024], BF16) for i in range(2)]
            ptb = [pst(ph, f"ptbA{i}", [128, 1024], BF16) for i in range(2)]
            for i1 in range(128):
                s = i1 % 2
                dma(uf[s][:], peer_u[i1 * 128:(i1 + 1) * 128, :], w=[("uf", s)])
                dma(vf[s][:], peer_v[i1 * 128:(i1 + 1) * 128, :], w=[("vf", s)])
                V(lambda e: e.tensor_copy(out=ub[s][:], in_=uf[s][:]), r=[("uf", s)], w=[("ub", s)])
                for kc in range(8):
                    P(lambda e: e.transpose(out=ptb[s][:, kc * 128:(kc + 1) * 128], in_=ub[s][:, kc * 128:(kc + 1) * 128], identity=identb[:]),
                      r=[("ub", s), "identb"], w=[("ptbA", s)])
                A(lambda e: e.activation(out=uts[s][:], in_=ptb[s][:], func=AF.Copy), r=[("ptbA", s)], w=[("uts", s)])
                dma(UT[i1 // 2][:, (i1 % 2) * 1024:(i1 % 2 + 1) * 1024], uts[s][:], r=[("uts", s)], w=["UT"])
                G(lambda e: e.tensor_copy(out=vb[s][:], in_=vf[s][:]), r=[("vf", s)], w=[("vb", s)])
                dma(VB[i1 // 2][:, (i1 % 2) * 1024:(i1 % 2 + 1) * 1024], vb[s][:], r=[("vb", s)], w=["VB"])

            condT = sbt(ph, "condT", [128, 8, NB], F32)
            adab = sbt(ph, "adab", [1, 6 * D], F32)
            modrow = sbt(ph, "modrow", [1, NB, 6 * D], F32)
            wada = [sbt(ph, f"wada{i}", [128, 8, 512], F32) for i in range(2)]
            psm = [pst(ph, f"psm{i}", [128, 512], F32) for i in range(2)]
            for b in range(NB):
                dma(condT[:, :, b], c_in[b].rearrange("(kc p) -> p kc", p=128), w=["condT"], allow_slow_non_contiguous=True)
            dma(adab[:], ada_b, w=["adab"])
            A(lambda e: e.activation(out=condT[:], in_=condT[:], func=AF.Silu), r=["condT"], w=["condT"])
            adaw_v = ada_w.rearrange("(kc p) n -> p kc n", p=128)
            for ncx in range(12):
                s = ncx % 2
                dma(wada[s][:], adaw_v[:, :, ncx * 512:(ncx + 1) * 512], w=[("wada", s)])
                for b in range(NB):
                    pb = (ncx * NB + b) % 2
                    for kc in range(8):
                        P(lambda e: e.matmul(psm[pb][0:1, :], lhsT=condT[:, kc, b:b + 1], rhs=wada[s][:, kc, :], start=(kc == 0), stop=(kc == 7)),
                          r=["condT", ("wada", s)], w=[("psm", pb)])
                    V(lambda e: e.tensor_tensor(out=modrow[0:1, b, ncx * 512:(ncx + 1) * 512], in0=psm[pb][0:1, :], in1=adab[0:1, ncx * 512:(ncx + 1) * 512], op=ALU.add),
                      r=[("psm", pb), "adab"], w=["modrow"])
            for b in range(NB):
                dma(MOD[b:b + 1, :], modrow[0:1, b, :], r=["modrow"], w=["MOD"])

            lq = sbt(ph, "lq", [1, 4, 64], F32)
            lsc = sbt(ph, "lsc", [1, 8], F32)
            dma(lq[0:1, 0, :], lam_q1, w=["lq0"])
            dma(lq[0:1, 1, :], lam_k1, w=["lq1"])
            dma(lq[0:1, 2, :], lam_q2, w=["lq2"])
            dma(lq[0:1, 3, :], lam_k2, w=["lq3"])
            V(lambda e: e.tensor_tensor(out=lq[0:1, 0, :], in0=lq[0:1, 0, :], in1=lq[0:1, 1, :], op=ALU.mult), r=["lq0", "lq1"], w=["lq0"])
            V(lambda e: e.tensor_tensor(out=lq[0:1, 2, :], in0=lq[0:1, 2, :], in1=lq[0:1, 3, :], op=ALU.mult), r=["lq2", "lq3"], w=["lq2"])
            V(lambda e: e.reduce_sum(out=lsc[0:1, 0:1], in_=lq[0:1, 0, :], axis=AX.X), r=["lq0"], w=["lsc0"])
            V(lambda e: e.reduce_sum(out=lsc[0:1, 1:2], in_=lq[0:1, 2, :], axis=AX.X), r=["lq2"], w=["lsc1"])
            A(lambda e: e.activation(out=lsc[0:1, 2:4], in_=lsc[0:1, 0:2], func=AF.Exp), r=["lsc0", "lsc1"], w=["lsc2"])
            lam_init = 0.8 - 0.6 * math.exp(0.0)
            V(lambda e: e.tensor_tensor(out=lsc[0:1, 4:5], in0=lsc[0:1, 3:4], in1=lsc[0:1, 2:3], op=ALU.subtract), r=["lsc2"], w=["lsc4"])
            V(lambda e: e.tensor_scalar(out=lsc[0:1, 5:6], in0=lsc[0:1, 4:5], scalar1=-lam_init, scalar2=None, op0=ALU.add), r=["lsc4"], w=["lsc5"])
            P(lambda e: e.matmul(psm[0][:, 0:1], lhsT=onesf[0:1, :], rhs=lsc[0:1, 5:6], start=True, stop=True), r=["lsc5", "cst", ("psm", 0)], w=[("psm", 0)])
            V(lambda e: e.tensor_copy(out=lamt[:, 0:1], in_=psm[0][:, 0:1]), r=[("psm", 0)], w=["lamt"])
            k.barrier()

        win_v = w_in.rearrange("(kc p) n -> p kc n", p=128)
        for b in range(NB):
            with ExitStack() as ph:
                hT = sbt(ph, "hT", [128, 8, S], BF16)
                A1 = sbt(ph, "A1", [128, D], F32)
                B1 = sbt(ph, "B1", [128, D], F32)
                w1b = sbt(ph, "w1b", [128, D], F32)
                xt = [sbt(ph, f"xt{i}", [128, D], F32) for i in range(2)]
                hxs = [sbt(ph, f"hx{i}", [128, D], F32) for i in range(2)]
                hb = [sbt(ph, f"hb{i}", [128, D], BF16) for i in range(2)]
                junk = sbt(ph, "junkB", [128, D], BF16)
                st4s = [sbt(ph, f"st4{i}", [128, 8], F32) for i in range(2)]
                ptb = [pst(ph, f"ptbB{i}", [128, 1024], BF16) for i in range(2)]
                psb = [pst(ph, f"psB{i}", [128, 512], F32) for i in range(4)]
                dma(A1[:], MOD[b:b + 1, D:2 * D].partition_broadcast(128), r=["MOD"], w=["A1"])
                dma(B1[:], MOD[b:b + 1, 0:D].partition_broadcast(128), r=["MOD"], w=["B1"])
                dma(w1b[:], norm1_w.partition_broadcast(128), w=["w1b"])
                V(lambda e: e.scalar_tensor_tensor(out=A1[:], in0=A1[:], scalar=1.0, in1=w1b[:], op0=ALU.add, op1=ALU.mult), r=["A1", "w1b"], w=["A1"])
                V(lambda e: e.memset(nmx[:], 0.0), w=["nmx"])
                for tt in range(NT):
                    s = tt % 2
                    hx = hxs[s]
                    st4 = st4s[s]
                    dma(xt[s][:], x[b, tt * 128:(tt + 1) * 128, :], w=[("xt", s)])
                    A(lambda e: e.activation(out=junk[:], in_=xt[s][:], func=AF.Square, accum_out=st4[:, 0:1]), r=[("xt", s)], w=["junkB", ("st0", s)])
                    V(lambda e: e.tensor_scalar(out=st4[:, 1:2], in0=st4[:, 0:1], scalar1=1.0 / D, scalar2=EPS, op0=ALU.mult, op1=ALU.add), r=[("st0", s)], w=[("st1", s)])
                    A(lambda e: e.activation(out=st4[:, 2:3], in_=st4[:, 1:2], func=AF.Sqrt), r=[("st1", s)], w=[("st2", s)])
                    V(lambda e: e.reciprocal(out=st4[:, 3:4], in_=st4[:, 2:3]), r=[("st2", s)], w=[("st3", s)])
                    V(lambda e: e.scalar_tensor_tensor(out=hx[:], in0=xt[s][:], scalar=st4[:, 3:4], in1=A1[:], op0=ALU.mult, op1=ALU.mult), r=[("xt", s), ("st3", s), "A1"], w=[("hx", s)])
                    G(lambda e: e.tensor_tensor(out=hb[s][:], in0=hx[:], in1=B1[:], op=ALU.add), r=[("hx", s), "B1"], w=[("hb", s)])
                    for kc in range(8):
                        P(lambda e: e.transpose(out=ptb[s][:, kc * 128:(kc + 1) * 128], in_=hb[s][:, kc * 128:(kc + 1) * 128], identity=identb[:]),
                          r=[("hb", s), "identb"], w=[("ptbB", s)])
                    A(lambda e: e.activation(out=hT[:, :, tt * 128:(tt + 1) * 128], in_=ptb[s][:].rearrange("p (k t) -> p k t", k=8), func=AF.Copy),
                      r=[("ptbB", s)], w=[("hT", tt)])

                wf = [sbt(ph, f"wf{i}", [128, 8, 512], F32) for i in range(2)]
                wb = [sbt(ph, f"wb{i}", [128, 8, 512], BF16) for i in range(2)]
                qfs = [sbt(ph, f"qf{i}", [128, 512], F32) for i in range(4)]
                sqjs = [sbt(ph, f"sqj{i}", [128, 512], F32) for i in range(2)]
                rts = [sbt(ph, f"rt{i}", [128, 4, 8, 8], F32) for i in range(4)]
                nsqs = [sbt(ph, f"nsq{i}", [128, 8], F32) for i in range(4)]
                qb = [sbt(ph, f"qb{i}", [128, 512], BF16) for i in range(4)]
                stage = [sbt(ph, f"stage{i}", [128, 4, 512], BF16) for i in range(2)]
                ob = [sbt(ph, f"ob{i}", [128, 512], BF16) for i in range(3)]
                xbuf = sbt(ph, "xbuf", [128, 4, 515], F32)
                caccs = [sbt(ph, f"cacc{i}", [128, 512], F32) for i in range(3)]
                csil = sbt(ph, "csil", [128, 512], F32)
                cstg = [sbt(ph, f"cstg{i}", [128, 512], BF16) for i in range(2)]
                cw = sbt(ph, "cw", [128, 8, 4], F32)
                cbi = sbt(ph, "cbi", [128, 8], F32)
                gbias = sbt(ph, "gbias", [128, 16], F32)
                gz = sbt(ph, "gz", [128, 16], F32)
                gt = [sbt(ph, f"gtB{i}", [128, 16], F32) for i in range(2)]
                for j in range(4):
                    dma(cw[:, :, j], conv_w[j].rearrange("(blk p) -> p blk", p=128), w=["cw"], allow_slow_non_contiguous=True)
                dma(cbi[:], conv_b.rearrange("o (blk p) -> p (o blk)", p=128), w=["cbi"], allow_slow_non_contiguous=True)
                dma(gbias[:, 0:8], ml_ib.partition_broadcast(128), w=["gbias0"])
                dma(gbias[:, 8:16], ml_fb.partition_broadcast(128), w=["gbias1"])

                chunks = []
                for i in range(2):
                    chunks.append((i * 512, 512, "qa", i))
                for i in range(2):
                    chunks.append((1024 + i * 512, 512, "ka", i))
                for i in range(2):
                    chunks.append((2048 + i * 512, 512, "va", i))
                chunks.append((3072, 512, "qm", 0))
                chunks.append((3584, 512, "km", 0))
                for i in range(2):
                    chunks.append((4096 + i * 512, 512, "vm", i))
                for i in range(2):
                    chunks.append((5120 + i * 512, 512, "om", i))
                chunks.append((6144, 16, "gate", 0))
                for i in range(2):
                    chunks.append((6160 + i * 512, 512, "ga", i))
                for i in range(2):
                    chunks.append((7184 + i * 512, 512, "gm", i))

                pcount = 0
                ocount = 0
                qcount = 0
                for ci, (c0, ncol, kind, idx) in enumerate(chunks):
                    s = ci % 2
                    dma(wf[s][:, :, 0:ncol], win_v[:, :, c0:c0 + ncol], w=[("wf", s)])
                    G(lambda e: e.tensor_copy(out=wb[s][:, :, 0:ncol], in_=wf[s][:, :, 0:ncol]), r=[("wf", s)], w=[("wb", s)])
                    if kind in ("qm", "km"):
                        sc_ = 1.0 if kind == "qm" else 0.125
                        dst = QMT if kind == "qm" else KMT
                        boff = 0 if kind == "qm" else 4
                        for cb in range(4):
                            V(lambda e: e.memset(xbuf[:, cb, 0:3], 0.0), w=[("xbuf", cb)])
                        for tg in range(NG):
                            for cb in range(4):
                                pb = pcount % 4
                                pcount += 1
                                for kc in range(8):
                                    P(lambda e: e.matmul(psb[pb][:], lhsT=wb[s][:, kc, cb * 128:(cb + 1) * 128], rhs=hT[:, kc, tg * 512:(tg + 1) * 512], start=(kc == 0), stop=(kc == 7)),
                                      r=[("wb", s)] + [("hT", t_) for t_ in range(tg * 4, tg * 4 + 4)], w=[("psB", pb)])
                                if tg > 0:
                                    V(lambda e: e.tensor_copy(out=xbuf[:, cb, 0:3], in_=xbuf[:, cb, 512:515]), r=[("xbuf", cb)], w=[("xbuf", cb)])
                                A(lambda e: e.activation(out=xbuf[:, cb, 3:515], in_=psb[pb][:], func=AF.Copy), r=[("psB", pb)], w=[("xbuf", cb)])
                                ca = caccs[qcount % 3]
                                kca = ("cacc", qcount % 3)
                                A(lambda e: e.activation(out=ca[:], in_=xbuf[:, cb, 0:512], func=AF.Identity, scale=cw[:, boff + cb, 0:1], bias=cbi[:, boff + cb:boff + cb + 1]), r=[("xbuf", cb), "cw", "cbi"], w=[kca])
                                V(lambda e: e.scalar_tensor_tensor(out=ca[:], in0=xbuf[:, cb, 1:513], scalar=cw[:, boff + cb, 1:2], in1=ca[:], op0=ALU.mult, op1=ALU.add), r=[("xbuf", cb), "cw", kca], w=[kca])
                                V(lambda e: e.scalar_tensor_tensor(out=ca[:], in0=xbuf[:, cb, 2:514], scalar=cw[:, boff + cb, 2:3], in1=ca[:], op0=ALU.mult, op1=ALU.add), r=[("xbuf", cb), "cw", kca], w=[kca])
                                V(lambda e: e.scalar_tensor_tensor(out=ca[:], in0=xbuf[:, cb, 3:515], scalar=cw[:, boff + cb, 3:4], in1=ca[:], op0=ALU.mult, op1=ALU.add), r=[("xbuf", cb), "cw", kca], w=[kca])
                                cs = qcount % 2
                                qcount += 1
                                if kind == "qm":
                                    A(lambda e: e.activation(out=cstg[cs][:], in_=ca[:], func=AF.Silu), r=[kca], w=[("cstg", cs)])
                                else:
                                    A(lambda e: e.activation(out=csil[:], in_=ca[:], func=AF.Silu), r=[kca], w=["csil"])
                                    G(lambda e: e.tensor_scalar(out=cstg[cs][:], in0=csil[:], scalar1=sc_, scalar2=None, op0=ALU.mult), r=["csil"], w=[("cstg", cs)])
                                dma(dst[b, cb, :, tg * 512:(tg + 1) * 512], cstg[cs][:], r=[("cstg", cs)], w=["QKMT"])
                        continue
                    for tt in range(NT):
                        pb = pcount % 4
                        pcount += 1
                        for kc in range(8):
                            P(lambda e: e.matmul(psb[pb][:, 0:ncol], lhsT=hT[:, kc, tt * 128:(tt + 1) * 128], rhs=wb[s][:, kc, 0:ncol], start=(kc == 0), stop=(kc == 7)),
                              r=[("wb", s), ("hT", tt)], w=[("psB", pb)])
                        if kind in ("qa", "ka"):
                            z = tt % 4
                            z2 = tt % 2
                            qf, sqj, rt, nsq = qfs[z], sqjs[z2], rts[z], nsqs[z]
                            kq = ("qf", z)
                            A(lambda e: e.activation(out=qf[:], in_=psb[pb][:], func=AF.Copy), r=[("psB", pb)], w=[kq])
                            qv = qf[:].rearrange("p (g d) -> p g d", g=8)
                            x1 = qv[:, :, 0:8]
                            x2 = qv[:, :, 8:16]
                            cosb = cosf[:, tt * 8:(tt + 1) * 8].unsqueeze(1).to_broadcast([128, 8, 8])
                            sinb = sinf[:, tt * 8:(tt + 1) * 8].unsqueeze(1).to_broadcast([128, 8, 8])
                            V(lambda e: e.tensor_tensor(out=rt[:, 0], in0=x1, in1=cosb, op=ALU.mult), r=[kq, "cst"], w=[("rt0", z)])
                            V(lambda e: e.tensor_tensor(out=rt[:, 1], in0=x2, in1=sinb, op=ALU.mult), r=[kq, "cst"], w=[("rt1", z)])
                            V(lambda e: e.tensor_tensor(out=rt[:, 2], in0=x2, in1=cosb, op=ALU.mult), r=[kq, "cst"], w=[("rt2", z)])
                            V(lambda e: e.tensor_tensor(out=rt[:, 3], in0=x1, in1=sinb, op=ALU.mult), r=[kq, "cst"], w=[("rt3", z)])
                            V(lambda e: e.tensor_tensor(out=x1, in0=rt[:, 0], in1=rt[:, 1], op=ALU.subtract), r=[("rt0", z), ("rt1", z), ("rt2", z), ("rt3", z)], w=[kq])
                            V(lambda e: e.tensor_tensor(out=x2, in0=rt[:, 2], in1=rt[:, 3], op=ALU.add), r=[("rt2", z), ("rt3", z)], w=[kq])
                            G(lambda e: e.tensor_tensor(out=sqj[:], in0=qf[:], in1=qf[:], op=ALU.mult), r=[kq], w=[("sqj", z2)])
                            V(lambda e: e.reduce_sum(out=nsq[:], in_=sqj[:].rearrange("p (g d) -> p g d", g=8), axis=AX.X), r=[("sqj", z2)], w=[("nsq", z)])
                            ncol0 = (0 if kind == "qa" else 16) + idx * 8
                            V(lambda e: e.tensor_tensor(out=nmx[:, ncol0:ncol0 + 8], in0=nmx[:, ncol0:ncol0 + 8], in1=nsq[:], op=ALU.max), r=[("nsq", z), "nmx"], w=["nmx"])
                            qs = qcount % 4
                            qcount += 1
                            A(lambda e: e.activation(out=qb[qs][:], in_=qf[:], func=AF.Copy, scale=(0.125 if kind == "qa" else 1.0)), r=[kq], w=[("qb", qs)])
                            ps_ = tt % 2
                            for hd in range(4):
                                P(lambda e: e.transpose(out=ptb[ps_][:, hd * 128:(hd + 1) * 128], in_=qb[qs][:, hd * 128:(hd + 1) * 128], identity=identb[:]),
                                  r=[("qb", qs), "identb"], w=[("ptbB", ps_)])
                            sg = (tt // 4) % 2
                            V(lambda e: e.tensor_copy(out=stage[sg][:, :, (tt % 4) * 128:(tt % 4 + 1) * 128], in_=ptb[ps_][:, 0:512].rearrange("p (h t) -> p h t", h=4)),
                              r=[("ptbB", ps_)], w=[("stage", sg)])
                            if tt % 4 == 3:
                                dstT = QT if kind == "qa" else KT
                                t0 = (tt // 4) * 512
                                dma(dstT[b, idx * 4:(idx + 1) * 4, :, t0:t0 + 512].rearrange("h p t -> p h t"), stage[sg][:], r=[("stage", sg)], w=["QKT"])
                        elif kind in ("va", "vm"):
                            os_ = ocount % 3
                            ocount += 1
                            A(lambda e: e.activation(out=ob[os_][:], in_=psb[pb][:], func=AF.Copy), r=[("psB", pb)], w=[("ob", os_)])
                            dstv = VA if kind == "va" else VM
                            dma(dstv[b, tt * 128:(tt + 1) * 128, idx * 512:(idx + 1) * 512], ob[os_][:], r=[("ob", os_)], w=["VAVM"])
                        elif kind in ("om", "ga", "gm"):
                            os_ = ocount % 3
                            ocount += 1
                            A(lambda e: e.activation(out=ob[os_][:], in_=psb[pb][:], func=AF.Sigmoid), r=[("psB", pb)], w=[("ob", os_)])
                            dsts = {"om": OMS, "ga": GAS, "gm": GMS}[kind]
                            dma(dsts[b, tt * 128:(tt + 1) * 128, idx * 512:(idx + 1) * 512], ob[os_][:], r=[("ob", os_)], w=["SIGS"])
                        else:
                            gs = tt % 2
                            V(lambda e: e.tensor_tensor(out=gz[:], in0=psb[pb][:, 0:16], in1=gbias[:], op=ALU.add), r=[("psB", pb), "gbias0", "gbias1"], w=["gz"])
                            A(lambda e: e.activation(out=gz[:, 8:16], in_=gz[:, 8:16], func=AF.Exp, scale=-1.0), r=["gz"], w=["gz"])
                            A(lambda e: e.activation(out=gz[:, 8:16], in_=gz[:, 8:16], func=AF.Ln, bias=1.0, scale=1.0), r=["gz"], w=["gz"])
                            V(lambda e: e.tensor_copy(out=gt[gs][:, 0:8], in_=gz[:, 0:8]), r=["gz"], w=[("gtB", gs)])
                            V(lambda e: e.tensor_scalar(out=gt[gs][:, 8:16], in0=gz[:, 8:16], scalar1=-1.0, scalar2=None, op0=ALU.mult), r=["gz", ("gtB", gs)], w=[("gtB", gs)])
                            dma(GATES[b, tt * 128:(tt + 1) * 128, :], gt[gs][:], r=[("gtB", gs)], w=["GATES"])
                k.barrier()

            with ExitStack() as ph:
                qmt = sbt(ph, "qmt", [128, 4, S], BF16)
                kmt = sbt(ph, "kmt", [128, 4, S], BF16)
                Cst = sbt(ph, "Cst", [128, 4, 129], F32)
                Cbf = sbt(ph, "Cbf", [128, 4, 129], BF16)
                mlw = sbt(ph, "mlw", [128, D], F32)
                gtl = [sbt(ph, f"gtl{i}", [128, 16], F32) for i in range(2)]
                vmt = [sbt(ph, f"vmt{i}", [128, 8, 129], BF16) for i in range(2)]
                omt = [sbt(ph, f"omt{i}", [128, D], BF16) for i in range(2)]
                gmt = [sbt(ph, f"gmt{i}", [128, D], BF16) for i in range(2)]
                sms = [sbt(ph, f"sm{i}", [128, 8, 8], F32) for i in range(2)]
                Kws = [sbt(ph, f"Kw{i}", [128, 4, 128], BF16) for i in range(2)]
                PT = [sbt(ph, f"PT{i}", [128, 128], BF16) for i in range(2)]
                hn = sbt(ph, "hn", [128, 8, 128], F32)
                hj = sbt(ph, "hj", [128, 8, 128], F32)
                n8 = sbt(ph, "n8", [128, 8, 8], F32)
                ymb = [sbt(ph, f"ymb{i}", [128, D], BF16) for i in range(2)]
                psGK = pst(ph, "psGK", [128, 1024], BF16)
                psGf = psGK[:, 512:1024].bitcast(F32)
                psS = [pst(ph, f"psS{i}", [128, 512], F32) for i in range(2)]
                psTot = [pst(ph, f"psTot{i}", [128, 512], F32) for i in range(3)]
                psC = [pst(ph, f"psC{i}", [128, 512], F32) for i in range(2)]
                dma(qmt[:], QMT[b].rearrange("c p t -> p c t"), w=["qmt"])
                dma(kmt[:], KMT[b].rearrange("c p t -> p c t"), w=["kmt"])
                dma(mlw[:], ml_nw.partition_broadcast(128), w=["mlw"])
                V(lambda e: e.memset(Cst[:], 0.0), w=[("Cst", h) for h in range(8)])
                V(lambda e: e.memset(Cbf[:], 0.0), w=[("Cbf", h) for h in range(8)])
                for i in range(2):
                    V(lambda e: e.memset(vmt[i][:, :, 128:129], 1.0), w=[("vmt", i)])
                for tt in range(NT):
                    s = tt % 2
                    sm = sms[s]
                    Kw = Kws[s]
                    tsl = slice(tt * 128, (tt + 1) * 128)
                    dma(gtl[s][:], GATES[b, tsl, :], w=[("gtl", s)])
                    dma(vmt[s][:, :, 0:128], VM[b, tsl, :].rearrange("p (h e) -> p h e", h=8), w=[("vmt", s)])
                    dma(omt[s][:], OMS[b, tsl, :], w=[("omt", s)])
                    dma(gmt[s][:], GMS[b, tsl, :], w=[("gmt", s)])
                    lf = gtl[s][:, 8:16]
                    ig = gtl[s][:, 0:8]
                    P(lambda e: e.matmul(psGf[:, 0:8], lhsT=trif, rhs=lf, start=True, stop=True), r=[("gtl", s), "cst"], w=["psGK"])
                    P(lambda e: e.matmul(psGf[:, 8:16], lhsT=onesf, rhs=lf, start=True, stop=True), r=[("gtl", s), "cst"], w=["psGK"])
                    V(lambda e: e.tensor_copy(out=sm[:, 0:2, :], in_=psGf[:, 0:16].rearrange("p (a h) -> p a h", a=2)), r=["psGK"], w=[("sm01", s)])
                    V(lambda e: e.tensor_tensor(out=sm[:, 2, :], in0=ig, in1=sm[:, 0, :], op=ALU.subtract), r=[("gtl", s), ("sm01", s)], w=[("sm2", s)])
                    V(lambda e: e.tensor_tensor(out=sm[:, 6, :], in0=sm[:, 1, :], in1=sm[:, 2, :], op=ALU.add), r=[("sm01", s), ("sm2", s)], w=[("sm6", s)])
                    A(lambda e: e.activation(out=sm[:, 3, :], in_=sm[:, 2, :], func=AF.Exp), r=[("sm2", s)], w=[("sm3", s)])
                    A(lambda e: e.activation(out=sm[:, 4, :], in_=sm[:, 6, :], func=AF.Exp), r=[("sm6", s)], w=[("sm4", s)])
                    A(lambda e: e.activation(out=sm[:, 5, :], in_=sm[:, 1, :], func=AF.Exp), r=[("sm01", s)], w=[("sm5", s)])
                    A(lambda e: e.activation(out=sm[:, 7, :], in_=sm[:, 0, :], func=AF.Exp, scale=-1.0), r=[("sm01", s)], w=[("sm7", s)])
                    for cb in range(4):
                        P(lambda e: e.transpose(out=psGK[:, cb * 128:(cb + 1) * 128], in_=kmt[:, cb, tsl], identity=identb[:]), r=["kmt", "identb"], w=["psGK"])
                    V(lambda e: e.tensor_tensor(out=Kw[:].rearrange("p c (h d) -> p (c h) d", h=2), in0=psGK[:, 0:512].rearrange("p (g d) -> p g d", g=8),
                                                in1=sm[:, 4, :].unsqueeze(2).to_broadcast([128, 8, 64]), op=ALU.mult), r=["psGK", ("sm4", s)], w=[("Kw", s)])
                    started = [False, False, False]
                    for h in range(8):
                        cb, r0 = h // 2, (h % 2) * 64
                        hs = h % 2
                        bank, slot = h // 3, h % 3
                        tcol = slice(slot * 129, (slot + 1) * 129)
                        P(lambda e: e.matmul(psS[hs][:, 0:128], lhsT=kmt[r0:r0 + 64, cb, tsl], rhs=qmt[r0:r0 + 64, cb, tsl], start=True, stop=True), r=["kmt", "qmt"], w=[("psS", hs)])
                        V(lambda e: e.scalar_tensor_tensor(out=PT[hs][:], in0=psS[hs][:, 0:128], scalar=sm[:, 3, h:h + 1], in1=cmf, op0=ALU.mult, op1=ALU.mult),
                          r=[("psS", hs), ("sm3", s), "cst"], w=[("PT", hs)])
                        st_ = not started[bank]
                        started[bank] = True
                        P(lambda e: e.matmul(psTot[bank][:, tcol], lhsT=PT[hs][:], rhs=vmt[s][:, h, :], start=st_, stop=False, skip_group_check=True), r=[("PT", hs), ("vmt", s)], w=[("psTot", bank)])
                        P(lambda e: e.matmul(psTot[bank][:, tcol], lhsT=qmt[r0:r0 + 64, cb, tsl], rhs=Cbf[r0:r0 + 64, cb, :], start=False, stop=True, skip_group_check=True), r=["qmt", ("Cbf", h)], w=[("psTot", bank)])
                        P(lambda e: e.matmul(psC[hs][:, 0:129], lhsT=Kw[:, cb, :], rhs=vmt[s][:, h, :], start=True, stop=True), r=[("Kw", s), ("vmt", s)], w=[("psC", hs)])
                        V(lambda e: e.scalar_tensor_tensor(out=Cst[r0:r0 + 64, cb, :], in0=Cst[r0:r0 + 64, cb, :], scalar=sm[r0:r0 + 64, 5, h:h + 1], in1=psC[hs][r0:r0 + 64, 0:129], op0=ALU.mult, op1=ALU.add),
                          r=[("psC", hs), ("sm5", s), ("Cst", h)], w=[("Cst", h)])
                        G(lambda e: e.tensor_copy(out=Cbf[r0:r0 + 64, cb, :], in_=Cst[r0:r0 + 64, cb, :]), r=[("Cst", h)], w=[("Cbf", h)])
                    for bank in range(3):
                        nh_ = 3 if bank < 2 else 2
                        V(lambda e: e.tensor_copy(out=n8[:, 0, bank * 3:bank * 3 + nh_], in_=psTot[bank][:, 0:nh_ * 129].rearrange("p (s e) -> p s e", e=129)[:, :, 128]), r=[("psTot", bank)], w=[("n80", bank)])
                    k80 = [("n80", i) for i in range(3)]
                    V(lambda e: e.scalar_tensor_tensor(out=n8[:, 1, :], in0=n8[:, 0, :], scalar=-1.0, in1=n8[:, 0, :], op0=ALU.mult, op1=ALU.max), r=k80, w=["n81"])
                    V(lambda e: e.tensor_tensor(out=n8[:, 2, :], in0=n8[:, 1, :], in1=sm[:, 7, :], op=ALU.max), r=["n81", ("sm7", s)], w=["n82"])
                    V(lambda e: e.reciprocal(out=n8[:, 3, :], in_=n8[:, 2, :]), r=["n82"], w=["n83"])
                    for bank in range(3):
                        nh_ = 3 if bank < 2 else 2
                        V(lambda e: e.tensor_tensor(out=hn[:, bank * 3:bank * 3 + nh_, :], in0=psTot[bank][:, 0:nh_ * 129].rearrange("p (s e) -> p s e", e=129)[:, :, 0:128],
                                                    in1=n8[:, 3, bank * 3:bank * 3 + nh_].unsqueeze(2).to_broadcast([128, nh_, 128]), op=ALU.mult), r=[("psTot", bank), "n83"], w=[("hn", bank)])
                    khn = [("hn", i) for i in range(3)]
                    G(lambda e: e.tensor_tensor(out=hj[:], in0=hn[:], in1=hn[:], op=ALU.mult), r=khn, w=["hj"])
                    V(lambda e: e.reduce_sum(out=n8[:, 4, :], in_=hj[:], axis=AX.X), r=["hj"], w=["n84"])
                    V(lambda e: e.tensor_scalar(out=n8[:, 5, :], in0=n8[:, 4, :], scalar1=1.0 / 128, scalar2=EPS, op0=ALU.mult, op1=ALU.add), r=["n84"], w=["n85"])
                    A(lambda e: e.activation(out=n8[:, 6, :], in_=n8[:, 5, :], func=AF.Sqrt), r=["n85"], w=["n86"])
                    V(lambda e: e.reciprocal(out=n8[:, 7, :], in_=n8[:, 6, :]), r=["n86"], w=["n87"])
                    V(lambda e: e.tensor_tensor(out=hj[:], in0=hn[:], in1=n8[:, 7, :].unsqueeze(2).to_broadcast([128, 8, 128]), op=ALU.mult), r=khn + ["n87", "hj"], w=["hj"])
                    hjf = hj[:].rearrange("p h e -> p (h e)")
                    G(lambda e: e.tensor_tensor(out=hjf, in0=hjf, in1=mlw[:], op=ALU.mult), r=["hj", "mlw"], w=["hj"])
                    V(lambda e: e.tensor_tensor(out=hjf, in0=hjf, in1=omt[s][:], op=ALU.mult), r=["hj", ("omt", s)], w=["hj"])
                    G(lambda e: e.tensor_tensor(out=ymb[s][:], in0=hjf, in1=gmt[s][:], op=ALU.mult), r=["hj", ("gmt", s)], w=[("ymb", s)])
                    dma(YM[b, tsl, :], ymb[s][:], r=[("ymb", s)], w=["YM"])
                k.barrier()

            with ExitStack() as ph:
                qts = [sbt(ph, f"qts{i}", [128, S], BF16) for i in range(2)]
                kts = [sbt(ph, f"kts{i}", [128, S], BF16) for i in range(2)]
                vh = [sbt(ph, f"vh{i}", [128, NT, 129], BF16) for i in range(2)]
                negMb = sbt(ph, "negMb", [128, 16], F32)
                nw = sbt(ph, "nw", [16, 8], F32)
                dg = sbt(ph, "dg", [16, 16], F32)
                swb = sbt(ph, "swb", [128, 128], F32)
                PTa = [sbt(ph, f"PTa{i}", [128, 2, 512], BF16) for i in range(2)]
                negMh = sbt(ph, "negMh", [128, 8], F32)
                gat = [sbt(ph, f"gat{i}", [128, 128], BF16) for i in range(2)]
                ymt = [sbt(ph, f"ymt{i}", [128, 128], BF16) for i in range(2)]
                mb = [sbt(ph, f"mb{i}", [128, 128], BF16) for i in range(2)]
                mstage = [sbt(ph, f"mstage{i}", [128, 512], BF16) for i in range(2)]
                psS = [pst(ph, f"psSa{i}", [128, 1024], F32) for i in range(2)]
                psA = [pst(ph, f"psA{i}", [128, 512], F32) for i in range(3)]
                psT = pst(ph, "psT", [128, 1024], BF16)
                P(lambda e: e.transpose(out=psA[0][0:16, 0:128], in_=nmx[:, 0:16], identity=identf), r=["nmx", "cst"], w=[("psA", 0)])
                P(lambda e: e.transpose(out=psA[0][0:16, 128:256], in_=nmx[:, 16:32], identity=identf), r=["nmx", "cst"], w=[("psA", 0)])
                V(lambda e: e.reduce_max(out=nw[:, 0:2], in_=psA[0][0:16, 0:256].rearrange("p (a t) -> p a t", a=2), axis=AX.X), r=[("psA", 0)], w=["nw0"])
                V(lambda e: e.tensor_tensor(out=nw[:, 2:3], in0=nw[:, 0:1], in1=nw[:, 1:2], op=ALU.mult), r=["nw0"], w=["nw2"])
                A(lambda e: e.activation(out=nw[:, 3:4], in_=nw[:, 2:3], func=AF.Sqrt), r=["nw2"], w=["nw3"])
                V(lambda e: e.tensor_scalar(out=nw[:, 4:5], in0=nw[:, 3:4], scalar1=-0.125, scalar2=None, op0=ALU.mult), r=["nw3"], w=["nw4"])
                V(lambda e: e.tensor_scalar(out=dg[:], in0=identf[0:16, 0:16], scalar1=nw[:, 4:5], scalar2=None, op0=ALU.mult), r=["nw4", "cst"], w=["dg"])
                P(lambda e: e.matmul(psA[1][:, 0:16], lhsT=onesf[0:16, :], rhs=dg[:], start=True, stop=True), r=["dg", "cst"], w=[("psA", 1)])
                V(lambda e: e.tensor_copy(out=negMb[:], in_=psA[1][:, 0:16]), r=[("psA", 1)], w=["negMb"])
                nv2 = negMb[:].rearrange("p (h c) -> p h c", c=2)
                V(lambda e: e.tensor_tensor(out=negMh[:], in0=nv2[:, :, 0], in1=nv2[:, :, 1], op=ALU.min), r=["negMb"], w=["negMh"])
                dma(swb[:], subln_w.partition_broadcast(128), w=["swb"])
                V(lambda e: e.tensor_scalar(out=swb[:], in0=swb[:], scalar1=(1.0 - lam_init), scalar2=None, op0=ALU.mult), r=["swb"], w=["swb"])
                for i in range(2):
                    V(lambda e: e.memset(vh[i][:, :, 128:129], 1.0), w=[("vh", i)])
                fcount = 0
                accS = [sbt(ph, f"accS{i}", [128, 3, 387], F32) for i in range(2)]
                rrs = [sbt(ph, f"rrs{i}", [128, 8], F32) for i in range(2)]
                o1s = [sbt(ph, f"o1s{i}", [128, 128], F32) for i in range(2)]
                o2s = [sbt(ph, f"o2s{i}", [128, 128], F32) for i in range(2)]
                ojs = [sbt(ph, f"ojs{i}", [128, 128], F32) for i in range(2)]
                gcnt = 0
                def load_head(hd_):
                    s_ = hd_ % 2
                    dma(qts[s_][:], QT[b, hd_], w=[("qts", s_)])
                    dma(kts[s_][:], KT[b, hd_], w=[("kts", s_)])
                    for t8 in range(0, NT, 8):
                        te = min(NT, t8 + 8)
                        dma(vh[s_][:, t8:te, 0:128], VA[b].rearrange("(tt p) c -> p tt c", p=128)[:, t8:te, hd_ * 128:(hd_ + 1) * 128], w=[("vh", s_)])

                load_head(0)
                for hd in range(8):
                    s = hd % 2
                    if hd + 1 < 8:
                        load_head(hd + 1)
                    blocks = [(g, kt) for g in range(NG) for kt in range(4 * g + 4)]

                    def qk_exp(bi):
                        g, kt = blocks[bi]
                        bs = bi % 2
                        for c in range(2):
                            P(lambda e: e.matmul(psS[bs][:, c * 512:(c + 1) * 512], lhsT=kts[s][c * 64:(c + 1) * 64, kt * 128:(kt + 1) * 128], rhs=qts[s][c * 64:(c + 1) * 64, g * 512:(g + 1) * 512], start=True, stop=True),
                              r=[("kts", s), ("qts", s)], w=[("psSa", bs)])
                        A(lambda e: e.activation(out=PTa[bs][:].rearrange("p c q -> p (c q)"), in_=psS[bs][:], func=AF.Exp, bias=negMh[:, hd:hd + 1], scale=1.0),
                          r=[("psSa", bs), "negMh"], w=[("PTa", bs)])

                    qk_exp(0)
                    started = [False, False, False]
                    for bi, (g, kt) in enumerate(blocks):
                        bs = bi % 2
                        if bi + 1 < len(blocks):
                            qk_exp(bi + 1)
                        if kt == 0:
                            started = [False, False, False]
                        for qi in range(4):
                            qt_ = 4 * g + qi
                            if kt > qt_:
                                continue
                            for c in range(2):
                                if kt == qt_ and c == 0:
                                    V(lambda e: e.tensor_tensor(out=PTa[bs][:, :, qi * 128:(qi + 1) * 128], in0=PTa[bs][:, :, qi * 128:(qi + 1) * 128], in1=cmb[:].unsqueeze(1).to_broadcast([128, 2, 128]), op=ALU.mult),
                                      r=[("PTa", bs), "cmb"], w=[("PTa", bs)])
                                ai = c * 4 + qi
                                bank, slot = ai // 3, ai % 3
                                st_ = not started[bank]
                                started[bank] = True
                                P(lambda e: e.matmul(psA[bank][:, slot * 129:(slot + 1) * 129], lhsT=PTa[bs][:, c, qi * 128:(qi + 1) * 128], rhs=vh[s][:, kt, :], start=st_, stop=(kt == qt_), skip_group_check=True),
                                  r=[("PTa", bs), ("vh", s)], w=[("psA", bank)])
                        if kt != 4 * g + 3:
                            continue
                        gs_ = gcnt % 2
                        gcnt += 1
                        for bank in range(3):
                            ncol_ = 387 if bank < 2 else 258
                            A(lambda e: e.activation(out=accS[gs_][:, bank, 0:ncol_], in_=psA[bank][:, 0:ncol_], func=AF.Copy), r=[("psA", bank)], w=[("accS", gs_, bank)])
                        ms = g % 2
                        for qi in range(4):
                            qt_ = 4 * g + qi
                            tsl = slice(qt_ * 128, (qt_ + 1) * 128)
                            fs = fcount % 2
                            fcount += 1
                            rr, o1, o2, oj = rrs[fs], o1s[fs], o2s[fs], ojs[fs]
                            dma(gat[fs][:], GAS[b, tsl, hd * 128:(hd + 1) * 128], w=[("gat", fs)])
                            dma(ymt[fs][:], YM[b, tsl, hd * 128:(hd + 1) * 128], w=[("ymt", fs)])
                            a1 = accS[gs_][:, qi // 3, (qi % 3) * 129:(qi % 3 + 1) * 129]
                            a2i = 4 + qi
                            a2 = accS[gs_][:, a2i // 3, (a2i % 3) * 129:(a2i % 3 + 1) * 129]
                            rk = [("accS", gs_, qi // 3), ("accS", gs_, a2i // 3)]
                            V(lambda e: e.reciprocal(out=rr[:, 0:1], in_=a1[:, 128:129]), r=rk, w=[("rr0", fs)])
                            V(lambda e: e.reciprocal(out=rr[:, 1:2], in_=a2[:, 128:129]), r=rk, w=[("rr1", fs)])
                            V(lambda e: e.tensor_tensor(out=rr[:, 2:3], in0=rr[:, 1:2], in1=lamt[:, 0:1], op=ALU.mult), r=[("rr1", fs), "lamt"], w=[("rr2", fs)])
                            A(lambda e: e.activation(out=o1[:], in_=a1[:, 0:128], func=AF.Copy, scale=rr[:, 0:1]), r=rk + [("rr0", fs)], w=[("o1", fs)])
                            V(lambda e: e.scalar_tensor_tensor(out=o2[:], in0=a2[:, 0:128], scalar=rr[:, 2:3], in1=o1[:], op0=ALU.mult, op1=ALU.add), r=rk + [("rr2", fs), ("o1", fs)], w=[("o2", fs)])
                            A(lambda e: e.activation(out=oj[:], in_=o2[:], func=AF.Square, accum_out=rr[:, 3:4]), r=[("o2", fs)], w=[("oj", fs), ("rr3", fs)])
                            V(lambda e: e.tensor_scalar(out=rr[:, 4:5], in0=rr[:, 3:4], scalar1=1.0 / 128, scalar2=EPS, op0=ALU.mult, op1=ALU.add), r=[("rr3", fs)], w=[("rr4", fs)])
                            A(lambda e: e.activation(out=rr[:, 5:6], in_=rr[:, 4:5], func=AF.Sqrt), r=[("rr4", fs)], w=[("rr5", fs)])
                            V(lambda e: e.reciprocal(out=rr[:, 6:7], in_=rr[:, 5:6]), r=[("rr5", fs)], w=[("rr6", fs)])
                            V(lambda e: e.scalar_tensor_tensor(out=oj[:], in0=o2[:], scalar=rr[:, 6:7], in1=swb[:], op0=ALU.mult, op1=ALU.mult), r=[("o2", fs), ("rr6", fs), "swb", ("oj", fs)], w=[("oj", fs)])
                            G(lambda e: e.tensor_tensor(out=oj[:], in0=oj[:], in1=gat[fs][:], op=ALU.mult), r=[("oj", fs), ("gat", fs)], w=[("oj", fs)])
                            G(lambda e: e.tensor_tensor(out=mb[fs][:], in0=oj[:], in1=ymt[fs][:], op=ALU.add), r=[("oj", fs), ("ymt", fs)], w=[("mb", fs)])
                            P(lambda e: e.transpose(out=psT[:, qi * 128:(qi + 1) * 128], in_=mb[fs][:], identity=identb[:]), r=[("mb", fs), "identb"], w=["psT"])
                        V(lambda e: e.tensor_copy(out=mstage[ms][:], in_=psT[:, 0:512]), r=["psT"], w=[("mstage", ms)])
                        dma(MT[b, hd, :, g * 512:(g + 1) * 512], mstage[ms][:], r=[("mstage", ms)], w=["MT"])
                k.barrier()

        wq_v = peer_wq.rearrange("(kc p) n -> p kc n", p=128)
        wo_v = w_out.rearrange("(kc p) n -> p kc n", p=128)
        with ExitStack() as ph:
            wob = sbt(ph, "wob", [128, 8, D], BF16)
            wqb = sbt(ph, "wqb", [128, 8, 2048], BF16)
            keyT = sbt(ph, "keyT", [128, 16, 128], BF16)
            psT = [pst(ph, f"psTE{i}", [128, 1024], BF16) for i in range(2)]
            ph2 = ExitStack()
            wtmp = [sbt(ph2, f"wtmp{i}", [128, 8, 512], F32) for i in range(2)]
            ktmp = sbt(ph2, "ktmp", [128, 16, 128], F32)
            ktb = sbt(ph2, "ktb", [128, 16, 128], BF16)
            wi = 0
            for n0 in range(0, D, 512):
                s = wi % 2
                wi += 1
                dma(wtmp[s][:], wo_v[:, :, n0:n0 + 512], w=[("wtmp", s)])
                V(lambda e: e.tensor_copy(out=wob[:, :, n0:n0 + 512], in_=wtmp[s][:]), r=[("wtmp", s)], w=["wob"])
            for n0 in range(0, 2048, 512):
                s = wi % 2
                wi += 1
                dma(wtmp[s][:], wq_v[:, :, n0:n0 + 512], w=[("wtmp", s)])
                G(lambda e: e.tensor_copy(out=wqb[:, :, n0:n0 + 512], in_=wtmp[s][:]), r=[("wtmp", s)], w=["wqb"])
            dma(ktmp[:], peer_keys.rearrange("g n d -> n g d"), w=["ktmp"])
            V(lambda e: e.tensor_copy(out=ktb[:], in_=ktmp[:]), r=["ktmp"], w=["ktb"])
            for g8 in range(2):
                for j in range(8):
                    gi = g8 * 8 + j
                    P(lambda e: e.transpose(out=psT[g8][:, j * 128:(j + 1) * 128], in_=ktb[:, gi, :], identity=identb[:]), r=["ktb", "identb"], w=[("psTE", g8)])
                V(lambda e: e.tensor_copy(out=keyT[:, g8 * 8:(g8 + 1) * 8, :], in_=psT[g8][:].rearrange("p (g n) -> p g n", g=8)), r=[("psTE", g8)], w=["keyT"])
            k.barrier()
            ph2.close()
            gt1 = sbt(ph, "gt1", [128, D], F32)
            A2 = sbt(ph, "A2", [128, D], F32)
            B2 = sbt(ph, "B2", [128, D], F32)
            w2b = sbt(ph, "w2b", [128, D], F32)
            mt = [sbt(ph, f"mt{i}", [128, 8, 128], BF16) for i in range(2)]
            xt = [sbt(ph, f"xtE{i}", [128, D], F32) for i in range(2)]
            x1 = [sbt(ph, f"x1E{i}", [128, D], F32) for i in range(2)]
            junk = sbt(ph, "junkE", [128, D], BF16)
            hx = sbt(ph, "hxE", [128, D], F32)
            hb = sbt(ph, "hbE", [128, D], BF16)
            h2t = [sbt(ph, f"h2t{i}", [128, 8, 128], BF16) for i in range(2)]
            qpt = sbt(ph, "qpt", [128, 16, 128], BF16)
            scs = [sbt(ph, f"sc{i}", [128, 16, 128], F32) for i in range(2)]
            wk = sbt(ph, "wkE", [128, 16, 128], F32)
            v16 = sbt(ph, "v16", [128, 16, 16], F32)
            i16u = sbt(ph, "i16u", [128, 16, 16], U32)
            i16v = sbt(ph, "i16v", [128, 16, 16], F32)
            cand = sbt(ph, "cand", [128, 8, 256], F32)
            cwk = sbt(ph, "cwk", [128, 8, 256], F32)
            c16 = sbt(ph, "c16", [128, 8, 16], F32)
            p16u = sbt(ph, "p16u", [128, 8, 16], U32)
            p16 = sbt(ph, "p16", [128, 8, 16], F32)
            d1 = sbt(ph, "d1", [128, 8, 16, 16], F32)
            d2 = sbt(ph, "d2", [128, 8, 16, 16], F32)
            ar = sbt(ph, "ar", [128, 8, 16], F32)
            br = sbt(ph, "br", [128, 8, 16], F32)
            igt = [sbt(ph, f"igt{i}", [128, 3, 128], F32) for i in range(2)]
            st4 = sbt(ph, "st4E", [128, 16], F32)
            psO = [pst(ph, f"psO{i}", [128, 512], F32) for i in range(2)]
            psQ = [pst(ph, f"psQ{i}", [128, 512], F32) for i in range(2)]
            psX = [pst(ph, f"psX{i}", [128, 512], F32) for i in range(2)]
            dma(w2b[:], norm2_w.partition_broadcast(128), w=["w2b"])
            def e_front(b, tt):
                s = tt % 2
                tsl = slice(tt * 128, (tt + 1) * 128)
                if tt == 0:
                    dma(gt1[:], MOD[b:b + 1, 2 * D:3 * D].partition_broadcast(128), r=["MOD"], w=["gt1"])
                    dma(B2[:], MOD[b:b + 1, 3 * D:4 * D].partition_broadcast(128), r=["MOD"], w=["B2"])
                    dma(A2[:], MOD[b:b + 1, 4 * D:5 * D].partition_broadcast(128), r=["MOD"], w=["A2"])
                    V(lambda e: e.scalar_tensor_tensor(out=A2[:], in0=A2[:], scalar=1.0, in1=w2b[:], op0=ALU.add, op1=ALU.mult), r=["A2", "w2b"], w=["A2"])
                dma(mt[s][:], MT[b, :, :, tsl].rearrange("h p t -> p h t"), r=["MT"], w=[("mt", s)])
                dma(xt[s][:], x[b, tsl, :], w=[("xtE", s)])
                for nh in range(2):
                    for kc in range(8):
                        P(lambda e: e.matmul(psO[nh][:], lhsT=mt[s][:, kc, :], rhs=wob[:, kc, nh * 512:(nh + 1) * 512], start=(kc == 0), stop=(kc == 7)),
                          r=[("mt", s), "wob"], w=[("psO", nh)])
                    V(lambda e: e.tensor_tensor(out=x1[s][:, nh * 512:(nh + 1) * 512], in0=psO[nh][:], in1=gt1[:, nh * 512:(nh + 1) * 512], op=ALU.mult), r=[("psO", nh), "gt1"], w=[("x1E", s)])
                G(lambda e: e.tensor_tensor(out=x1[s][:], in0=x1[s][:], in1=xt[s][:], op=ALU.add), r=[("x1E", s), ("xtE", s)], w=[("x1E", s)])
                dma(X1[b, tsl, :], x1[s][:], r=[("x1E", s)], w=["X1"])
                A(lambda e: e.activation(out=junk[:], in_=x1[s][:], func=AF.Square, accum_out=st4[:, 0:1]), r=[("x1E", s)], w=["junkE", "sE0"])
                V(lambda e: e.tensor_scalar(out=st4[:, 1:2], in0=st4[:, 0:1], scalar1=1.0 / D, scalar2=EPS, op0=ALU.mult, op1=ALU.add), r=["sE0"], w=["sE1"])
                A(lambda e: e.activation(out=st4[:, 2:3], in_=st4[:, 1:2], func=AF.Sqrt), r=["sE1"], w=["sE2"])
                V(lambda e: e.reciprocal(out=st4[:, 3:4], in_=st4[:, 2:3]), r=["sE2"], w=["sE3"])
                V(lambda e: e.scalar_tensor_tensor(out=hx[:], in0=x1[s][:], scalar=st4[:, 3:4], in1=A2[:], op0=ALU.mult, op1=ALU.mult), r=[("x1E", s), "sE3", "A2"], w=["hxE"])
                G(lambda e: e.tensor_tensor(out=hb[:], in0=hx[:], in1=B2[:], op=ALU.add), r=["hxE", "B2"], w=["hbE"])
                for kc in range(8):
                    P(lambda e: e.transpose(out=psT[0][:, kc * 128:(kc + 1) * 128], in_=hb[:, kc * 128:(kc + 1) * 128], identity=identb[:]), r=["hbE", "identb"], w=[("psTE", 0)])
                A(lambda e: e.activation(out=h2t[s][:], in_=psT[0][:].rearrange("p (k t) -> p k t", k=8), func=AF.Copy), r=[("psTE", 0)], w=[("h2t", s)])
                dma(H2T[b, :, :, tsl].rearrange("k p t -> p k t"), h2t[s][:], r=[("h2t", s)], w=["H2T"])
                for gq in range(4):
                    pq = gq % 2
                    for j in range(4):
                        gi = gq * 4 + j
                        for kc in range(8):
                            P(lambda e: e.matmul(psQ[pq][:, j * 128:(j + 1) * 128], lhsT=wqb[:, kc, gi * 128:(gi + 1) * 128], rhs=h2t[s][:, kc, :], start=(kc == 0), stop=(kc == 7)),
                              r=["wqb", ("h2t", s)], w=[("psQ", pq)])
                    A(lambda e: e.activation(out=qpt[:, gq * 4:(gq + 1) * 4, :], in_=psQ[pq][:].rearrange("p (g t) -> p g t", g=4), func=AF.Copy), r=[("psQ", pq)], w=["qpt"])
                sc = scs[s]
                for gq in range(4):
                    px = gq % 2
                    for j in range(4):
                        gi = gq * 4 + j
                        P(lambda e: e.matmul(psX[px][:, j * 128:(j + 1) * 128], lhsT=qpt[:, gi, :], rhs=keyT[:, gi, :], start=True, stop=True), r=["qpt", "keyT"], w=[("psX", px)])
                    A(lambda e: e.activation(out=sc[:, gq * 4:(gq + 1) * 4, :], in_=psX[px][:].rearrange("p (g n) -> p g n", g=4), func=AF.Copy), r=[("psX", px)], w=[("sc", s, gq)])

            def e_back(b, tt):
                s = tt % 2
                tsl = slice(tt * 128, (tt + 1) * 128)
                sc = scs[s]
                for gi in range(16):
                    V(lambda e: e.max(out=v16[:, gi, 0:8], in_=sc[:, gi, :]), r=[("sc", s, gi // 4)], w=[("v16a", gi)])
                for gi in range(16):
                    V(lambda e: e.max_index(out=i16u[:, gi, 0:8], in_max=v16[:, gi, 0:8], in_values=sc[:, gi, :]), r=[("sc", s, gi // 4), ("v16a", gi)], w=[("i16a", gi)])
                for gi in range(16):
                    V(lambda e: e.match_replace(out=wk[:, gi, :], in_to_replace=v16[:, gi, 0:8], in_values=sc[:, gi, :], imm_value=-1e30), r=[("sc", s, gi // 4), ("v16a", gi)], w=[("wkE", gi)])
                for gi in range(16):
                    V(lambda e: e.max(out=v16[:, gi, 8:16], in_=wk[:, gi, :]), r=[("wkE", gi)], w=[("v16b", gi)])
                for gi in range(16):
                    V(lambda e: e.max_index(out=i16u[:, gi, 8:16], in_max=v16[:, gi, 8:16], in_values=wk[:, gi, :]), r=[("wkE", gi), ("v16b", gi)], w=[("i16b", gi)])
                allv = [("v16a", gi) for gi in range(16)] + [("v16b", gi) for gi in range(16)]
                alli = [("i16a", gi) for gi in range(16)] + [("i16b", gi) for gi in range(16)]
                V(lambda e: e.tensor_copy(out=i16v[:], in_=i16u[:]), r=alli, w=["i16v"])
                v16v = v16[:].rearrange("p (h q) k -> p h q k", q=2)
                i16vv = i16v[:].rearrange("p (h q) k -> p h q k", q=2)
                V(lambda e: e.tensor_tensor(out=cand[:].rearrange("p h (a b) -> p h a b", a=16), in0=v16v[:, :, 0, :].unsqueeze(3).to_broadcast([128, 8, 16, 16]),
                                            in1=v16v[:, :, 1, :].unsqueeze(2).to_broadcast([128, 8, 16, 16]), op=ALU.add), r=allv, w=["cand"])
                for h in range(8):
                    V(lambda e: e.max(out=c16[:, h, 0:8], in_=cand[:, h, :]), r=["cand"], w=[("c16a", h)])
                for h in range(8):
                    V(lambda e: e.max_index(out=p16u[:, h, 0:8], in_max=c16[:, h, 0:8], in_values=cand[:, h, :]), r=["cand", ("c16a", h)], w=[("p16a", h)])
                for h in range(8):
                    V(lambda e: e.match_replace(out=cwk[:, h, :], in_to_replace=c16[:, h, 0:8], in_values=cand[:, h, :], imm_value=-1e30), r=["cand", ("c16a", h)], w=[("cwk", h)])
                for h in range(8):
                    V(lambda e: e.max(out=c16[:, h, 8:16], in_=cwk[:, h, :]), r=[("cwk", h)], w=[("c16b", h)])
                for h in range(8):
                    V(lambda e: e.max_index(out=p16u[:, h, 8:16], in_max=c16[:, h, 8:16], in_values=cwk[:, h, :]), r=[("cwk", h), ("c16b", h)], w=[("p16b", h)])
                allc = [("c16a", h) for h in range(8)] + [("c16b", h) for h in range(8)]
                allp = [("p16a", h) for h in range(8)] + [("p16b", h) for h in range(8)]
                V(lambda e: e.tensor_copy(out=p16[:], in_=p16u[:]), r=allp, w=["p16"])
                ig0 = igt[s][:, 0, :].rearrange("p (h r) -> p h r", h=8)
                ig1 = igt[s][:, 1, :].rearrange("p (h r) -> p h r", h=8)
                HS = [slice(0, 4), slice(4, 8)]

                def bc4(ap3, axis):
                    return ap3.unsqueeze(axis).to_broadcast([128, 4, 16, 16])

                a16b = a16f.unsqueeze(1).unsqueeze(1).to_broadcast([128, 4, 16, 16])
                i16b = i16f.unsqueeze(1).unsqueeze(1).to_broadcast([128, 4, 16, 16])
                for z, hsl in enumerate(HS):
                    V(lambda e: e.tensor_tensor(out=d1[:, hsl], in0=bc4(p16[:, hsl, :], 3), in1=a16b, op=ALU.subtract), r=["p16", "cst"], w=[("d1", z)])
                for z, hsl in enumerate(HS):
                    V(lambda e: e.tensor_scalar(out=d2[:, hsl], in0=d1[:, hsl], scalar1=0.0, scalar2=None, op0=ALU.is_ge), r=[("d1", z)], w=[("d2", z)])
                for z, hsl in enumerate(HS):
                    V(lambda e: e.tensor_scalar(out=d1[:, hsl], in0=d1[:, hsl], scalar1=15.5, scalar2=None, op0=ALU.is_lt), r=[("d1", z), ("d2", z)], w=[("d1", z)])
                for z, hsl in enumerate(HS):
                    V(lambda e: e.tensor_tensor(out=d1[:, hsl], in0=d1[:, hsl], in1=d2[:, hsl], op=ALU.mult), r=[("d1", z), ("d2", z)], w=[("d1", z)])
                for z, hsl in enumerate(HS):
                    G(lambda e: e.tensor_tensor(out=d2[:, hsl], in0=d1[:, hsl], in1=a16b, op=ALU.mult), r=[("d1", z), "cst"], w=[("d2", z)])
                for z, hsl in enumerate(HS):
                    V(lambda e: e.reduce_sum(out=ar[:, hsl, :], in_=d2[:, hsl], axis=AX.X), r=[("d2", z)], w=[("ar", z)])
                for z, hsl in enumerate(HS):
                    G(lambda e: e.tensor_tensor(out=d2[:, hsl], in0=d1[:, hsl], in1=bc4(i16vv[:, hsl, 0, :], 2), op=ALU.mult), r=[("d1", z), "i16v", ("ar", z)], w=[("d2", z)])
                for z, hsl in enumerate(HS):
                    V(lambda e: e.reduce_sum(out=ig0[:, hsl, :], in_=d2[:, hsl], axis=AX.X), r=[("d2", z)], w=[("igt", s, z)])
                for z, hsl in enumerate(HS):
                    V(lambda e: e.tensor_tensor(out=br[:, hsl, :], in0=p16[:, hsl, :], in1=ar[:, hsl, :], op=ALU.subtract), r=["p16", ("ar", z)], w=[("br", z)])
                for z, hsl in enumerate(HS):
                    V(lambda e: e.tensor_tensor(out=d1[:, hsl], in0=bc4(br[:, hsl, :], 3), in1=i16b, op=ALU.is_equal), r=[("br", z), "cst", ("d2", z)], w=[("d1", z)])
                for z, hsl in enumerate(HS):
                    G(lambda e: e.tensor_tensor(out=d2[:, hsl], in0=d1[:, hsl], in1=bc4(i16vv[:, hsl, 1, :], 2), op=ALU.mult), r=[("d1", z), "i16v"], w=[("d2", z)])
                for z, hsl in enumerate(HS):
                    V(lambda e: e.reduce_sum(out=ig1[:, hsl, :], in_=d2[:, hsl], axis=AX.X), r=[("d2", z), ("igt", s, z)], w=[("igt", s, z)])
                V(lambda e: e.tensor_tensor(out=ar[:], in0=c16[:], in1=c16[:, :, 0:1].to_broadcast([128, 8, 16]), op=ALU.subtract), r=allc + [("br", 0), ("br", 1), ("ar", 0), ("ar", 1)], w=[("ar", 0), ("ar", 1)])
                A(lambda e: e.activation(out=ar[:], in_=ar[:], func=AF.Exp), r=[("ar", 0), ("ar", 1)], w=[("ar", 0), ("ar", 1)])
                V(lambda e: e.reduce_sum(out=st4[:, 8:16], in_=ar[:], axis=AX.X), r=[("ar", 0), ("ar", 1)], w=["sE8"])
                V(lambda e: e.reciprocal(out=st4[:, 8:16], in_=st4[:, 8:16]), r=["sE8"], w=["sE8"])
                V(lambda e: e.tensor_tensor(out=igt[s][:, 2, :].rearrange("p (h r) -> p h r", h=8), in0=ar[:], in1=st4[:, 8:16].unsqueeze(2).to_broadcast([128, 8, 16]), op=ALU.mult),
                  r=[("ar", 0), ("ar", 1), "sE8", ("igt", s, 0), ("igt", s, 1)], w=[("igt", s, 0), ("igt", s, 1)])
                dma(IG[b, tsl, :, :], igt[s][:], r=[("igt", s, 0), ("igt", s, 1)], w=["IG"])

            tiles = [(b, tt) for b in range(NB) for tt in range(NT)]
            e_front(*tiles[0])
            for ti, (b, tt) in enumerate(tiles):
                if ti + 1 < len(tiles):
                    e_front(*tiles[ti + 1])
                e_back(b, tt)
            k.barrier()

        TG = 256
        NGR = T // TG
        X1f = X1.rearrange("b s d -> (b s) d")
        outf = out.rearrange("b s d -> (b s) d")
        IGf = IG.rearrange("b s a r -> (b s) a r")
        with ExitStack() as ph:
            Gm = [sbt(ph, f"Gm{i}", [128, TG, 128], BF16) for i in range(2)]
            ust = [sbt(ph, f"ust{i}", [128, 2, 1024], BF16) for i in range(3)]
            vst = [sbt(ph, f"vst{i}", [128, 2, 1024], BF16) for i in range(3)]
            h2s = [sbt(ph, f"h2F{i}", [128, 8, TG], BF16) for i in range(2)]
            igl = sbt(ph, "igl", [128, 2, 3, 128], F32)
            igT = sbt(ph, "igT", [128, 3, TG], F32)
            ohA = [sbt(ph, f"ohA{i}", [128, 8, 128], BF16) for i in range(2)]
            ohB = [sbt(ph, f"ohB{i}", [128, 8, 128], BF16) for i in range(2)]
            ohT = sbt(ph, "ohT", [128, 8, 128], BF16)
            act = [sbt(ph, f"actF{i}", [128, TG], BF16) for i in range(4)]
            ga = [sbt(ph, f"gaF{i}", [128, TG], BF16) for i in range(4)]
            gt2 = sbt(ph, "gt2", [128, D], F32)
            fwb = sbt(ph, "fwb", [128, D], F32)
            x1l = sbt(ph, "x1l", [128, D], F32)
            yo = sbt(ph, "yo", [128, D], F32)
            oo = sbt(ph, "oo", [128, D], F32)
            st4 = sbt(ph, "st4F", [128, 8], F32)
            psY = [pst(ph, f"psY{i}", [128, 512], F32) for i in range(4)]
            psSc = [pst(ph, f"psSc{i}", [128, 512], F32) for i in range(2)]
            psGb = [pst(ph, f"psGb{i}", [128, 512], F32) for i in range(2)]
            dma(fwb[:], fin_w.partition_broadcast(128), w=["fwb"])
            UTv = UT.rearrange("i p f -> p i f")
            VBv = VB.rearrange("i p f -> p i f")
            ldc = [0]
            gcount = [0]
            slots = {}
            iob = iotaf.unsqueeze(1).to_broadcast([128, 8, 128])

            def prep_group(gr):
                t0 = gr * TG
                b = t0 // S
                s0 = t0 % S
                hs = gr % 2
                dma(h2s[hs][:], H2T[b, :, :, s0:s0 + TG].rearrange("k p t -> p k t"), w=[("h2F", hs)])
                dma(igl[:].rearrange("p j a r -> p j (a r)"), IGf[t0:t0 + TG].rearrange("(j p) a r -> p j (a r)", p=128), w=["igl"])
                for j in range(2):
                    for a in range(3):
                        P(lambda e: e.transpose(out=psGb[0][:, 0:128], in_=igl[:, j, a, :], identity=identf), r=["igl", "cst"], w=[("psGb", 0)])
                        V(lambda e: e.tensor_copy(out=igT[:, a, j * 128:(j + 1) * 128], in_=psGb[0][:, 0:128]), r=[("psGb", 0)], w=["igT"])

            def gb_onehots(gr, sub):
                os_ = sub % 2
                tq = slice(sub * 8, (sub + 1) * 8)
                V(lambda e: e.tensor_tensor(out=ohA[os_][:], in0=iob, in1=igT[:, 0, tq].unsqueeze(2).to_broadcast([128, 8, 128]), op=ALU.is_equal), r=["cst", "igT"], w=[("ohA", os_)])
                V(lambda e: e.tensor_tensor(out=ohT[:], in0=iob, in1=igT[:, 1, tq].unsqueeze(2).to_broadcast([128, 8, 128]), op=ALU.is_equal), r=["cst", "igT"], w=["ohT"])
                G(lambda e: e.tensor_tensor(out=ohB[os_][:], in0=ohT[:], in1=igT[:, 2, tq].unsqueeze(2).to_broadcast([128, 8, 128]), op=ALU.mult), r=["ohT", "igT"], w=[("ohB", os_)])

            def gb_mm(gr, sub):
                os_ = sub % 2
                gb = gr % 2
                for q4 in range(2):
                    pg = gcount[0] % 2
                    gcount[0] += 1
                    for j in range(4):
                        tl = q4 * 4 + j
                        P(lambda e: e.matmul(psGb[pg][:, j * 128:(j + 1) * 128], lhsT=ohB[os_][:, tl, :], rhs=ohA[os_][:, tl, :], start=True, stop=True),
                          r=[("ohA", os_), ("ohB", os_)], w=[("psGb", pg)])
                    tb = sub * 8 + q4 * 4
                    A(lambda e: e.activation(out=Gm[gb][:, tb:tb + 4, :], in_=psGb[pg][:].rearrange("p (t i) -> p t i", t=4), func=AF.Copy), r=[("psGb", pg)], w=[("Gm", gb)])

            def load_blk(i2b):
                ldc[0] += 1
                ss_ = ldc[0] % 3
                slots[i2b] = ss_
                dma(ust[ss_][:].rearrange("p j f -> p (j f)"), UT[i2b], r=["UT"], w=[("ust", ss_)])
                dma(vst[ss_][:].rearrange("p j f -> p (j f)"), VB[i2b], r=["VB"], w=[("vst", ss_)])

            def scores(gr, i1):
                ss_ = slots[i1 // 2]
                j = i1 % 2
                q = i1 % 2
                h2 = h2s[gr % 2]
                for kc in range(8):
                    P(lambda e: e.matmul(psSc[q][:, 0:TG], lhsT=ust[ss_][:, j, kc * 128:(kc + 1) * 128], rhs=h2[:, kc, :], start=(kc == 0), stop=(kc == 7)),
                      r=[("ust", ss_), ("h2F", gr % 2)], w=[("psSc", q)])

            prep_group(0)
            for sub in range(TG // 8):
                gb_onehots(0, sub)
                gb_mm(0, sub)
            if NGR > 1:
                prep_group(1)
            for gr in range(NGR):
                t0 = gr * TG
                b = t0 // S
                s0 = t0 % S
                gb = gr % 2
                if s0 == 0:
                    dma(gt2[:], MOD[b:b + 1, 5 * D:6 * D].partition_broadcast(128), r=["MOD"], w=["gt2"])
                nxt = gr + 1 < NGR
                if gr == 0:
                    load_blk(0)
                    load_blk(1)
                    scores(gr, 0)

                def ymm(i1):
                    ss_y = slots_y[i1]
                    j_ = i1 % 2
                    q_ = i1 % 4
                    for tj in range(2):
                        for nh in range(2):
                            P(lambda e: e.matmul(psY[tj * 2 + nh][:], lhsT=ga[q_][:, tj * 128:(tj + 1) * 128], rhs=vst[ss_y][:, j_, nh * 512:(nh + 1) * 512], start=(i1 == 0), stop=(i1 == 127)),
                              r=[("gaF", q_), ("vst", ss_y)], w=[("psY", tj * 2 + nh)])

                slots_y = {}
                for i1 in range(128):
                    ss_ = slots[i1 // 2]
                    slots_y[i1] = ss_
                    q = i1 % 2
                    q4 = i1 % 4
                    if i1 + 1 < 128:
                        scores(gr, i1 + 1)
                    if i1 >= 1:
                        ymm(i1 - 1)
                    if i1 % 2 == 1 and (i1 + 1) // 2 + 1 < 64:
                        load_blk((i1 + 1) // 2 + 1)
                    if nxt and i1 % 4 == 0:
                        sub = i1 // 4
                        gb_onehots(gr + 1, sub)
                        if sub >= 1:
                            gb_mm(gr + 1, sub - 1)
                    A(lambda e: e.activation(out=act[q][:], in_=psSc[q][:, 0:TG], func=AF.Gelu), r=[("psSc", q)], w=[("actF", q)])
                    eng = V if (i1 % 2 == 0) else G
                    eng(lambda e: e.tensor_tensor(out=ga[q4][:], in0=act[q][:], in1=Gm[gb][:, :, i1], op=ALU.mult), r=[("actF", q), ("Gm", gb)], w=[("gaF", q4)])
                ymm(127)
                if nxt:
                    gb_mm(gr + 1, 31)
                if gr + 2 < NGR:
                    prep_group(gr + 2)
                if nxt:
                    load_blk(0)
                    load_blk(1)
                    scores(gr + 1, 0)
                Yb = [yo, oo]
                for tj in range(2):
                    for nh in range(2):
                        V(lambda e: e.tensor_tensor(out=Yb[tj][:, nh * 512:(nh + 1) * 512], in0=psY[tj * 2 + nh][:], in1=gt2[:, nh * 512:(nh + 1) * 512], op=ALU.mult), r=[("psY", tj * 2 + nh), "gt2"], w=[("Yb", tj)])
                for tj in range(2):
                    Y = Yb[tj]
                    ky = ("Yb", tj)
                    dma(x1l[:], X1f[t0 + tj * 128:t0 + (tj + 1) * 128, :], w=["x1l"])
                    G(lambda e: e.tensor_tensor(out=Y[:], in0=Y[:], in1=x1l[:], op=ALU.add), r=[ky, "x1l"], w=[ky])
                    A(lambda e: e.activation(out=x1l[:], in_=Y[:], func=AF.Square, accum_out=st4[:, 0:1]), r=[ky, "x1l"], w=["x1l", "sF0"])
                    V(lambda e: e.tensor_scalar(out=st4[:, 1:2], in0=st4[:, 0:1], scalar1=1.0 / D, scalar2=EPS, op0=ALU.mult, op1=ALU.add), r=["sF0"], w=["sF1"])
                    A(lambda e: e.activation(out=st4[:, 2:3], in_=st4[:, 1:2], func=AF.Sqrt), r=["sF1"], w=["sF2"])
                    V(lambda e: e.reciprocal(out=st4[:, 3:4], in_=st4[:, 2:3]), r=["sF2"], w=["sF3"])
                    V(lambda e: e.scalar_tensor_tensor(out=Y[:], in0=Y[:], scalar=st4[:, 3:4], in1=fwb[:], op0=ALU.mult, op1=ALU.mult), r=[ky, "sF3", "fwb"], w=[ky])
                    dma(outf[t0 + tj * 128:t0 + (tj + 1) * 128, :], Y[:], r=[ky], w=["OUT"])
            k.barrier()
    return nc


_INPUT_ORDER = ["x", "c", "ada_w", "ada_b", "norm1_w", "norm2_w", "w_in", "conv_w", "conv_b", "ml_i_bias", "ml_f_bias",
                "ml_norm_w", "lam_q1", "lam_k1", "lam_q2", "lam_k2", "subln_w", "w_out", "peer_wq", "peer_keys",
                "peer_u", "peer_v", "final_norm_w"]


def make_in_maps(cfg, inputs, n_cores):
    f = lambda a: np.ascontiguousarray(np.asarray(a, dtype=np.float32))
    NB = cfg.NB
    shared = {
        "ada_w": f(inputs["ada_w"][0]), "ada_b": f(inputs["ada_b"][0]).reshape(1, -1),
        "norm1_w": f(inputs["norm1_w"][0]).reshape(1, -1), "norm2_w": f(inputs["norm2_w"][0]).reshape(1, -1),
        "w_in": f(inputs["w_in"][0]), "conv_w": f(inputs["conv_w"][0]), "conv_b": f(inputs["conv_b"][0]).reshape(1, -1),
        "ml_i_bias": f(inputs["ml_i_bias"][0]).reshape(1, -1), "ml_f_bias": f(inputs["ml_f_bias"][0]).reshape(1, -1),
        "ml_norm_w": f(inputs["ml_norm_w"][0]).reshape(1, -1),
        "lam_q1": f(inputs["lam_q1"][0]).reshape(1, -1), "lam_k1": f(inputs["lam_k1"][0]).reshape(1, -1),
        "lam_q2": f(inputs["lam_q2"][0]).reshape(1, -1), "lam_k2": f(inputs["lam_k2"][0]).reshape(1, -1),
        "subln_w": f(inputs["subln_w"][0]).reshape(1, -1), "w_out": f(inputs["w_out"][0]),
        "peer_wq": f(inputs["peer_wq"][0]), "peer_keys": f(inputs["peer_keys"][0]).reshape(16, 128, 128),
        "peer_u": f(inputs["peer_u"][0]), "peer_v": f(inputs["peer_v"][0]),
        "final_norm_w": f(inputs["final_norm_w"]).reshape(1, -1),
        "cst": host_consts(cfg),
    }
    xs = f(inputs["x"])
    cs = f(inputs["c"])
    maps = []
    for i in range(n_cores):
        m = dict(shared)
        m["x"] = np.ascontiguousarray(xs[i * NB:(i + 1) * NB])
        m["c"] = np.ascontiguousarray(cs[i * NB:(i + 1) * NB])
        maps.append(m)
    return maps


def kernel(**inputs):
    n_cores = 8
    cfg = Cfg(S=4096, NB=2)
    nc = build(cfg)
    maps = make_in_maps(cfg, inputs, n_cores)
    res = run_bass_kernel_spmd(nc, maps, core_ids=list(range(n_cores)))
    outs = [np.asarray(r["out"], dtype=np.float32) for r in res.results]
    return np.concatenate(outs, axis=0)
```
